# Optimizing a Trainium2 kernel written in Bass

```python
import numpy as np
import jax
import jax.numpy as jnp
from jax import lax

D_MODEL = 1024
BATCH = 8
SEQ = 8192
DEPTH = 1

GRID_W = 64
NA_HEAD_DIM = 64
NA_HEADS = D_MODEL // 128
NA_WIDTH = NA_HEADS * NA_HEAD_DIM
NA_KH_MAX = 8
NA_KW = 16
HG_DK = 128
HG_DV = 128
HG_HEADS = D_MODEL // 256
HG_WIDTH = HG_HEADS * HG_DK
HG_VWIDTH = HG_HEADS * HG_DV
HG_CHUNK = 64
N_GROUPS = 4
EXPERTS_PER_GROUP = 8
N_EXPERTS = N_GROUPS * EXPERTS_PER_GROUP
TOP_K = 2
D_FF_EXPERT = D_MODEL // 2
MOE_BLOCK = 256
DN_ALPHA = (2 * DEPTH) ** 0.25
DN_BETA = (8 * DEPTH) ** -0.25
LN_EPS = 1e-5
RMS_EPS = 1e-6
IN_SIZES = (NA_WIDTH, NA_WIDTH, NA_WIDTH, HG_WIDTH, HG_WIDTH, HG_WIDTH, HG_VWIDTH, HG_VWIDTH, D_MODEL, D_MODEL)
D_IN = sum(IN_SIZES)

kernel_name = 'hybrid_na_hgrn2_hmoe_encoder'


def layer_norm(x, g, b):
    xf = x.astype(jnp.float32)
    mu = jnp.mean(xf, axis=-1, keepdims=True)
    xc = xf - mu
    var = jnp.mean(xc * xc, axis=-1, keepdims=True)
    return (xc * lax.rsqrt(var + LN_EPS) * g.astype(jnp.float32) + b.astype(jnp.float32)).astype(x.dtype)


def neighbourhood_attention(q, k, v, rpb):
    b, s, h, dh = q.shape
    rows = s // GRID_W
    kh = min(NA_KH_MAX, rows)

    def to_grid(t):
        return t.reshape(b, rows, GRID_W, h, dh).transpose(0, 3, 1, 2, 4)

    qg = to_grid(q * (dh ** -0.5))
    kg = to_grid(k)
    vg = to_grid(v)
    cols = np.arange(GRID_W)
    col_start = np.clip(cols - NA_KW // 2, 0, GRID_W - NA_KW)
    col_idx = col_start[:, None] + np.arange(NA_KW)[None, :]
    col_off = col_idx - cols[:, None] + (NA_KW - 1)

    def row_block(r):
        rs = jnp.clip(r - kh // 2, 0, rows - kh)
        q_row = lax.dynamic_index_in_dim(qg, r, axis=2, keepdims=False)
        k_band = lax.dynamic_slice_in_dim(kg, rs, kh, axis=2)
        v_band = lax.dynamic_slice_in_dim(vg, rs, kh, axis=2)
        k_win = k_band[:, :, :, col_idx]
        v_win = v_band[:, :, :, col_idx]
        row_off = rs + jnp.arange(kh) - r + (NA_KH_MAX - 1)
        bias = rpb[:, row_off][:, :, col_off].transpose(0, 2, 1, 3)
        scores = jnp.einsum('bhcd,bhrckd->bhcr k'.replace(' ', ''), q_row, k_win).astype(jnp.float32)
        scores = scores + bias[None].astype(jnp.float32)
        p = jax.nn.softmax(scores.reshape(b, h, GRID_W, kh * NA_KW), axis=-1)
        p = p.reshape(b, h, GRID_W, kh, NA_KW).astype(v.dtype)
        return jnp.einsum('bhcrk,bhrckd->bhcd', p, v_win)

    out = lax.map(row_block, jnp.arange(rows))
    return out.transpose(1, 0, 3, 2, 4).reshape(b, s, h * dh)


def gla_chunk_scan(q, k, v, log_f):
    _, b, n, c, dk = q.shape
    dv = v.shape[-1]
    causal = jnp.tril(jnp.ones((c, c), dtype=bool))

    def step(state, inp):
        qc, kc, vc, gc = inp
        bcum = jnp.cumsum(gc, axis=2)
        b_last = bcum[:, :, -1:, :]
        o_inter = jnp.einsum('bntk,bnkv->bntv', qc * jnp.exp(bcum), state)
        diff = bcum[:, :, :, None, :] - bcum[:, :, None, :, :]
        decay = jnp.exp(jnp.where(causal[:, :, None], diff, -jnp.inf))
        attn = jnp.einsum('bntk,bnsk,bntsk->bnts', qc, kc, decay)
        o_intra = jnp.einsum('bnts,bnsv->bntv', attn, vc)
        new_state = jnp.exp(b_last[:, :, 0, :])[..., None] * state + jnp.einsum(
            'bnsk,bnsv->bnkv', kc * jnp.exp(b_last - bcum), vc)
        return new_state, o_inter + o_intra

    init = jnp.zeros((b, n, dk, dv), jnp.float32)
    _, o = lax.scan(step, init, (q, k, v, log_f))
    return o


def hgrn2_bidirectional(q, f_fwd, f_bwd, i, g, lb, norm_g):
    b, s, h, dk = q.shape
    dv = i.shape[-1]
    f32 = jnp.float32
    qf = jax.nn.silu(q.astype(f32))
    lbr = lb.astype(f32).reshape(2, 1, 1, h, dk)
    forget = lbr + (1.0 - lbr) * jax.nn.sigmoid(jnp.stack([f_fwd, f_bwd]).astype(f32))
    log_f = jnp.log(forget)
    kk = 1.0 - forget
    vf = i.astype(f32)

    def rev(t):
        return jnp.flip(t, axis=1)

    qd = jnp.stack([qf, rev(qf)])
    kd = jnp.stack([kk[0], rev(kk[1])])
    vd = jnp.stack([vf, rev(vf)])
    gd = jnp.stack([log_f[0], rev(log_f[1])])
    nc = s // HG_CHUNK

    def to_chunks(t):
        d = t.shape[-1]
        return t.reshape(2, b, nc, HG_CHUNK, h, d).transpose(2, 1, 0, 4, 3, 5).reshape(nc, b, 2 * h, HG_CHUNK, d)

    o = gla_chunk_scan(to_chunks(qd), to_chunks(kd), to_chunks(vd), to_chunks(gd))
    o = o.reshape(nc, b, 2, h, HG_CHUNK, dv).transpose(2, 1, 0, 4, 3, 5).reshape(2, b, s, h, dv)
    o = o[0] + rev(o[1])
    o = o * lax.rsqrt(jnp.mean(o * o, axis=-1, keepdims=True) + RMS_EPS)
    o = o * norm_g.astype(f32).reshape(h, dv) * jax.nn.silu(g.astype(f32))
    return o.reshape(b, s, h * dv).astype(i.dtype)


def hierarchical_moe(x2d, w_rg, b_rg, w_re, b_re, w_gate, w_up, w_down):
    t, d = x2d.shape
    f32 = jnp.float32
    xf = x2d.astype(f32)
    g_logits = xf @ w_rg.astype(f32) + b_rg.astype(f32)
    g_prob = jax.nn.softmax(g_logits, axis=-1)
    g_sel = jnp.argmax(g_logits, axis=-1)
    g_w = jnp.take_along_axis(g_prob, g_sel[:, None], axis=-1)
    e_logits = (xf @ w_re.astype(f32) + b_re.astype(f32)).reshape(t, N_GROUPS, EXPERTS_PER_GROUP)
    e_logits = jnp.take_along_axis(e_logits, g_sel[:, None, None], axis=1)[:, 0]
    top_p, top_i = lax.top_k(jax.nn.softmax(e_logits, axis=-1), TOP_K)
    weights = g_w * top_p / jnp.sum(top_p, axis=-1, keepdims=True)
    expert_id = (g_sel[:, None] * EXPERTS_PER_GROUP + top_i).reshape(-1)
    n_assign = t * TOP_K
    order = jnp.argsort(expert_id)
    sorted_e = expert_id[order]
    counts = jnp.bincount(expert_id, length=N_EXPERTS)
    padded = (counts + MOE_BLOCK - 1) // MOE_BLOCK * MOE_BLOCK
    starts = jnp.cumsum(counts) - counts
    pends = jnp.cumsum(padded)
    pstarts = pends - padded
    dest = pstarts[sorted_e] + jnp.arange(n_assign) - starts[sorted_e]
    n_blocks = -(-n_assign // MOE_BLOCK) + N_EXPERTS
    tok = order // TOP_K
    buf_tok = jnp.zeros((n_blocks * MOE_BLOCK,), tok.dtype).at[dest].set(tok)
    block_expert = jnp.minimum(
        jnp.searchsorted(pends, jnp.arange(n_blocks) * MOE_BLOCK, side='right'), N_EXPERTS - 1)
    xbuf = x2d[buf_tok].reshape(n_blocks, MOE_BLOCK, d)

    def expert_block(args):
        xb, e = args
        hid = jax.nn.silu(xb @ w_gate[e]) * (xb @ w_up[e])
        return hid @ w_down[e]

    ybuf = lax.map(expert_block, (xbuf, block_expert)).reshape(-1, d)
    w_sorted = weights.reshape(-1)[order].astype(x2d.dtype)
    return jnp.zeros_like(x2d).at[tok].add(w_sorted[:, None] * ybuf[dest])


def setup_inputs(seed: int = 0) -> dict:
    key = jax.random.key(seed)
    ks = jax.random.split(key, 24)
    f32 = jnp.float32

    def nrm(k, shape, scale):
        return jax.random.normal(k, shape, f32) * scale

    col_scale = np.concatenate(
        [np.full((n,), DN_BETA if j in (2, 6) else 1.0, np.float32) for j, n in enumerate(IN_SIZES)])
    return {
        'x': nrm(ks[0], (BATCH, SEQ, D_MODEL), 1.0),
        'emb_ln_g': 1.0 + nrm(ks[1], (D_MODEL,), 0.05),
        'emb_ln_b': nrm(ks[2], (D_MODEL,), 0.02),
        'w_in': nrm(ks[3], (DEPTH, D_MODEL, D_IN), D_MODEL ** -0.5) * jnp.asarray(col_scale),
        'na_rpb': nrm(ks[4], (DEPTH, NA_HEADS, 2 * NA_KH_MAX - 1, 2 * NA_KW - 1), 0.02),
        'hg_lb': 1.0 + nrm(ks[5], (2, DEPTH + 1, HG_WIDTH), 0.1),
        'hg_norm_g': 1.0 + nrm(ks[6], (DEPTH, HG_VWIDTH), 0.05),
        'w_proj_a': nrm(ks[7], (DEPTH, NA_WIDTH, D_MODEL), NA_WIDTH ** -0.5 * DN_BETA),
        'w_proj_b': nrm(ks[8], (DEPTH, HG_VWIDTH, D_MODEL), HG_VWIDTH ** -0.5 * DN_BETA),
        'w_out': nrm(ks[9], (DEPTH, D_MODEL, D_MODEL), D_MODEL ** -0.5 * DN_BETA),
        'ln1_g': 1.0 + nrm(ks[10], (DEPTH, D_MODEL), 0.05),
        'ln1_b': nrm(ks[11], (DEPTH, D_MODEL), 0.02),
        'w_router_group': nrm(ks[12], (DEPTH, D_MODEL, N_GROUPS), D_MODEL ** -0.5),
        'b_router_group': nrm(ks[13], (DEPTH, N_GROUPS), 0.01),
        'w_router_expert': nrm(ks[14], (DEPTH, D_MODEL, N_EXPERTS), D_MODEL ** -0.5),
        'b_router_expert': nrm(ks[15], (DEPTH, N_EXPERTS), 0.01),
        'w_gate': nrm(ks[16], (DEPTH, N_EXPERTS, D_MODEL, D_FF_EXPERT), D_MODEL ** -0.5 * DN_BETA),
        'w_up': nrm(ks[17], (DEPTH, N_EXPERTS, D_MODEL, D_FF_EXPERT), D_MODEL ** -0.5 * DN_BETA),
        'w_down': nrm(ks[18], (DEPTH, N_EXPERTS, D_FF_EXPERT, D_MODEL), D_FF_EXPERT ** -0.5 * DN_BETA),
        'ln2_g': 1.0 + nrm(ks[19], (DEPTH, D_MODEL), 0.05),
        'ln2_b': nrm(ks[20], (DEPTH, D_MODEL), 0.02),
    }


def reference(x, emb_ln_g, emb_ln_b, w_in, na_rpb, hg_lb, hg_norm_g, w_proj_a, w_proj_b, w_out,
              ln1_g, ln1_b, w_router_group, b_router_group, w_router_expert, b_router_expert,
              w_gate, w_up, w_down, ln2_g, ln2_b):
    b, s, d = x.shape
    split_points = np.cumsum(IN_SIZES)[:-1].tolist()
    lb_all = jnp.cumsum(jax.nn.softmax(hg_lb.astype(jnp.float32), axis=1), axis=1)
    h = layer_norm(x, emb_ln_g, emb_ln_b)
    for l in range(DEPTH):
        u = h @ w_in[l]
        (na_q, na_k, na_v, hg_q, hg_ff, hg_fb, hg_i, hg_g, gate_a, gate_b) = jnp.split(u, split_points, axis=-1)
        a = neighbourhood_attention(na_q.reshape(b, s, NA_HEADS, NA_HEAD_DIM),
                                    na_k.reshape(b, s, NA_HEADS, NA_HEAD_DIM),
                                    na_v.reshape(b, s, NA_HEADS, NA_HEAD_DIM), na_rpb[l])
        c = hgrn2_bidirectional(hg_q.reshape(b, s, HG_HEADS, HG_DK),
                                hg_ff.reshape(b, s, HG_HEADS, HG_DK),
                                hg_fb.reshape(b, s, HG_HEADS, HG_DK),
                                hg_i.reshape(b, s, HG_HEADS, HG_DV),
                                hg_g.reshape(b, s, HG_HEADS, HG_DV),
                                lb_all[:, l], hg_norm_g[l])
        merged = jax.nn.sigmoid(gate_a) * (a @ w_proj_a[l]) + jax.nn.sigmoid(gate_b) * (c @ w_proj_b[l])
        h = layer_norm(DN_ALPHA * h + merged @ w_out[l], ln1_g[l], ln1_b[l])
        moe = hierarchical_moe(h.reshape(b * s, d), w_router_group[l], b_router_group[l],
                               w_router_expert[l], b_router_expert[l], w_gate[l], w_up[l], w_down[l])
        h = layer_norm(DN_ALPHA * h + moe.reshape(b, s, d), ln2_g[l], ln2_b[l])
    return h
```

```python
from contextlib import ExitStack
import numpy as np
import ml_dtypes
import concourse.bass as bass
import concourse.mybir as mybir
from concourse.bass_utils import run_bass_kernel_spmd

F32 = mybir.dt.float32
BF16 = mybir.dt.bfloat16
I32 = mybir.dt.int32
AF = mybir.ActivationFunctionType
ALU = mybir.AluOpType
AX = mybir.AxisListType

T = 8192
D = 1024
NBLK = 16
NTILE = 64
CAP = 768
NSLOT = 32 * CAP + 128
ALPHA = 2.0 ** 0.25
LN_EPS = 1e-5
RMS_EPS = 1e-6
NEG = -30000.0

ENGS = ("pe", "dve", "act", "pool", "sp")
EPOCH = 3000
NDMA_SEM = 8


class Op:
    __slots__ = ("eng", "fn", "deps", "sig", "sem", "val", "dma", "n")

    def __init__(self, eng, fn, dma):
        self.eng = eng
        self.fn = fn
        self.dma = dma
        self.deps = []
        self.sig = dma
        self.sem = None
        self.val = 0


class Sched:
    def __init__(self):
        self.q = {e: [] for e in ENGS}
        self.last_w = {}
        self.readers = {}
        self.dma_hist = {e: [] for e in ENGS}
        self.nops = 0
        self.bar = None
        self.bar_pending = set()
        self.rec = None

    def barrier(self):
        ops = []
        for e in ENGS:
            last = None
            for op in reversed(self.q[e]):
                if not op.dma:
                    last = op
                    break
            if last is not None:
                ops.append(last)
            ops.extend(self.dma_hist[e][-NDMA_SEM:])
        self.bar = ops
        self.bar_pending = set(ENGS)

    def record(self):
        self.rec = []

    def stop(self):
        r = self.rec
        self.rec = None
        return r

    def merge(self, *streams):
        streams = [st for st in streams if st]
        pos = [0] * len(streams)
        total = sum(len(st) for st in streams)
        for _ in range(total):
            best = None
            for i, st in enumerate(streams):
                if pos[i] < len(st):
                    f = (pos[i] + 0.5) / len(st)
                    if best is None or f < best[0]:
                        best = (f, i)
            i = best[1]
            self.add(*streams[i][pos[i]])
            pos[i] += 1

    def add(self, eng, fn, r=(), w=(), dma=False):
        if self.rec is not None:
            self.rec.append((eng, fn, tuple(r), tuple(w), dma))
            return None
        op = Op(eng, fn, dma)
        op.n = self.nops
        self.nops += 1
        deps = {}
        if eng in self.bar_pending:
            self.bar_pending.discard(eng)
            for p in self.bar:
                if p.dma or p.eng != eng:
                    deps[id(p)] = p
        for k in r:
            p = self.last_w.get(k)
            if p is not None and p is not op:
                if p.eng == eng and not p.dma and not dma:
                    if eng != "pe":
                        deps[id(p)] = p
                else:
                    deps[id(p)] = p
        for k in w:
            p = self.last_w.get(k)
            if p is not None and p is not op and (p.dma or dma or p.eng != eng or eng != "pe"):
                deps[id(p)] = p
            rd = self.readers.get(k)
            if rd:
                for p in rd.values():
                    if p is not op and (p.dma or dma or p.eng != eng or eng != "pe"):
                        deps[id(p)] = p
        for k in w:
            self.last_w[k] = op
            self.readers[k] = {}
        for k in r:
            d = self.readers.setdefault(k, {})
            d[("dma", op.n) if dma else eng] = op
        if dma:
            h = self.dma_hist[eng]
            if len(h) >= NDMA_SEM:
                p = h[-NDMA_SEM]
                deps[id(p)] = p
            h.append(op)
        op.deps = list(deps.values())
        for p in op.deps:
            p.sig = True
        self.q[eng].append(op)
        return op

    def emit(self, nc):
        with ExitStack() as es:
            for e in ENGS:
                cnt = 0
                sem = None
                dsem = [None] * NDMA_SEM
                dval = [0] * NDMA_SEM
                nd = 0
                ns = 0
                for op in self.q[e]:
                    if op.dma:
                        s = nd % NDMA_SEM
                        if dsem[s] is None:
                            dsem[s] = es.enter_context(nc.semaphore(f"d_{e}_{s}"))
                        dval[s] += 16
                        op.sem = dsem[s]
                        op.val = dval[s]
                        nd += 1
                    elif op.sig:
                        if sem is None or cnt >= EPOCH:
                            sem = es.enter_context(nc.semaphore(f"c_{e}_{ns}"))
                            ns += 1
                            cnt = 0
                        cnt += 1
                        op.sem = sem
                        op.val = cnt
            block = es.enter_context(nc.Block())
            engmap = {"pe": block.tensor, "dve": block.vector, "act": block.scalar,
                      "pool": block.gpsimd, "sp": block.sync}
            for e in ENGS:
                ops = self.q[e]
                if not ops:
                    continue

                def body(engine, ops=ops):
                    waited = {}
                    for op in ops:
                        for p in op.deps:
                            key = id(p.sem)
                            if waited.get(key, 0) >= p.val:
                                continue
                            waited[key] = p.val
                            engine.wait_ge(p.sem, p.val)
                        ins = op.fn(engine)
                        if op.sig:
                            ins.then_inc(op.sem, 16 if op.dma else 1)
                    last = {}
                    for op in ops:
                        if op.dma:
                            last[id(op.sem)] = op
                    for op in last.values():
                        if waited.get(id(op.sem), 0) < op.val:
                            engine.wait_ge(op.sem, op.val)

                engmap[e](body)


IN_OFF = dict(na_q=0, na_k=512, na_v=1024, hg_q=1536, hg_ff=2048, hg_fb=2560, hg_i=3072, hg_g=3584,
              gate_a=4096, gate_b=5120)


def build(dbg=(), phases="0BNFMEG"):
    nc = bass.Bass("TRN2", target_bir_lowering=False)
    S = Sched()

    def din(name, shape, dt):
        return nc.dram_tensor(name, list(shape), dt, kind="ExternalInput").ap()

    def dscr(name, shape, dt):
        kind = "ExternalOutput" if name in dbg else "Internal"
        return nc.dram_tensor(name, list(shape), dt, kind=kind).ap()

    x_d = din("x", [T, D], F32)
    w_in_d = din("w_in", [D, 6144], F32)
    w_pa_d = din("w_proj_a", [512, D], F32)
    w_pb_d = din("w_proj_b", [512, D], F32)
    w_out_d = din("w_out", [D, D], F32)
    w_gate_d = din("w_gate", [32, D, 512], F32)
    w_up_d = din("w_up", [32, D, 512], F32)
    w_down_d = din("w_down", [32, 512, D], F32)
    lnp_d = din("lnp", [128, 6, D], F32)
    embp_d = din("embp", [128, 16], F32)
    lbraw_d = din("lbraw", [128, 16], F32)
    normg_d = din("normg", [128, 4], F32)
    wr_d = din("wr", [128, 8, 36], F32)
    rb_d = din("rb", [128, 4, 36], F32)
    tt_d = din("tt", [128, 16, 512], BF16)
    cst_d = din("cst", [128, 5, 512], BF16)
    cst32_d = din("cst32", [128, 3, 512], F32)
    out_d = nc.dram_tensor("out", [T, D], F32, kind="ExternalOutput").ap()

    h0T_d = dscr("h0T_d", [D, T], BF16)
    obw_d = dscr("obw_d", [512, T], F32)
    aT_d = dscr("aT_d", [512, T], BF16)
    cT_d = dscr("cT_d", [512, T], BF16)
    h1_d = dscr("h1_d", [T, D], F32)
    xbuf_d = dscr("xbuf_d", [NSLOT, D], BF16)
    ybuf_d = dscr("ybuf_d", [NSLOT, D], F32)

    h0T_v = h0T_d.rearrange("(k p) t -> p k t", p=128)
    obw_v = obw_d.rearrange("(h p) t -> p h t", p=128)
    aT_v = aT_d.rearrange("(k p) t -> p k t", p=128)
    cT_v = cT_d.rearrange("(k p) t -> p k t", p=128)
    x_v = x_d.rearrange("(b t p) d -> b p t d", t=4, p=128)
    h1_v = h1_d.rearrange("(b t p) d -> b p t d", t=4, p=128)
    out_v = out_d.rearrange("(n p) d -> n p d", p=128)
    h1_tv = h1_d.rearrange("(n p) d -> n p d", p=128)
    w_in_v = w_in_d.rearrange("(k p) n -> p k n", p=128)

    with ExitStack() as ges:
        uid = [0]

        def sbuf(es, name, shape, dt):
            uid[0] += 1
            return es.enter_context(nc.sbuf_tensor(f"s{uid[0]}_{name}", list(shape), dt))

        PS = ges.enter_context(nc.psum_tensor("PS", [128, 4096], F32))

        def bank(i, n=1):
            return PS[:, i * 512:(i + n) * 512]

        def bank16(i):
            return PS[:, i * 512:(i + 1) * 512].bitcast(BF16)

        cst = sbuf(ges, "cst", [128, 5, 512], BF16)
        cst32 = sbuf(ges, "cst32", [128, 3, 512], F32)
        embp = sbuf(ges, "embp", [128, 16], F32)
        lbraw = sbuf(ges, "lbraw", [128, 16], F32)
        lbp = sbuf(ges, "lbp", [128, 3, 8], F32)
        normg = sbuf(ges, "normg", [128, 4], F32)
        posall = sbuf(ges, "posall", [128, NTILE, 2], I32)
        wall = sbuf(ges, "wall", [128, NTILE, 2], F32)
        xstat = sbuf(ges, "xstat", [128, NTILE, 2], F32)
        ident = cst[:, 0, 0:128]
        ones_bf = cst[:, 4, 0:128]

        def DMA(eng, out, in_, r, w):
            S.add(eng, lambda e: e.dma_start(out=out, in_=in_), r=r, w=w, dma=True)

        def MM(out, lhsT, rhs, start, stop, r, w):
            S.add("pe", lambda e: e.matmul(out, lhsT=lhsT, rhs=rhs, start=start, stop=stop), r=r, w=w)

        def TR(out, in_, idn, r, w):
            S.add("pe", lambda e: e.transpose(out, in_, idn), r=r, w=w)

        def ACT(out, in_, func, r, w, scale=1.0, bias=0.0):
            S.add("act", lambda e: e.activation(out=out, in_=in_, func=func, bias=bias, scale=scale), r=r, w=w)

        def TT(eng, out, in0, in1, op, r, w):
            S.add(eng, lambda e: e.tensor_tensor(out=out, in0=in0, in1=in1, op=op), r=r, w=w)

        def TS(eng, out, in0, s1, s2, op0, op1, r, w):
            if s2 is None:
                S.add(eng, lambda e: e.tensor_scalar(out=out, in0=in0, scalar1=s1, scalar2=None, op0=op0), r=r, w=w)
            else:
                S.add(eng, lambda e: e.tensor_scalar(out=out, in0=in0, scalar1=s1, scalar2=s2, op0=op0, op1=op1), r=r, w=w)

        def STT(out, in0, scalar, in1, op0, op1, r, w):
            S.add("dve", lambda e: e.scalar_tensor_tensor(out=out, in0=in0, scalar=scalar, in1=in1, op0=op0, op1=op1), r=r, w=w)

        def CP(eng, out, in_, r, w):
            if eng == "act":
                S.add("act", lambda e: e.activation(out=out, in_=in_, func=AF.Copy), r=r, w=w)
            else:
                S.add(eng, lambda e: e.tensor_copy(out=out, in_=in_), r=r, w=w)

        def MSET(eng, ap, val, w):
            S.add(eng, lambda e: e.memset(ap, val), w=w)

        DMA("sp", cst[:], cst_d, [], ["cst"])
        DMA("sp", cst32[:], cst32_d, [], ["cst32"])
        DMA("sp", embp[:], embp_d, [], ["embp"])
        DMA("sp", lbraw[:], lbraw_d, [], ["lbraw"])
        DMA("sp", normg[:], normg_d, [], ["normg"])
        lbr = lbraw[:].rearrange("p (d l h) -> p d l h", d=2, l=2)
        lb_v = lbp[:, 0, :].rearrange("p (d h) -> p d h", d=2)
        TT("dve", lb_v, lbr[:, :, 1, :], lbr[:, :, 0, :], ALU.subtract, ["lbraw"], ["lbp"])
        ACT(lbp[:, 0, :], lbp[:, 0, :], AF.Exp, ["lbp"], ["lbp"])
        ACT(lbp[:, 0, :], lbp[:, 0, :], AF.Ln, ["lbp"], ["lbp"], bias=1.0)
        ACT(lbp[:, 0, :], lbp[:, 0, :], AF.Exp, ["lbp"], ["lbp"], scale=-1.0)
        TS("dve", lbp[:, 1, :], lbp[:, 0, :], -1.0, 1.0, ALU.mult, ALU.add, ["lbp"], ["lbp"])
        TS("dve", lbp[:, 2, :], lbp[:, 1, :], -1.0, None, ALU.mult, None, ["lbp"], ["lbp"])

        def ln_stats(es_name, xt, xkey, stats, mv, rstd, nmr, tiles=4):
            for t in range(tiles):
                for c in range(2):
                    S.add("dve", lambda e, t=t, c=c: e.bn_stats(out=stats[:, t, c * 6:(c + 1) * 6], in_=xt[:, t, c * 512:(c + 1) * 512]),
                          r=[xkey], w=[es_name + "st"])
                S.add("dve", lambda e, t=t: e.bn_aggr(out=mv[:, t, :], in_=stats[:, t, :]), r=[es_name + "st"], w=[es_name + "mv"])
            ACT(rstd[:, 0:tiles], mv[:, 0:tiles, 1], AF.Ln, [es_name + "mv"], [es_name + "rs", "xstat"], bias=LN_EPS)
            ACT(rstd[:, 0:tiles], rstd[:, 0:tiles], AF.Exp, [es_name + "rs"], [es_name + "rs", "xstat"], scale=-0.5)
            STT(nmr[:, 0:tiles], mv[:, 0:tiles, 0], -1.0, rstd[:, 0:tiles], ALU.mult, ALU.mult, [es_name + "mv", es_name + "rs"], [es_name + "nm", "xstat"])

        def phase0():
            with ExitStack() as es:
                xt = [sbuf(es, f"p0_xt{i}", [128, 4, D], F32) for i in range(2)]
                xn2 = [sbuf(es, f"p0_xn{i}", [128, 4, D], BF16) for i in range(2)]
                hT = [sbuf(es, f"p0_hT{i}", [128, 8, 512], BF16) for i in range(2)]
                stats2 = [sbuf(es, f"p0_stats{i}", [128, 4, 12], F32) for i in range(2)]
                mv2 = [sbuf(es, f"p0_mv{i}", [128, 4, 2], F32) for i in range(2)]
                def stageA(blk):
                    b = blk % 2
                    if blk + 1 < NBLK:
                        DMA("sp", xt[1 - b][:], x_v[blk + 1], [], [f"xt{1 - b}"])
                    rstd = xstat[:, blk * 4:(blk + 1) * 4, 0]
                    nmr = xstat[:, blk * 4:(blk + 1) * 4, 1]
                    ln_stats(f"p0{b}", xt[b], f"xt{b}", stats2[b], mv2[b], rstd, nmr)
                    for t in range(4):
                        ACT(xn2[b][:, t, :], xt[b][:, t, :], AF.Identity, [f"xt{b}", f"p0{b}rs", f"p0{b}nm"], [("xn", b, t)],
                            scale=rstd[:, t:t + 1], bias=nmr[:, t:t + 1])

                def stageB(blk):
                    b = blk % 2
                    xn = xn2[b]
                    for k in range(8):
                        pb = bank16(k % 2)
                        for t in range(4):
                            TR(pb[:, t * 128:(t + 1) * 128], xn[:, t, k * 128:(k + 1) * 128], ident, [("xn", b, t), "cst"], [("B", k % 2)])
                        if k % 2 == 0:
                            ACT(hT[b][:, k, :], pb[:, 0:512], AF.Identity, [("B", k % 2), "embp"], [f"hT{b}"],
                                scale=embp[:, k:k + 1], bias=embp[:, 8 + k:9 + k])
                        else:
                            TS("dve", hT[b][:, k, :], pb[:, 0:512], embp[:, k:k + 1], embp[:, 8 + k:9 + k], ALU.mult, ALU.add,
                               [("B", k % 2), "embp"], [f"hT{b}"])
                    DMA("sp", h0T_v[:, :, blk * 512:(blk + 1) * 512], hT[b][:], [f"hT{b}"], ["h0T_d"])

                DMA("sp", xt[0][:], x_v[0], [], ["xt0"])
                stageA(0)
                for blk in range(NBLK):
                    S.record()
                    if blk + 1 < NBLK:
                        stageA(blk + 1)
                    ra = S.stop()
                    S.record()
                    stageB(blk)
                    rb_ = S.stop()
                    S.merge(ra, rb_)

        def load_w(es, name, col0, ncol):
            w = sbuf(es, name, [128, 8, ncol], BF16)
            for k in range(8):
                DMA("pool", w[:, k, :], w_in_v[:, k, col0:col0 + ncol], [], [name])
            return w

        def hgrn_phase(direction):
            fwd = direction == 0
            with ExitStack() as es:
                Wq = load_w(es, "hg_Wq", IN_OFF["hg_q"], 512)
                Wf = load_w(es, "hg_Wf", IN_OFF["hg_ff"] if fwd else IN_OFF["hg_fb"], 512)
                Wi = load_w(es, "hg_Wi", IN_OFF["hg_i"], 512)
                Wg = load_w(es, "hg_Wg", IN_OFF["hg_g"], 512) if fwd else None
                hT = [sbuf(es, f"hg_hT{i}", [128, 8, 512], BF16) for i in range(2)]
                tnames = ["e1", "l1", "l2", "sq", "qf", "g", "fg", "kk", "bc", "eb", "enb", "kd32"]
                tmps = [{n: sbuf(es, f"hg_{n}{i}", [128, 512], F32) for n in tnames} for i in range(2)]
                ebl2 = [sbuf(es, f"hg_ebl{i}", [128, 4, 4], F32) for i in range(2)]
                Qd2 = [sbuf(es, f"hg_Qd{i}", [128, 4, 512], BF16) for i in range(2)]
                Kd2 = [sbuf(es, f"hg_Kd{i}", [128, 4, 512], BF16) for i in range(2)]
                Kl2 = [sbuf(es, f"hg_Kl{i}", [128, 4, 512], BF16) for i in range(2)]
                vsb2 = [sbuf(es, f"hg_v{i}", [128, 4, 512], BF16) for i in range(2)]
                klt = sbuf(es, "hg_klt", [128, 512], BF16)
                at = sbuf(es, "hg_at", [128, 512], BF16)
                osb = [sbuf(es, f"hg_o{i}", [128, 4, 512], F32) for i in range(2)]
                St = sbuf(es, "hg_S", [128, 4, 128], F32)
                Sb2 = [sbuf(es, f"hg_Sb{i}", [128, 4, 128], BF16) for i in range(2)]
                if fwd:
                    obw = sbuf(es, "hg_obw", [128, 4, 512], F32)
                    osq = sbuf(es, "hg_osq", [128, 4, 512], BF16)
                    rs = sbuf(es, "hg_rs", [128, 512], F32)
                    sg2 = [sbuf(es, f"hg_sg{i}", [128, 4, 512], F32) for i in range(2)]
                    cT = [sbuf(es, f"hg_cT{i}", [128, 4, 512], BF16) for i in range(2)]
                MSET("dve", St[:], 0.0, ["hg_S"])
                MSET("dve", Sb2[0][:], 0.0, ["hg_Sb0"])
                MSET("dve", Sb2[1][:], 0.0, ["hg_Sb1"])
                gcount = [0]
                di = 0 if fwd else 1
                mask = cst[:, 1 if fwd else 2, :]
                blocks = list(range(NBLK)) if fwd else list(range(NBLK - 1, -1, -1))
                tiles = [0, 1, 2, 3] if fwd else [3, 2, 1, 0]
                DMA("sp", hT[0][:], h0T_v[:, :, blocks[0] * 512:(blocks[0] + 1) * 512], ["h0T_d"], ["hg_hT0"])

                def vproj(bi, tl, bk):
                    b = bi % 2
                    hk = f"hg_hT{b}"
                    vsb = vsb2[b]
                    for t in tl:
                        pv = bank(bk)
                        for k in range(8):
                            MM(pv, hT[b][:, k, t * 128:(t + 1) * 128], Wi[:, k, :], k == 0, k == 7, [hk, "hg_Wi"], [("B", bk)])
                        CP("act" if t % 2 == 0 else "dve", vsb[:, t, :], pv, [("B", bk)], [("hg_v", b, t)])

                def head(bi, h, si):
                    b = bi % 2
                    hk = f"hg_hT{b}"
                    Qd, Kd, Kl, ebl = Qd2[b], Kd2[b], Kl2[b], ebl2[b]
                    tm = tmps[si]
                    e1, l1, l2, sq, qf, gg, fg, kk, bc, eb, enb, kd32 = [tm[n] for n in tnames]
                    K_ = lambda n: f"hg_{n}{si}"
                    pf = bank(2 * si)
                    pq = bank(2 * si + 1)
                    pfk = ("B", 2 * si)
                    pqk = ("B", 2 * si + 1)
                    for k in range(8):
                        MM(pf, Wf[:, k, h * 128:(h + 1) * 128], hT[b][:, k, :], k == 0, k == 7, [hk, "hg_Wf"], [pfk])
                    for k in range(8):
                        MM(pq, Wq[:, k, h * 128:(h + 1) * 128], hT[b][:, k, :], k == 0, k == 7, [hk, "hg_Wq"], [pqk])
                    lb = lbp[:, 0, di * 4 + h:di * 4 + h + 1]
                    ACT(e1[:], pf, AF.Exp, [pfk], [K_("e1")], scale=-1.0)
                    ACT(l1[:], e1[:], AF.Ln, [K_("e1")], [K_("l1")], bias=1.0)
                    ACT(l2[:], e1[:], AF.Ln, [K_("e1"), "lbp"], [K_("l2")], scale=lb, bias=1.0)
                    TT("pool", gg[:], l2[:], l1[:], ALU.subtract, [K_("l1"), K_("l2")], [K_("g")])
                    ACT(fg[:], gg[:], AF.Exp, [K_("g")], [K_("fg")])
                    TS("pool", kk[:], fg[:], -1.0, 1.0, ALU.mult, ALU.add, [K_("fg")], [K_("kk")])
                    ACT(e1[:], pq, AF.Exp, [pqk], [K_("e1")], scale=-1.0)
                    ACT(l1[:], e1[:], AF.Ln, [K_("e1")], [K_("l1")], bias=1.0)
                    ACT(sq[:], l1[:], AF.Exp, [K_("l1")], [K_("sq")], scale=-1.0)
                    TT("dve", qf[:], pq, sq[:], ALU.mult, [pqk, K_("sq")], [K_("qf")])
                    if fwd:
                        S.add("dve", lambda e: e.tensor_tensor_scan(out=bc[:], data0=cst32[:, 0, :], data1=gg[:], initial=0.0,
                                                                    op0=ALU.mult, op1=ALU.add), r=[K_("g"), "cst32"], w=[K_("bc")])
                        blast = bc[:].rearrange("p (c t) -> p c t", t=128)[:, :, 127]
                    else:
                        S.add("dve", lambda e: e.tensor_tensor_scan(out=bc[:, ::-1], data0=cst32[:, 1, ::-1], data1=gg[:, ::-1], initial=0.0,
                                                                    op0=ALU.mult, op1=ALU.add), r=[K_("g"), "cst32"], w=[K_("bc")])
                        blast = bc[:].rearrange("p (c t) -> p c t", t=128)[:, :, 0]
                    ACT(eb[:], bc[:], AF.Exp, [K_("bc")], [K_("eb")])
                    ACT(enb[:], bc[:], AF.Exp, [K_("bc")], [K_("enb")], scale=-1.0)
                    ACT(ebl[:, h, :], blast, AF.Exp, [K_("bc")], [("hg_ebl", b, h)])
                    TT("dve", Qd[:, h, :], qf[:], eb[:], ALU.mult, [K_("qf"), K_("eb")], [("hg_Qd", b, h)])
                    TT("pool", kd32[:], kk[:], enb[:], ALU.mult, [K_("kk"), K_("enb")], [K_("kd32")])
                    CP("pool", Kd[:, h, :], kd32[:], [K_("kd32")], [("hg_Kd", b, h)])
                    TT("dve", Kl[:, h, :].rearrange("p (c t) -> p c t", t=128), kd32[:].rearrange("p (c t) -> p c t", t=128),
                       ebl[:, h, :].unsqueeze(2).to_broadcast([128, 4, 128]), ALU.mult, [K_("kd32"), ("hg_ebl", b, h)], [("hg_Kl", b, h)])
                    if fwd:
                        for k in range(8):
                            MM(pq, Wg[:, k, h * 128:(h + 1) * 128], hT[b][:, k, :], k == 0, k == 7, [hk, "hg_Wg"], [pqk])
                        ACT(e1[:], pq, AF.Exp, [pqk], [K_("e1")], scale=-1.0)
                        ACT(l1[:], e1[:], AF.Ln, [K_("e1")], [K_("l1")], bias=1.0)
                        ACT(sq[:], l1[:], AF.Exp, [K_("l1")], [K_("sq")], scale=-1.0)
                        TT("dve", sg2[b][:, h, :], pq, sq[:], ALU.mult, [pqk, K_("sq")], [("hg_sg", b, h)])

                def streamsA(bi):
                    b = bi % 2
                    out = []
                    for si in range(2):
                        S.record()
                        if si == 0 and bi + 1 < NBLK:
                            nb = blocks[bi + 1]
                            DMA("sp", hT[1 - b][:], h0T_v[:, :, nb * 512:(nb + 1) * 512], ["h0T_d"], [f"hg_hT{1 - b}"])
                        vproj(bi, [2 * si, 2 * si + 1], 2 * si)
                        head(bi, si, si)
                        head(bi, si + 2, si)
                        out.append(S.stop())
                    return out

                def stageB(bi):
                    b = bi % 2
                    blk = blocks[bi]
                    Qd, Kd, Kl, vsb, ebl = Qd2[b], Kd2[b], Kl2[b], vsb2[b], ebl2[b]
                    allq = [("hg_Qd", b, h) for h in range(4)]
                    allk = [("hg_Kd", b, h) for h in range(4)]
                    alll = [("hg_Kl", b, h) for h in range(4)]
                    ob = osb[b]
                    if fwd:
                        DMA("sp", obw[:], obw_v[:, :, blk * 512:(blk + 1) * 512], ["obw_d"], ["hg_obw"])
                    for t in tiles:
                        ts = slice(t * 128, (t + 1) * 128)
                        g = gcount[0]
                        gcount[0] += 1
                        Sold, Snew = Sb2[(g + 1) % 2], Sb2[g % 2]
                        soldk, snewk = f"hg_Sb{(g + 1) % 2}", f"hg_Sb{g % 2}"
                        pk = bank16(4)
                        for h in range(4):
                            TR(pk[:, h * 128:(h + 1) * 128], Kl[:, h, ts], ident, alll + ["cst"], [("B", 4)])
                        CP("act", klt[:], pk[:, 0:512], [("B", 4)], ["hg_klt"])
                        pa = bank(5)
                        for h in range(4):
                            MM(pa[:, h * 128:(h + 1) * 128], Kd[:, h, ts], Qd[:, h, ts], True, True, allq + allk, [("B", 5)])
                        TT("dve", at[:], pa, mask, ALU.mult, [("B", 5), "cst"], ["hg_at"])
                        pS = bank(7)
                        for h in range(4):
                            MM(pS[:, h * 128:(h + 1) * 128], klt[:, h * 128:(h + 1) * 128], vsb[:, t, h * 128:(h + 1) * 128], True, True,
                               ["hg_klt", ("hg_v", b, t)], [("B", 7)])
                        po = bank(6)
                        for h in range(4):
                            MM(po[:, h * 128:(h + 1) * 128], vsb[:, t, h * 128:(h + 1) * 128], at[:, h * 128:(h + 1) * 128], True, False,
                               [("hg_v", b, t), "hg_at"], [("B", 6)])
                            MM(po[:, h * 128:(h + 1) * 128], Sold[:, h, :], Qd[:, h, ts], False, True, [soldk] + allq, [("B", 6)])
                        for h in range(4):
                            STT(St[:, h, :], St[:, h, :], ebl[:, h, t:t + 1], pS[:, h * 128:(h + 1) * 128], ALU.mult, ALU.add,
                                ["hg_S", ("hg_ebl", b, h), ("B", 7)], ["hg_S"])
                        CP("act", Snew[:], St[:], ["hg_S"], [snewk])
                        CP("act", ob[:, :, ts], po.rearrange("p (h t) -> p h t", h=4), [("B", 6)], [f"hg_o{b}"])
                    if not fwd:
                        DMA("sp", obw_v[:, :, blk * 512:(blk + 1) * 512], ob[:], [f"hg_o{b}"], ["obw_d"])
                    else:
                        TT("pool", ob[:], ob[:], obw[:], ALU.add, [f"hg_o{b}", "hg_obw"], [f"hg_o{b}"])
                        TT("pool", osq[:], ob[:], ob[:], ALU.mult, [f"hg_o{b}"], ["hg_osq"])
                        for h in range(4):
                            pr = bank(4)
                            MM(pr, ones_bf, osq[:, h, :], True, True, ["hg_osq", "cst"], [("B", 4)])
                            ACT(rs[:], pr, AF.Ln, [("B", 4)], ["hg_rs"], scale=1.0 / 128.0, bias=RMS_EPS)
                            ACT(rs[:], rs[:], AF.Exp, ["hg_rs"], ["hg_rs"], scale=-0.5)
                            TT("dve", rs[:], rs[:], ob[:, h, :], ALU.mult, ["hg_rs", f"hg_o{b}"], ["hg_rs"])
                            STT(cT[b][:, h, :], rs[:], normg[:, h:h + 1], sg2[b][:, h, :], ALU.mult, ALU.mult,
                                ["hg_rs", ("hg_sg", b, h), "normg"], [f"hg_cT{b}"])
                        DMA("sp", cT_v[:, :, blk * 512:(blk + 1) * 512], cT[b][:], [f"hg_cT{b}"], ["cT_d"])

                for st_ in streamsA(0):
                    S.merge(st_)
                for bi in range(NBLK):
                    sts = streamsA(bi + 1) if bi + 1 < NBLK else []
                    S.record()
                    stageB(bi)
                    rb_ = S.stop()
                    S.merge(*(sts + [rb_]))

        def phaseN():
            with ExitStack() as es:
                kT = sbuf(es, "na_kT", [128, 4, T], BF16)
                Vx = sbuf(es, "na_V", [128, NTILE, 8, 65], BF16)
                hT = [sbuf(es, f"na_hT{i}", [128, 8, 512], BF16) for i in range(2)]
                MSET("pool", Vx[:, :, :, 64:65], 1.0, ["na_V"])
                with ExitStack() as es1:
                    Wk = load_w(es1, "na_Wk", IN_OFF["na_k"], 512)
                    Wv = load_w(es1, "na_Wv", IN_OFF["na_v"], 512)
                    DMA("sp", hT[0][:], h0T_v[:, :, 0:512], ["h0T_d"], ["na_hT0"])
                    for blk in range(NBLK):
                        b = blk % 2
                        hk = f"na_hT{b}"
                        if blk + 1 < NBLK:
                            DMA("sp", hT[1 - b][:], h0T_v[:, :, (blk + 1) * 512:(blk + 2) * 512], ["h0T_d"], [f"na_hT{1 - b}"])
                        for p in range(4):
                            pk = bank(p % 2)
                            for k in range(8):
                                MM(pk, Wk[:, k, p * 128:(p + 1) * 128], hT[b][:, k, :], k == 0, k == 7, [hk, "na_Wk"], [("B", p % 2)])
                            CP("act" if p % 2 == 0 else "dve", kT[:, p, blk * 512:(blk + 1) * 512], pk, [("B", p % 2)], ["na_kT"])
                        for t in range(4):
                            pv = bank(2 + t % 2)
                            for k in range(8):
                                MM(pv, hT[b][:, k, t * 128:(t + 1) * 128], Wv[:, k, :], k == 0, k == 7, [hk, "na_Wv"], [("B", 2 + t % 2)])
                            CP("dve" if t % 2 == 0 else "act", Vx[:, blk * 4 + t, :, 0:64], pv.rearrange("p (h d) -> p h d", h=8),
                               [("B", 2 + t % 2)], ["na_V"])
                S.barrier()
                with ExitStack() as es2:
                    tts = sbuf(es2, "na_tt", [128, 16, 512], BF16)
                    DMA("sp", tts[:], tt_d, [], ["na_tt"])
                    Wq = load_w(es2, "na_Wq", IN_OFF["na_q"], 512)
                    qbd = sbuf(es2, "na_qbd", [128, 4, 8, 128], BF16)
                    Eb = [sbuf(es2, f"na_E{i}", [128, 1280], BF16) for i in range(2)]
                    rec = sbuf(es2, "na_rec", [128, 4], F32)
                    asb = [sbuf(es2, f"na_a{i}", [128, 512], BF16) for i in range(2)]
                    aT1 = sbuf(es2, "na_aT", [128, 4, 512], BF16)
                    aT = [aT1, aT1]
                    MSET("pool", qbd[:], 0.0, ["na_qbd"])
                    DMA("sp", hT[0][:], h0T_v[:, :, 0:512], ["h0T_d"], ["na_hT0"])

                    def tiles_of(r):
                        rs = min(max(r - 4, 0), 120)
                        if rs % 2 == 0:
                            return [(rs // 2 + i, 2 * (rs // 2 + i) - r + 7) for i in range(4)]
                        m0 = (rs - 1) // 2
                        return [(m0, 14)] + [(m0 + i, 2 * (m0 + i) - r + 7) for i in range(1, 4)] + [(m0 + 4, 15)]

                    def qk(u, rl):
                        r, h2 = u // 2, u % 2
                        tl = tiles_of(r)
                        nch = len(tl)
                        sbk = PS[:, (u % 2) * 1536:(u % 2) * 1536 + 1536]
                        keys = [("B", (u % 2) * 3 + i) for i in range(3)]
                        for pi in range(2):
                            p = 2 * h2 + pi
                            for ci, (m, slab) in enumerate(tl):
                                slot = pi * nch + ci
                                o = sbk[:, slot * 128:(slot + 1) * 128]
                                MM(o, kT[:, p, m * 128:(m + 1) * 128], qbd[:, p, rl, :], True, False, ["na_kT", "na_qbd"], keys)
                                MM(o, ident, tts[:, slab, p * 128:(p + 1) * 128], False, True, ["cst", "na_tt"], keys)

                    def rest(u):
                        r, h2 = u // 2, u % 2
                        tl = tiles_of(r)
                        nch = len(tl)
                        sbk = PS[:, (u % 2) * 1536:(u % 2) * 1536 + 1536]
                        keys = [("B", (u % 2) * 3 + i) for i in range(3)]
                        E = Eb[u % 2]
                        ek = f"na_E{u % 2}"
                        n = 2 * nch * 128
                        ACT(E[:, 0:n], sbk[:, 0:n], AF.Exp, keys, [ek])
                        par = r % 2
                        prt = slice(par * 64, (par + 1) * 64)
                        pO = bank(6)
                        for pi in range(2):
                            for hh in range(2):
                                hq = pi * 2 + hh
                                head = 4 * h2 + hq
                                for ci, (m, slab) in enumerate(tl):
                                    c0 = (pi * nch + ci) * 128 + hh * 64
                                    MM(pO[prt, hq * 65:(hq + 1) * 65], E[:, c0:c0 + 64], Vx[:, m, head, :], ci == 0, ci == nch - 1,
                                       [ek, "na_V"], [("B", 6)])
                        pov = pO[prt, 0:260].rearrange("p (h d) -> p h d", d=65)
                        S.add("dve", lambda e: e.reciprocal(out=rec[prt, :], in_=pov[:, :, 64]), r=[("B", 6)], w=["na_rec"])
                        tb = (r // 2) % 2
                        TT("dve", asb[tb][prt, h2 * 256:(h2 + 1) * 256].rearrange("p (h d) -> p h d", d=64), pov[:, :, 0:64],
                           rec[prt, :].unsqueeze(2).to_broadcast([64, 4, 64]), ALU.mult, [("B", 6), "na_rec"], [f"na_a{tb}"])

                    for blk in range(NBLK):
                        b = blk % 2
                        hk = f"na_hT{b}"
                        if blk + 1 < NBLK:
                            DMA("sp", hT[1 - b][:], h0T_v[:, :, (blk + 1) * 512:(blk + 2) * 512], ["h0T_d"], [f"na_hT{1 - b}"])
                        for p in range(4):
                            pq = bank(7)
                            for k in range(8):
                                MM(pq, Wq[:, k, p * 128:(p + 1) * 128], hT[b][:, k, :], k == 0, k == 7, [hk, "na_Wq"], [("B", 7)])
                            ACT(qbd[0:64, p, :, 0:64], pq[0:64, :].rearrange("p (r c) -> p r c", r=8), AF.Copy, [("B", 7)], ["na_qbd"], scale=0.125)
                            TS("dve", qbd[64:128, p, :, 64:128], pq[64:128, :].rearrange("p (r c) -> p r c", r=8), 0.125, None, ALU.mult, None,
                               [("B", 7)], ["na_qbd"])
                        u0 = blk * 16
                        qk(u0, 0)
                        for ul in range(16):
                            u = u0 + ul
                            if ul + 1 < 16:
                                qk(u + 1, (ul + 1) // 2)
                            rest(u)
                            if ul % 4 == 3:
                                t = ul // 4
                                tb = ((u // 2) // 2) % 2
                                pT = bank16(7)
                                for c in range(4):
                                    TR(pT[:, c * 128:(c + 1) * 128], asb[tb][:, c * 128:(c + 1) * 128], ident, [f"na_a{tb}", "cst"], [("B", 7)])
                                CP("act", aT[b][:, :, t * 128:(t + 1) * 128], pT[:, 0:512].rearrange("p (c t) -> p c t", c=4), [("B", 7)], ["na_aT"])
                        DMA("sp", aT_v[:, :, blk * 512:(blk + 1) * 512], aT[b][:], ["na_aT"], ["aT_d"])

        def phaseM():
            with ExitStack() as es:
                ba_bf = sbuf(es, "m_ba", [1, D], BF16)
                with ExitStack() as est:
                    tmpb = sbuf(est, "m_tmpb", [1, D], F32)
                    DMA("sp", tmpb[:], lnp_d[0:1, 1, :], [], ["m_tmpb"])
                    TS("dve", tmpb[:], tmpb[:], ALPHA, None, ALU.mult, None, ["m_tmpb"], ["m_tmpb"])
                    CP("dve", ba_bf[:], tmpb[:], ["m_tmpb"], ["m_ba"])
                S.barrier()
                lnp = sbuf(es, "m_lnp", [128, 3, D], F32)
                DMA("sp", lnp[:, 0, :], lnp_d[:, 0, :], [], ["m_lnp"])
                DMA("sp", lnp[:, 1:3, :], lnp_d[:, 2:4, :], [], ["m_lnp"])
                TS("pool", lnp[:, 0, :], lnp[:, 0, :], ALPHA, None, ALU.mult, None, ["m_lnp"], ["m_lnp"])
                Wga = load_w(es, "m_Wga", IN_OFF["gate_a"], 1024)
                Wgb = load_w(es, "m_Wgb", IN_OFF["gate_b"], 1024)
                Wa = sbuf(es, "m_Wa", [128, 4, D], BF16)
                Wb = sbuf(es, "m_Wb", [128, 4, D], BF16)
                Wo = sbuf(es, "m_Wo", [128, 8, D], BF16)
                for k in range(4):
                    DMA("pool", Wa[:, k, :], w_pa_d[k * 128:(k + 1) * 128, :], [], ["m_Wa"])
                    DMA("pool", Wb[:, k, :], w_pb_d[k * 128:(k + 1) * 128, :], [], ["m_Wb"])
                for k in range(8):
                    DMA("pool", Wo[:, k, :], w_out_d[k * 128:(k + 1) * 128, :], [], ["m_Wo"])
                wr = sbuf(es, "m_wr", [128, 8, 36], F32)
                rb = sbuf(es, "m_rb", [128, 4, 36], F32)
                DMA("sp", wr[:], wr_d, [], ["m_wr"])
                DMA("sp", rb[:], rb_d, [], ["m_rb"])
                hT = [sbuf(es, f"m_hT{i}", [128, 8, 512], BF16) for i in range(2)]
                aTs1 = sbuf(es, "m_aT", [128, 4, 512], BF16)
                aTs = [aTs1, aTs1]
                cTs1 = sbuf(es, "m_cT", [128, 4, 512], BF16)
                cTs = [cTs1, cTs1]
                xt = sbuf(es, "m_xt", [128, 4, D], F32)
                sgb_ = sbuf(es, "m_sgb", [128, 512], F32)
                sg4 = [(sbuf(es, f"m_sga{i}", [128, 512], F32), sgb_) for i in range(2)]
                m1 = sbuf(es, "m_m1", [128, 512], F32)
                m2 = sbuf(es, "m_m2", [128, 512], F32)
                mT = sbuf(es, "m_mT", [128, 8, 512], BF16)
                zz = [sbuf(es, f"m_z{i}", [128, 4, D], F32) for i in range(2)]
                hx2 = [sbuf(es, f"m_hx{i}", [128, D], F32) for i in range(2)]
                h1b = sbuf(es, "m_h1b", [128, 4, D], BF16)
                h1T = sbuf(es, "m_h1T", [128, 4, 128], F32)
                st1 = sbuf(es, "m_st1", [128, 4, 12], F32)
                mv1 = sbuf(es, "m_mv1", [128, 4, 2], F32)
                rs1 = sbuf(es, "m_rs1", [128, 4], F32)
                nm1 = sbuf(es, "m_nm1", [128, 4], F32)
                L = sbuf(es, "m_L", [128, 4, 36], F32)
                gmax = sbuf(es, "m_gmax", [128, 4], F32)
                gm = sbuf(es, "m_gm", [128, 4, 4], F32)
                gd = sbuf(es, "m_gd", [128, 4, 4], F32)
                gw = sbuf(es, "m_gw", [128, 4], F32)
                EM = sbuf(es, "m_EM", [128, 4, 32], F32)
                EM2 = sbuf(es, "m_EM2", [128, 4, 32], F32)
                top1 = sbuf(es, "m_top1", [128, 4], F32)
                top2 = sbuf(es, "m_top2", [128, 4], F32)
                oh = [sbuf(es, f"m_oh{i}", [128, 4, 32], F32) for i in range(2)]
                cnt = sbuf(es, "m_cnt", [128, 4, 32], BF16)
                rank = sbuf(es, "m_rank", [128, 4, 32], F32)
                slot = sbuf(es, "m_slot", [128, 4, 32], F32)
                tmp = sbuf(es, "m_tmp", [128, 4, 32], F32)
                tot = sbuf(es, "m_tot", [128, 32], F32)
                sm = sbuf(es, "m_sm", [128, 8, 4], F32)
                MSET("dve", tot[:], 0.0, ["m_tot"])
                if "fakeidx" in dbg:
                    fake_ix = sbuf(es, "m_fake", [128, NTILE, 2], I32)
                    S.add("pool", lambda e: e.iota(fake_ix[:].rearrange("p n k -> p (n k)"), pattern=[[128, 128]], base=0, channel_multiplier=1),
                          w=["m_fake"])
                ident32 = cst32[:, 2, 0:128]
                ustr = cst[:, 3, 0:128]
                ebase = cst32[:, 2, 128:160]
                pidx = cst32[:, 2, 160:161]

                def loads_ac(blk):
                    DMA("sp", aTs[0][:], aT_v[:, :, blk * 512:(blk + 1) * 512], ["aT_d"], ["m_aT"])
                    DMA("sp", cTs[0][:], cT_v[:, :, blk * 512:(blk + 1) * 512], ["cT_d"], ["m_cT"])

                def loads(blk, b):
                    DMA("sp", hT[b][:], h0T_v[:, :, blk * 512:(blk + 1) * 512], ["h0T_d"], [f"m_hT{b}"])

                def stageA(blk):
                    b = blk % 2
                    z = zz[b]
                    if blk + 1 < NBLK:
                        loads(blk + 1, 1 - b)
                    rs0 = xstat[:, blk * 4:(blk + 1) * 4, 0]
                    nm0 = xstat[:, blk * 4:(blk + 1) * 4, 1]
                    for c in range(8):
                        ga_, gb_ = 0, 1
                        pa_, pb_ = (2, 3) if c % 2 == 0 else (4, 5)
                        cs = slice(c * 128, (c + 1) * 128)
                        for k in range(8):
                            MM(bank(ga_), Wga[:, k, cs], hT[b][:, k, :], k == 0, k == 7, [f"m_hT{b}", "m_Wga"], [("B", ga_)])
                        for k in range(4):
                            MM(bank(pa_), Wa[:, k, cs], aTs[b][:, k, :], k == 0, k == 3, ["m_aT", "m_Wa"], [("B", pa_)])
                        for k in range(8):
                            MM(bank(gb_), Wgb[:, k, cs], hT[b][:, k, :], k == 0, k == 7, [f"m_hT{b}", "m_Wgb"], [("B", gb_)])
                        for k in range(4):
                            MM(bank(pb_), Wb[:, k, cs], cTs[b][:, k, :], k == 0, k == 3, ["m_cT", "m_Wb"], [("B", pb_)])
                        sga, sgb = sg4[c % 2]
                        ACT(sga[:], bank(ga_), AF.Sigmoid, [("B", ga_)], [f"m_sga{c % 2}"])
                        ACT(sgb[:], bank(gb_), AF.Sigmoid, [("B", gb_)], ["m_sgb"])
                        TT("dve", m1[:], bank(pa_), sga[:], ALU.mult, [("B", pa_), f"m_sga{c % 2}"], ["m_m1"])
                        TT("dve", m2[:], bank(pb_), sgb[:], ALU.mult, [("B", pb_), "m_sgb"], ["m_m2"])
                        TT("pool", mT[:, c, :], m1[:], m2[:], ALU.add, ["m_m1", "m_m2"], [("m_mT", c)])
                    if blk + 1 < NBLK:
                        loads_ac(blk + 1)
                    mtk = [("m_mT", c) for c in range(8)]
                    for t in range(4):
                        hxx = hx2[t % 2]
                        hk_ = f"m_hx{t % 2}"
                        ACT(hxx[:], xt[:, t, :], AF.Identity, ["m_xt", "xstat"], [hk_], scale=rs0[:, t:t + 1], bias=nm0[:, t:t + 1])
                        TT("dve", hxx[:], hxx[:], lnp[:, 0, :], ALU.mult, [hk_, "m_lnp"], [hk_])
                        for n in range(2):
                            o = (2, 3, 4, 5)[(t * 2 + n) % 4]
                            ns = slice(n * 512, (n + 1) * 512)
                            MM(bank(o), cst[0:1, 4, 0:128], ba_bf[0:1, ns], True, False, ["cst", "m_ba"], [("B", o)])
                            for k in range(8):
                                MM(bank(o), mT[:, k, t * 128:(t + 1) * 128], Wo[:, k, ns], False, k == 7, mtk + ["m_Wo"], [("B", o)])
                            TT("dve", z[:, t, ns], hxx[:, ns], bank(o), ALU.add, [hk_, ("B", o)], [("m_z", b, t)])
                    if blk + 1 < NBLK:
                        DMA("sp", xt[:], x_v[blk + 1], [], ["m_xt"])

                def stageB(blk):
                    b = blk % 2
                    z = zz[b]
                    zk = [("m_z", b, t) for t in range(4)]
                    for t in range(4):
                        for c in range(2):
                            S.add("dve", lambda e, t=t, c=c: e.bn_stats(out=st1[:, t, c * 6:(c + 1) * 6], in_=z[:, t, c * 512:(c + 1) * 512]),
                                  r=[("m_z", b, t)], w=["m1st"])
                        S.add("dve", lambda e, t=t: e.bn_aggr(out=mv1[:, t, :], in_=st1[:, t, :]), r=["m1st"], w=["m1mv"])
                    ACT(rs1[:], mv1[:, :, 1], AF.Ln, ["m1mv"], ["m1rs"], bias=LN_EPS)
                    ACT(rs1[:], rs1[:], AF.Exp, ["m1rs"], ["m1rs"], scale=-0.5)
                    STT(nm1[:], mv1[:, :, 0], -1.0, rs1[:], ALU.mult, ALU.mult, ["m1mv", "m1rs"], ["m1nm"])
                    for t in range(4):
                        ACT(z[:, t, :], z[:, t, :], AF.Identity, [("m_z", b, t), "m1rs", "m1nm"], [("m_z", b, t)], scale=rs1[:, t:t + 1], bias=nm1[:, t:t + 1])
                        TT("dve", z[:, t, :], z[:, t, :], lnp[:, 1, :], ALU.mult, [("m_z", b, t), "m_lnp"], [("m_z", b, t)])
                        TT("pool", z[:, t, :], z[:, t, :], lnp[:, 2, :], ALU.add, [("m_z", b, t), "m_lnp"], [("m_z", b, t)])
                        CP("act", h1b[:, t, :], z[:, t, :], [("m_z", b, t)], [("m_h1b", t)])
                    DMA("sp", h1_v[blk], z[:], zk, ["h1_d"])
                    pL = bank(7)
                    for t in range(4):
                        for g in range(2):
                            for kk_ in range(4):
                                k = g * 4 + kk_
                                TR(bank(6)[:, kk_ * 128:(kk_ + 1) * 128], z[:, t, k * 128:(k + 1) * 128], ident32, [("m_z", b, t), "cst32"], [("B", 6)])
                            CP("act" if g == 0 else "dve", h1T[:, 0:4, :], bank(6).rearrange("p (k t) -> p k t", k=4),
                               [("B", 6)], ["m_h1T"])
                            for kk_ in range(4):
                                k = g * 4 + kk_
                                MM(pL[:, t * 36:(t + 1) * 36], h1T[:, kk_, :], wr[:, k, :], k == 0, k == 7, ["m_h1T", "m_wr"], [("B", 7)])
                    TT("dve", L[:], pL[:, 0:144].rearrange("p (t n) -> p t n", n=36), rb[:], ALU.add, [("B", 7), "m_rb"], ["m_L"])
                    GL = L[:, :, 0:4]
                    EL = L[:, :, 4:36].rearrange("p t (g e) -> p t g e", g=4)
                    S.add("dve", lambda e: e.tensor_reduce(out=gmax[:], in_=GL, axis=AX.X, op=ALU.max), r=["m_L"], w=["m_gmax"])
                    TT("dve", gm[:], GL, gmax[:].unsqueeze(2).to_broadcast([128, 4, 4]), ALU.is_equal, ["m_L", "m_gmax"], ["m_gm"])
                    TT("dve", gd[:], GL, gmax[:].unsqueeze(2).to_broadcast([128, 4, 4]), ALU.subtract, ["m_L", "m_gmax"], ["m_gd"])
                    ACT(gd[:], gd[:], AF.Exp, ["m_gd"], ["m_gd"])
                    S.add("dve", lambda e: e.tensor_reduce(out=gw[:], in_=gd[:], axis=AX.X, op=ALU.add), r=["m_gd"], w=["m_gw"])
                    S.add("dve", lambda e: e.reciprocal(out=gw[:], in_=gw[:]), r=["m_gw"], w=["m_gw"])
                    TS("dve", gm[:], gm[:], 1e9, -1e9, ALU.mult, ALU.add, ["m_gm"], ["m_gm"])
                    TT("dve", EM[:].rearrange("p t (g e) -> p t g e", g=4), EL, gm[:].unsqueeze(3).to_broadcast([128, 4, 4, 8]), ALU.add,
                       ["m_L", "m_gm"], ["m_EM"])
                    S.add("dve", lambda e: e.tensor_reduce(out=top1[:], in_=EM[:], axis=AX.X, op=ALU.max), r=["m_EM"], w=["m_top1"])
                    TT("dve", oh[0][:], EM[:], top1[:].unsqueeze(2).to_broadcast([128, 4, 32]), ALU.is_equal, ["m_EM", "m_top1"], ["m_oh0"])
                    STT(EM2[:], oh[0][:], -1e9, EM[:], ALU.mult, ALU.add, ["m_oh0", "m_EM"], ["m_EM2"])
                    S.add("dve", lambda e: e.tensor_reduce(out=top2[:], in_=EM2[:], axis=AX.X, op=ALU.max), r=["m_EM2"], w=["m_top2"])
                    TT("dve", oh[1][:], EM2[:], top2[:].unsqueeze(2).to_broadcast([128, 4, 32]), ALU.is_equal, ["m_EM2", "m_top2"], ["m_oh1"])
                    w1 = wall[:, blk * 4:(blk + 1) * 4, 0]
                    w2 = wall[:, blk * 4:(blk + 1) * 4, 1]
                    TT("dve", sm[:, 0, :], top2[:], top1[:], ALU.subtract, ["m_top1", "m_top2"], ["m_sm0"])
                    ACT(sm[:, 0, :], sm[:, 0, :], AF.Exp, ["m_sm0"], ["m_sm0"])
                    TS("dve", sm[:, 0, :], sm[:, 0, :], 1.0, None, ALU.add, None, ["m_sm0"], ["m_sm0"])
                    S.add("dve", lambda e: e.reciprocal(out=sm[:, 1, :], in_=sm[:, 0, :]), r=["m_sm0"], w=["m_sm1"])
                    TT("dve", w1, sm[:, 1, :], gw[:], ALU.mult, ["m_sm1", "m_gw"], [("wall", blk)])
                    TT("dve", w2, gw[:], w1, ALU.subtract, ["m_gw", ("wall", blk)], [("wall", blk)])
                    TT("dve", cnt[:], oh[0][:], oh[1][:], ALU.add, ["m_oh0", "m_oh1"], ["m_cnt"])
                    pR = bank(7)[:, 256:512]
                    for t in range(4):
                        MM(pR[:, t * 32:(t + 1) * 32], ustr, cnt[:, t, :], True, t == 0, ["m_cnt", "cst"], [("B", 7)])
                        for t2 in range(t):
                            MM(pR[:, t * 32:(t + 1) * 32], ones_bf, cnt[:, t2, :], False, t2 == t - 1, ["m_cnt", "cst"], [("B", 7)])
                    for t in range(4):
                        MM(pR[:, 128:160], ones_bf, cnt[:, t, :], t == 0, t == 3, ["m_cnt", "cst"], [("B", 7)])
                    TT("dve", rank[:], pR[:, 0:128].rearrange("p (t n) -> p t n", n=32), tot[:].unsqueeze(1).to_broadcast([128, 4, 32]), ALU.add,
                       [("B", 7), "m_tot"], ["m_rank"])
                    TT("dve", tot[:], tot[:], pR[:, 128:160], ALU.add, ["m_tot", ("B", 7)], ["m_tot"])
                    TT("dve", slot[:], rank[:], ebase.unsqueeze(1).to_broadcast([128, 4, 32]), ALU.add, ["m_rank", "cst32"], ["m_slot"])
                    for kx in range(2):
                        ohk = f"m_oh{kx}"
                        TT("dve", tmp[:], oh[kx][:], slot[:], ALU.mult, [ohk, "m_slot"], ["m_tmp"])
                        S.add("dve", lambda e: e.tensor_reduce(out=sm[:, 2, :], in_=tmp[:], axis=AX.X, op=ALU.add), r=["m_tmp"], w=["m_sm2"])
                        TT("dve", tmp[:], oh[kx][:], rank[:], ALU.mult, [ohk, "m_rank"], ["m_tmp"])
                        S.add("dve", lambda e: e.tensor_reduce(out=sm[:, 3, :], in_=tmp[:], axis=AX.X, op=ALU.add), r=["m_tmp"], w=["m_sm3"])
                        TS("dve", sm[:, 3, :], sm[:, 3, :], float(CAP), None, ALU.is_ge, None, ["m_sm3"], ["m_sm3"])
                        TS("dve", sm[:, 4, :], sm[:, 2, :], -1.0, pidx, ALU.mult, ALU.add, ["m_sm2", "cst32"], ["m_sm4"])
                        TT("dve", sm[:, 4, :], sm[:, 4, :], sm[:, 3, :], ALU.mult, ["m_sm4", "m_sm3"], ["m_sm4"])
                        TT("dve", sm[:, 2, :], sm[:, 2, :], sm[:, 4, :], ALU.add, ["m_sm2", "m_sm4"], ["m_sm2"])
                        CP("pool", posall[:, blk * 4:(blk + 1) * 4, kx], sm[:, 2, :], ["m_sm2"], [("posall", blk)])
                    for t in range(4):
                        if "noscatter" in dbg:
                            break
                        for kx in range(2):
                            ixap = posall[:, blk * 4 + t, kx:kx + 1]
                            if "fakeidx" in dbg:
                                ixap = fake_ix[:, blk * 4 + t, kx:kx + 1]
                            S.add("pool", lambda e, t=t, kx=kx, ixap=ixap: e.indirect_dma_start(
                                out=xbuf_d, out_offset=bass.IndirectOffsetOnAxis(ap=ixap, axis=0),
                                in_=h1b[:, t, :], in_offset=None), r=[("m_h1b", t), ("posall", blk)], w=["xbuf_d"], dma=True)

                loads(0, 0)
                loads_ac(0)
                DMA("sp", xt[:], x_v[0], [], ["m_xt"])
                stageA(0)
                for blk in range(NBLK):
                    ra = None
                    if blk + 1 < NBLK:
                        S.record()
                        stageA(blk + 1)
                        ra = S.stop()
                    S.record()
                    stageB(blk)
                    rb_ = S.stop()
                    S.merge(ra, rb_)

        def dump_routing():
            pos_d = nc.dram_tensor("pos_d", [128, NTILE * 2], I32, kind="ExternalOutput").ap()
            wall_d = nc.dram_tensor("wall_d", [128, NTILE * 2], F32, kind="ExternalOutput").ap()
            allk = [("posall", b) for b in range(NBLK)]
            allw = [("wall", b) for b in range(NBLK)]
            DMA("sp", pos_d, posall[:].rearrange("p n k -> p (n k)"), allk, ["pos_d"])
            DMA("sp", wall_d, wall[:].rearrange("p n k -> p (n k)"), allw, ["wall_d"])

        def phaseE():
            with ExitStack() as es:
                wg = [sbuf(es, f"e_wg{i}", [128, 8, 512], BF16) for i in range(2)]
                wu = [sbuf(es, f"e_wu{i}", [128, 8, 512], BF16) for i in range(2)]
                wd = [sbuf(es, f"e_wd{i}", [128, 4, D], BF16) for i in range(2)]
                xs = [sbuf(es, f"e_xs{i}", [128, 6, D], BF16) for i in range(2)]
                xT2 = [sbuf(es, f"e_xT{i}", [128, 8, CAP], BF16) for i in range(2)]
                sg = sbuf(es, "e_sg", [128, 384], F32)
                tg = sbuf(es, "e_tg", [128, 384], F32)
                hTt = sbuf(es, "e_hT", [128, 4, CAP], BF16)
                ysb = [sbuf(es, f"e_y{i}", [128, D], F32) for i in range(2)]

                def wload(e, b):
                    DMA("pool", wg[b][:], w_gate_d[e].rearrange("(k p) n -> p k n", p=128), [], [f"e_wg{b}"])
                    DMA("pool", wu[b][:], w_up_d[e].rearrange("(k p) n -> p k n", p=128), [], [f"e_wu{b}"])
                    DMA("pool", wd[b][:], w_down_d[e].rearrange("(k p) n -> p k n", p=128), [], [f"e_wd{b}"])
                    DMA("sp", xs[b][:], xbuf_d[e * CAP:(e + 1) * CAP, :].rearrange("(s p) d -> p s d", p=128), ["xbuf_d"], [f"e_xs{b}"])

                def stageA(e):
                    b = e % 2
                    for k in range(8):
                        pb = bank16(k % 2)
                        for s_ in range(6):
                            TR(pb[:, s_ * 128:(s_ + 1) * 128], xs[b][:, s_, k * 128:(k + 1) * 128], ident, [f"e_xs{b}", "cst"], [("B", k % 2)])
                        CP("act" if k % 2 == 0 else "dve", xT2[b][:, k, :], pb[:, 0:CAP], [("B", k % 2)], [("e_xT", b, k)])

                def stageB(e):
                    b = e % 2
                    xT = xT2[b]
                    xtk = [("e_xT", b, k) for k in range(8)]
                    for m in range(4):
                        for half in range(2):
                            u = m * 2 + half
                            o = 2 + 2 * (u % 2)
                            hs = slice(half * 384, (half + 1) * 384)
                            ms = slice(m * 128, (m + 1) * 128)
                            for k in range(8):
                                MM(bank(o)[:, 0:384], wg[b][:, k, ms], xT[:, k, hs], k == 0, k == 7, xtk + [f"e_wg{b}"], [("B", o)])
                            for k in range(8):
                                MM(bank(o + 1)[:, 0:384], wu[b][:, k, ms], xT[:, k, hs], k == 0, k == 7, xtk + [f"e_wu{b}"], [("B", o + 1)])
                            ACT(sg[:], bank(o)[:, 0:384], AF.Sigmoid, [("B", o)], ["e_sg"])
                            TT("dve", tg[:], bank(o)[:, 0:384], sg[:], ALU.mult, [("B", o), "e_sg"], ["e_tg"])
                            TT("dve", hTt[:, m, hs], bank(o + 1)[:, 0:384], tg[:], ALU.mult, [("B", o + 1), "e_tg"], [("e_hT", m)])
                    htk = [("e_hT", m) for m in range(4)]
                    for s_ in range(6):
                        yb = (e * 6 + s_) % 2
                        for n in range(2):
                            for m in range(4):
                                MM(bank(6 + n), hTt[:, m, s_ * 128:(s_ + 1) * 128], wd[b][:, m, n * 512:(n + 1) * 512], m == 0, m == 3,
                                   htk + [f"e_wd{b}"], [("B", 6 + n)])
                            CP("act" if n == 0 else "dve", ysb[yb][:, n * 512:(n + 1) * 512], bank(6 + n), [("B", 6 + n)], [f"e_y{yb}"])
                        DMA("sp", ybuf_d[e * CAP + s_ * 128:e * CAP + (s_ + 1) * 128, :], ysb[yb][:], [f"e_y{yb}"], ["ybuf_d"])

                wload(0, 0)
                for e in range(32):
                    if e + 1 < 32:
                        wload(e + 1, (e + 1) % 2)
                    stageA(e)
                    stageB(e)

        def phaseG():
            with ExitStack() as es:
                lnp = sbuf(es, "g_lnp", [128, 2, D], F32)
                DMA("sp", lnp[:], lnp_d[:, 4:6, :], [], ["g_lnp"])
                y0 = [sbuf(es, f"g_y0{i}", [128, D], F32) for i in range(2)]
                y1 = [sbuf(es, f"g_y1{i}", [128, D], F32) for i in range(2)]
                h1t = [sbuf(es, f"g_h1{i}", [128, D], F32) for i in range(2)]
                zz = [sbuf(es, f"g_z{i}", [128, 1, D], F32) for i in range(2)]
                st = sbuf(es, "g_st", [128, 1, 12], F32)
                mv = sbuf(es, "g_mv", [128, 1, 2], F32)
                rs2 = [sbuf(es, f"g_rs{i}", [128, 1], F32) for i in range(2)]
                nm2 = [sbuf(es, f"g_nm{i}", [128, 1], F32) for i in range(2)]

                def gl(n, b):
                    S.add("pool", lambda e: e.indirect_dma_start(out=y0[b][:], out_offset=None, in_=ybuf_d,
                                                                 in_offset=bass.IndirectOffsetOnAxis(ap=posall[:, n, 0:1], axis=0)),
                          r=["ybuf_d"], w=[f"g_y0{b}"], dma=True)
                    S.add("pool", lambda e: e.indirect_dma_start(out=y1[b][:], out_offset=None, in_=ybuf_d,
                                                                 in_offset=bass.IndirectOffsetOnAxis(ap=posall[:, n, 1:2], axis=0)),
                          r=["ybuf_d"], w=[f"g_y1{b}"], dma=True)
                    DMA("sp", h1t[b][:], h1_tv[n], ["h1_d"], [f"g_h1{b}"])

                def stageA(n):
                    b = n % 2
                    zt = zz[b]
                    zk = f"g_z{b}"
                    TS("dve", zt[:, 0, :], y0[b][:], wall[:, n, 0:1], None, ALU.mult, None, [f"g_y0{b}"], [zk, zk + "a", zk + "b"])
                    STT(zt[:, 0, :], y1[b][:], wall[:, n, 1:2], zt[:, 0, :], ALU.mult, ALU.add, [f"g_y1{b}", zk], [zk])
                    STT(zt[:, 0, :], h1t[b][:], ALPHA, zt[:, 0, :], ALU.mult, ALU.add, [f"g_h1{b}", zk], [zk])
                    ln_stats(f"g{b}", zt, zk, st, mv, rs2[b], nm2[b], tiles=1)

                def stageB(n):
                    b = n % 2
                    zt = zz[b]
                    zk = f"g_z{b}"
                    ACT(zt[:, 0, :], zt[:, 0, :], AF.Identity, [zk, f"g{b}rs", f"g{b}nm"], [zk], scale=rs2[b][:, 0:1], bias=nm2[b][:, 0:1])
                    TT("dve", zt[:, 0, :], zt[:, 0, :], lnp[:, 0, :], ALU.mult, [zk, "g_lnp"], [zk])
                    TT("pool", zt[:, 0, 0:512], zt[:, 0, 0:512], lnp[:, 1, 0:512], ALU.add, [zk, "g_lnp"], [zk + "a"])
                    TT("dve", zt[:, 0, 512:1024], zt[:, 0, 512:1024], lnp[:, 1, 512:1024], ALU.add, [zk, "g_lnp"], [zk + "b"])
                    DMA("sp", out_v[n], zt[:, 0, :], [zk, zk + "a", zk + "b"], ["out"])

                gl(0, 0)
                for n in range(NTILE):
                    if n + 1 < NTILE:
                        gl(n + 1, (n + 1) % 2)
                    stageA(n)
                    stageB(n)

        for ph in phases:
            if ph == "0":
                phase0()
            elif ph == "B":
                hgrn_phase(1)
            elif ph == "F":
                hgrn_phase(0)
            elif ph == "N":
                phaseN()
            elif ph == "M":
                phaseM()
                if "pos_d" in dbg:
                    dump_routing()
            elif ph == "E":
                phaseE()
            elif ph == "G":
                phaseG()
            S.barrier()
        S.emit(nc)
    return nc


def host_consts():
    bf = ml_dtypes.bfloat16
    cst = np.zeros((128, 5, 512), np.float32)
    eye = np.eye(128, dtype=np.float32)
    s = np.arange(128)[:, None]
    t = np.arange(128)[None, :]
    cst[:, 0, :] = np.tile(eye, (1, 4))
    cst[:, 1, :] = np.tile((s <= t).astype(np.float32), (1, 4))
    cst[:, 2, :] = np.tile((s >= t).astype(np.float32), (1, 4))
    cst[:, 3, :] = np.tile((s < t).astype(np.float32), (1, 4))
    cst[:, 4, :] = 1.0
    cst32 = np.zeros((128, 3, 512), np.float32)
    tt = np.arange(512)
    cst32[:, 0, :] = (tt % 128 != 0).astype(np.float32)[None, :]
    cst32[:, 1, :] = (tt % 128 != 127).astype(np.float32)[None, :]
    cst32[:, 2, 0:128] = eye
    cst32[:, 2, 128:160] = (np.arange(32) * CAP).astype(np.float32)[None, :]
    cst32[:, 2, 160] = 32 * CAP + np.arange(128)
    return cst.astype(bf), cst32


def host_tt(rpb):
    bf = ml_dtypes.bfloat16
    cols = np.arange(64)
    col_start = np.clip(cols - 8, 0, 48)
    c = cols[None, :]
    kc = cols[:, None]
    valid = (kc >= col_start[None, :]) & (kc < col_start[None, :] + 16)
    off = np.clip(kc - c + 15, 0, 30)
    full = np.full((15, 64, 8, 64), NEG, np.float32)
    for ro in range(15):
        for h in range(8):
            full[ro, :, h, :] = np.where(valid, rpb[h, ro][off], NEG)
    full = full.reshape(15, 64, 512)
    neg = np.full((64, 512), NEG, np.float32)
    tt = np.zeros((128, 16, 512), np.float32)
    for a in range(14):
        tt[0:64, a] = full[a]
        tt[64:128, a] = full[a + 1]
    tt[0:64, 14] = neg
    tt[64:128, 14] = full[3]
    tt[0:64, 15] = full[10]
    tt[64:128, 15] = neg
    return tt.astype(bf)


def make_in_maps(inp):
    cst, cst32 = host_consts()
    f = lambda a: np.ascontiguousarray(np.asarray(a, np.float32))
    lnp = np.stack([inp["emb_ln_g"], inp["emb_ln_b"], inp["ln1_g"][0], inp["ln1_b"][0], inp["ln2_g"][0], inp["ln2_b"][0]], 0)
    lnp = f(np.broadcast_to(lnp[None], (128, 6, D)))
    embp = f(np.concatenate([np.asarray(inp["emb_ln_g"]).reshape(8, 128).T, np.asarray(inp["emb_ln_b"]).reshape(8, 128).T], 1))
    lbraw = f(np.asarray(inp["hg_lb"]).reshape(2, 2, 4, 128).transpose(3, 0, 1, 2).reshape(128, 16))
    normg = f(np.asarray(inp["hg_norm_g"])[0].reshape(4, 128).T)
    wr = np.concatenate([np.asarray(inp["w_router_group"])[0], np.asarray(inp["w_router_expert"])[0]], 1)
    wr = f(wr.reshape(8, 128, 36).transpose(1, 0, 2))
    rb = np.concatenate([np.asarray(inp["b_router_group"])[0], np.asarray(inp["b_router_expert"])[0]], 0)
    rb = f(np.broadcast_to(rb[None, None], (128, 4, 36)))
    tt = host_tt(np.asarray(inp["na_rpb"], np.float32)[0])
    shared = dict(w_in=f(inp["w_in"][0]), w_proj_a=f(inp["w_proj_a"][0]), w_proj_b=f(inp["w_proj_b"][0]), w_out=f(inp["w_out"][0]),
                  w_gate=f(inp["w_gate"][0]), w_up=f(inp["w_up"][0]), w_down=f(inp["w_down"][0]),
                  lnp=lnp, embp=embp, lbraw=lbraw, normg=normg, wr=wr, rb=rb, tt=tt, cst=cst, cst32=cst32)
    x = np.asarray(inp["x"], np.float32)
    return [dict(shared, x=np.ascontiguousarray(x[b])) for b in range(x.shape[0])]


def kernel(**inputs):
    nc = build()
    in_maps = make_in_maps(inputs)
    res = run_bass_kernel_spmd(nc, in_maps, core_ids=list(range(8)))
    return np.stack([np.asarray(r["out"], np.float32) for r in res.results], 0)
```

```python
from contextlib import ExitStack
import numpy as np
import ml_dtypes
import concourse.bass as bass
import concourse.mybir as mybir
from concourse.bass_utils import run_bass_kernel_spmd

F32 = mybir.dt.float32
BF16 = mybir.dt.bfloat16
I32 = mybir.dt.int32
AF = mybir.ActivationFunctionType
ALU = mybir.AluOpType
AX = mybir.AxisListType

T = 8192
D = 1024
NBLK = 16
NTILE = 64
CAP = 768
NSLOT = 32 * CAP + 128
ALPHA = 2.0 ** 0.25
LN_EPS = 1e-5
RMS_EPS = 1e-6
NEG = -30000.0

ENGS = ("pe", "dve", "act", "pool", "sp")
EPOCH = 3000
NDMA_SEM = 8


class Op:
    __slots__ = ("eng", "fn", "deps", "sig", "sem", "val", "dma", "n")

    def __init__(self, eng, fn, dma):
        self.eng = eng
        self.fn = fn
        self.dma = dma
        self.deps = []
        self.sig = dma
        self.sem = None
        self.val = 0


class Sched:
    def __init__(self):
        self.q = {e: [] for e in ENGS}
        self.last_w = {}
        self.readers = {}
        self.dma_hist = {e: [] for e in ENGS}
        self.nops = 0
        self.bar = None
        self.bar_pending = set()
        self.rec = None

    def barrier(self):
        ops = []
        for e in ENGS:
            last = None
            for op in reversed(self.q[e]):
                if not op.dma:
                    last = op
                    break
            if last is not None:
                ops.append(last)
            ops.extend(self.dma_hist[e][-NDMA_SEM:])
        self.bar = ops
        self.bar_pending = set(ENGS)

    def record(self):
        self.rec = []

    def stop(self):
        r = self.rec
        self.rec = None
        return r

    def merge(self, *streams):
        streams = [st for st in streams if st]
        pos = [0] * len(streams)
        total = sum(len(st) for st in streams)
        for _ in range(total):
            best = None
            for i, st in enumerate(streams):
                if pos[i] < len(st):
                    f = (pos[i] + 0.5) / len(st)
                    if best is None or f < best[0]:
                        best = (f, i)
            i = best[1]
            self.add(*streams[i][pos[i]])
            pos[i] += 1

    def add(self, eng, fn, r=(), w=(), dma=False):
        if self.rec is not None:
            self.rec.append((eng, fn, tuple(r), tuple(w), dma))
            return None
        op = Op(eng, fn, dma)
        op.n = self.nops
        self.nops += 1
        deps = {}
        if eng in self.bar_pending:
            self.bar_pending.discard(eng)
            for p in self.bar:
                if p.dma or p.eng != eng:
                    deps[id(p)] = p
        for k in r:
            p = self.last_w.get(k)
            if p is not None and p is not op:
                if p.eng == eng and not p.dma and not dma:
                    if eng != "pe":
                        deps[id(p)] = p
                else:
                    deps[id(p)] = p
        for k in w:
            p = self.last_w.get(k)
            if p is not None and p is not op and (p.dma or dma or p.eng != eng or eng != "pe"):
                deps[id(p)] = p
            rd = self.readers.get(k)
            if rd:
                for p in rd.values():
                    if p is not op and (p.dma or dma or p.eng != eng or eng != "pe"):
                        deps[id(p)] = p
        for k in w:
            self.last_w[k] = op
            self.readers[k] = {}
        for k in r:
            d = self.readers.setdefault(k, {})
            d[("dma", op.n) if dma else eng] = op
        if dma:
            h = self.dma_hist[eng]
            if len(h) >= NDMA_SEM:
                p = h[-NDMA_SEM]
                deps[id(p)] = p
            h.append(op)
        op.deps = list(deps.values())
        for p in op.deps:
            p.sig = True
        self.q[eng].append(op)
        return op

    def emit(self, nc):
        with ExitStack() as es:
            for e in ENGS:
                cnt = 0
                sem = None
                dsem = [None] * NDMA_SEM
                dval = [0] * NDMA_SEM
                nd = 0
                ns = 0
                for op in self.q[e]:
                    if op.dma:
                        s = nd % NDMA_SEM
                        if dsem[s] is None:
                            dsem[s] = es.enter_context(nc.semaphore(f"d_{e}_{s}"))
                        dval[s] += 16
                        op.sem = dsem[s]
                        op.val = dval[s]
                        nd += 1
                    elif op.sig:
                        if sem is None or cnt >= EPOCH:
                            sem = es.enter_context(nc.semaphore(f"c_{e}_{ns}"))
                            ns += 1
                            cnt = 0
                        cnt += 1
                        op.sem = sem
                        op.val = cnt
            block = es.enter_context(nc.Block())
            engmap = {"pe": block.tensor, "dve": block.vector, "act": block.scalar,
                      "pool": block.gpsimd, "sp": block.sync}
            for e in ENGS:
                ops = self.q[e]
                if not ops:
                    continue

                def body(engine, ops=ops):
                    waited = {}
                    for op in ops:
                        for p in op.deps:
                            key = id(p.sem)
                            if waited.get(key, 0) >= p.val:
                                continue
                            waited[key] = p.val
                            engine.wait_ge(p.sem, p.val)
                        ins = op.fn(engine)
                        if op.sig:
                            ins.then_inc(op.sem, 16 if op.dma else 1)
                    last = {}
                    for op in ops:
                        if op.dma:
                            last[id(op.sem)] = op
                    for op in last.values():
                        if waited.get(id(op.sem), 0) < op.val:
                            engine.wait_ge(op.sem, op.val)

                engmap[e](body)


IN_OFF = dict(na_q=0, na_k=512, na_v=1024, hg_q=1536, hg_ff=2048, hg_fb=2560, hg_i=3072, hg_g=3584,
              gate_a=4096, gate_b=5120)


def build(dbg=(), phases="0BNFMEG"):
    nc = bass.Bass("TRN2", target_bir_lowering=False)
    S = Sched()

    def din(name, shape, dt):
        return nc.dram_tensor(name, list(shape), dt, kind="ExternalInput").ap()

    def dscr(name, shape, dt):
        kind = "ExternalOutput" if name in dbg else "Internal"
        return nc.dram_tensor(name, list(shape), dt, kind=kind).ap()

    x_d = din("x", [T, D], F32)
    w_in_d = din("w_in", [D, 6144], F32)
    w_pa_d = din("w_proj_a", [512, D], F32)
    w_pb_d = din("w_proj_b", [512, D], F32)
    w_out_d = din("w_out", [D, D], F32)
    w_gate_d = din("w_gate", [32, D, 512], F32)
    w_up_d = din("w_up", [32, D, 512], F32)
    w_down_d = din("w_down", [32, 512, D], F32)
    lnp_d = din("lnp", [128, 6, D], F32)
    embp_d = din("embp", [128, 16], F32)
    lbraw_d = din("lbraw", [128, 16], F32)
    normg_d = din("normg", [128, 4], F32)
    wr_d = din("wr", [128, 8, 36], F32)
    rb_d = din("rb", [128, 4, 36], F32)
    tt_d = din("tt", [128, 16, 512], BF16)
    cst_d = din("cst", [128, 5, 512], BF16)
    cst32_d = din("cst32", [128, 3, 512], F32)
    out_d = nc.dram_tensor("out", [T, D], F32, kind="ExternalOutput").ap()

    h0T_d = dscr("h0T_d", [D, T], BF16)
    obw_d = dscr("obw_d", [512, T], F32)
    aT_d = dscr("aT_d", [512, T], BF16)
    cT_d = dscr("cT_d", [512, T], BF16)
    h1_d = dscr("h1_d", [T, D], F32)
    xbuf_d = dscr("xbuf_d", [NSLOT, D], BF16)
    ybuf_d = dscr("ybuf_d", [NSLOT, D], F32)

    h0T_v = h0T_d.rearrange("(k p) t -> p k t", p=128)
    obw_v = obw_d.rearrange("(h p) t -> p h t", p=128)
    aT_v = aT_d.rearrange("(k p) t -> p k t", p=128)
    cT_v = cT_d.rearrange("(k p) t -> p k t", p=128)
    x_v = x_d.rearrange("(b t p) d -> b p t d", t=4, p=128)
    h1_v = h1_d.rearrange("(b t p) d -> b p t d", t=4, p=128)
    out_v = out_d.rearrange("(n p) d -> n p d", p=128)
    h1_tv = h1_d.rearrange("(n p) d -> n p d", p=128)
    w_in_v = w_in_d.rearrange("(k p) n -> p k n", p=128)

    with ExitStack() as ges:
        uid = [0]

        def sbuf(es, name, shape, dt):
            uid[0] += 1
            return es.enter_context(nc.sbuf_tensor(f"s{uid[0]}_{name}", list(shape), dt))

        PS = ges.enter_context(nc.psum_tensor("PS", [128, 4096], F32))

        def bank(i, n=1):
            return PS[:, i * 512:(i + n) * 512]

        def bank16(i):
            return PS[:, i * 512:(i + 1) * 512].bitcast(BF16)

        cst = sbuf(ges, "cst", [128, 5, 512], BF16)
        cst32 = sbuf(ges, "cst32", [128, 3, 512], F32)
        embp = sbuf(ges, "embp", [128, 16], F32)
        lbraw = sbuf(ges, "lbraw", [128, 16], F32)
        lbp = sbuf(ges, "lbp", [128, 3, 8], F32)
        normg = sbuf(ges, "normg", [128, 4], F32)
        posall = sbuf(ges, "posall", [128, NTILE, 2], I32)
        wall = sbuf(ges, "wall", [128, NTILE, 2], F32)
        xstat = sbuf(ges, "xstat", [128, NTILE, 2], F32)
        ident = cst[:, 0, 0:128]
        ones_bf = cst[:, 4, 0:128]

        def DMA(eng, out, in_, r, w):
            S.add(eng, lambda e: e.dma_start(out=out, in_=in_), r=r, w=w, dma=True)

        def MM(out, lhsT, rhs, start, stop, r, w):
            S.add("pe", lambda e: e.matmul(out, lhsT=lhsT, rhs=rhs, start=start, stop=stop), r=r, w=w)

        def TR(out, in_, idn, r, w):
            S.add("pe", lambda e: e.transpose(out, in_, idn), r=r, w=w)

        def ACT(out, in_, func, r, w, scale=1.0, bias=0.0):
            S.add("act", lambda e: e.activation(out=out, in_=in_, func=func, bias=bias, scale=scale), r=r, w=w)

        def TT(eng, out, in0, in1, op, r, w):
            S.add(eng, lambda e: e.tensor_tensor(out=out, in0=in0, in1=in1, op=op), r=r, w=w)

        def TS(eng, out, in0, s1, s2, op0, op1, r, w):
            if s2 is None:
                S.add(eng, lambda e: e.tensor_scalar(out=out, in0=in0, scalar1=s1, scalar2=None, op0=op0), r=r, w=w)
            else:
                S.add(eng, lambda e: e.tensor_scalar(out=out, in0=in0, scalar1=s1, scalar2=s2, op0=op0, op1=op1), r=r, w=w)

        def STT(out, in0, scalar, in1, op0, op1, r, w):
            S.add("dve", lambda e: e.scalar_tensor_tensor(out=out, in0=in0, scalar=scalar, in1=in1, op0=op0, op1=op1), r=r, w=w)

        def CP(eng, out, in_, r, w):
            if eng == "act":
                S.add("act", lambda e: e.activation(out=out, in_=in_, func=AF.Copy), r=r, w=w)
            else:
                S.add(eng, lambda e: e.tensor_copy(out=out, in_=in_), r=r, w=w)

        def MSET(eng, ap, val, w):
            S.add(eng, lambda e: e.memset(ap, val), w=w)

        DMA("sp", cst[:], cst_d, [], ["cst"])
        DMA("sp", cst32[:], cst32_d, [], ["cst32"])
        DMA("sp", embp[:], embp_d, [], ["embp"])
        DMA("sp", lbraw[:], lbraw_d, [], ["lbraw"])
        DMA("sp", normg[:], normg_d, [], ["normg"])
        lbr = lbraw[:].rearrange("p (d l h) -> p d l h", d=2, l=2)
        lb_v = lbp[:, 0, :].rearrange("p (d h) -> p d h", d=2)
        TT("dve", lb_v, lbr[:, :, 1, :], lbr[:, :, 0, :], ALU.subtract, ["lbraw"], ["lbp"])
        ACT(lbp[:, 0, :], lbp[:, 0, :], AF.Exp, ["lbp"], ["lbp"])
        ACT(lbp[:, 0, :], lbp[:, 0, :], AF.Ln, ["lbp"], ["lbp"], bias=1.0)
        ACT(lbp[:, 0, :], lbp[:, 0, :], AF.Exp, ["lbp"], ["lbp"], scale=-1.0)
        TS("dve", lbp[:, 1, :], lbp[:, 0, :], -1.0, 1.0, ALU.mult, ALU.add, ["lbp"], ["lbp"])
        TS("dve", lbp[:, 2, :], lbp[:, 1, :], -1.0, None, ALU.mult, None, ["lbp"], ["lbp"])

        def ln_stats(es_name, xt, xkey, stats, mv, rstd, nmr, tiles=4):
            for t in range(tiles):
                for c in range(2):
                    S.add("dve", lambda e, t=t, c=c: e.bn_stats(out=stats[:, t, c * 6:(c + 1) * 6], in_=xt[:, t, c * 512:(c + 1) * 512]),
                          r=[xkey], w=[es_name + "st"])
                S.add("dve", lambda e, t=t: e.bn_aggr(out=mv[:, t, :], in_=stats[:, t, :]), r=[es_name + "st"], w=[es_name + "mv"])
            ACT(rstd[:, 0:tiles], mv[:, 0:tiles, 1], AF.Ln, [es_name + "mv"], [es_name + "rs", "xstat"], bias=LN_EPS)
            ACT(rstd[:, 0:tiles], rstd[:, 0:tiles], AF.Exp, [es_name + "rs"], [es_name + "rs", "xstat"], scale=-0.5)
            STT(nmr[:, 0:tiles], mv[:, 0:tiles, 0], -1.0, rstd[:, 0:tiles], ALU.mult, ALU.mult, [es_name + "mv", es_name + "rs"], [es_name + "nm", "xstat"])

        def phase0():
            with ExitStack() as es:
                xt = [sbuf(es, f"p0_xt{i}", [128, 4, D], F32) for i in range(2)]
                xn2 = [sbuf(es, f"p0_xn{i}", [128, 4, D], BF16) for i in range(2)]
                hT = [sbuf(es, f"p0_hT{i}", [128, 8, 512], BF16) for i in range(2)]
                stats2 = [sbuf(es, f"p0_stats{i}", [128, 4, 12], F32) for i in range(2)]
                mv2 = [sbuf(es, f"p0_mv{i}", [128, 4, 2], F32) for i in range(2)]
                def stageA(blk):
                    b = blk % 2
                    if blk + 1 < NBLK:
                        DMA("sp", xt[1 - b][:], x_v[blk + 1], [], [f"xt{1 - b}"])
                    rstd = xstat[:, blk * 4:(blk + 1) * 4, 0]
                    nmr = xstat[:, blk * 4:(blk + 1) * 4, 1]
                    ln_stats(f"p0{b}", xt[b], f"xt{b}", stats2[b], mv2[b], rstd, nmr)
                    for t in range(4):
                        ACT(xn2[b][:, t, :], xt[b][:, t, :], AF.Identity, [f"xt{b}", f"p0{b}rs", f"p0{b}nm"], [("xn", b, t)],
                            scale=rstd[:, t:t + 1], bias=nmr[:, t:t + 1])

                def stageB(blk):
                    b = blk % 2
                    xn = xn2[b]
                    for k in range(8):
                        pb = bank16(k % 2)
                        for t in range(4):
                            TR(pb[:, t * 128:(t + 1) * 128], xn[:, t, k * 128:(k + 1) * 128], ident, [("xn", b, t), "cst"], [("B", k % 2)])
                        if k % 2 == 0:
                            ACT(hT[b][:, k, :], pb[:, 0:512], AF.Identity, [("B", k % 2), "embp"], [f"hT{b}"],
                                scale=embp[:, k:k + 1], bias=embp[:, 8 + k:9 + k])
                        else:
                            TS("dve", hT[b][:, k, :], pb[:, 0:512], embp[:, k:k + 1], embp[:, 8 + k:9 + k], ALU.mult, ALU.add,
                               [("B", k % 2), "embp"], [f"hT{b}"])
                    DMA("sp", h0T_v[:, :, blk * 512:(blk + 1) * 512], hT[b][:], [f"hT{b}"], ["h0T_d"])

                DMA("sp", xt[0][:], x_v[0], [], ["xt0"])
                stageA(0)
                for blk in range(NBLK):
                    S.record()
                    if blk + 1 < NBLK:
                        stageA(blk + 1)
                    ra = S.stop()
                    S.record()
                    stageB(blk)
                    rb_ = S.stop()
                    S.merge(ra, rb_)

        def load_w(es, name, col0, ncol):
            w = sbuf(es, name, [128, 8, ncol], BF16)
            for k in range(8):
                DMA("pool", w[:, k, :], w_in_v[:, k, col0:col0 + ncol], [], [name])
            return w

        def hgrn_phase(direction):
            fwd = direction == 0
            with ExitStack() as es:
                Wq = load_w(es, "hg_Wq", IN_OFF["hg_q"], 512)
                Wf = load_w(es, "hg_Wf", IN_OFF["hg_ff"] if fwd else IN_OFF["hg_fb"], 512)
                Wi = load_w(es, "hg_Wi", IN_OFF["hg_i"], 512)
                Wg = load_w(es, "hg_Wg", IN_OFF["hg_g"], 512) if fwd else None
                hT = [sbuf(es, f"hg_hT{i}", [128, 8, 512], BF16) for i in range(2)]
                tnames = ["e1", "l1", "l2", "sq", "qf", "g", "fg", "kk", "bc", "eb", "enb", "kd32"]
                tmps = [{n: sbuf(es, f"hg_{n}{i}", [128, 512], F32) for n in tnames} for i in range(2)]
                ebl2 = [sbuf(es, f"hg_ebl{i}", [128, 4, 4], F32) for i in range(2)]
                Qd2 = [sbuf(es, f"hg_Qd{i}", [128, 4, 512], BF16) for i in range(2)]
                Kd2 = [sbuf(es, f"hg_Kd{i}", [128, 4, 512], BF16) for i in range(2)]
                Kl2 = [sbuf(es, f"hg_Kl{i}", [128, 4, 512], BF16) for i in range(2)]
                vsb2 = [sbuf(es, f"hg_v{i}", [128, 4, 512], BF16) for i in range(2)]
                klt = sbuf(es, "hg_klt", [128, 512], BF16)
                at = sbuf(es, "hg_at", [128, 512], BF16)
                osb = [sbuf(es, f"hg_o{i}", [128, 4, 512], F32) for i in range(2)]
                St = sbuf(es, "hg_S", [128, 4, 128], F32)
                Sb2 = [sbuf(es, f"hg_Sb{i}", [128, 4, 128], BF16) for i in range(2)]
                if fwd:
                    obw = sbuf(es, "hg_obw", [128, 4, 512], F32)
                    osq = sbuf(es, "hg_osq", [128, 4, 512], BF16)
                    rs = sbuf(es, "hg_rs", [128, 512], F32)
                    sg2 = [sbuf(es, f"hg_sg{i}", [128, 4, 512], F32) for i in range(2)]
                    cT = [sbuf(es, f"hg_cT{i}", [128, 4, 512], BF16) for i in range(2)]
                zlist = []
                if not fwd:
                    zt = sbuf(es, "hg_zero", [128, 8192], BF16)
                    MSET("pool", zt[:], 0.0, ["hg_zero"])
                    xb_flat = xbuf_d.rearrange("(p a) d -> p (a d)", p=128)
                    ncol = NSLOT * D // 128
                    c0 = 0
                    while c0 < ncol:
                        cw = min(8192, ncol - c0)
                        zlist.append((c0, cw))
                        c0 += cw
                MSET("dve", St[:], 0.0, ["hg_S"])
                MSET("dve", Sb2[0][:], 0.0, ["hg_Sb0"])
                MSET("dve", Sb2[1][:], 0.0, ["hg_Sb1"])
                gcount = [0]
                di = 0 if fwd else 1
                mask = cst[:, 1 if fwd else 2, :]
                blocks = list(range(NBLK)) if fwd else list(range(NBLK - 1, -1, -1))
                tiles = [0, 1, 2, 3] if fwd else [3, 2, 1, 0]
                DMA("sp", hT[0][:], h0T_v[:, :, blocks[0] * 512:(blocks[0] + 1) * 512], ["h0T_d"], ["hg_hT0"])

                def vproj(bi, tl, bk):
                    b = bi % 2
                    hk = f"hg_hT{b}"
                    vsb = vsb2[b]
                    for t in tl:
                        pv = bank(bk)
                        for k in range(8):
                            MM(pv, hT[b][:, k, t * 128:(t + 1) * 128], Wi[:, k, :], k == 0, k == 7, [hk, "hg_Wi"], [("B", bk)])
                        CP("act" if t % 2 == 0 else "dve", vsb[:, t, :], pv, [("B", bk)], [("hg_v", b, t)])

                def head(bi, h, si):
                    b = bi % 2
                    hk = f"hg_hT{b}"
                    Qd, Kd, Kl, ebl = Qd2[b], Kd2[b], Kl2[b], ebl2[b]
                    tm = tmps[si]
                    e1, l1, l2, sq, qf, gg, fg, kk, bc, eb, enb, kd32 = [tm[n] for n in tnames]
                    K_ = lambda n: f"hg_{n}{si}"
                    pf = bank(2 * si)
                    pq = bank(2 * si + 1)
                    pfk = ("B", 2 * si)
                    pqk = ("B", 2 * si + 1)
                    for k in range(8):
                        MM(pf, Wf[:, k, h * 128:(h + 1) * 128], hT[b][:, k, :], k == 0, k == 7, [hk, "hg_Wf"], [pfk])
                    for k in range(8):
                        MM(pq, Wq[:, k, h * 128:(h + 1) * 128], hT[b][:, k, :], k == 0, k == 7, [hk, "hg_Wq"], [pqk])
                    lb = lbp[:, 0, di * 4 + h:di * 4 + h + 1]
                    ACT(e1[:], pf, AF.Exp, [pfk], [K_("e1")], scale=-1.0)
                    ACT(l1[:], e1[:], AF.Ln, [K_("e1")], [K_("l1")], bias=1.0)
                    ACT(l2[:], e1[:], AF.Ln, [K_("e1"), "lbp"], [K_("l2")], scale=lb, bias=1.0)
                    TT("pool", gg[:], l2[:], l1[:], ALU.subtract, [K_("l1"), K_("l2")], [K_("g")])
                    ACT(fg[:], gg[:], AF.Exp, [K_("g")], [K_("fg")])
                    TS("pool", kk[:], fg[:], -1.0, 1.0, ALU.mult, ALU.add, [K_("fg")], [K_("kk")])
                    ACT(e1[:], pq, AF.Exp, [pqk], [K_("e1")], scale=-1.0)
                    ACT(l1[:], e1[:], AF.Ln, [K_("e1")], [K_("l1")], bias=1.0)
                    ACT(sq[:], l1[:], AF.Exp, [K_("l1")], [K_("sq")], scale=-1.0)
                    TT("dve", qf[:], pq, sq[:], ALU.mult, [pqk, K_("sq")], [K_("qf")])
                    if fwd:
                        S.add("dve", lambda e: e.tensor_tensor_scan(out=bc[:], data0=cst32[:, 0, :], data1=gg[:], initial=0.0,
                                                                    op0=ALU.mult, op1=ALU.add), r=[K_("g"), "cst32"], w=[K_("bc")])
                        blast = bc[:].rearrange("p (c t) -> p c t", t=128)[:, :, 127]
                    else:
                        S.add("dve", lambda e: e.tensor_tensor_scan(out=bc[:, ::-1], data0=cst32[:, 1, ::-1], data1=gg[:, ::-1], initial=0.0,
                                                                    op0=ALU.mult, op1=ALU.add), r=[K_("g"), "cst32"], w=[K_("bc")])
                        blast = bc[:].rearrange("p (c t) -> p c t", t=128)[:, :, 0]
                    ACT(eb[:], bc[:], AF.Exp, [K_("bc")], [K_("eb")])
                    ACT(enb[:], bc[:], AF.Exp, [K_("bc")], [K_("enb")], scale=-1.0)
                    ACT(ebl[:, h, :], blast, AF.Exp, [K_("bc")], [("hg_ebl", b, h)])
                    TT("dve", Qd[:, h, :], qf[:], eb[:], ALU.mult, [K_("qf"), K_("eb")], [("hg_Qd", b, h)])
                    TT("pool", kd32[:], kk[:], enb[:], ALU.mult, [K_("kk"), K_("enb")], [K_("kd32")])
                    CP("pool", Kd[:, h, :], kd32[:], [K_("kd32")], [("hg_Kd", b, h)])
                    TT("dve", Kl[:, h, :].rearrange("p (c t) -> p c t", t=128), kd32[:].rearrange("p (c t) -> p c t", t=128),
                       ebl[:, h, :].unsqueeze(2).to_broadcast([128, 4, 128]), ALU.mult, [K_("kd32"), ("hg_ebl", b, h)], [("hg_Kl", b, h)])
                    if fwd:
                        for k in range(8):
                            MM(pq, Wg[:, k, h * 128:(h + 1) * 128], hT[b][:, k, :], k == 0, k == 7, [hk, "hg_Wg"], [pqk])
                        ACT(e1[:], pq, AF.Exp, [pqk], [K_("e1")], scale=-1.0)
                        ACT(l1[:], e1[:], AF.Ln, [K_("e1")], [K_("l1")], bias=1.0)
                        ACT(sq[:], l1[:], AF.Exp, [K_("l1")], [K_("sq")], scale=-1.0)
                        TT("dve", sg2[b][:, h, :], pq, sq[:], ALU.mult, [pqk, K_("sq")], [("hg_sg", b, h)])

                def streamsA(bi):
                    b = bi % 2
                    out = []
                    for si in range(2):
                        S.record()
                        if si == 0 and bi + 1 < NBLK:
                            nb = blocks[bi + 1]
                            DMA("sp", hT[1 - b][:], h0T_v[:, :, nb * 512:(nb + 1) * 512], ["h0T_d"], [f"hg_hT{1 - b}"])
                        vproj(bi, [2 * si, 2 * si + 1], 2 * si)
                        head(bi, si, si)
                        head(bi, si + 2, si)
                        out.append(S.stop())
                    return out

                def stageB(bi):
                    b = bi % 2
                    blk = blocks[bi]
                    Qd, Kd, Kl, vsb, ebl = Qd2[b], Kd2[b], Kl2[b], vsb2[b], ebl2[b]
                    allq = [("hg_Qd", b, h) for h in range(4)]
                    allk = [("hg_Kd", b, h) for h in range(4)]
                    alll = [("hg_Kl", b, h) for h in range(4)]
                    ob = osb[b]
                    if fwd:
                        DMA("sp", obw[:], obw_v[:, :, blk * 512:(blk + 1) * 512], ["obw_d"], ["hg_obw"])
                    for t in tiles:
                        ts = slice(t * 128, (t + 1) * 128)
                        g = gcount[0]
                        gcount[0] += 1
                        Sold, Snew = Sb2[(g + 1) % 2], Sb2[g % 2]
                        soldk, snewk = f"hg_Sb{(g + 1) % 2}", f"hg_Sb{g % 2}"
                        pk = bank16(4)
                        for h in range(4):
                            TR(pk[:, h * 128:(h + 1) * 128], Kl[:, h, ts], ident, alll + ["cst"], [("B", 4)])
                        CP("act", klt[:], pk[:, 0:512], [("B", 4)], ["hg_klt"])
                        pa = bank(5)
                        for h in range(4):
                            MM(pa[:, h * 128:(h + 1) * 128], Kd[:, h, ts], Qd[:, h, ts], True, True, allq + allk, [("B", 5)])
                        TT("dve", at[:], pa, mask, ALU.mult, [("B", 5), "cst"], ["hg_at"])
                        pS = bank(7)
                        for h in range(4):
                            MM(pS[:, h * 128:(h + 1) * 128], klt[:, h * 128:(h + 1) * 128], vsb[:, t, h * 128:(h + 1) * 128], True, True,
                               ["hg_klt", ("hg_v", b, t)], [("B", 7)])
                        po = bank(6)
                        for h in range(4):
                            MM(po[:, h * 128:(h + 1) * 128], vsb[:, t, h * 128:(h + 1) * 128], at[:, h * 128:(h + 1) * 128], True, False,
                               [("hg_v", b, t), "hg_at"], [("B", 6)])
                            MM(po[:, h * 128:(h + 1) * 128], Sold[:, h, :], Qd[:, h, ts], False, True, [soldk] + allq, [("B", 6)])
                        for h in range(4):
                            STT(St[:, h, :], St[:, h, :], ebl[:, h, t:t + 1], pS[:, h * 128:(h + 1) * 128], ALU.mult, ALU.add,
                                ["hg_S", ("hg_ebl", b, h), ("B", 7)], ["hg_S"])
                        CP("act", Snew[:], St[:], ["hg_S"], [snewk])
                        CP("act", ob[:, :, ts], po.rearrange("p (h t) -> p h t", h=4), [("B", 6)], [f"hg_o{b}"])
                    if not fwd:
                        DMA("sp", obw_v[:, :, blk * 512:(blk + 1) * 512], ob[:], [f"hg_o{b}"], ["obw_d"])
                        for _ in range(2):
                            if zlist:
                                c0, cw = zlist.pop(0)
                                DMA("sp", xb_flat[:, c0:c0 + cw], zt[:, 0:cw], ["hg_zero"], [("xbuf_z", c0)])
                    else:
                        TT("pool", ob[:], ob[:], obw[:], ALU.add, [f"hg_o{b}", "hg_obw"], [f"hg_o{b}"])
                        TT("pool", osq[:], ob[:], ob[:], ALU.mult, [f"hg_o{b}"], ["hg_osq"])
                        for h in range(4):
                            pr = bank(4)
                            MM(pr, ones_bf, osq[:, h, :], True, True, ["hg_osq", "cst"], [("B", 4)])
                            ACT(rs[:], pr, AF.Ln, [("B", 4)], ["hg_rs"], scale=1.0 / 128.0, bias=RMS_EPS)
                            ACT(rs[:], rs[:], AF.Exp, ["hg_rs"], ["hg_rs"], scale=-0.5)
                            TT("dve", rs[:], rs[:], ob[:, h, :], ALU.mult, ["hg_rs", f"hg_o{b}"], ["hg_rs"])
                            STT(cT[b][:, h, :], rs[:], normg[:, h:h + 1], sg2[b][:, h, :], ALU.mult, ALU.mult,
                                ["hg_rs", ("hg_sg", b, h), "normg"], [f"hg_cT{b}"])
                        DMA("sp", cT_v[:, :, blk * 512:(blk + 1) * 512], cT[b][:], [f"hg_cT{b}"], ["cT_d"])

                for st_ in streamsA(0):
                    S.merge(st_)
                for bi in range(NBLK):
                    sts = streamsA(bi + 1) if bi + 1 < NBLK else []
                    S.record()
                    stageB(bi)
                    rb_ = S.stop()
                    S.merge(*(sts + [rb_]))

        def phaseN():
            with ExitStack() as es:
                kT = sbuf(es, "na_kT", [128, 4, T], BF16)
                Vx = sbuf(es, "na_V", [128, NTILE, 8, 65], BF16)
                hT = [sbuf(es, f"na_hT{i}", [128, 8, 512], BF16) for i in range(2)]
                MSET("pool", Vx[:, :, :, 64:65], 1.0, ["na_V"])
                with ExitStack() as es1:
                    Wk = load_w(es1, "na_Wk", IN_OFF["na_k"], 512)
                    Wv = load_w(es1, "na_Wv", IN_OFF["na_v"], 512)
                    DMA("sp", hT[0][:], h0T_v[:, :, 0:512], ["h0T_d"], ["na_hT0"])
                    for blk in range(NBLK):
                        b = blk % 2
                        hk = f"na_hT{b}"
                        if blk + 1 < NBLK:
                            DMA("sp", hT[1 - b][:], h0T_v[:, :, (blk + 1) * 512:(blk + 2) * 512], ["h0T_d"], [f"na_hT{1 - b}"])
                        for p in range(4):
                            pk = bank(p % 2)
                            for k in range(8):
                                MM(pk, Wk[:, k, p * 128:(p + 1) * 128], hT[b][:, k, :], k == 0, k == 7, [hk, "na_Wk"], [("B", p % 2)])
                            CP("act" if p % 2 == 0 else "dve", kT[:, p, blk * 512:(blk + 1) * 512], pk, [("B", p % 2)], ["na_kT"])
                        for t in range(4):
                            pv = bank(2 + t % 2)
                            for k in range(8):
                                MM(pv, hT[b][:, k, t * 128:(t + 1) * 128], Wv[:, k, :], k == 0, k == 7, [hk, "na_Wv"], [("B", 2 + t % 2)])
                            CP("dve" if t % 2 == 0 else "act", Vx[:, blk * 4 + t, :, 0:64], pv.rearrange("p (h d) -> p h d", h=8),
                               [("B", 2 + t % 2)], ["na_V"])
                S.barrier()
                with ExitStack() as es2:
                    tts = sbuf(es2, "na_tt", [128, 16, 512], BF16)
                    DMA("sp", tts[:], tt_d, [], ["na_tt"])
                    Wq = load_w(es2, "na_Wq", IN_OFF["na_q"], 512)
                    qbd = sbuf(es2, "na_qbd", [128, 4, 8, 128], BF16)
                    Eb = [sbuf(es2, f"na_E{i}", [128, 1280], BF16) for i in range(2)]
                    rec = sbuf(es2, "na_rec", [128, 4], F32)
                    asb = [sbuf(es2, f"na_a{i}", [128, 512], BF16) for i in range(2)]
                    aT1 = sbuf(es2, "na_aT", [128, 4, 512], BF16)
                    aT = [aT1, aT1]
                    MSET("pool", qbd[:], 0.0, ["na_qbd"])
                    DMA("sp", hT[0][:], h0T_v[:, :, 0:512], ["h0T_d"], ["na_hT0"])

                    def tiles_of(r):
                        rs = min(max(r - 4, 0), 120)
                        if rs % 2 == 0:
                            return [(rs // 2 + i, 2 * (rs // 2 + i) - r + 7) for i in range(4)]
                        m0 = (rs - 1) // 2
                        return [(m0, 14)] + [(m0 + i, 2 * (m0 + i) - r + 7) for i in range(1, 4)] + [(m0 + 4, 15)]

                    def qk(u, rl):
                        r, h2 = u // 2, u % 2
                        tl = tiles_of(r)
                        nch = len(tl)
                        sbk = PS[:, (u % 2) * 1536:(u % 2) * 1536 + 1536]
                        keys = [("B", (u % 2) * 3 + i) for i in range(3)]
                        for pi in range(2):
                            p = 2 * h2 + pi
                            for ci, (m, slab) in enumerate(tl):
                                slot = pi * nch + ci
                                o = sbk[:, slot * 128:(slot + 1) * 128]
                                MM(o, kT[:, p, m * 128:(m + 1) * 128], qbd[:, p, rl, :], True, False, ["na_kT", "na_qbd"], keys)
                                MM(o, ident, tts[:, slab, p * 128:(p + 1) * 128], False, True, ["cst", "na_tt"], keys)

                    def rest(u):
                        r, h2 = u // 2, u % 2
                        tl = tiles_of(r)
                        nch = len(tl)
                        sbk = PS[:, (u % 2) * 1536:(u % 2) * 1536 + 1536]
                        keys = [("B", (u % 2) * 3 + i) for i in range(3)]
                        E = Eb[u % 2]
                        ek = f"na_E{u % 2}"
                        n = 2 * nch * 128
                        ACT(E[:, 0:n], sbk[:, 0:n], AF.Exp, keys, [ek])
                        par = r % 2
                        prt = slice(par * 64, (par + 1) * 64)
                        pO = bank(6)
                        for pi in range(2):
                            for hh in range(2):
                                hq = pi * 2 + hh
                                head = 4 * h2 + hq
                                for ci, (m, slab) in enumerate(tl):
                                    c0 = (pi * nch + ci) * 128 + hh * 64
                                    MM(pO[prt, hq * 65:(hq + 1) * 65], E[:, c0:c0 + 64], Vx[:, m, head, :], ci == 0, ci == nch - 1,
                                       [ek, "na_V"], [("B", 6)])
                        pov = pO[prt, 0:260].rearrange("p (h d) -> p h d", d=65)
                        S.add("dve", lambda e: e.reciprocal(out=rec[prt, :], in_=pov[:, :, 64]), r=[("B", 6)], w=["na_rec"])
                        tb = (r // 2) % 2
                        TT("dve", asb[tb][prt, h2 * 256:(h2 + 1) * 256].rearrange("p (h d) -> p h d", d=64), pov[:, :, 0:64],
                           rec[prt, :].unsqueeze(2).to_broadcast([64, 4, 64]), ALU.mult, [("B", 6), "na_rec"], [f"na_a{tb}"])

                    for blk in range(NBLK):
                        b = blk % 2
                        hk = f"na_hT{b}"
                        if blk + 1 < NBLK:
                            DMA("sp", hT[1 - b][:], h0T_v[:, :, (blk + 1) * 512:(blk + 2) * 512], ["h0T_d"], [f"na_hT{1 - b}"])
                        for p in range(4):
                            pq = bank(7)
                            for k in range(8):
                                MM(pq, Wq[:, k, p * 128:(p + 1) * 128], hT[b][:, k, :], k == 0, k == 7, [hk, "na_Wq"], [("B", 7)])
                            ACT(qbd[0:64, p, :, 0:64], pq[0:64, :].rearrange("p (r c) -> p r c", r=8), AF.Copy, [("B", 7)], ["na_qbd"], scale=0.125)
                            TS("dve", qbd[64:128, p, :, 64:128], pq[64:128, :].rearrange("p (r c) -> p r c", r=8), 0.125, None, ALU.mult, None,
                               [("B", 7)], ["na_qbd"])
                        u0 = blk * 16
                        qk(u0, 0)
                        for ul in range(16):
                            u = u0 + ul
                            if ul + 1 < 16:
                                qk(u + 1, (ul + 1) // 2)
                            rest(u)
                            if ul % 4 == 3:
                                t = ul // 4
                                tb = ((u // 2) // 2) % 2
                                pT = bank16(7)
                                for c in range(4):
                                    TR(pT[:, c * 128:(c + 1) * 128], asb[tb][:, c * 128:(c + 1) * 128], ident, [f"na_a{tb}", "cst"], [("B", 7)])
                                CP("act", aT[b][:, :, t * 128:(t + 1) * 128], pT[:, 0:512].rearrange("p (c t) -> p c t", c=4), [("B", 7)], ["na_aT"])
                        DMA("sp", aT_v[:, :, blk * 512:(blk + 1) * 512], aT[b][:], ["na_aT"], ["aT_d"])

        def phaseM():
            with ExitStack() as es:
                ba_bf = sbuf(es, "m_ba", [1, D], BF16)
                with ExitStack() as est:
                    tmpb = sbuf(est, "m_tmpb", [1, D], F32)
                    DMA("sp", tmpb[:], lnp_d[0:1, 1, :], [], ["m_tmpb"])
                    TS("dve", tmpb[:], tmpb[:], ALPHA, None, ALU.mult, None, ["m_tmpb"], ["m_tmpb"])
                    CP("dve", ba_bf[:], tmpb[:], ["m_tmpb"], ["m_ba"])
                S.barrier()
                lnp = sbuf(es, "m_lnp", [128, 3, D], F32)
                DMA("sp", lnp[:, 0, :], lnp_d[:, 0, :], [], ["m_lnp"])
                DMA("sp", lnp[:, 1:3, :], lnp_d[:, 2:4, :], [], ["m_lnp"])
                TS("pool", lnp[:, 0, :], lnp[:, 0, :], ALPHA, None, ALU.mult, None, ["m_lnp"], ["m_lnp"])
                Wga = load_w(es, "m_Wga", IN_OFF["gate_a"], 1024)
                Wgb = load_w(es, "m_Wgb", IN_OFF["gate_b"], 1024)
                Wa = sbuf(es, "m_Wa", [128, 4, D], BF16)
                Wb = sbuf(es, "m_Wb", [128, 4, D], BF16)
                Wo = sbuf(es, "m_Wo", [128, 8, D], BF16)
                for k in range(4):
                    DMA("pool", Wa[:, k, :], w_pa_d[k * 128:(k + 1) * 128, :], [], ["m_Wa"])
                    DMA("pool", Wb[:, k, :], w_pb_d[k * 128:(k + 1) * 128, :], [], ["m_Wb"])
                for k in range(8):
                    DMA("pool", Wo[:, k, :], w_out_d[k * 128:(k + 1) * 128, :], [], ["m_Wo"])
                wr = sbuf(es, "m_wr", [128, 8, 36], F32)
                rb = sbuf(es, "m_rb", [128, 4, 36], F32)
                DMA("sp", wr[:], wr_d, [], ["m_wr"])
                DMA("sp", rb[:], rb_d, [], ["m_rb"])
                hT = [sbuf(es, f"m_hT{i}", [128, 8, 512], BF16) for i in range(2)]
                aTs1 = sbuf(es, "m_aT", [128, 4, 512], BF16)
                aTs = [aTs1, aTs1]
                cTs1 = sbuf(es, "m_cT", [128, 4, 512], BF16)
                cTs = [cTs1, cTs1]
                xt = sbuf(es, "m_xt", [128, 4, D], F32)
                sgb_ = sbuf(es, "m_sgb", [128, 512], F32)
                sg4 = [(sbuf(es, f"m_sga{i}", [128, 512], F32), sgb_) for i in range(2)]
                m1 = sbuf(es, "m_m1", [128, 512], F32)
                m2 = sbuf(es, "m_m2", [128, 512], F32)
                mT = sbuf(es, "m_mT", [128, 8, 512], BF16)
                zz = [sbuf(es, f"m_z{i}", [128, 4, D], F32) for i in range(2)]
                hx2 = [sbuf(es, f"m_hx{i}", [128, D], F32) for i in range(2)]
                h1b = sbuf(es, "m_h1b", [128, 4, D], BF16)
                h1T = sbuf(es, "m_h1T", [128, 4, 128], F32)
                st1 = sbuf(es, "m_st1", [128, 4, 12], F32)
                mv1 = sbuf(es, "m_mv1", [128, 4, 2], F32)
                rs1 = sbuf(es, "m_rs1", [128, 4], F32)
                nm1 = sbuf(es, "m_nm1", [128, 4], F32)
                L = sbuf(es, "m_L", [128, 4, 36], F32)
                gmax = sbuf(es, "m_gmax", [128, 4], F32)
                gm = sbuf(es, "m_gm", [128, 4, 4], F32)
                gd = sbuf(es, "m_gd", [128, 4, 4], F32)
                gw = sbuf(es, "m_gw", [128, 4], F32)
                EM = sbuf(es, "m_EM", [128, 4, 32], F32)
                EM2 = sbuf(es, "m_EM2", [128, 4, 32], F32)
                top1 = sbuf(es, "m_top1", [128, 4], F32)
                top2 = sbuf(es, "m_top2", [128, 4], F32)
                oh = [sbuf(es, f"m_oh{i}", [128, 4, 32], F32) for i in range(2)]
                cnt = sbuf(es, "m_cnt", [128, 4, 32], BF16)
                rank = sbuf(es, "m_rank", [128, 4, 32], F32)
                slot = sbuf(es, "m_slot", [128, 4, 32], F32)
                tmp = sbuf(es, "m_tmp", [128, 4, 32], F32)
                tot = sbuf(es, "m_tot", [128, 32], F32)
                sm = sbuf(es, "m_sm", [128, 8, 4], F32)
                MSET("dve", tot[:], 0.0, ["m_tot"])
                if "fakeidx" in dbg:
                    fake_ix = sbuf(es, "m_fake", [128, NTILE, 2], I32)
                    S.add("pool", lambda e: e.iota(fake_ix[:].rearrange("p n k -> p (n k)"), pattern=[[128, 128]], base=0, channel_multiplier=1),
                          w=["m_fake"])
                ident32 = cst32[:, 2, 0:128]
                ustr = cst[:, 3, 0:128]
                ebase = cst32[:, 2, 128:160]
                pidx = cst32[:, 2, 160:161]

                def loads_ac(blk):
                    DMA("sp", aTs[0][:], aT_v[:, :, blk * 512:(blk + 1) * 512], ["aT_d"], ["m_aT"])
                    DMA("sp", cTs[0][:], cT_v[:, :, blk * 512:(blk + 1) * 512], ["cT_d"], ["m_cT"])

                def loads(blk, b):
                    DMA("sp", hT[b][:], h0T_v[:, :, blk * 512:(blk + 1) * 512], ["h0T_d"], [f"m_hT{b}"])

                def stageA(blk):
                    b = blk % 2
                    z = zz[b]
                    if blk + 1 < NBLK:
                        loads(blk + 1, 1 - b)
                    rs0 = xstat[:, blk * 4:(blk + 1) * 4, 0]
                    nm0 = xstat[:, blk * 4:(blk + 1) * 4, 1]
                    for c in range(8):
                        ga_, gb_ = 0, 1
                        pa_, pb_ = (2, 3) if c % 2 == 0 else (4, 5)
                        cs = slice(c * 128, (c + 1) * 128)
                        for k in range(8):
                            MM(bank(ga_), Wga[:, k, cs], hT[b][:, k, :], k == 0, k == 7, [f"m_hT{b}", "m_Wga"], [("B", ga_)])
                        for k in range(4):
                            MM(bank(pa_), Wa[:, k, cs], aTs[b][:, k, :], k == 0, k == 3, ["m_aT", "m_Wa"], [("B", pa_)])
                        for k in range(8):
                            MM(bank(gb_), Wgb[:, k, cs], hT[b][:, k, :], k == 0, k == 7, [f"m_hT{b}", "m_Wgb"], [("B", gb_)])
                        for k in range(4):
                            MM(bank(pb_), Wb[:, k, cs], cTs[b][:, k, :], k == 0, k == 3, ["m_cT", "m_Wb"], [("B", pb_)])
                        sga, sgb = sg4[c % 2]
                        ACT(sga[:], bank(ga_), AF.Sigmoid, [("B", ga_)], [f"m_sga{c % 2}"])
                        ACT(sgb[:], bank(gb_), AF.Sigmoid, [("B", gb_)], ["m_sgb"])
                        TT("dve", m1[:], bank(pa_), sga[:], ALU.mult, [("B", pa_), f"m_sga{c % 2}"], ["m_m1"])
                        TT("dve", m2[:], bank(pb_), sgb[:], ALU.mult, [("B", pb_), "m_sgb"], ["m_m2"])
                        TT("pool", mT[:, c, :], m1[:], m2[:], ALU.add, ["m_m1", "m_m2"], [("m_mT", c)])
                    if blk + 1 < NBLK:
                        loads_ac(blk + 1)
                    mtk = [("m_mT", c) for c in range(8)]
                    for t in range(4):
                        hxx = hx2[t % 2]
                        hk_ = f"m_hx{t % 2}"
                        ACT(hxx[:], xt[:, t, :], AF.Identity, ["m_xt", "xstat"], [hk_], scale=rs0[:, t:t + 1], bias=nm0[:, t:t + 1])
                        TT("dve", hxx[:], hxx[:], lnp[:, 0, :], ALU.mult, [hk_, "m_lnp"], [hk_])
                        for n in range(2):
                            o = (2, 3, 4, 5)[(t * 2 + n) % 4]
                            ns = slice(n * 512, (n + 1) * 512)
                            MM(bank(o), cst[0:1, 4, 0:128], ba_bf[0:1, ns], True, False, ["cst", "m_ba"], [("B", o)])
                            for k in range(8):
                                MM(bank(o), mT[:, k, t * 128:(t + 1) * 128], Wo[:, k, ns], False, k == 7, mtk + ["m_Wo"], [("B", o)])
                            TT("dve", z[:, t, ns], hxx[:, ns], bank(o), ALU.add, [hk_, ("B", o)], [("m_z", b, t)])
                    if blk + 1 < NBLK:
                        DMA("sp", xt[:], x_v[blk + 1], [], ["m_xt"])

                def stageB(blk):
                    b = blk % 2
                    z = zz[b]
                    zk = [("m_z", b, t) for t in range(4)]
                    for t in range(4):
                        for c in range(2):
                            S.add("dve", lambda e, t=t, c=c: e.bn_stats(out=st1[:, t, c * 6:(c + 1) * 6], in_=z[:, t, c * 512:(c + 1) * 512]),
                                  r=[("m_z", b, t)], w=["m1st"])
                        S.add("dve", lambda e, t=t: e.bn_aggr(out=mv1[:, t, :], in_=st1[:, t, :]), r=["m1st"], w=["m1mv"])
                    ACT(rs1[:], mv1[:, :, 1], AF.Ln, ["m1mv"], ["m1rs"], bias=LN_EPS)
                    ACT(rs1[:], rs1[:], AF.Exp, ["m1rs"], ["m1rs"], scale=-0.5)
                    STT(nm1[:], mv1[:, :, 0], -1.0, rs1[:], ALU.mult, ALU.mult, ["m1mv", "m1rs"], ["m1nm"])
                    for t in range(4):
                        ACT(z[:, t, :], z[:, t, :], AF.Identity, [("m_z", b, t), "m1rs", "m1nm"], [("m_z", b, t)], scale=rs1[:, t:t + 1], bias=nm1[:, t:t + 1])
                        TT("dve", z[:, t, :], z[:, t, :], lnp[:, 1, :], ALU.mult, [("m_z", b, t), "m_lnp"], [("m_z", b, t)])
                        TT("pool", z[:, t, :], z[:, t, :], lnp[:, 2, :], ALU.add, [("m_z", b, t), "m_lnp"], [("m_z", b, t)])
                        CP("act", h1b[:, t, :], z[:, t, :], [("m_z", b, t)], [("m_h1b", t)])
                    DMA("sp", h1_v[blk], z[:], zk, ["h1_d"])
                    pL = bank(7)
                    for t in range(4):
                        for g in range(2):
                            for kk_ in range(4):
                                k = g * 4 + kk_
                                TR(bank(6)[:, kk_ * 128:(kk_ + 1) * 128], z[:, t, k * 128:(k + 1) * 128], ident32, [("m_z", b, t), "cst32"], [("B", 6)])
                            CP("act" if g == 0 else "dve", h1T[:, 0:4, :], bank(6).rearrange("p (k t) -> p k t", k=4),
                               [("B", 6)], ["m_h1T"])
                            for kk_ in range(4):
                                k = g * 4 + kk_
                                MM(pL[:, t * 36:(t + 1) * 36], h1T[:, kk_, :], wr[:, k, :], k == 0, k == 7, ["m_h1T", "m_wr"], [("B", 7)])
                    TT("dve", L[:], pL[:, 0:144].rearrange("p (t n) -> p t n", n=36), rb[:], ALU.add, [("B", 7), "m_rb"], ["m_L"])
                    GL = L[:, :, 0:4]
                    EL = L[:, :, 4:36].rearrange("p t (g e) -> p t g e", g=4)
                    S.add("dve", lambda e: e.tensor_reduce(out=gmax[:], in_=GL, axis=AX.X, op=ALU.max), r=["m_L"], w=["m_gmax"])
                    TT("dve", gm[:], GL, gmax[:].unsqueeze(2).to_broadcast([128, 4, 4]), ALU.is_equal, ["m_L", "m_gmax"], ["m_gm"])
                    TT("dve", gd[:], GL, gmax[:].unsqueeze(2).to_broadcast([128, 4, 4]), ALU.subtract, ["m_L", "m_gmax"], ["m_gd"])
                    ACT(gd[:], gd[:], AF.Exp, ["m_gd"], ["m_gd"])
                    S.add("dve", lambda e: e.tensor_reduce(out=gw[:], in_=gd[:], axis=AX.X, op=ALU.add), r=["m_gd"], w=["m_gw"])
                    S.add("dve", lambda e: e.reciprocal(out=gw[:], in_=gw[:]), r=["m_gw"], w=["m_gw"])
                    TS("dve", gm[:], gm[:], 1e9, -1e9, ALU.mult, ALU.add, ["m_gm"], ["m_gm"])
                    TT("dve", EM[:].rearrange("p t (g e) -> p t g e", g=4), EL, gm[:].unsqueeze(3).to_broadcast([128, 4, 4, 8]), ALU.add,
                       ["m_L", "m_gm"], ["m_EM"])
                    S.add("dve", lambda e: e.tensor_reduce(out=top1[:], in_=EM[:], axis=AX.X, op=ALU.max), r=["m_EM"], w=["m_top1"])
                    TT("dve", oh[0][:], EM[:], top1[:].unsqueeze(2).to_broadcast([128, 4, 32]), ALU.is_equal, ["m_EM", "m_top1"], ["m_oh0"])
                    STT(EM2[:], oh[0][:], -1e9, EM[:], ALU.mult, ALU.add, ["m_oh0", "m_EM"], ["m_EM2"])
                    S.add("dve", lambda e: e.tensor_reduce(out=top2[:], in_=EM2[:], axis=AX.X, op=ALU.max), r=["m_EM2"], w=["m_top2"])
                    TT("dve", oh[1][:], EM2[:], top2[:].unsqueeze(2).to_broadcast([128, 4, 32]), ALU.is_equal, ["m_EM2", "m_top2"], ["m_oh1"])
                    w1 = wall[:, blk * 4:(blk + 1) * 4, 0]
                    w2 = wall[:, blk * 4:(blk + 1) * 4, 1]
                    TT("dve", sm[:, 0, :], top2[:], top1[:], ALU.subtract, ["m_top1", "m_top2"], ["m_sm0"])
                    ACT(sm[:, 0, :], sm[:, 0, :], AF.Exp, ["m_sm0"], ["m_sm0"])
                    TS("dve", sm[:, 0, :], sm[:, 0, :], 1.0, None, ALU.add, None, ["m_sm0"], ["m_sm0"])
                    S.add("dve", lambda e: e.reciprocal(out=sm[:, 1, :], in_=sm[:, 0, :]), r=["m_sm0"], w=["m_sm1"])
                    TT("dve", w1, sm[:, 1, :], gw[:], ALU.mult, ["m_sm1", "m_gw"], [("wall", blk)])
                    TT("dve", w2, gw[:], w1, ALU.subtract, ["m_gw", ("wall", blk)], [("wall", blk)])
                    TT("dve", cnt[:], oh[0][:], oh[1][:], ALU.add, ["m_oh0", "m_oh1"], ["m_cnt"])
                    pR = bank(7)[:, 256:512]
                    for t in range(4):
                        MM(pR[:, t * 32:(t + 1) * 32], ustr, cnt[:, t, :], True, t == 0, ["m_cnt", "cst"], [("B", 7)])
                        for t2 in range(t):
                            MM(pR[:, t * 32:(t + 1) * 32], ones_bf, cnt[:, t2, :], False, t2 == t - 1, ["m_cnt", "cst"], [("B", 7)])
                    for t in range(4):
                        MM(pR[:, 128:160], ones_bf, cnt[:, t, :], t == 0, t == 3, ["m_cnt", "cst"], [("B", 7)])
                    TT("dve", rank[:], pR[:, 0:128].rearrange("p (t n) -> p t n", n=32), tot[:].unsqueeze(1).to_broadcast([128, 4, 32]), ALU.add,
                       [("B", 7), "m_tot"], ["m_rank"])
                    TT("dve", tot[:], tot[:], pR[:, 128:160], ALU.add, ["m_tot", ("B", 7)], ["m_tot"])
                    TT("dve", slot[:], rank[:], ebase.unsqueeze(1).to_broadcast([128, 4, 32]), ALU.add, ["m_rank", "cst32"], ["m_slot"])
                    for kx in range(2):
                        ohk = f"m_oh{kx}"
                        TT("dve", tmp[:], oh[kx][:], slot[:], ALU.mult, [ohk, "m_slot"], ["m_tmp"])
                        S.add("dve", lambda e: e.tensor_reduce(out=sm[:, 2, :], in_=tmp[:], axis=AX.X, op=ALU.add), r=["m_tmp"], w=["m_sm2"])
                        TT("dve", tmp[:], oh[kx][:], rank[:], ALU.mult, [ohk, "m_rank"], ["m_tmp"])
                        S.add("dve", lambda e: e.tensor_reduce(out=sm[:, 3, :], in_=tmp[:], axis=AX.X, op=ALU.add), r=["m_tmp"], w=["m_sm3"])
                        TS("dve", sm[:, 3, :], sm[:, 3, :], float(CAP), None, ALU.is_ge, None, ["m_sm3"], ["m_sm3"])
                        TS("dve", sm[:, 4, :], sm[:, 2, :], -1.0, pidx, ALU.mult, ALU.add, ["m_sm2", "cst32"], ["m_sm4"])
                        TT("dve", sm[:, 4, :], sm[:, 4, :], sm[:, 3, :], ALU.mult, ["m_sm4", "m_sm3"], ["m_sm4"])
                        TT("dve", sm[:, 2, :], sm[:, 2, :], sm[:, 4, :], ALU.add, ["m_sm2", "m_sm4"], ["m_sm2"])
                        CP("pool", posall[:, blk * 4:(blk + 1) * 4, kx], sm[:, 2, :], ["m_sm2"], [("posall", blk)])
                    for t in range(4):
                        if "noscatter" in dbg:
                            break
                        for kx in range(2):
                            ixap = posall[:, blk * 4 + t, kx:kx + 1]
                            if "fakeidx" in dbg:
                                ixap = fake_ix[:, blk * 4 + t, kx:kx + 1]
                            S.add("pool", lambda e, t=t, kx=kx, ixap=ixap: e.indirect_dma_start(
                                out=xbuf_d, out_offset=bass.IndirectOffsetOnAxis(ap=ixap, axis=0),
                                in_=h1b[:, t, :], in_offset=None), r=[("m_h1b", t), ("posall", blk)], w=["xbuf_d"], dma=True)

                loads(0, 0)
                loads_ac(0)
                DMA("sp", xt[:], x_v[0], [], ["m_xt"])
                stageA(0)
                for blk in range(NBLK):
                    ra = None
                    if blk + 1 < NBLK:
                        S.record()
                        stageA(blk + 1)
                        ra = S.stop()
                    S.record()
                    stageB(blk)
                    rb_ = S.stop()
                    S.merge(ra, rb_)

        def dump_routing():
            pos_d = nc.dram_tensor("pos_d", [128, NTILE * 2], I32, kind="ExternalOutput").ap()
            wall_d = nc.dram_tensor("wall_d", [128, NTILE * 2], F32, kind="ExternalOutput").ap()
            allk = [("posall", b) for b in range(NBLK)]
            allw = [("wall", b) for b in range(NBLK)]
            DMA("sp", pos_d, posall[:].rearrange("p n k -> p (n k)"), allk, ["pos_d"])
            DMA("sp", wall_d, wall[:].rearrange("p n k -> p (n k)"), allw, ["wall_d"])

        def phaseE():
            with ExitStack() as es:
                wg = [sbuf(es, f"e_wg{i}", [128, 8, 512], BF16) for i in range(2)]
                wu = [sbuf(es, f"e_wu{i}", [128, 8, 512], BF16) for i in range(2)]
                wd = [sbuf(es, f"e_wd{i}", [128, 4, D], BF16) for i in range(2)]
                xs = [sbuf(es, f"e_xs{i}", [128, 6, D], BF16) for i in range(2)]
                xT2 = [sbuf(es, f"e_xT{i}", [128, 8, CAP], BF16) for i in range(2)]
                sg = sbuf(es, "e_sg", [128, 384], F32)
                tg = sbuf(es, "e_tg", [128, 384], F32)
                hTt = sbuf(es, "e_hT", [128, 4, CAP], BF16)
                ysb = [sbuf(es, f"e_y{i}", [128, D], F32) for i in range(2)]

                def wload(e, b):
                    DMA("pool", wg[b][:], w_gate_d[e].rearrange("(k p) n -> p k n", p=128), [], [f"e_wg{b}"])
                    DMA("pool", wu[b][:], w_up_d[e].rearrange("(k p) n -> p k n", p=128), [], [f"e_wu{b}"])
                    DMA("pool", wd[b][:], w_down_d[e].rearrange("(k p) n -> p k n", p=128), [], [f"e_wd{b}"])
                    DMA("sp", xs[b][:], xbuf_d[e * CAP:(e + 1) * CAP, :].rearrange("(s p) d -> p s d", p=128), ["xbuf_d"], [f"e_xs{b}"])

                def stageA(e):
                    b = e % 2
                    for k in range(8):
                        pb = bank16(k % 2)
                        for s_ in range(6):
                            TR(pb[:, s_ * 128:(s_ + 1) * 128], xs[b][:, s_, k * 128:(k + 1) * 128], ident, [f"e_xs{b}", "cst"], [("B", k % 2)])
                        CP("act" if k % 2 == 0 else "dve", xT2[b][:, k, :], pb[:, 0:CAP], [("B", k % 2)], [("e_xT", b, k)])

                def stageB(e):
                    b = e % 2
                    xT = xT2[b]
                    xtk = [("e_xT", b, k) for k in range(8)]
                    for m in range(4):
                        for half in range(2):
                            u = m * 2 + half
                            o = 2 + 2 * (u % 2)
                            hs = slice(half * 384, (half + 1) * 384)
                            ms = slice(m * 128, (m + 1) * 128)
                            for k in range(8):
                                MM(bank(o)[:, 0:384], wg[b][:, k, ms], xT[:, k, hs], k == 0, k == 7, xtk + [f"e_wg{b}"], [("B", o)])
                            for k in range(8):
                                MM(bank(o + 1)[:, 0:384], wu[b][:, k, ms], xT[:, k, hs], k == 0, k == 7, xtk + [f"e_wu{b}"], [("B", o + 1)])
                            ACT(sg[:], bank(o)[:, 0:384], AF.Sigmoid, [("B", o)], ["e_sg"])
                            TT("dve", tg[:], bank(o)[:, 0:384], sg[:], ALU.mult, [("B", o), "e_sg"], ["e_tg"])
                            TT("dve", hTt[:, m, hs], bank(o + 1)[:, 0:384], tg[:], ALU.mult, [("B", o + 1), "e_tg"], [("e_hT", m)])
                    htk = [("e_hT", m) for m in range(4)]
                    for s_ in range(6):
                        yb = (e * 6 + s_) % 2
                        for n in range(2):
                            for m in range(4):
                                MM(bank(6 + n), hTt[:, m, s_ * 128:(s_ + 1) * 128], wd[b][:, m, n * 512:(n + 1) * 512], m == 0, m == 3,
                                   htk + [f"e_wd{b}"], [("B", 6 + n)])
                            CP("act" if n == 0 else "dve", ysb[yb][:, n * 512:(n + 1) * 512], bank(6 + n), [("B", 6 + n)], [f"e_y{yb}"])
                        DMA("sp", ybuf_d[e * CAP + s_ * 128:e * CAP + (s_ + 1) * 128, :], ysb[yb][:], [f"e_y{yb}"], ["ybuf_d"])

                wload(0, 0)
                for e in range(32):
                    if e + 1 < 32:
                        wload(e + 1, (e + 1) % 2)
                    stageA(e)
                    stageB(e)

        def phaseG():
            with ExitStack() as es:
                lnp = sbuf(es, "g_lnp", [128, 2, D], F32)
                DMA("sp", lnp[:], lnp_d[:, 4:6, :], [], ["g_lnp"])
                y0 = [sbuf(es, f"g_y0{i}", [128, D], F32) for i in range(2)]
                y1 = [sbuf(es, f"g_y1{i}", [128, D], F32) for i in range(2)]
                h1t = [sbuf(es, f"g_h1{i}", [128, D], F32) for i in range(2)]
                zz = [sbuf(es, f"g_z{i}", [128, 1, D], F32) for i in range(2)]
                st = sbuf(es, "g_st", [128, 1, 12], F32)
                mv = sbuf(es, "g_mv", [128, 1, 2], F32)
                rs2 = [sbuf(es, f"g_rs{i}", [128, 1], F32) for i in range(2)]
                nm2 = [sbuf(es, f"g_nm{i}", [128, 1], F32) for i in range(2)]

                def gl(n, b):
                    S.add("pool", lambda e: e.indirect_dma_start(out=y0[b][:], out_offset=None, in_=ybuf_d,
                                                                 in_offset=bass.IndirectOffsetOnAxis(ap=posall[:, n, 0:1], axis=0)),
                          r=["ybuf_d"], w=[f"g_y0{b}"], dma=True)
                    S.add("pool", lambda e: e.indirect_dma_start(out=y1[b][:], out_offset=None, in_=ybuf_d,
                                                                 in_offset=bass.IndirectOffsetOnAxis(ap=posall[:, n, 1:2], axis=0)),
                          r=["ybuf_d"], w=[f"g_y1{b}"], dma=True)
                    DMA("sp", h1t[b][:], h1_tv[n], ["h1_d"], [f"g_h1{b}"])

                def stageA(n):
                    b = n % 2
                    zt = zz[b]
                    zk = f"g_z{b}"
                    TS("dve", zt[:, 0, :], y0[b][:], wall[:, n, 0:1], None, ALU.mult, None, [f"g_y0{b}"], [zk, zk + "a", zk + "b"])
                    STT(zt[:, 0, :], y1[b][:], wall[:, n, 1:2], zt[:, 0, :], ALU.mult, ALU.add, [f"g_y1{b}", zk], [zk])
                    STT(zt[:, 0, :], h1t[b][:], ALPHA, zt[:, 0, :], ALU.mult, ALU.add, [f"g_h1{b}", zk], [zk])
                    ln_stats(f"g{b}", zt, zk, st, mv, rs2[b], nm2[b], tiles=1)

                def stageB(n):
                    b = n % 2
                    zt = zz[b]
                    zk = f"g_z{b}"
                    ACT(zt[:, 0, :], zt[:, 0, :], AF.Identity, [zk, f"g{b}rs", f"g{b}nm"], [zk], scale=rs2[b][:, 0:1], bias=nm2[b][:, 0:1])
                    TT("dve", zt[:, 0, :], zt[:, 0, :], lnp[:, 0, :], ALU.mult, [zk, "g_lnp"], [zk])
                    TT("pool", zt[:, 0, 0:512], zt[:, 0, 0:512], lnp[:, 1, 0:512], ALU.add, [zk, "g_lnp"], [zk + "a"])
                    TT("dve", zt[:, 0, 512:1024], zt[:, 0, 512:1024], lnp[:, 1, 512:1024], ALU.add, [zk, "g_lnp"], [zk + "b"])
                    DMA("sp", out_v[n], zt[:, 0, :], [zk, zk + "a", zk + "b"], ["out"])

                gl(0, 0)
                for n in range(NTILE):
                    if n + 1 < NTILE:
                        gl(n + 1, (n + 1) % 2)
                    stageA(n)
                    stageB(n)

        for ph in phases:
            if ph == "0":
                phase0()
            elif ph == "B":
                hgrn_phase(1)
            elif ph == "F":
                hgrn_phase(0)
            elif ph == "N":
                phaseN()
            elif ph == "M":
                phaseM()
                if "pos_d" in dbg:
                    dump_routing()
            elif ph == "E":
                phaseE()
            elif ph == "G":
                phaseG()
            S.barrier()
        S.emit(nc)
    return nc


def host_consts():
    bf = ml_dtypes.bfloat16
    cst = np.zeros((128, 5, 512), np.float32)
    eye = np.eye(128, dtype=np.float32)
    s = np.arange(128)[:, None]
    t = np.arange(128)[None, :]
    cst[:, 0, :] = np.tile(eye, (1, 4))
    cst[:, 1, :] = np.tile((s <= t).astype(np.float32), (1, 4))
    cst[:, 2, :] = np.tile((s >= t).astype(np.float32), (1, 4))
    cst[:, 3, :] = np.tile((s < t).astype(np.float32), (1, 4))
    cst[:, 4, :] = 1.0
    cst32 = np.zeros((128, 3, 512), np.float32)
    tt = np.arange(512)
    cst32[:, 0, :] = (tt % 128 != 0).astype(np.float32)[None, :]
    cst32[:, 1, :] = (tt % 128 != 127).astype(np.float32)[None, :]
    cst32[:, 2, 0:128] = eye
    cst32[:, 2, 128:160] = (np.arange(32) * CAP).astype(np.float32)[None, :]
    cst32[:, 2, 160] = 32 * CAP + np.arange(128)
    return cst.astype(bf), cst32


def host_tt(rpb):
    bf = ml_dtypes.bfloat16
    cols = np.arange(64)
    col_start = np.clip(cols - 8, 0, 48)
    c = cols[None, :]
    kc = cols[:, None]
    valid = (kc >= col_start[None, :]) & (kc < col_start[None, :] + 16)
    off = np.clip(kc - c + 15, 0, 30)
    full = np.full((15, 64, 8, 64), NEG, np.float32)
    for ro in range(15):
        for h in range(8):
            full[ro, :, h, :] = np.where(valid, rpb[h, ro][off], NEG)
    full = full.reshape(15, 64, 512)
    neg = np.full((64, 512), NEG, np.float32)
    tt = np.zeros((128, 16, 512), np.float32)
    for a in range(14):
        tt[0:64, a] = full[a]
        tt[64:128, a] = full[a + 1]
    tt[0:64, 14] = neg
    tt[64:128, 14] = full[3]
    tt[0:64, 15] = full[10]
    tt[64:128, 15] = neg
    return tt.astype(bf)


def make_in_maps(inp):
    cst, cst32 = host_consts()
    f = lambda a: np.ascontiguousarray(np.asarray(a, np.float32))
    lnp = np.stack([inp["emb_ln_g"], inp["emb_ln_b"], inp["ln1_g"][0], inp["ln1_b"][0], inp["ln2_g"][0], inp["ln2_b"][0]], 0)
    lnp = f(np.broadcast_to(lnp[None], (128, 6, D)))
    embp = f(np.concatenate([np.asarray(inp["emb_ln_g"]).reshape(8, 128).T, np.asarray(inp["emb_ln_b"]).reshape(8, 128).T], 1))
    lbraw = f(np.asarray(inp["hg_lb"]).reshape(2, 2, 4, 128).transpose(3, 0, 1, 2).reshape(128, 16))
    normg = f(np.asarray(inp["hg_norm_g"])[0].reshape(4, 128).T)
    wr = np.concatenate([np.asarray(inp["w_router_group"])[0], np.asarray(inp["w_router_expert"])[0]], 1)
    wr = f(wr.reshape(8, 128, 36).transpose(1, 0, 2))
    rb = np.concatenate([np.asarray(inp["b_router_group"])[0], np.asarray(inp["b_router_expert"])[0]], 0)
    rb = f(np.broadcast_to(rb[None, None], (128, 4, 36)))
    tt = host_tt(np.asarray(inp["na_rpb"], np.float32)[0])
    shared = dict(w_in=f(inp["w_in"][0]), w_proj_a=f(inp["w_proj_a"][0]), w_proj_b=f(inp["w_proj_b"][0]), w_out=f(inp["w_out"][0]),
                  w_gate=f(inp["w_gate"][0]), w_up=f(inp["w_up"][0]), w_down=f(inp["w_down"][0]),
                  lnp=lnp, embp=embp, lbraw=lbraw, normg=normg, wr=wr, rb=rb, tt=tt, cst=cst, cst32=cst32)
    x = np.asarray(inp["x"], np.float32)
    return [dict(shared, x=np.ascontiguousarray(x[b])) for b in range(x.shape[0])]


def kernel(**inputs):
    nc = build()
    in_maps = make_in_maps(inputs)
    res = run_bass_kernel_spmd(nc, in_maps, core_ids=list(range(8)))
    return np.stack([np.asarray(r["out"], np.float32) for r in res.results], 0)
```

```python
from contextlib import ExitStack
import numpy as np
import ml_dtypes
import concourse.bass as bass
import concourse.mybir as mybir
from concourse.bass_utils import run_bass_kernel_spmd

F32 = mybir.dt.float32
BF16 = mybir.dt.bfloat16
I32 = mybir.dt.int32
AF = mybir.ActivationFunctionType
ALU = mybir.AluOpType
AX = mybir.AxisListType

T = 8192
D = 1024
NBLK = 16
NTILE = 64
CAP = 768
NSLOT = 32 * CAP + 128
ALPHA = 2.0 ** 0.25
LN_EPS = 1e-5
RMS_EPS = 1e-6
NEG = -30000.0

ENGS = ("pe", "dve", "act", "pool", "sp")
EPOCH = 3000
NDMA_SEM = 8


class Op:
    __slots__ = ("eng", "fn", "deps", "sig", "sem", "val", "dma", "n")

    def __init__(self, eng, fn, dma):
        self.eng = eng
        self.fn = fn
        self.dma = dma
        self.deps = []
        self.sig = dma
        self.sem = None
        self.val = 0


class Sched:
    def __init__(self):
        self.q = {e: [] for e in ENGS}
        self.last_w = {}
        self.readers = {}
        self.dma_hist = {e: [] for e in ENGS}
        self.nops = 0
        self.bar = None
        self.bar_pending = set()
        self.rec = None
        self.m_wfin = {}
        self.m_rfin = {}
        self.m_efree = {}

    def barrier(self):
        ops = []
        for e in ENGS:
            last = None
            for op in reversed(self.q[e]):
                if not op.dma:
                    last = op
                    break
            if last is not None:
                ops.append(last)
            ops.extend(self.dma_hist[e][-NDMA_SEM:])
        self.bar = ops
        self.bar_pending = set(ENGS)

    def record(self):
        self.rec = []

    def stop(self):
        r = self.rec
        self.rec = None
        return r

    def merge(self, *streams):
        streams = [st for st in streams if st]
        pos = [0] * len(streams)
        total = sum(len(st) for st in streams)
        wfin = self.m_wfin
        rfin = self.m_rfin
        efree = self.m_efree
        for _ in range(total):
            best = None
            for i, st in enumerate(streams):
                if pos[i] >= len(st):
                    continue
                eng, fn, r, w, dma, cost = st[pos[i]]
                rdy = 0.0
                for k in r:
                    rdy = max(rdy, wfin.get(k, 0.0))
                for k in w:
                    rdy = max(rdy, wfin.get(k, 0.0), rfin.get(k, 0.0))
                start = max(efree.get(eng, 0.0), rdy + 0.1)
                rem = len(st) - pos[i]
                cand = (start, -rem, i)
                if best is None or cand < best[0]:
                    best = (cand, i, start)
            _, i, start = best
            eng, fn, r, w, dma, cost = streams[i][pos[i]]
            pos[i] += 1
            if dma:
                efree[eng] = start + 0.06
                fin = start + cost
            else:
                fin = start + cost
                efree[eng] = fin
            for k in r:
                rfin[k] = max(rfin.get(k, 0.0), fin)
            for k in w:
                wfin[k] = fin
                rfin[k] = 0.0
            self.add(eng, fn, r, w, dma, cost)

    def add(self, eng, fn, r=(), w=(), dma=False, cost=0.5):
        if self.rec is not None:
            self.rec.append((eng, fn, tuple(r), tuple(w), dma, cost))
            return None
        op = Op(eng, fn, dma)
        op.n = self.nops
        self.nops += 1
        deps = {}
        if eng in self.bar_pending:
            self.bar_pending.discard(eng)
            for p in self.bar:
                if p.dma or p.eng != eng:
                    deps[id(p)] = p
        for k in r:
            p = self.last_w.get(k)
            if p is not None and p is not op:
                if p.eng == eng and not p.dma and not dma:
                    if eng != "pe":
                        deps[id(p)] = p
                else:
                    deps[id(p)] = p
        for k in w:
            p = self.last_w.get(k)
            if p is not None and p is not op and (p.dma or dma or p.eng != eng or eng != "pe"):
                deps[id(p)] = p
            rd = self.readers.get(k)
            if rd:
                for p in rd.values():
                    if p is not op and (p.dma or dma or p.eng != eng or eng != "pe"):
                        deps[id(p)] = p
        for k in w:
            self.last_w[k] = op
            self.readers[k] = {}
        for k in r:
            d = self.readers.setdefault(k, {})
            d[("dma", op.n) if dma else eng] = op
        if dma:
            h = self.dma_hist[eng]
            if len(h) >= NDMA_SEM:
                p = h[-NDMA_SEM]
                deps[id(p)] = p
            h.append(op)
        op.deps = list(deps.values())
        for p in op.deps:
            p.sig = True
        self.q[eng].append(op)
        return op

    def emit(self, nc):
        with ExitStack() as es:
            for e in ENGS:
                cnt = 0
                sem = None
                dsem = [None] * NDMA_SEM
                dval = [0] * NDMA_SEM
                nd = 0
                ns = 0
                for op in self.q[e]:
                    if op.dma:
                        s = nd % NDMA_SEM
                        if dsem[s] is None:
                            dsem[s] = es.enter_context(nc.semaphore(f"d_{e}_{s}"))
                        dval[s] += 16
                        op.sem = dsem[s]
                        op.val = dval[s]
                        nd += 1
                    elif op.sig:
                        if sem is None or cnt >= EPOCH:
                            sem = es.enter_context(nc.semaphore(f"c_{e}_{ns}"))
                            ns += 1
                            cnt = 0
                        cnt += 1
                        op.sem = sem
                        op.val = cnt
            block = es.enter_context(nc.Block())
            engmap = {"pe": block.tensor, "dve": block.vector, "act": block.scalar,
                      "pool": block.gpsimd, "sp": block.sync}
            for e in ENGS:
                ops = self.q[e]
                if not ops:
                    continue

                def body(engine, ops=ops):
                    waited = {}
                    for op in ops:
                        for p in op.deps:
                            key = id(p.sem)
                            if waited.get(key, 0) >= p.val:
                                continue
                            waited[key] = p.val
                            engine.wait_ge(p.sem, p.val)
                        ins = op.fn(engine)
                        if op.sig:
                            ins.then_inc(op.sem, 16 if op.dma else 1)
                    last = {}
                    for op in ops:
                        if op.dma:
                            last[id(op.sem)] = op
                    for op in last.values():
                        if waited.get(id(op.sem), 0) < op.val:
                            engine.wait_ge(op.sem, op.val)

                engmap[e](body)


IN_OFF = dict(na_q=0, na_k=512, na_v=1024, hg_q=1536, hg_ff=2048, hg_fb=2560, hg_i=3072, hg_g=3584,
              gate_a=4096, gate_b=5120)


def build(dbg=(), phases="0BNFMEG"):
    nc = bass.Bass("TRN2", target_bir_lowering=False)
    S = Sched()

    def din(name, shape, dt):
        return nc.dram_tensor(name, list(shape), dt, kind="ExternalInput").ap()

    def dscr(name, shape, dt):
        kind = "ExternalOutput" if name in dbg else "Internal"
        return nc.dram_tensor(name, list(shape), dt, kind=kind).ap()

    x_d = din("x", [T, D], F32)
    w_in_d = din("w_in", [D, 6144], F32)
    w_pa_d = din("w_proj_a", [512, D], F32)
    w_pb_d = din("w_proj_b", [512, D], F32)
    w_out_d = din("w_out", [D, D], F32)
    w_gate_d = din("w_gate", [32, D, 512], F32)
    w_up_d = din("w_up", [32, D, 512], F32)
    w_down_d = din("w_down", [32, 512, D], F32)
    lnp_d = din("lnp", [128, 6, D], F32)
    embp_d = din("embp", [128, 16], F32)
    lbraw_d = din("lbraw", [128, 16], F32)
    normg_d = din("normg", [128, 4], F32)
    wr_d = din("wr", [128, 8, 36], F32)
    rb_d = din("rb", [128, 4, 36], F32)
    tt_d = din("tt", [128, 16, 512], BF16)
    cst_d = din("cst", [128, 5, 512], BF16)
    cst32_d = din("cst32", [128, 3, 512], F32)
    out_d = nc.dram_tensor("out", [T, D], F32, kind="ExternalOutput").ap()

    h0T_d = dscr("h0T_d", [D, T], BF16)
    obw_d = dscr("obw_d", [512, T], F32)
    aT_d = dscr("aT_d", [512, T], BF16)
    cT_d = dscr("cT_d", [512, T], BF16)
    h1_d = dscr("h1_d", [T, D], F32)
    xbuf_d = dscr("xbuf_d", [NSLOT, D], BF16)
    ybuf_d = dscr("ybuf_d", [NSLOT, D], F32)

    h0T_v = h0T_d.rearrange("(k p) t -> p k t", p=128)
    obw_v = obw_d.rearrange("(h p) t -> p h t", p=128)
    aT_v = aT_d.rearrange("(k p) t -> p k t", p=128)
    cT_v = cT_d.rearrange("(k p) t -> p k t", p=128)
    x_v = x_d.rearrange("(b t p) d -> b p t d", t=4, p=128)
    h1_v = h1_d.rearrange("(b t p) d -> b p t d", t=4, p=128)
    out_v = out_d.rearrange("(n p) d -> n p d", p=128)
    h1_tv = h1_d.rearrange("(n p) d -> n p d", p=128)
    w_in_v = w_in_d.rearrange("(k p) n -> p k n", p=128)

    with ExitStack() as ges:
        uid = [0]

        def sbuf(es, name, shape, dt):
            uid[0] += 1
            return es.enter_context(nc.sbuf_tensor(f"s{uid[0]}_{name}", list(shape), dt))

        PS = ges.enter_context(nc.psum_tensor("PS", [128, 4096], F32))

        def bank(i, n=1):
            return PS[:, i * 512:(i + n) * 512]

        def bank16(i):
            return PS[:, i * 512:(i + 1) * 512].bitcast(BF16)

        cst = sbuf(ges, "cst", [128, 5, 512], BF16)
        cst32 = sbuf(ges, "cst32", [128, 3, 512], F32)
        embp = sbuf(ges, "embp", [128, 16], F32)
        lbraw = sbuf(ges, "lbraw", [128, 16], F32)
        lbp = sbuf(ges, "lbp", [128, 3, 8], F32)
        normg = sbuf(ges, "normg", [128, 4], F32)
        posall = sbuf(ges, "posall", [128, NTILE, 2], I32)
        wall = sbuf(ges, "wall", [128, NTILE, 2], F32)
        xstat = sbuf(ges, "xstat", [128, NTILE, 2], F32)
        ident = cst[:, 0, 0:128]
        ones_bf = cst[:, 4, 0:128]

        def nfree(ap):
            n = 1
            for d in ap.shape[1:]:
                n *= d
            return n

        def DMA(eng, out, in_, r, w):
            S.add(eng, lambda e: e.dma_start(out=out, in_=in_), r=r, w=w, dma=True, cost=2.5 + nfree(out) * 128 * 4 / 150e3)

        def MM(out, lhsT, rhs, start, stop, r, w):
            c_ = max(64, nfree(rhs)) / 2400.0 * (4.0 if rhs.dtype == F32 else 1.0) + 0.01
            S.add("pe", lambda e: e.matmul(out, lhsT=lhsT, rhs=rhs, start=start, stop=stop), r=r, w=w, cost=c_)

        def TR(out, in_, idn, r, w):
            S.add("pe", lambda e: e.transpose(out, in_, idn), r=r, w=w, cost=0.08)

        def ACT(out, in_, func, r, w, scale=1.0, bias=0.0):
            S.add("act", lambda e: e.activation(out=out, in_=in_, func=func, bias=bias, scale=scale), r=r, w=w, cost=0.25 + nfree(out) / 1200.0)

        def vcost(eng, out):
            return (0.3 + nfree(out) / 330.0) if eng == "pool" else (0.16 + nfree(out) / 960.0)

        def TT(eng, out, in0, in1, op, r, w):
            S.add(eng, lambda e: e.tensor_tensor(out=out, in0=in0, in1=in1, op=op), r=r, w=w, cost=vcost(eng, out))

        def TS(eng, out, in0, s1, s2, op0, op1, r, w):
            if s2 is None:
                S.add(eng, lambda e: e.tensor_scalar(out=out, in0=in0, scalar1=s1, scalar2=None, op0=op0), r=r, w=w, cost=vcost(eng, out))
            else:
                S.add(eng, lambda e: e.tensor_scalar(out=out, in0=in0, scalar1=s1, scalar2=s2, op0=op0, op1=op1), r=r, w=w, cost=vcost(eng, out))

        def STT(out, in0, scalar, in1, op0, op1, r, w):
            S.add("dve", lambda e: e.scalar_tensor_tensor(out=out, in0=in0, scalar=scalar, in1=in1, op0=op0, op1=op1), r=r, w=w, cost=vcost("dve", out))

        def CP(eng, out, in_, r, w):
            if eng == "act":
                S.add("act", lambda e: e.activation(out=out, in_=in_, func=AF.Copy), r=r, w=w, cost=0.25 + nfree(out) / 1200.0)
            else:
                S.add(eng, lambda e: e.tensor_copy(out=out, in_=in_), r=r, w=w, cost=vcost(eng, out))

        def MSET(eng, ap, val, w):
            S.add(eng, lambda e: e.memset(ap, val), w=w)

        DMA("sp", cst[:], cst_d, [], ["cst"])
        DMA("sp", cst32[:], cst32_d, [], ["cst32"])
        DMA("sp", embp[:], embp_d, [], ["embp"])
        DMA("sp", lbraw[:], lbraw_d, [], ["lbraw"])
        DMA("sp", normg[:], normg_d, [], ["normg"])
        lbr = lbraw[:].rearrange("p (d l h) -> p d l h", d=2, l=2)
        lb_v = lbp[:, 0, :].rearrange("p (d h) -> p d h", d=2)
        TT("dve", lb_v, lbr[:, :, 1, :], lbr[:, :, 0, :], ALU.subtract, ["lbraw"], ["lbp"])
        ACT(lbp[:, 0, :], lbp[:, 0, :], AF.Exp, ["lbp"], ["lbp"])
        ACT(lbp[:, 0, :], lbp[:, 0, :], AF.Ln, ["lbp"], ["lbp"], bias=1.0)
        ACT(lbp[:, 0, :], lbp[:, 0, :], AF.Exp, ["lbp"], ["lbp"], scale=-1.0)
        TS("dve", lbp[:, 1, :], lbp[:, 0, :], -1.0, 1.0, ALU.mult, ALU.add, ["lbp"], ["lbp"])
        TS("dve", lbp[:, 2, :], lbp[:, 1, :], -1.0, None, ALU.mult, None, ["lbp"], ["lbp"])

        def ln_stats(es_name, xt, xkey, stats, mv, rstd, nmr, tiles=4):
            for t in range(tiles):
                for c in range(2):
                    S.add("dve", lambda e, t=t, c=c: e.bn_stats(out=stats[:, t, c * 6:(c + 1) * 6], in_=xt[:, t, c * 512:(c + 1) * 512]),
                          r=[xkey], w=[es_name + "st"])
                S.add("dve", lambda e, t=t: e.bn_aggr(out=mv[:, t, :], in_=stats[:, t, :]), r=[es_name + "st"], w=[es_name + "mv"])
            ACT(rstd[:, 0:tiles], mv[:, 0:tiles, 1], AF.Ln, [es_name + "mv"], [es_name + "rs", "xstat"], bias=LN_EPS)
            ACT(rstd[:, 0:tiles], rstd[:, 0:tiles], AF.Exp, [es_name + "rs"], [es_name + "rs", "xstat"], scale=-0.5)
            STT(nmr[:, 0:tiles], mv[:, 0:tiles, 0], -1.0, rstd[:, 0:tiles], ALU.mult, ALU.mult, [es_name + "mv", es_name + "rs"], [es_name + "nm", "xstat"])

        def phase0():
            with ExitStack() as es:
                xt = [sbuf(es, f"p0_xt{i}", [128, 4, D], F32) for i in range(2)]
                xn2 = [sbuf(es, f"p0_xn{i}", [128, 4, D], BF16) for i in range(2)]
                hT = [sbuf(es, f"p0_hT{i}", [128, 8, 512], BF16) for i in range(2)]
                stats2 = [sbuf(es, f"p0_stats{i}", [128, 4, 12], F32) for i in range(2)]
                mv2 = [sbuf(es, f"p0_mv{i}", [128, 4, 2], F32) for i in range(2)]
                def stageA(blk):
                    b = blk % 2
                    if blk + 1 < NBLK:
                        DMA("sp", xt[1 - b][:], x_v[blk + 1], [], [f"xt{1 - b}"])
                    rstd = xstat[:, blk * 4:(blk + 1) * 4, 0]
                    nmr = xstat[:, blk * 4:(blk + 1) * 4, 1]
                    ln_stats(f"p0{b}", xt[b], f"xt{b}", stats2[b], mv2[b], rstd, nmr)
                    for t in range(4):
                        ACT(xn2[b][:, t, :], xt[b][:, t, :], AF.Identity, [f"xt{b}", f"p0{b}rs", f"p0{b}nm"], [("xn", b, t)],
                            scale=rstd[:, t:t + 1], bias=nmr[:, t:t + 1])

                def stageB(blk):
                    b = blk % 2
                    xn = xn2[b]
                    for k in range(8):
                        pb = bank16(k % 2)
                        for t in range(4):
                            TR(pb[:, t * 128:(t + 1) * 128], xn[:, t, k * 128:(k + 1) * 128], ident, [("xn", b, t), "cst"], [("B", k % 2)])
                        if k % 2 == 0:
                            ACT(hT[b][:, k, :], pb[:, 0:512], AF.Identity, [("B", k % 2), "embp"], [f"hT{b}"],
                                scale=embp[:, k:k + 1], bias=embp[:, 8 + k:9 + k])
                        else:
                            TS("dve", hT[b][:, k, :], pb[:, 0:512], embp[:, k:k + 1], embp[:, 8 + k:9 + k], ALU.mult, ALU.add,
                               [("B", k % 2), "embp"], [f"hT{b}"])
                    DMA("sp", h0T_v[:, :, blk * 512:(blk + 1) * 512], hT[b][:], [f"hT{b}"], ["h0T_d"])

                DMA("sp", xt[0][:], x_v[0], [], ["xt0"])
                stageA(0)
                for blk in range(NBLK):
                    S.record()
                    if blk + 1 < NBLK:
                        stageA(blk + 1)
                    ra = S.stop()
                    S.record()
                    stageB(blk)
                    rb_ = S.stop()
                    S.merge(ra, rb_)

        def load_w(es, name, col0, ncol):
            w = sbuf(es, name, [128, 8, ncol], BF16)
            for k in range(8):
                DMA("pool", w[:, k, :], w_in_v[:, k, col0:col0 + ncol], [], [name])
            return w

        def hgrn_phase(direction):
            fwd = direction == 0
            with ExitStack() as es:
                Wq = load_w(es, "hg_Wq", IN_OFF["hg_q"], 512)
                Wf = load_w(es, "hg_Wf", IN_OFF["hg_ff"] if fwd else IN_OFF["hg_fb"], 512)
                Wi = load_w(es, "hg_Wi", IN_OFF["hg_i"], 512)
                Wg = load_w(es, "hg_Wg", IN_OFF["hg_g"], 512) if fwd else None
                hT = [sbuf(es, f"hg_hT{i}", [128, 8, 512], BF16) for i in range(2)]
                tnames = ["e1", "l1", "l2", "sq", "qf", "g", "fg", "kk", "bc", "eb", "enb", "kd32"]
                tmps = [{n: sbuf(es, f"hg_{n}{i}", [128, 512], F32) for n in tnames} for i in range(2)]
                ebl2 = [sbuf(es, f"hg_ebl{i}", [128, 4, 4], F32) for i in range(2)]
                Qd2 = [sbuf(es, f"hg_Qd{i}", [128, 4, 512], BF16) for i in range(2)]
                Kd2 = [sbuf(es, f"hg_Kd{i}", [128, 4, 512], BF16) for i in range(2)]
                Kl2 = [sbuf(es, f"hg_Kl{i}", [128, 4, 512], BF16) for i in range(2)]
                vsb2 = [sbuf(es, f"hg_v{i}", [128, 4, 512], BF16) for i in range(2)]
                klt = sbuf(es, "hg_klt", [128, 512], BF16)
                at = sbuf(es, "hg_at", [128, 512], BF16)
                osb = [sbuf(es, f"hg_o{i}", [128, 4, 512], F32) for i in range(2)]
                St = sbuf(es, "hg_S", [128, 4, 128], F32)
                Sb2 = [sbuf(es, f"hg_Sb{i}", [128, 4, 128], BF16) for i in range(2)]
                if fwd:
                    obw = sbuf(es, "hg_obw", [128, 4, 512], F32)
                    osq = sbuf(es, "hg_osq", [128, 4, 512], BF16)
                    rs = sbuf(es, "hg_rs", [128, 512], F32)
                    sg2 = [sbuf(es, f"hg_sg{i}", [128, 4, 512], F32) for i in range(2)]
                    cT = [sbuf(es, f"hg_cT{i}", [128, 4, 512], BF16) for i in range(2)]
                zlist = []
                if not fwd:
                    zt = sbuf(es, "hg_zero", [128, 8192], BF16)
                    MSET("pool", zt[:], 0.0, ["hg_zero"])
                    xb_flat = xbuf_d.rearrange("(p a) d -> p (a d)", p=128)
                    ncol = NSLOT * D // 128
                    c0 = 0
                    while c0 < ncol:
                        cw = min(8192, ncol - c0)
                        zlist.append((c0, cw))
                        c0 += cw
                MSET("dve", St[:], 0.0, ["hg_S"])
                MSET("dve", Sb2[0][:], 0.0, ["hg_Sb0"])
                MSET("dve", Sb2[1][:], 0.0, ["hg_Sb1"])
                gcount = [0]
                di = 0 if fwd else 1
                mask = cst[:, 1 if fwd else 2, :]
                blocks = list(range(NBLK)) if fwd else list(range(NBLK - 1, -1, -1))
                tiles = [0, 1, 2, 3] if fwd else [3, 2, 1, 0]
                DMA("sp", hT[0][:], h0T_v[:, :, blocks[0] * 512:(blocks[0] + 1) * 512], ["h0T_d"], ["hg_hT0"])

                def vproj(bi, tl, bk):
                    b = bi % 2
                    hk = f"hg_hT{b}"
                    vsb = vsb2[b]
                    for t in tl:
                        pv = bank(bk)
                        for k in range(8):
                            MM(pv, hT[b][:, k, t * 128:(t + 1) * 128], Wi[:, k, :], k == 0, k == 7, [hk, "hg_Wi"], [("B", bk)])
                        CP("act" if t % 2 == 0 else "dve", vsb[:, t, :], pv, [("B", bk)], [("hg_v", b, t)])

                def head(bi, h, si):
                    b = bi % 2
                    hk = f"hg_hT{b}"
                    Qd, Kd, Kl, ebl = Qd2[b], Kd2[b], Kl2[b], ebl2[b]
                    tm = tmps[si]
                    e1, l1, l2, sq, qf, gg, fg, kk, bc, eb, enb, kd32 = [tm[n] for n in tnames]
                    K_ = lambda n: f"hg_{n}{si}"
                    pf = bank(2 * si)
                    pq = bank(2 * si + 1)
                    pfk = ("B", 2 * si)
                    pqk = ("B", 2 * si + 1)
                    for k in range(8):
                        MM(pf, Wf[:, k, h * 128:(h + 1) * 128], hT[b][:, k, :], k == 0, k == 7, [hk, "hg_Wf"], [pfk])
                    for k in range(8):
                        MM(pq, Wq[:, k, h * 128:(h + 1) * 128], hT[b][:, k, :], k == 0, k == 7, [hk, "hg_Wq"], [pqk])
                    lb = lbp[:, 0, di * 4 + h:di * 4 + h + 1]
                    ACT(e1[:], pf, AF.Exp, [pfk], [K_("e1")], scale=-1.0)
                    ACT(l1[:], e1[:], AF.Ln, [K_("e1")], [K_("l1")], bias=1.0)
                    ACT(l2[:], e1[:], AF.Ln, [K_("e1"), "lbp"], [K_("l2")], scale=lb, bias=1.0)
                    TT("pool", gg[:], l2[:], l1[:], ALU.subtract, [K_("l1"), K_("l2")], [K_("g")])
                    ACT(fg[:], gg[:], AF.Exp, [K_("g")], [K_("fg")])
                    TS("pool", kk[:], fg[:], -1.0, 1.0, ALU.mult, ALU.add, [K_("fg")], [K_("kk")])
                    ACT(e1[:], pq, AF.Exp, [pqk], [K_("e1")], scale=-1.0)
                    ACT(l1[:], e1[:], AF.Ln, [K_("e1")], [K_("l1")], bias=1.0)
                    ACT(sq[:], l1[:], AF.Exp, [K_("l1")], [K_("sq")], scale=-1.0)
                    TT("dve", qf[:], pq, sq[:], ALU.mult, [pqk, K_("sq")], [K_("qf")])
                    if fwd:
                        S.add("dve", lambda e: e.tensor_tensor_scan(out=bc[:], data0=cst32[:, 0, :], data1=gg[:], initial=0.0,
                                                                    op0=ALU.mult, op1=ALU.add), r=[K_("g"), "cst32"], w=[K_("bc")], cost=1.25)
                        blast = bc[:].rearrange("p (c t) -> p c t", t=128)[:, :, 127]
                    else:
                        S.add("dve", lambda e: e.tensor_tensor_scan(out=bc[:, ::-1], data0=cst32[:, 1, ::-1], data1=gg[:, ::-1], initial=0.0,
                                                                    op0=ALU.mult, op1=ALU.add), r=[K_("g"), "cst32"], w=[K_("bc")], cost=1.25)
                        blast = bc[:].rearrange("p (c t) -> p c t", t=128)[:, :, 0]
                    ACT(eb[:], bc[:], AF.Exp, [K_("bc")], [K_("eb")])
                    ACT(enb[:], bc[:], AF.Exp, [K_("bc")], [K_("enb")], scale=-1.0)
                    ACT(ebl[:, h, :], blast, AF.Exp, [K_("bc")], [("hg_ebl", b, h)])
                    TT("dve", Qd[:, h, :], qf[:], eb[:], ALU.mult, [K_("qf"), K_("eb")], [("hg_Qd", b, h)])
                    TT("pool", kd32[:], kk[:], enb[:], ALU.mult, [K_("kk"), K_("enb")], [K_("kd32")])
                    CP("pool", Kd[:, h, :], kd32[:], [K_("kd32")], [("hg_Kd", b, h)])
                    TT("dve", Kl[:, h, :].rearrange("p (c t) -> p c t", t=128), kd32[:].rearrange("p (c t) -> p c t", t=128),
                       ebl[:, h, :].unsqueeze(2).to_broadcast([128, 4, 128]), ALU.mult, [K_("kd32"), ("hg_ebl", b, h)], [("hg_Kl", b, h)])
                    if fwd:
                        for k in range(8):
                            MM(pq, Wg[:, k, h * 128:(h + 1) * 128], hT[b][:, k, :], k == 0, k == 7, [hk, "hg_Wg"], [pqk])
                        ACT(e1[:], pq, AF.Exp, [pqk], [K_("e1")], scale=-1.0)
                        ACT(l1[:], e1[:], AF.Ln, [K_("e1")], [K_("l1")], bias=1.0)
                        ACT(sq[:], l1[:], AF.Exp, [K_("l1")], [K_("sq")], scale=-1.0)
                        TT("dve", sg2[b][:, h, :], pq, sq[:], ALU.mult, [pqk, K_("sq")], [("hg_sg", b, h)])

                def streamsA(bi):
                    b = bi % 2
                    out = []
                    for si in range(2):
                        S.record()
                        if si == 0 and bi + 1 < NBLK:
                            nb = blocks[bi + 1]
                            DMA("sp", hT[1 - b][:], h0T_v[:, :, nb * 512:(nb + 1) * 512], ["h0T_d"], [f"hg_hT{1 - b}"])
                        vproj(bi, [2 * si, 2 * si + 1], 2 * si)
                        head(bi, si, si)
                        head(bi, si + 2, si)
                        out.append(S.stop())
                    return out

                def stageB(bi):
                    b = bi % 2
                    blk = blocks[bi]
                    Qd, Kd, Kl, vsb, ebl = Qd2[b], Kd2[b], Kl2[b], vsb2[b], ebl2[b]
                    allq = [("hg_Qd", b, h) for h in range(4)]
                    allk = [("hg_Kd", b, h) for h in range(4)]
                    alll = [("hg_Kl", b, h) for h in range(4)]
                    ob = osb[b]
                    if fwd:
                        DMA("sp", obw[:], obw_v[:, :, blk * 512:(blk + 1) * 512], ["obw_d"], ["hg_obw"])
                    for t in tiles:
                        ts = slice(t * 128, (t + 1) * 128)
                        g = gcount[0]
                        gcount[0] += 1
                        Sold, Snew = Sb2[(g + 1) % 2], Sb2[g % 2]
                        soldk, snewk = f"hg_Sb{(g + 1) % 2}", f"hg_Sb{g % 2}"
                        pk = bank16(4)
                        for h in range(4):
                            TR(pk[:, h * 128:(h + 1) * 128], Kl[:, h, ts], ident, alll + ["cst"], [("B", 4)])
                        CP("act", klt[:], pk[:, 0:512], [("B", 4)], ["hg_klt"])
                        pa = bank(5)
                        for h in range(4):
                            MM(pa[:, h * 128:(h + 1) * 128], Kd[:, h, ts], Qd[:, h, ts], True, True, allq + allk, [("B", 5)])
                        TT("dve", at[:], pa, mask, ALU.mult, [("B", 5), "cst"], ["hg_at"])
                        pS = bank(7)
                        for h in range(4):
                            MM(pS[:, h * 128:(h + 1) * 128], klt[:, h * 128:(h + 1) * 128], vsb[:, t, h * 128:(h + 1) * 128], True, True,
                               ["hg_klt", ("hg_v", b, t)], [("B", 7)])
                        po = bank(6)
                        for h in range(4):
                            MM(po[:, h * 128:(h + 1) * 128], vsb[:, t, h * 128:(h + 1) * 128], at[:, h * 128:(h + 1) * 128], True, False,
                               [("hg_v", b, t), "hg_at"], [("B", 6)])
                            MM(po[:, h * 128:(h + 1) * 128], Sold[:, h, :], Qd[:, h, ts], False, True, [soldk] + allq, [("B", 6)])
                        for h in range(4):
                            STT(St[:, h, :], St[:, h, :], ebl[:, h, t:t + 1], pS[:, h * 128:(h + 1) * 128], ALU.mult, ALU.add,
                                ["hg_S", ("hg_ebl", b, h), ("B", 7)], ["hg_S"])
                        CP("act", Snew[:], St[:], ["hg_S"], [snewk])
                        CP("act", ob[:, :, ts], po.rearrange("p (h t) -> p h t", h=4), [("B", 6)], [f"hg_o{b}"])
                    if not fwd:
                        DMA("sp", obw_v[:, :, blk * 512:(blk + 1) * 512], ob[:], [f"hg_o{b}"], ["obw_d"])
                        for _ in range(2):
                            if zlist:
                                c0, cw = zlist.pop(0)
                                DMA("sp", xb_flat[:, c0:c0 + cw], zt[:, 0:cw], ["hg_zero"], [("xbuf_z", c0)])
                    else:
                        TT("pool", ob[:], ob[:], obw[:], ALU.add, [f"hg_o{b}", "hg_obw"], [f"hg_o{b}"])
                        TT("pool", osq[:], ob[:], ob[:], ALU.mult, [f"hg_o{b}"], ["hg_osq"])
                        for h in range(4):
                            pr = bank(4)
                            MM(pr, ones_bf, osq[:, h, :], True, True, ["hg_osq", "cst"], [("B", 4)])
                            ACT(rs[:], pr, AF.Ln, [("B", 4)], ["hg_rs"], scale=1.0 / 128.0, bias=RMS_EPS)
                            ACT(rs[:], rs[:], AF.Exp, ["hg_rs"], ["hg_rs"], scale=-0.5)
                            TT("dve", rs[:], rs[:], ob[:, h, :], ALU.mult, ["hg_rs", f"hg_o{b}"], ["hg_rs"])
                            STT(cT[b][:, h, :], rs[:], normg[:, h:h + 1], sg2[b][:, h, :], ALU.mult, ALU.mult,
                                ["hg_rs", ("hg_sg", b, h), "normg"], [f"hg_cT{b}"])
                        DMA("sp", cT_v[:, :, blk * 512:(blk + 1) * 512], cT[b][:], [f"hg_cT{b}"], ["cT_d"])

                for st_ in streamsA(0):
                    S.merge(st_)
                for bi in range(NBLK):
                    sts = streamsA(bi + 1) if bi + 1 < NBLK else []
                    S.record()
                    stageB(bi)
                    rb_ = S.stop()
                    S.merge(*(sts + [rb_]))

        def phaseN():
            with ExitStack() as es:
                kT = sbuf(es, "na_kT", [128, 4, T], BF16)
                Vx = sbuf(es, "na_V", [128, NTILE, 8, 65], BF16)
                hT = [sbuf(es, f"na_hT{i}", [128, 8, 512], BF16) for i in range(2)]
                MSET("pool", Vx[:, :, :, 64:65], 1.0, ["na_V"])
                with ExitStack() as es1:
                    Wk = load_w(es1, "na_Wk", IN_OFF["na_k"], 512)
                    Wv = load_w(es1, "na_Wv", IN_OFF["na_v"], 512)
                    DMA("sp", hT[0][:], h0T_v[:, :, 0:512], ["h0T_d"], ["na_hT0"])
                    for blk in range(NBLK):
                        b = blk % 2
                        hk = f"na_hT{b}"
                        if blk + 1 < NBLK:
                            DMA("sp", hT[1 - b][:], h0T_v[:, :, (blk + 1) * 512:(blk + 2) * 512], ["h0T_d"], [f"na_hT{1 - b}"])
                        for p in range(4):
                            pk = bank(p % 2)
                            for k in range(8):
                                MM(pk, Wk[:, k, p * 128:(p + 1) * 128], hT[b][:, k, :], k == 0, k == 7, [hk, "na_Wk"], [("B", p % 2)])
                            CP("act" if p % 2 == 0 else "dve", kT[:, p, blk * 512:(blk + 1) * 512], pk, [("B", p % 2)], ["na_kT"])
                        for t in range(4):
                            pv = bank(2 + t % 2)
                            for k in range(8):
                                MM(pv, hT[b][:, k, t * 128:(t + 1) * 128], Wv[:, k, :], k == 0, k == 7, [hk, "na_Wv"], [("B", 2 + t % 2)])
                            CP("dve" if t % 2 == 0 else "act", Vx[:, blk * 4 + t, :, 0:64], pv.rearrange("p (h d) -> p h d", h=8),
                               [("B", 2 + t % 2)], ["na_V"])
                S.barrier()
                with ExitStack() as es2:
                    tts = sbuf(es2, "na_tt", [128, 16, 512], BF16)
                    DMA("sp", tts[:], tt_d, [], ["na_tt"])
                    Wq = load_w(es2, "na_Wq", IN_OFF["na_q"], 512)
                    qbd = sbuf(es2, "na_qbd", [128, 4, 8, 128], BF16)
                    Eb = [sbuf(es2, f"na_E{i}", [128, 1280], BF16) for i in range(2)]
                    rec = sbuf(es2, "na_rec", [128, 4], F32)
                    asb = [sbuf(es2, f"na_a{i}", [128, 512], BF16) for i in range(2)]
                    aT1 = sbuf(es2, "na_aT", [128, 4, 512], BF16)
                    aT = [aT1, aT1]
                    MSET("pool", qbd[:], 0.0, ["na_qbd"])
                    DMA("sp", hT[0][:], h0T_v[:, :, 0:512], ["h0T_d"], ["na_hT0"])

                    def tiles_of(r):
                        rs = min(max(r - 4, 0), 120)
                        if rs % 2 == 0:
                            return [(rs // 2 + i, 2 * (rs // 2 + i) - r + 7) for i in range(4)]
                        m0 = (rs - 1) // 2
                        return [(m0, 14)] + [(m0 + i, 2 * (m0 + i) - r + 7) for i in range(1, 4)] + [(m0 + 4, 15)]

                    def qk(u, rl):
                        r, h2 = u // 2, u % 2
                        tl = tiles_of(r)
                        nch = len(tl)
                        sbk = PS[:, (u % 2) * 1536:(u % 2) * 1536 + 1536]
                        keys = [("B", (u % 2) * 3 + i) for i in range(3)]
                        for pi in range(2):
                            p = 2 * h2 + pi
                            for ci, (m, slab) in enumerate(tl):
                                slot = pi * nch + ci
                                o = sbk[:, slot * 128:(slot + 1) * 128]
                                MM(o, kT[:, p, m * 128:(m + 1) * 128], qbd[:, p, rl, :], True, False, ["na_kT", "na_qbd"], keys)
                                MM(o, ident, tts[:, slab, p * 128:(p + 1) * 128], False, True, ["cst", "na_tt"], keys)

                    def rest(u):
                        r, h2 = u // 2, u % 2
                        tl = tiles_of(r)
                        nch = len(tl)
                        sbk = PS[:, (u % 2) * 1536:(u % 2) * 1536 + 1536]
                        keys = [("B", (u % 2) * 3 + i) for i in range(3)]
                        E = Eb[u % 2]
                        ek = f"na_E{u % 2}"
                        n = 2 * nch * 128
                        ACT(E[:, 0:n], sbk[:, 0:n], AF.Exp, keys, [ek])
                        par = r % 2
                        prt = slice(par * 64, (par + 1) * 64)
                        pO = bank(6)
                        for pi in range(2):
                            for hh in range(2):
                                hq = pi * 2 + hh
                                head = 4 * h2 + hq
                                for ci, (m, slab) in enumerate(tl):
                                    c0 = (pi * nch + ci) * 128 + hh * 64
                                    MM(pO[prt, hq * 65:(hq + 1) * 65], E[:, c0:c0 + 64], Vx[:, m, head, :], ci == 0, ci == nch - 1,
                                       [ek, "na_V"], [("B", 6)])
                        pov = pO[prt, 0:260].rearrange("p (h d) -> p h d", d=65)
                        S.add("dve", lambda e: e.reciprocal(out=rec[prt, :], in_=pov[:, :, 64]), r=[("B", 6)], w=["na_rec"])
                        tb = (r // 2) % 2
                        TT("dve", asb[tb][prt, h2 * 256:(h2 + 1) * 256].rearrange("p (h d) -> p h d", d=64), pov[:, :, 0:64],
                           rec[prt, :].unsqueeze(2).to_broadcast([64, 4, 64]), ALU.mult, [("B", 6), "na_rec"], [f"na_a{tb}"])

                    for blk in range(NBLK):
                        b = blk % 2
                        hk = f"na_hT{b}"
                        if blk + 1 < NBLK:
                            DMA("sp", hT[1 - b][:], h0T_v[:, :, (blk + 1) * 512:(blk + 2) * 512], ["h0T_d"], [f"na_hT{1 - b}"])
                        for p in range(4):
                            pq = bank(7)
                            for k in range(8):
                                MM(pq, Wq[:, k, p * 128:(p + 1) * 128], hT[b][:, k, :], k == 0, k == 7, [hk, "na_Wq"], [("B", 7)])
                            ACT(qbd[0:64, p, :, 0:64], pq[0:64, :].rearrange("p (r c) -> p r c", r=8), AF.Copy, [("B", 7)], ["na_qbd"], scale=0.125)
                            TS("dve", qbd[64:128, p, :, 64:128], pq[64:128, :].rearrange("p (r c) -> p r c", r=8), 0.125, None, ALU.mult, None,
                               [("B", 7)], ["na_qbd"])
                        u0 = blk * 16
                        qk(u0, 0)
                        for ul in range(16):
                            u = u0 + ul
                            if ul + 1 < 16:
                                qk(u + 1, (ul + 1) // 2)
                            rest(u)
                            if ul % 4 == 3:
                                t = ul // 4
                                tb = ((u // 2) // 2) % 2
                                pT = bank16(7)
                                for c in range(4):
                                    TR(pT[:, c * 128:(c + 1) * 128], asb[tb][:, c * 128:(c + 1) * 128], ident, [f"na_a{tb}", "cst"], [("B", 7)])
                                CP("act", aT[b][:, :, t * 128:(t + 1) * 128], pT[:, 0:512].rearrange("p (c t) -> p c t", c=4), [("B", 7)], ["na_aT"])
                        DMA("sp", aT_v[:, :, blk * 512:(blk + 1) * 512], aT[b][:], ["na_aT"], ["aT_d"])

        def phaseM():
            with ExitStack() as es:
                ba_bf = sbuf(es, "m_ba", [1, D], BF16)
                with ExitStack() as est:
                    tmpb = sbuf(est, "m_tmpb", [1, D], F32)
                    DMA("sp", tmpb[:], lnp_d[0:1, 1, :], [], ["m_tmpb"])
                    TS("dve", tmpb[:], tmpb[:], ALPHA, None, ALU.mult, None, ["m_tmpb"], ["m_tmpb"])
                    CP("dve", ba_bf[:], tmpb[:], ["m_tmpb"], ["m_ba"])
                S.barrier()
                lnp = sbuf(es, "m_lnp", [128, 3, D], F32)
                DMA("sp", lnp[:, 0, :], lnp_d[:, 0, :], [], ["m_lnp"])
                DMA("sp", lnp[:, 1:3, :], lnp_d[:, 2:4, :], [], ["m_lnp"])
                TS("pool", lnp[:, 0, :], lnp[:, 0, :], ALPHA, None, ALU.mult, None, ["m_lnp"], ["m_lnp"])
                Wga = load_w(es, "m_Wga", IN_OFF["gate_a"], 1024)
                Wgb = load_w(es, "m_Wgb", IN_OFF["gate_b"], 1024)
                Wa = sbuf(es, "m_Wa", [128, 4, D], BF16)
                Wb = sbuf(es, "m_Wb", [128, 4, D], BF16)
                Wo = sbuf(es, "m_Wo", [128, 8, D], BF16)
                for k in range(4):
                    DMA("pool", Wa[:, k, :], w_pa_d[k * 128:(k + 1) * 128, :], [], ["m_Wa"])
                    DMA("pool", Wb[:, k, :], w_pb_d[k * 128:(k + 1) * 128, :], [], ["m_Wb"])
                for k in range(8):
                    DMA("pool", Wo[:, k, :], w_out_d[k * 128:(k + 1) * 128, :], [], ["m_Wo"])
                wr = sbuf(es, "m_wr", [128, 8, 36], F32)
                rb = sbuf(es, "m_rb", [128, 4, 36], F32)
                DMA("sp", wr[:], wr_d, [], ["m_wr"])
                DMA("sp", rb[:], rb_d, [], ["m_rb"])
                hT = [sbuf(es, f"m_hT{i}", [128, 8, 512], BF16) for i in range(2)]
                aTs1 = sbuf(es, "m_aT", [128, 4, 512], BF16)
                aTs = [aTs1, aTs1]
                cTs1 = sbuf(es, "m_cT", [128, 4, 512], BF16)
                cTs = [cTs1, cTs1]
                xt = sbuf(es, "m_xt", [128, 4, D], F32)
                sgb_ = sbuf(es, "m_sgb", [128, 512], F32)
                sg4 = [(sbuf(es, f"m_sga{i}", [128, 512], F32), sgb_) for i in range(2)]
                m1 = sbuf(es, "m_m1", [128, 512], F32)
                m2 = sbuf(es, "m_m2", [128, 512], F32)
                mT = sbuf(es, "m_mT", [128, 8, 512], BF16)
                zz = [sbuf(es, f"m_z{i}", [128, 4, D], F32) for i in range(2)]
                hx2 = [sbuf(es, f"m_hx{i}", [128, D], F32) for i in range(2)]
                h1b = sbuf(es, "m_h1b", [128, 4, D], BF16)
                h1T = sbuf(es, "m_h1T", [128, 4, 128], F32)
                st1 = sbuf(es, "m_st1", [128, 4, 12], F32)
                mv1 = sbuf(es, "m_mv1", [128, 4, 2], F32)
                rs1 = sbuf(es, "m_rs1", [128, 4], F32)
                nm1 = sbuf(es, "m_nm1", [128, 4], F32)
                L = sbuf(es, "m_L", [128, 4, 36], F32)
                gmax = sbuf(es, "m_gmax", [128, 4], F32)
                gm = sbuf(es, "m_gm", [128, 4, 4], F32)
                gd = sbuf(es, "m_gd", [128, 4, 4], F32)
                gw = sbuf(es, "m_gw", [128, 4], F32)
                EM = sbuf(es, "m_EM", [128, 4, 32], F32)
                EM2 = sbuf(es, "m_EM2", [128, 4, 32], F32)
                top1 = sbuf(es, "m_top1", [128, 4], F32)
                top2 = sbuf(es, "m_top2", [128, 4], F32)
                oh = [sbuf(es, f"m_oh{i}", [128, 4, 32], F32) for i in range(2)]
                cnt = sbuf(es, "m_cnt", [128, 4, 32], BF16)
                rank = sbuf(es, "m_rank", [128, 4, 32], F32)
                slot = sbuf(es, "m_slot", [128, 4, 32], F32)
                tmp = sbuf(es, "m_tmp", [128, 4, 32], F32)
                tot = sbuf(es, "m_tot", [128, 32], F32)
                sm = sbuf(es, "m_sm", [128, 8, 4], F32)
                MSET("dve", tot[:], 0.0, ["m_tot"])
                if "fakeidx" in dbg:
                    fake_ix = sbuf(es, "m_fake", [128, NTILE, 2], I32)
                    S.add("pool", lambda e: e.iota(fake_ix[:].rearrange("p n k -> p (n k)"), pattern=[[128, 128]], base=0, channel_multiplier=1),
                          w=["m_fake"])
                ident32 = cst32[:, 2, 0:128]
                ustr = cst[:, 3, 0:128]
                ebase = cst32[:, 2, 128:160]
                pidx = cst32[:, 2, 160:161]

                def loads_ac(blk):
                    DMA("sp", aTs[0][:], aT_v[:, :, blk * 512:(blk + 1) * 512], ["aT_d"], ["m_aT"])
                    DMA("sp", cTs[0][:], cT_v[:, :, blk * 512:(blk + 1) * 512], ["cT_d"], ["m_cT"])

                def loads(blk, b):
                    DMA("sp", hT[b][:], h0T_v[:, :, blk * 512:(blk + 1) * 512], ["h0T_d"], [f"m_hT{b}"])

                def stageA(blk):
                    b = blk % 2
                    z = zz[b]
                    if blk + 1 < NBLK:
                        loads(blk + 1, 1 - b)
                    rs0 = xstat[:, blk * 4:(blk + 1) * 4, 0]
                    nm0 = xstat[:, blk * 4:(blk + 1) * 4, 1]
                    for c in range(8):
                        ga_, gb_ = 0, 1
                        pa_, pb_ = (2, 3) if c % 2 == 0 else (4, 5)
                        cs = slice(c * 128, (c + 1) * 128)
                        for k in range(8):
                            MM(bank(ga_), Wga[:, k, cs], hT[b][:, k, :], k == 0, k == 7, [f"m_hT{b}", "m_Wga"], [("B", ga_)])
                        for k in range(4):
                            MM(bank(pa_), Wa[:, k, cs], aTs[b][:, k, :], k == 0, k == 3, ["m_aT", "m_Wa"], [("B", pa_)])
                        for k in range(8):
                            MM(bank(gb_), Wgb[:, k, cs], hT[b][:, k, :], k == 0, k == 7, [f"m_hT{b}", "m_Wgb"], [("B", gb_)])
                        for k in range(4):
                            MM(bank(pb_), Wb[:, k, cs], cTs[b][:, k, :], k == 0, k == 3, ["m_cT", "m_Wb"], [("B", pb_)])
                        sga, sgb = sg4[c % 2]
                        ACT(sga[:], bank(ga_), AF.Sigmoid, [("B", ga_)], [f"m_sga{c % 2}"])
                        ACT(sgb[:], bank(gb_), AF.Sigmoid, [("B", gb_)], ["m_sgb"])
                        TT("dve", m1[:], bank(pa_), sga[:], ALU.mult, [("B", pa_), f"m_sga{c % 2}"], ["m_m1"])
                        TT("dve", m2[:], bank(pb_), sgb[:], ALU.mult, [("B", pb_), "m_sgb"], ["m_m2"])
                        TT("pool", mT[:, c, :], m1[:], m2[:], ALU.add, ["m_m1", "m_m2"], [("m_mT", c)])
                    if blk + 1 < NBLK:
                        loads_ac(blk + 1)
                    mtk = [("m_mT", c) for c in range(8)]
                    for t in range(4):
                        hxx = hx2[t % 2]
                        hk_ = f"m_hx{t % 2}"
                        ACT(hxx[:], xt[:, t, :], AF.Identity, ["m_xt", "xstat"], [hk_], scale=rs0[:, t:t + 1], bias=nm0[:, t:t + 1])
                        TT("dve", hxx[:], hxx[:], lnp[:, 0, :], ALU.mult, [hk_, "m_lnp"], [hk_])
                        for n in range(2):
                            o = (2, 3, 4, 5)[(t * 2 + n) % 4]
                            ns = slice(n * 512, (n + 1) * 512)
                            MM(bank(o), cst[0:1, 4, 0:128], ba_bf[0:1, ns], True, False, ["cst", "m_ba"], [("B", o)])
                            for k in range(8):
                                MM(bank(o), mT[:, k, t * 128:(t + 1) * 128], Wo[:, k, ns], False, k == 7, mtk + ["m_Wo"], [("B", o)])
                            TT("dve", z[:, t, ns], hxx[:, ns], bank(o), ALU.add, [hk_, ("B", o)], [("m_z", b, t)])
                    if blk + 1 < NBLK:
                        DMA("sp", xt[:], x_v[blk + 1], [], ["m_xt"])

                def stageB(blk):
                    b = blk % 2
                    z = zz[b]
                    zk = [("m_z", b, t) for t in range(4)]
                    for t in range(4):
                        for c in range(2):
                            S.add("dve", lambda e, t=t, c=c: e.bn_stats(out=st1[:, t, c * 6:(c + 1) * 6], in_=z[:, t, c * 512:(c + 1) * 512]),
                                  r=[("m_z", b, t)], w=["m1st"])
                        S.add("dve", lambda e, t=t: e.bn_aggr(out=mv1[:, t, :], in_=st1[:, t, :]), r=["m1st"], w=["m1mv"])
                    ACT(rs1[:], mv1[:, :, 1], AF.Ln, ["m1mv"], ["m1rs"], bias=LN_EPS)
                    ACT(rs1[:], rs1[:], AF.Exp, ["m1rs"], ["m1rs"], scale=-0.5)
                    STT(nm1[:], mv1[:, :, 0], -1.0, rs1[:], ALU.mult, ALU.mult, ["m1mv", "m1rs"], ["m1nm"])
                    for t in range(4):
                        ACT(z[:, t, :], z[:, t, :], AF.Identity, [("m_z", b, t), "m1rs", "m1nm"], [("m_z", b, t)], scale=rs1[:, t:t + 1], bias=nm1[:, t:t + 1])
                        TT("dve", z[:, t, :], z[:, t, :], lnp[:, 1, :], ALU.mult, [("m_z", b, t), "m_lnp"], [("m_z", b, t)])
                        TT("pool", z[:, t, :], z[:, t, :], lnp[:, 2, :], ALU.add, [("m_z", b, t), "m_lnp"], [("m_z", b, t)])
                        CP("act", h1b[:, t, :], z[:, t, :], [("m_z", b, t)], [("m_h1b", t)])
                    DMA("sp", h1_v[blk], z[:], zk, ["h1_d"])
                    pL = bank(7)
                    for t in range(4):
                        for g in range(2):
                            for kk_ in range(4):
                                k = g * 4 + kk_
                                TR(bank(6)[:, kk_ * 128:(kk_ + 1) * 128], z[:, t, k * 128:(k + 1) * 128], ident32, [("m_z", b, t), "cst32"], [("B", 6)])
                            CP("act" if g == 0 else "dve", h1T[:, 0:4, :], bank(6).rearrange("p (k t) -> p k t", k=4),
                               [("B", 6)], ["m_h1T"])
                            for kk_ in range(4):
                                k = g * 4 + kk_
                                MM(pL[:, t * 36:(t + 1) * 36], h1T[:, kk_, :], wr[:, k, :], k == 0, k == 7, ["m_h1T", "m_wr"], [("B", 7)])
                    TT("dve", L[:], pL[:, 0:144].rearrange("p (t n) -> p t n", n=36), rb[:], ALU.add, [("B", 7), "m_rb"], ["m_L"])
                    GL = L[:, :, 0:4]
                    EL = L[:, :, 4:36].rearrange("p t (g e) -> p t g e", g=4)
                    S.add("dve", lambda e: e.tensor_reduce(out=gmax[:], in_=GL, axis=AX.X, op=ALU.max), r=["m_L"], w=["m_gmax"])
                    TT("dve", gm[:], GL, gmax[:].unsqueeze(2).to_broadcast([128, 4, 4]), ALU.is_equal, ["m_L", "m_gmax"], ["m_gm"])
                    TT("dve", gd[:], GL, gmax[:].unsqueeze(2).to_broadcast([128, 4, 4]), ALU.subtract, ["m_L", "m_gmax"], ["m_gd"])
                    ACT(gd[:], gd[:], AF.Exp, ["m_gd"], ["m_gd"])
                    S.add("dve", lambda e: e.tensor_reduce(out=gw[:], in_=gd[:], axis=AX.X, op=ALU.add), r=["m_gd"], w=["m_gw"])
                    S.add("dve", lambda e: e.reciprocal(out=gw[:], in_=gw[:]), r=["m_gw"], w=["m_gw"])
                    TS("dve", gm[:], gm[:], 1e9, -1e9, ALU.mult, ALU.add, ["m_gm"], ["m_gm"])
                    TT("dve", EM[:].rearrange("p t (g e) -> p t g e", g=4), EL, gm[:].unsqueeze(3).to_broadcast([128, 4, 4, 8]), ALU.add,
                       ["m_L", "m_gm"], ["m_EM"])
                    S.add("dve", lambda e: e.tensor_reduce(out=top1[:], in_=EM[:], axis=AX.X, op=ALU.max), r=["m_EM"], w=["m_top1"])
                    TT("dve", oh[0][:], EM[:], top1[:].unsqueeze(2).to_broadcast([128, 4, 32]), ALU.is_equal, ["m_EM", "m_top1"], ["m_oh0"])
                    STT(EM2[:], oh[0][:], -1e9, EM[:], ALU.mult, ALU.add, ["m_oh0", "m_EM"], ["m_EM2"])
                    S.add("dve", lambda e: e.tensor_reduce(out=top2[:], in_=EM2[:], axis=AX.X, op=ALU.max), r=["m_EM2"], w=["m_top2"])
                    TT("dve", oh[1][:], EM2[:], top2[:].unsqueeze(2).to_broadcast([128, 4, 32]), ALU.is_equal, ["m_EM2", "m_top2"], ["m_oh1"])
                    w1 = wall[:, blk * 4:(blk + 1) * 4, 0]
                    w2 = wall[:, blk * 4:(blk + 1) * 4, 1]
                    TT("dve", sm[:, 0, :], top2[:], top1[:], ALU.subtract, ["m_top1", "m_top2"], ["m_sm0"])
                    ACT(sm[:, 0, :], sm[:, 0, :], AF.Exp, ["m_sm0"], ["m_sm0"])
                    TS("dve", sm[:, 0, :], sm[:, 0, :], 1.0, None, ALU.add, None, ["m_sm0"], ["m_sm0"])
                    S.add("dve", lambda e: e.reciprocal(out=sm[:, 1, :], in_=sm[:, 0, :]), r=["m_sm0"], w=["m_sm1"])
                    TT("dve", w1, sm[:, 1, :], gw[:], ALU.mult, ["m_sm1", "m_gw"], [("wall", blk)])
                    TT("dve", w2, gw[:], w1, ALU.subtract, ["m_gw", ("wall", blk)], [("wall", blk)])
                    TT("dve", cnt[:], oh[0][:], oh[1][:], ALU.add, ["m_oh0", "m_oh1"], ["m_cnt"])
                    pR = bank(7)[:, 256:512]
                    for t in range(4):
                        MM(pR[:, t * 32:(t + 1) * 32], ustr, cnt[:, t, :], True, t == 0, ["m_cnt", "cst"], [("B", 7)])
                        for t2 in range(t):
                            MM(pR[:, t * 32:(t + 1) * 32], ones_bf, cnt[:, t2, :], False, t2 == t - 1, ["m_cnt", "cst"], [("B", 7)])
                    for t in range(4):
                        MM(pR[:, 128:160], ones_bf, cnt[:, t, :], t == 0, t == 3, ["m_cnt", "cst"], [("B", 7)])
                    TT("dve", rank[:], pR[:, 0:128].rearrange("p (t n) -> p t n", n=32), tot[:].unsqueeze(1).to_broadcast([128, 4, 32]), ALU.add,
                       [("B", 7), "m_tot"], ["m_rank"])
                    TT("dve", tot[:], tot[:], pR[:, 128:160], ALU.add, ["m_tot", ("B", 7)], ["m_tot"])
                    TT("dve", slot[:], rank[:], ebase.unsqueeze(1).to_broadcast([128, 4, 32]), ALU.add, ["m_rank", "cst32"], ["m_slot"])
                    for kx in range(2):
                        ohk = f"m_oh{kx}"
                        TT("dve", tmp[:], oh[kx][:], slot[:], ALU.mult, [ohk, "m_slot"], ["m_tmp"])
                        S.add("dve", lambda e: e.tensor_reduce(out=sm[:, 2, :], in_=tmp[:], axis=AX.X, op=ALU.add), r=["m_tmp"], w=["m_sm2"])
                        TT("dve", tmp[:], oh[kx][:], rank[:], ALU.mult, [ohk, "m_rank"], ["m_tmp"])
                        S.add("dve", lambda e: e.tensor_reduce(out=sm[:, 3, :], in_=tmp[:], axis=AX.X, op=ALU.add), r=["m_tmp"], w=["m_sm3"])
                        TS("dve", sm[:, 3, :], sm[:, 3, :], float(CAP), None, ALU.is_ge, None, ["m_sm3"], ["m_sm3"])
                        TS("dve", sm[:, 4, :], sm[:, 2, :], -1.0, pidx, ALU.mult, ALU.add, ["m_sm2", "cst32"], ["m_sm4"])
                        TT("dve", sm[:, 4, :], sm[:, 4, :], sm[:, 3, :], ALU.mult, ["m_sm4", "m_sm3"], ["m_sm4"])
                        TT("dve", sm[:, 2, :], sm[:, 2, :], sm[:, 4, :], ALU.add, ["m_sm2", "m_sm4"], ["m_sm2"])
                        CP("pool", posall[:, blk * 4:(blk + 1) * 4, kx], sm[:, 2, :], ["m_sm2"], [("posall", blk)])
                    for t in range(4):
                        if "noscatter" in dbg:
                            break
                        for kx in range(2):
                            ixap = posall[:, blk * 4 + t, kx:kx + 1]
                            if "fakeidx" in dbg:
                                ixap = fake_ix[:, blk * 4 + t, kx:kx + 1]
                            S.add("pool", lambda e, t=t, kx=kx, ixap=ixap: e.indirect_dma_start(
                                out=xbuf_d, out_offset=bass.IndirectOffsetOnAxis(ap=ixap, axis=0),
                                in_=h1b[:, t, :], in_offset=None), r=[("m_h1b", t), ("posall", blk)], w=["xbuf_d"], dma=True, cost=4.0)

                loads(0, 0)
                loads_ac(0)
                DMA("sp", xt[:], x_v[0], [], ["m_xt"])
                stageA(0)
                for blk in range(NBLK):
                    ra = None
                    if blk + 1 < NBLK:
                        S.record()
                        stageA(blk + 1)
                        ra = S.stop()
                    S.record()
                    stageB(blk)
                    rb_ = S.stop()
                    S.merge(ra, rb_)

        def dump_routing():
            pos_d = nc.dram_tensor("pos_d", [128, NTILE * 2], I32, kind="ExternalOutput").ap()
            wall_d = nc.dram_tensor("wall_d", [128, NTILE * 2], F32, kind="ExternalOutput").ap()
            allk = [("posall", b) for b in range(NBLK)]
            allw = [("wall", b) for b in range(NBLK)]
            DMA("sp", pos_d, posall[:].rearrange("p n k -> p (n k)"), allk, ["pos_d"])
            DMA("sp", wall_d, wall[:].rearrange("p n k -> p (n k)"), allw, ["wall_d"])

        def phaseE():
            with ExitStack() as es:
                wg = [sbuf(es, f"e_wg{i}", [128, 8, 512], BF16) for i in range(2)]
                wu = [sbuf(es, f"e_wu{i}", [128, 8, 512], BF16) for i in range(2)]
                wd = [sbuf(es, f"e_wd{i}", [128, 4, D], BF16) for i in range(2)]
                xs = [sbuf(es, f"e_xs{i}", [128, 6, D], BF16) for i in range(2)]
                xT2 = [sbuf(es, f"e_xT{i}", [128, 8, CAP], BF16) for i in range(2)]
                sg = sbuf(es, "e_sg", [128, 384], F32)
                tg = sbuf(es, "e_tg", [128, 384], F32)
                hTt = sbuf(es, "e_hT", [128, 4, CAP], BF16)
                ysb = [sbuf(es, f"e_y{i}", [128, D], F32) for i in range(2)]

                def wload(e, b):
                    DMA("pool", wg[b][:], w_gate_d[e].rearrange("(k p) n -> p k n", p=128), [], [f"e_wg{b}"])
                    DMA("pool", wu[b][:], w_up_d[e].rearrange("(k p) n -> p k n", p=128), [], [f"e_wu{b}"])
                    DMA("pool", wd[b][:], w_down_d[e].rearrange("(k p) n -> p k n", p=128), [], [f"e_wd{b}"])
                    DMA("sp", xs[b][:], xbuf_d[e * CAP:(e + 1) * CAP, :].rearrange("(s p) d -> p s d", p=128), ["xbuf_d"], [f"e_xs{b}"])

                def stageA(e):
                    b = e % 2
                    for k in range(8):
                        pb = bank16(k % 2)
                        for s_ in range(6):
                            TR(pb[:, s_ * 128:(s_ + 1) * 128], xs[b][:, s_, k * 128:(k + 1) * 128], ident, [f"e_xs{b}", "cst"], [("B", k % 2)])
                        CP("act" if k % 2 == 0 else "dve", xT2[b][:, k, :], pb[:, 0:CAP], [("B", k % 2)], [("e_xT", b, k)])

                def stageB(e):
                    b = e % 2
                    xT = xT2[b]
                    xtk = [("e_xT", b, k) for k in range(8)]
                    for m in range(4):
                        for half in range(2):
                            u = m * 2 + half
                            o = 2 + 2 * (u % 2)
                            hs = slice(half * 384, (half + 1) * 384)
                            ms = slice(m * 128, (m + 1) * 128)
                            for k in range(8):
                                MM(bank(o)[:, 0:384], wg[b][:, k, ms], xT[:, k, hs], k == 0, k == 7, xtk + [f"e_wg{b}"], [("B", o)])
                            for k in range(8):
                                MM(bank(o + 1)[:, 0:384], wu[b][:, k, ms], xT[:, k, hs], k == 0, k == 7, xtk + [f"e_wu{b}"], [("B", o + 1)])
                            ACT(sg[:], bank(o)[:, 0:384], AF.Sigmoid, [("B", o)], ["e_sg"])
                            TT("dve", tg[:], bank(o)[:, 0:384], sg[:], ALU.mult, [("B", o), "e_sg"], ["e_tg"])
                            TT("dve", hTt[:, m, hs], bank(o + 1)[:, 0:384], tg[:], ALU.mult, [("B", o + 1), "e_tg"], [("e_hT", m)])
                    htk = [("e_hT", m) for m in range(4)]
                    for s_ in range(6):
                        yb = (e * 6 + s_) % 2
                        for n in range(2):
                            for m in range(4):
                                MM(bank(6 + n), hTt[:, m, s_ * 128:(s_ + 1) * 128], wd[b][:, m, n * 512:(n + 1) * 512], m == 0, m == 3,
                                   htk + [f"e_wd{b}"], [("B", 6 + n)])
                            CP("act" if n == 0 else "dve", ysb[yb][:, n * 512:(n + 1) * 512], bank(6 + n), [("B", 6 + n)], [f"e_y{yb}"])
                        DMA("sp", ybuf_d[e * CAP + s_ * 128:e * CAP + (s_ + 1) * 128, :], ysb[yb][:], [f"e_y{yb}"], ["ybuf_d"])

                wload(0, 0)
                for e in range(32):
                    if e + 1 < 32:
                        wload(e + 1, (e + 1) % 2)
                    stageA(e)
                    stageB(e)

        def phaseG():
            with ExitStack() as es:
                lnp = sbuf(es, "g_lnp", [128, 2, D], F32)
                DMA("sp", lnp[:], lnp_d[:, 4:6, :], [], ["g_lnp"])
                y0 = [sbuf(es, f"g_y0{i}", [128, D], F32) for i in range(2)]
                y1 = [sbuf(es, f"g_y1{i}", [128, D], F32) for i in range(2)]
                h1t = [sbuf(es, f"g_h1{i}", [128, D], F32) for i in range(2)]
                zz = [sbuf(es, f"g_z{i}", [128, 1, D], F32) for i in range(2)]
                st2 = [sbuf(es, f"g_st{i}", [128, 1, 12], F32) for i in range(2)]
                mv2 = [sbuf(es, f"g_mv{i}", [128, 1, 2], F32) for i in range(2)]
                rs2 = [sbuf(es, f"g_rs{i}", [128, 1], F32) for i in range(2)]
                nm2 = [sbuf(es, f"g_nm{i}", [128, 1], F32) for i in range(2)]

                def gl(n, b):
                    S.add("pool", lambda e: e.indirect_dma_start(out=y0[b][:], out_offset=None, in_=ybuf_d,
                                                                 in_offset=bass.IndirectOffsetOnAxis(ap=posall[:, n, 0:1], axis=0)),
                          r=["ybuf_d"], w=[f"g_y0{b}"], dma=True, cost=5.0)
                    S.add("pool", lambda e: e.indirect_dma_start(out=y1[b][:], out_offset=None, in_=ybuf_d,
                                                                 in_offset=bass.IndirectOffsetOnAxis(ap=posall[:, n, 1:2], axis=0)),
                          r=["ybuf_d"], w=[f"g_y1{b}"], dma=True, cost=5.0)
                    DMA("sp", h1t[b][:], h1_tv[n], ["h1_d"], [f"g_h1{b}"])

                def stageA(n):
                    b = n % 2
                    zt = zz[b]
                    zk = f"g_z{b}"
                    TS("dve", zt[:, 0, :], y0[b][:], wall[:, n, 0:1], None, ALU.mult, None, [f"g_y0{b}"], [zk, zk + "a", zk + "b"])
                    STT(zt[:, 0, :], y1[b][:], wall[:, n, 1:2], zt[:, 0, :], ALU.mult, ALU.add, [f"g_y1{b}", zk], [zk])
                    STT(zt[:, 0, :], h1t[b][:], ALPHA, zt[:, 0, :], ALU.mult, ALU.add, [f"g_h1{b}", zk], [zk])
                    ln_stats(f"g{b}", zt, zk, st2[b], mv2[b], rs2[b], nm2[b], tiles=1)

                def stageB(n):
                    b = n % 2
                    zt = zz[b]
                    zk = f"g_z{b}"
                    ACT(zt[:, 0, :], zt[:, 0, :], AF.Identity, [zk, f"g{b}rs", f"g{b}nm"], [zk], scale=rs2[b][:, 0:1], bias=nm2[b][:, 0:1])
                    TT("dve", zt[:, 0, :], zt[:, 0, :], lnp[:, 0, :], ALU.mult, [zk, "g_lnp"], [zk])
                    TT("pool", zt[:, 0, 0:512], zt[:, 0, 0:512], lnp[:, 1, 0:512], ALU.add, [zk, "g_lnp"], [zk + "a"])
                    TT("dve", zt[:, 0, 512:1024], zt[:, 0, 512:1024], lnp[:, 1, 512:1024], ALU.add, [zk, "g_lnp"], [zk + "b"])
                    DMA("sp", out_v[n], zt[:, 0, :], [zk, zk + "a", zk + "b"], ["out"])

                gl(0, 0)
                gl(1, 1)
                for n in range(0, NTILE, 2):
                    sts = []
                    for n2 in (n, n + 1):
                        S.record()
                        stageA(n2)
                        if n2 + 2 < NTILE:
                            gl(n2 + 2, n2 % 2)
                        stageB(n2)
                        sts.append(S.stop())
                    S.merge(*sts)

        for ph in phases:
            if ph == "0":
                phase0()
            elif ph == "B":
                hgrn_phase(1)
            elif ph == "F":
                hgrn_phase(0)
            elif ph == "N":
                phaseN()
            elif ph == "M":
                phaseM()
                if "pos_d" in dbg:
                    dump_routing()
            elif ph == "E":
                phaseE()
            elif ph == "G":
                phaseG()
            S.barrier()
        S.emit(nc)
    return nc


def host_consts():
    bf = ml_dtypes.bfloat16
    cst = np.zeros((128, 5, 512), np.float32)
    eye = np.eye(128, dtype=np.float32)
    s = np.arange(128)[:, None]
    t = np.arange(128)[None, :]
    cst[:, 0, :] = np.tile(eye, (1, 4))
    cst[:, 1, :] = np.tile((s <= t).astype(np.float32), (1, 4))
    cst[:, 2, :] = np.tile((s >= t).astype(np.float32), (1, 4))
    cst[:, 3, :] = np.tile((s < t).astype(np.float32), (1, 4))
    cst[:, 4, :] = 1.0
    cst32 = np.zeros((128, 3, 512), np.float32)
    tt = np.arange(512)
    cst32[:, 0, :] = (tt % 128 != 0).astype(np.float32)[None, :]
    cst32[:, 1, :] = (tt % 128 != 127).astype(np.float32)[None, :]
    cst32[:, 2, 0:128] = eye
    cst32[:, 2, 128:160] = (np.arange(32) * CAP).astype(np.float32)[None, :]
    cst32[:, 2, 160] = 32 * CAP + np.arange(128)
    return cst.astype(bf), cst32


def host_tt(rpb):
    bf = ml_dtypes.bfloat16
    cols = np.arange(64)
    col_start = np.clip(cols - 8, 0, 48)
    c = cols[None, :]
    kc = cols[:, None]
    valid = (kc >= col_start[None, :]) & (kc < col_start[None, :] + 16)
    off = np.clip(kc - c + 15, 0, 30)
    full = np.full((15, 64, 8, 64), NEG, np.float32)
    for ro in range(15):
        for h in range(8):
            full[ro, :, h, :] = np.where(valid, rpb[h, ro][off], NEG)
    full = full.reshape(15, 64, 512)
    neg = np.full((64, 512), NEG, np.float32)
    tt = np.zeros((128, 16, 512), np.float32)
    for a in range(14):
        tt[0:64, a] = full[a]
        tt[64:128, a] = full[a + 1]
    tt[0:64, 14] = neg
    tt[64:128, 14] = full[3]
    tt[0:64, 15] = full[10]
    tt[64:128, 15] = neg
    return tt.astype(bf)


def make_in_maps(inp):
    cst, cst32 = host_consts()
    f = lambda a: np.ascontiguousarray(np.asarray(a, np.float32))
    lnp = np.stack([inp["emb_ln_g"], inp["emb_ln_b"], inp["ln1_g"][0], inp["ln1_b"][0], inp["ln2_g"][0], inp["ln2_b"][0]], 0)
    lnp = f(np.broadcast_to(lnp[None], (128, 6, D)))
    embp = f(np.concatenate([np.asarray(inp["emb_ln_g"]).reshape(8, 128).T, np.asarray(inp["emb_ln_b"]).reshape(8, 128).T], 1))
    lbraw = f(np.asarray(inp["hg_lb"]).reshape(2, 2, 4, 128).transpose(3, 0, 1, 2).reshape(128, 16))
    normg = f(np.asarray(inp["hg_norm_g"])[0].reshape(4, 128).T)
    wr = np.concatenate([np.asarray(inp["w_router_group"])[0], np.asarray(inp["w_router_expert"])[0]], 1)
    wr = f(wr.reshape(8, 128, 36).transpose(1, 0, 2))
    rb = np.concatenate([np.asarray(inp["b_router_group"])[0], np.asarray(inp["b_router_expert"])[0]], 0)
    rb = f(np.broadcast_to(rb[None, None], (128, 4, 36)))
    tt = host_tt(np.asarray(inp["na_rpb"], np.float32)[0])
    shared = dict(w_in=f(inp["w_in"][0]), w_proj_a=f(inp["w_proj_a"][0]), w_proj_b=f(inp["w_proj_b"][0]), w_out=f(inp["w_out"][0]),
                  w_gate=f(inp["w_gate"][0]), w_up=f(inp["w_up"][0]), w_down=f(inp["w_down"][0]),
                  lnp=lnp, embp=embp, lbraw=lbraw, normg=normg, wr=wr, rb=rb, tt=tt, cst=cst, cst32=cst32)
    x = np.asarray(inp["x"], np.float32)
    return [dict(shared, x=np.ascontiguousarray(x[b])) for b in range(x.shape[0])]


def kernel(**inputs):
    nc = build()
    in_maps = make_in_maps(inputs)
    res = run_bass_kernel_spmd(nc, in_maps, core_ids=list(range(8)))
    return np.stack([np.asarray(r["out"], np.float32) for r in res.results], 0)
```

```python
from contextlib import ExitStack
import numpy as np
import ml_dtypes
import concourse.bass as bass
import concourse.mybir as mybir
from concourse.bass_utils import run_bass_kernel_spmd

F32 = mybir.dt.float32
BF16 = mybir.dt.bfloat16
I32 = mybir.dt.int32
AF = mybir.ActivationFunctionType
ALU = mybir.AluOpType
AX = mybir.AxisListType

T = 8192
D = 1024
NBLK = 16
NTILE = 64
CAP = 768
NSLOT = 32 * CAP + 128
ALPHA = 2.0 ** 0.25
LN_EPS = 1e-5
RMS_EPS = 1e-6
NEG = -30000.0

ENGS = ("pe", "dve", "act", "pool", "sp")
EPOCH = 3000
NDMA_SEM = 8


class Op:
    __slots__ = ("eng", "fn", "deps", "sig", "sem", "val", "dma", "n")

    def __init__(self, eng, fn, dma):
        self.eng = eng
        self.fn = fn
        self.dma = dma
        self.deps = []
        self.sig = dma
        self.sem = None
        self.val = 0


class Sched:
    def __init__(self):
        self.q = {e: [] for e in ENGS}
        self.last_w = {}
        self.readers = {}
        self.dma_hist = {e: [] for e in ENGS}
        self.nops = 0
        self.bar = None
        self.bar_pending = set()
        self.rec = None
        self.m_wfin = {}
        self.m_rfin = {}
        self.m_efree = {}

    def barrier(self):
        ops = []
        for e in ENGS:
            last = None
            for op in reversed(self.q[e]):
                if not op.dma:
                    last = op
                    break
            if last is not None:
                ops.append(last)
            ops.extend(self.dma_hist[e][-NDMA_SEM:])
        self.bar = ops
        self.bar_pending = set(ENGS)

    def record(self):
        self.rec = []

    def stop(self):
        r = self.rec
        self.rec = None
        return r

    def merge(self, *streams):
        streams = [st for st in streams if st]
        pos = [0] * len(streams)
        total = sum(len(st) for st in streams)
        wfin = self.m_wfin
        rfin = self.m_rfin
        efree = self.m_efree
        for _ in range(total):
            best = None
            for i, st in enumerate(streams):
                if pos[i] >= len(st):
                    continue
                eng, fn, r, w, dma, cost = st[pos[i]]
                rdy = 0.0
                for k in r:
                    rdy = max(rdy, wfin.get(k, 0.0))
                for k in w:
                    rdy = max(rdy, wfin.get(k, 0.0), rfin.get(k, 0.0))
                start = max(efree.get(eng, 0.0), rdy + 0.1)
                rem = len(st) - pos[i]
                cand = (start, -rem, i)
                if best is None or cand < best[0]:
                    best = (cand, i, start)
            _, i, start = best
            eng, fn, r, w, dma, cost = streams[i][pos[i]]
            pos[i] += 1
            if dma:
                efree[eng] = start + 0.06
                fin = start + cost
            else:
                fin = start + cost
                efree[eng] = fin
            for k in r:
                rfin[k] = max(rfin.get(k, 0.0), fin)
            for k in w:
                wfin[k] = fin
                rfin[k] = 0.0
            self.add(eng, fn, r, w, dma, cost)

    def add(self, eng, fn, r=(), w=(), dma=False, cost=0.5):
        if self.rec is not None:
            self.rec.append((eng, fn, tuple(r), tuple(w), dma, cost))
            return None
        op = Op(eng, fn, dma)
        op.n = self.nops
        self.nops += 1
        deps = {}
        if eng in self.bar_pending:
            self.bar_pending.discard(eng)
            for p in self.bar:
                if p.dma or p.eng != eng:
                    deps[id(p)] = p
        for k in r:
            p = self.last_w.get(k)
            if p is not None and p is not op:
                if p.eng == eng and not p.dma and not dma:
                    if eng != "pe":
                        deps[id(p)] = p
                else:
                    deps[id(p)] = p
        for k in w:
            p = self.last_w.get(k)
            if p is not None and p is not op and (p.dma or dma or p.eng != eng or eng != "pe"):
                deps[id(p)] = p
            rd = self.readers.get(k)
            if rd:
                for p in rd.values():
                    if p is not op and (p.dma or dma or p.eng != eng or eng != "pe"):
                        deps[id(p)] = p
        for k in w:
            self.last_w[k] = op
            self.readers[k] = {}
        for k in r:
            d = self.readers.setdefault(k, {})
            d[("dma", op.n) if dma else eng] = op
        if dma:
            h = self.dma_hist[eng]
            if len(h) >= NDMA_SEM:
                p = h[-NDMA_SEM]
                deps[id(p)] = p
            h.append(op)
        op.deps = list(deps.values())
        for p in op.deps:
            p.sig = True
        self.q[eng].append(op)
        return op

    def emit(self, nc):
        with ExitStack() as es:
            for e in ENGS:
                cnt = 0
                sem = None
                dsem = [None] * NDMA_SEM
                dval = [0] * NDMA_SEM
                nd = 0
                ns = 0
                for op in self.q[e]:
                    if op.dma:
                        s = nd % NDMA_SEM
                        if dsem[s] is None:
                            dsem[s] = es.enter_context(nc.semaphore(f"d_{e}_{s}"))
                        dval[s] += 16
                        op.sem = dsem[s]
                        op.val = dval[s]
                        nd += 1
                    elif op.sig:
                        if sem is None or cnt >= EPOCH:
                            sem = es.enter_context(nc.semaphore(f"c_{e}_{ns}"))
                            ns += 1
                            cnt = 0
                        cnt += 1
                        op.sem = sem
                        op.val = cnt
            block = es.enter_context(nc.Block())
            engmap = {"pe": block.tensor, "dve": block.vector, "act": block.scalar,
                      "pool": block.gpsimd, "sp": block.sync}
            for e in ENGS:
                ops = self.q[e]
                if not ops:
                    continue

                def body(engine, ops=ops):
                    waited = {}
                    for op in ops:
                        for p in op.deps:
                            key = id(p.sem)
                            if waited.get(key, 0) >= p.val:
                                continue
                            waited[key] = p.val
                            engine.wait_ge(p.sem, p.val)
                        ins = op.fn(engine)
                        if op.sig:
                            ins.then_inc(op.sem, 16 if op.dma else 1)
                    last = {}
                    for op in ops:
                        if op.dma:
                            last[id(op.sem)] = op
                    for op in last.values():
                        if waited.get(id(op.sem), 0) < op.val:
                            engine.wait_ge(op.sem, op.val)

                engmap[e](body)


IN_OFF = dict(na_q=0, na_k=512, na_v=1024, hg_q=1536, hg_ff=2048, hg_fb=2560, hg_i=3072, hg_g=3584,
              gate_a=4096, gate_b=5120)


def build(dbg=(), phases="0BNFMEG"):
    nc = bass.Bass("TRN2", target_bir_lowering=False)
    S = Sched()

    def din(name, shape, dt):
        return nc.dram_tensor(name, list(shape), dt, kind="ExternalInput").ap()

    def dscr(name, shape, dt):
        kind = "ExternalOutput" if name in dbg else "Internal"
        return nc.dram_tensor(name, list(shape), dt, kind=kind).ap()

    x_d = din("x", [T, D], F32)
    w_in_d = din("w_in", [D, 6144], F32)
    w_pa_d = din("w_proj_a", [512, D], F32)
    w_pb_d = din("w_proj_b", [512, D], F32)
    w_out_d = din("w_out", [D, D], F32)
    w_gate_d = din("w_gate", [32, D, 512], F32)
    w_up_d = din("w_up", [32, D, 512], F32)
    w_down_d = din("w_down", [32, 512, D], F32)
    lnp_d = din("lnp", [128, 6, D], F32)
    embp_d = din("embp", [128, 16], F32)
    lbraw_d = din("lbraw", [128, 16], F32)
    normg_d = din("normg", [128, 4], F32)
    wr_d = din("wr", [128, 8, 36], F32)
    rb_d = din("rb", [128, 4, 36], F32)
    tt_d = din("tt", [128, 16, 512], BF16)
    cst_d = din("cst", [128, 5, 512], BF16)
    cst32_d = din("cst32", [128, 3, 512], F32)
    out_d = nc.dram_tensor("out", [T, D], F32, kind="ExternalOutput").ap()

    h0T_d = dscr("h0T_d", [D, T], BF16)
    obw_d = dscr("obw_d", [512, T], F32)
    aT_d = dscr("aT_d", [512, T], BF16)
    cT_d = dscr("cT_d", [512, T], BF16)
    h1_d = dscr("h1_d", [T, D], F32)
    xbuf_d = dscr("xbuf_d", [NSLOT, D], BF16)
    ybuf_d = dscr("ybuf_d", [NSLOT, D], F32)

    h0T_v = h0T_d.rearrange("(k p) t -> p k t", p=128)
    obw_v = obw_d.rearrange("(h p) t -> p h t", p=128)
    aT_v = aT_d.rearrange("(k p) t -> p k t", p=128)
    cT_v = cT_d.rearrange("(k p) t -> p k t", p=128)
    x_v = x_d.rearrange("(b t p) d -> b p t d", t=4, p=128)
    h1_v = h1_d.rearrange("(b t p) d -> b p t d", t=4, p=128)
    out_v = out_d.rearrange("(n p) d -> n p d", p=128)
    h1_tv = h1_d.rearrange("(n p) d -> n p d", p=128)
    w_in_v = w_in_d.rearrange("(k p) n -> p k n", p=128)

    with ExitStack() as ges:
        uid = [0]

        def sbuf(es, name, shape, dt):
            uid[0] += 1
            return es.enter_context(nc.sbuf_tensor(f"s{uid[0]}_{name}", list(shape), dt))

        PS = ges.enter_context(nc.psum_tensor("PS", [128, 4096], F32))

        def bank(i, n=1):
            return PS[:, i * 512:(i + n) * 512]

        def bank16(i):
            return PS[:, i * 512:(i + 1) * 512].bitcast(BF16)

        cst = sbuf(ges, "cst", [128, 5, 512], BF16)
        cst32 = sbuf(ges, "cst32", [128, 3, 512], F32)
        embp = sbuf(ges, "embp", [128, 16], F32)
        lbraw = sbuf(ges, "lbraw", [128, 16], F32)
        lbp = sbuf(ges, "lbp", [128, 3, 8], F32)
        normg = sbuf(ges, "normg", [128, 4], F32)
        posall = sbuf(ges, "posall", [128, NTILE, 2], I32)
        wall = sbuf(ges, "wall", [128, NTILE, 2], F32)
        xstat = sbuf(ges, "xstat", [128, NTILE, 2], F32)
        ident = cst[:, 0, 0:128]
        ones_bf = cst[:, 4, 0:128]

        def nfree(ap):
            n = 1
            for d in ap.shape[1:]:
                n *= d
            return n

        def DMA(eng, out, in_, r, w):
            S.add(eng, lambda e: e.dma_start(out=out, in_=in_), r=r, w=w, dma=True, cost=2.5 + nfree(out) * 128 * 4 / 150e3)

        def MM(out, lhsT, rhs, start, stop, r, w):
            c_ = max(64, nfree(rhs)) / 2400.0 * (4.0 if rhs.dtype == F32 else 1.0) + 0.01
            S.add("pe", lambda e: e.matmul(out, lhsT=lhsT, rhs=rhs, start=start, stop=stop), r=r, w=w, cost=c_)

        def TR(out, in_, idn, r, w):
            S.add("pe", lambda e: e.transpose(out, in_, idn), r=r, w=w, cost=0.08)

        def ACT(out, in_, func, r, w, scale=1.0, bias=0.0):
            S.add("act", lambda e: e.activation(out=out, in_=in_, func=func, bias=bias, scale=scale), r=r, w=w, cost=0.25 + nfree(out) / 1200.0)

        def vcost(eng, out):
            return (0.3 + nfree(out) / 330.0) if eng == "pool" else (0.16 + nfree(out) / 960.0)

        def TT(eng, out, in0, in1, op, r, w):
            S.add(eng, lambda e: e.tensor_tensor(out=out, in0=in0, in1=in1, op=op), r=r, w=w, cost=vcost(eng, out))

        def TS(eng, out, in0, s1, s2, op0, op1, r, w):
            if s2 is None:
                S.add(eng, lambda e: e.tensor_scalar(out=out, in0=in0, scalar1=s1, scalar2=None, op0=op0), r=r, w=w, cost=vcost(eng, out))
            else:
                S.add(eng, lambda e: e.tensor_scalar(out=out, in0=in0, scalar1=s1, scalar2=s2, op0=op0, op1=op1), r=r, w=w, cost=vcost(eng, out))

        def STT(out, in0, scalar, in1, op0, op1, r, w):
            S.add("dve", lambda e: e.scalar_tensor_tensor(out=out, in0=in0, scalar=scalar, in1=in1, op0=op0, op1=op1), r=r, w=w, cost=vcost("dve", out))

        def CP(eng, out, in_, r, w):
            if eng == "act":
                S.add("act", lambda e: e.activation(out=out, in_=in_, func=AF.Copy), r=r, w=w, cost=0.25 + nfree(out) / 1200.0)
            else:
                S.add(eng, lambda e: e.tensor_copy(out=out, in_=in_), r=r, w=w, cost=vcost(eng, out))

        def MSET(eng, ap, val, w):
            S.add(eng, lambda e: e.memset(ap, val), w=w)

        DMA("sp", cst[:], cst_d, [], ["cst"])
        DMA("sp", cst32[:], cst32_d, [], ["cst32"])
        DMA("sp", embp[:], embp_d, [], ["embp"])
        DMA("sp", lbraw[:], lbraw_d, [], ["lbraw"])
        DMA("sp", normg[:], normg_d, [], ["normg"])
        lbr = lbraw[:].rearrange("p (d l h) -> p d l h", d=2, l=2)
        lb_v = lbp[:, 0, :].rearrange("p (d h) -> p d h", d=2)
        TT("dve", lb_v, lbr[:, :, 1, :], lbr[:, :, 0, :], ALU.subtract, ["lbraw"], ["lbp"])
        ACT(lbp[:, 0, :], lbp[:, 0, :], AF.Exp, ["lbp"], ["lbp"])
        ACT(lbp[:, 0, :], lbp[:, 0, :], AF.Ln, ["lbp"], ["lbp"], bias=1.0)
        ACT(lbp[:, 0, :], lbp[:, 0, :], AF.Exp, ["lbp"], ["lbp"], scale=-1.0)
        TS("dve", lbp[:, 1, :], lbp[:, 0, :], -1.0, 1.0, ALU.mult, ALU.add, ["lbp"], ["lbp"])
        TS("dve", lbp[:, 2, :], lbp[:, 1, :], -1.0, None, ALU.mult, None, ["lbp"], ["lbp"])

        def ln_stats(es_name, xt, xkey, stats, mv, rstd, nmr, tiles=4):
            for t in range(tiles):
                for c in range(2):
                    S.add("dve", lambda e, t=t, c=c: e.bn_stats(out=stats[:, t, c * 6:(c + 1) * 6], in_=xt[:, t, c * 512:(c + 1) * 512]),
                          r=[xkey], w=[es_name + "st"])
                S.add("dve", lambda e, t=t: e.bn_aggr(out=mv[:, t, :], in_=stats[:, t, :]), r=[es_name + "st"], w=[es_name + "mv"])
            ACT(rstd[:, 0:tiles], mv[:, 0:tiles, 1], AF.Ln, [es_name + "mv"], [es_name + "rs", "xstat"], bias=LN_EPS)
            ACT(rstd[:, 0:tiles], rstd[:, 0:tiles], AF.Exp, [es_name + "rs"], [es_name + "rs", "xstat"], scale=-0.5)
            STT(nmr[:, 0:tiles], mv[:, 0:tiles, 0], -1.0, rstd[:, 0:tiles], ALU.mult, ALU.mult, [es_name + "mv", es_name + "rs"], [es_name + "nm", "xstat"])

        def phase0():
            with ExitStack() as es:
                xt = [sbuf(es, f"p0_xt{i}", [128, 4, D], F32) for i in range(2)]
                xn2 = [sbuf(es, f"p0_xn{i}", [128, 4, D], BF16) for i in range(2)]
                hT = [sbuf(es, f"p0_hT{i}", [128, 8, 512], BF16) for i in range(2)]
                stats2 = [sbuf(es, f"p0_stats{i}", [128, 4, 12], F32) for i in range(2)]
                mv2 = [sbuf(es, f"p0_mv{i}", [128, 4, 2], F32) for i in range(2)]
                def stageA(blk):
                    b = blk % 2
                    if blk + 1 < NBLK:
                        DMA("sp", xt[1 - b][:], x_v[blk + 1], [], [f"xt{1 - b}"])
                    rstd = xstat[:, blk * 4:(blk + 1) * 4, 0]
                    nmr = xstat[:, blk * 4:(blk + 1) * 4, 1]
                    ln_stats(f"p0{b}", xt[b], f"xt{b}", stats2[b], mv2[b], rstd, nmr)
                    for t in range(4):
                        ACT(xn2[b][:, t, :], xt[b][:, t, :], AF.Identity, [f"xt{b}", f"p0{b}rs", f"p0{b}nm"], [("xn", b, t)],
                            scale=rstd[:, t:t + 1], bias=nmr[:, t:t + 1])

                def stageB(blk):
                    b = blk % 2
                    xn = xn2[b]
                    for k in range(8):
                        pb = bank16(k % 2)
                        for t in range(4):
                            TR(pb[:, t * 128:(t + 1) * 128], xn[:, t, k * 128:(k + 1) * 128], ident, [("xn", b, t), "cst"], [("B", k % 2)])
                        if k % 2 == 0:
                            ACT(hT[b][:, k, :], pb[:, 0:512], AF.Identity, [("B", k % 2), "embp"], [f"hT{b}"],
                                scale=embp[:, k:k + 1], bias=embp[:, 8 + k:9 + k])
                        else:
                            TS("dve", hT[b][:, k, :], pb[:, 0:512], embp[:, k:k + 1], embp[:, 8 + k:9 + k], ALU.mult, ALU.add,
                               [("B", k % 2), "embp"], [f"hT{b}"])
                    DMA("sp", h0T_v[:, :, blk * 512:(blk + 1) * 512], hT[b][:], [f"hT{b}"], ["h0T_d"])

                DMA("sp", xt[0][:], x_v[0], [], ["xt0"])
                stageA(0)
                for blk in range(NBLK):
                    S.record()
                    if blk + 1 < NBLK:
                        stageA(blk + 1)
                    ra = S.stop()
                    S.record()
                    stageB(blk)
                    rb_ = S.stop()
                    S.merge(ra, rb_)

        def load_w(es, name, col0, ncol):
            w = sbuf(es, name, [128, 8, ncol], BF16)
            for k in range(8):
                DMA("pool", w[:, k, :], w_in_v[:, k, col0:col0 + ncol], [], [name])
            return w

        def hgrn_phase(direction):
            fwd = direction == 0
            with ExitStack() as es:
                Wq = load_w(es, "hg_Wq", IN_OFF["hg_q"], 512)
                Wf = load_w(es, "hg_Wf", IN_OFF["hg_ff"] if fwd else IN_OFF["hg_fb"], 512)
                Wi = load_w(es, "hg_Wi", IN_OFF["hg_i"], 512)
                Wg = load_w(es, "hg_Wg", IN_OFF["hg_g"], 512) if fwd else None
                hT = [sbuf(es, f"hg_hT{i}", [128, 8, 512], BF16) for i in range(2)]
                tnames = ["e1", "l1", "l2", "sq", "qf", "g", "fg", "kk", "bc", "eb", "enb", "kd32"]
                tmps = [{n: sbuf(es, f"hg_{n}{i}", [128, 512], F32) for n in tnames} for i in range(2)]
                ebl2 = [sbuf(es, f"hg_ebl{i}", [128, 4, 4], F32) for i in range(2)]
                Qd2 = [sbuf(es, f"hg_Qd{i}", [128, 4, 512], BF16) for i in range(2)]
                Kd2 = [sbuf(es, f"hg_Kd{i}", [128, 4, 512], BF16) for i in range(2)]
                Kl2 = [sbuf(es, f"hg_Kl{i}", [128, 4, 512], BF16) for i in range(2)]
                vsb2 = [sbuf(es, f"hg_v{i}", [128, 4, 512], BF16) for i in range(2)]
                klt = sbuf(es, "hg_klt", [128, 512], BF16)
                at = sbuf(es, "hg_at", [128, 512], BF16)
                osb = [sbuf(es, f"hg_o{i}", [128, 4, 512], F32) for i in range(2)]
                St = sbuf(es, "hg_S", [128, 4, 128], F32)
                Sb2 = [sbuf(es, f"hg_Sb{i}", [128, 4, 128], BF16) for i in range(2)]
                if fwd:
                    obw = sbuf(es, "hg_obw", [128, 4, 512], F32)
                    osq = sbuf(es, "hg_osq", [128, 4, 512], BF16)
                    rs = sbuf(es, "hg_rs", [128, 512], F32)
                    sg2 = [sbuf(es, f"hg_sg{i}", [128, 4, 512], F32) for i in range(2)]
                    cT = [sbuf(es, f"hg_cT{i}", [128, 4, 512], BF16) for i in range(2)]
                zlist = []
                if not fwd:
                    zt = sbuf(es, "hg_zero", [128, 8192], BF16)
                    MSET("pool", zt[:], 0.0, ["hg_zero"])
                    xb_flat = xbuf_d.rearrange("(p a) d -> p (a d)", p=128)
                    ncol = NSLOT * D // 128
                    c0 = 0
                    while c0 < ncol:
                        cw = min(8192, ncol - c0)
                        zlist.append((c0, cw))
                        c0 += cw
                MSET("dve", St[:], 0.0, ["hg_S"])
                MSET("dve", Sb2[0][:], 0.0, ["hg_Sb0"])
                MSET("dve", Sb2[1][:], 0.0, ["hg_Sb1"])
                gcount = [0]
                di = 0 if fwd else 1
                mask = cst[:, 1 if fwd else 2, :]
                blocks = list(range(NBLK)) if fwd else list(range(NBLK - 1, -1, -1))
                tiles = [0, 1, 2, 3] if fwd else [3, 2, 1, 0]
                DMA("sp", hT[0][:], h0T_v[:, :, blocks[0] * 512:(blocks[0] + 1) * 512], ["h0T_d"], ["hg_hT0"])

                def vproj(bi, tl, bk):
                    b = bi % 2
                    hk = f"hg_hT{b}"
                    vsb = vsb2[b]
                    for t in tl:
                        pv = bank(bk)
                        for k in range(8):
                            MM(pv, hT[b][:, k, t * 128:(t + 1) * 128], Wi[:, k, :], k == 0, k == 7, [hk, "hg_Wi"], [("B", bk)])
                        CP("act" if t % 2 == 0 else "dve", vsb[:, t, :], pv, [("B", bk)], [("hg_v", b, t)])

                def head(bi, h, si):
                    b = bi % 2
                    hk = f"hg_hT{b}"
                    Qd, Kd, Kl, ebl = Qd2[b], Kd2[b], Kl2[b], ebl2[b]
                    tm = tmps[si]
                    e1, l1, l2, sq, qf, gg, fg, kk, bc, eb, enb, kd32 = [tm[n] for n in tnames]
                    K_ = lambda n: f"hg_{n}{si}"
                    pf = bank(2 * si)
                    pq = bank(2 * si + 1)
                    pfk = ("B", 2 * si)
                    pqk = ("B", 2 * si + 1)
                    for k in range(8):
                        MM(pf, Wf[:, k, h * 128:(h + 1) * 128], hT[b][:, k, :], k == 0, k == 7, [hk, "hg_Wf"], [pfk])
                    for k in range(8):
                        MM(pq, Wq[:, k, h * 128:(h + 1) * 128], hT[b][:, k, :], k == 0, k == 7, [hk, "hg_Wq"], [pqk])
                    lb = lbp[:, 0, di * 4 + h:di * 4 + h + 1]
                    ACT(e1[:], pf, AF.Exp, [pfk], [K_("e1")], scale=-1.0)
                    ACT(l1[:], e1[:], AF.Ln, [K_("e1")], [K_("l1")], bias=1.0)
                    ACT(l2[:], e1[:], AF.Ln, [K_("e1"), "lbp"], [K_("l2")], scale=lb, bias=1.0)
                    TT("pool", gg[:], l2[:], l1[:], ALU.subtract, [K_("l1"), K_("l2")], [K_("g")])
                    ACT(fg[:], gg[:], AF.Exp, [K_("g")], [K_("fg")])
                    TS("pool", kk[:], fg[:], -1.0, 1.0, ALU.mult, ALU.add, [K_("fg")], [K_("kk")])
                    ACT(e1[:], pq, AF.Exp, [pqk], [K_("e1")], scale=-1.0)
                    ACT(l1[:], e1[:], AF.Ln, [K_("e1")], [K_("l1")], bias=1.0)
                    ACT(sq[:], l1[:], AF.Exp, [K_("l1")], [K_("sq")], scale=-1.0)
                    TT("dve", qf[:], pq, sq[:], ALU.mult, [pqk, K_("sq")], [K_("qf")])
                    if fwd:
                        S.add("dve", lambda e: e.tensor_tensor_scan(out=bc[:], data0=cst32[:, 0, :], data1=gg[:], initial=0.0,
                                                                    op0=ALU.mult, op1=ALU.add), r=[K_("g"), "cst32"], w=[K_("bc")], cost=1.25)
                        blast = bc[:].rearrange("p (c t) -> p c t", t=128)[:, :, 127]
                    else:
                        S.add("dve", lambda e: e.tensor_tensor_scan(out=bc[:, ::-1], data0=cst32[:, 1, ::-1], data1=gg[:, ::-1], initial=0.0,
                                                                    op0=ALU.mult, op1=ALU.add), r=[K_("g"), "cst32"], w=[K_("bc")], cost=1.25)
                        blast = bc[:].rearrange("p (c t) -> p c t", t=128)[:, :, 0]
                    ACT(eb[:], bc[:], AF.Exp, [K_("bc")], [K_("eb")])
                    ACT(enb[:], bc[:], AF.Exp, [K_("bc")], [K_("enb")], scale=-1.0)
                    ACT(ebl[:, h, :], blast, AF.Exp, [K_("bc")], [("hg_ebl", b, h)])
                    TT("dve", Qd[:, h, :], qf[:], eb[:], ALU.mult, [K_("qf"), K_("eb")], [("hg_Qd", b, h)])
                    TT("pool", kd32[:], kk[:], enb[:], ALU.mult, [K_("kk"), K_("enb")], [K_("kd32")])
                    CP("pool", Kd[:, h, :], kd32[:], [K_("kd32")], [("hg_Kd", b, h)])
                    TT("dve", Kl[:, h, :].rearrange("p (c t) -> p c t", t=128), kd32[:].rearrange("p (c t) -> p c t", t=128),
                       ebl[:, h, :].unsqueeze(2).to_broadcast([128, 4, 128]), ALU.mult, [K_("kd32"), ("hg_ebl", b, h)], [("hg_Kl", b, h)])
                    if fwd:
                        for k in range(8):
                            MM(pq, Wg[:, k, h * 128:(h + 1) * 128], hT[b][:, k, :], k == 0, k == 7, [hk, "hg_Wg"], [pqk])
                        ACT(e1[:], pq, AF.Exp, [pqk], [K_("e1")], scale=-1.0)
                        ACT(l1[:], e1[:], AF.Ln, [K_("e1")], [K_("l1")], bias=1.0)
                        ACT(sq[:], l1[:], AF.Exp, [K_("l1")], [K_("sq")], scale=-1.0)
                        TT("dve", sg2[b][:, h, :], pq, sq[:], ALU.mult, [pqk, K_("sq")], [("hg_sg", b, h)])

                def streamsA(bi):
                    b = bi % 2
                    out = []
                    for si in range(2):
                        S.record()
                        if si == 0 and bi + 1 < NBLK:
                            nb = blocks[bi + 1]
                            DMA("sp", hT[1 - b][:], h0T_v[:, :, nb * 512:(nb + 1) * 512], ["h0T_d"], [f"hg_hT{1 - b}"])
                        vproj(bi, [2 * si, 2 * si + 1], 2 * si)
                        head(bi, si, si)
                        head(bi, si + 2, si)
                        out.append(S.stop())
                    return out

                def stageB(bi):
                    b = bi % 2
                    blk = blocks[bi]
                    Qd, Kd, Kl, vsb, ebl = Qd2[b], Kd2[b], Kl2[b], vsb2[b], ebl2[b]
                    allq = [("hg_Qd", b, h) for h in range(4)]
                    allk = [("hg_Kd", b, h) for h in range(4)]
                    alll = [("hg_Kl", b, h) for h in range(4)]
                    ob = osb[b]
                    if fwd:
                        DMA("sp", obw[:], obw_v[:, :, blk * 512:(blk + 1) * 512], ["obw_d"], ["hg_obw"])
                    for t in tiles:
                        ts = slice(t * 128, (t + 1) * 128)
                        g = gcount[0]
                        gcount[0] += 1
                        Sold, Snew = Sb2[(g + 1) % 2], Sb2[g % 2]
                        soldk, snewk = f"hg_Sb{(g + 1) % 2}", f"hg_Sb{g % 2}"
                        pk = bank16(4)
                        for h in range(4):
                            TR(pk[:, h * 128:(h + 1) * 128], Kl[:, h, ts], ident, alll + ["cst"], [("B", 4)])
                        CP("act", klt[:], pk[:, 0:512], [("B", 4)], ["hg_klt"])
                        pa = bank(5)
                        for h in range(4):
                            MM(pa[:, h * 128:(h + 1) * 128], Kd[:, h, ts], Qd[:, h, ts], True, True, allq + allk, [("B", 5)])
                        TT("dve", at[:], pa, mask, ALU.mult, [("B", 5), "cst"], ["hg_at"])
                        pS = bank(7)
                        for h in range(4):
                            MM(pS[:, h * 128:(h + 1) * 128], klt[:, h * 128:(h + 1) * 128], vsb[:, t, h * 128:(h + 1) * 128], True, True,
                               ["hg_klt", ("hg_v", b, t)], [("B", 7)])
                        po = bank(6)
                        for h in range(4):
                            MM(po[:, h * 128:(h + 1) * 128], vsb[:, t, h * 128:(h + 1) * 128], at[:, h * 128:(h + 1) * 128], True, False,
                               [("hg_v", b, t), "hg_at"], [("B", 6)])
                            MM(po[:, h * 128:(h + 1) * 128], Sold[:, h, :], Qd[:, h, ts], False, True, [soldk] + allq, [("B", 6)])
                        for h in range(4):
                            STT(St[:, h, :], St[:, h, :], ebl[:, h, t:t + 1], pS[:, h * 128:(h + 1) * 128], ALU.mult, ALU.add,
                                ["hg_S", ("hg_ebl", b, h), ("B", 7)], ["hg_S"])
                        CP("act", Snew[:], St[:], ["hg_S"], [snewk])
                        CP("act", ob[:, :, ts], po.rearrange("p (h t) -> p h t", h=4), [("B", 6)], [f"hg_o{b}"])
                    if not fwd:
                        DMA("sp", obw_v[:, :, blk * 512:(blk + 1) * 512], ob[:], [f"hg_o{b}"], ["obw_d"])
                        for _ in range(2):
                            if zlist:
                                c0, cw = zlist.pop(0)
                                DMA("sp", xb_flat[:, c0:c0 + cw], zt[:, 0:cw], ["hg_zero"], [("xbuf_z", c0)])
                    else:
                        TT("pool", ob[:], ob[:], obw[:], ALU.add, [f"hg_o{b}", "hg_obw"], [f"hg_o{b}"])
                        TT("pool", osq[:], ob[:], ob[:], ALU.mult, [f"hg_o{b}"], ["hg_osq"])
                        for h in range(4):
                            pr = bank(4)
                            MM(pr, ones_bf, osq[:, h, :], True, True, ["hg_osq", "cst"], [("B", 4)])
                            ACT(rs[:], pr, AF.Ln, [("B", 4)], ["hg_rs"], scale=1.0 / 128.0, bias=RMS_EPS)
                            ACT(rs[:], rs[:], AF.Exp, ["hg_rs"], ["hg_rs"], scale=-0.5)
                            TT("dve", rs[:], rs[:], ob[:, h, :], ALU.mult, ["hg_rs", f"hg_o{b}"], ["hg_rs"])
                            STT(cT[b][:, h, :], rs[:], normg[:, h:h + 1], sg2[b][:, h, :], ALU.mult, ALU.mult,
                                ["hg_rs", ("hg_sg", b, h), "normg"], [f"hg_cT{b}"])
                        DMA("sp", cT_v[:, :, blk * 512:(blk + 1) * 512], cT[b][:], [f"hg_cT{b}"], ["cT_d"])

                for st_ in streamsA(0):
                    S.merge(st_)
                for bi in range(NBLK):
                    sts = streamsA(bi + 1) if bi + 1 < NBLK else []
                    S.record()
                    stageB(bi)
                    rb_ = S.stop()
                    S.merge(*(sts + [rb_]))

        def phaseN():
            with ExitStack() as es:
                kT = sbuf(es, "na_kT", [128, 4, T], BF16)
                Vx = sbuf(es, "na_V", [128, NTILE, 8, 65], BF16)
                hT = [sbuf(es, f"na_hT{i}", [128, 8, 512], BF16) for i in range(2)]
                MSET("pool", Vx[:, :, :, 64:65], 1.0, ["na_V"])
                with ExitStack() as es1:
                    Wk = load_w(es1, "na_Wk", IN_OFF["na_k"], 512)
                    Wv = load_w(es1, "na_Wv", IN_OFF["na_v"], 512)
                    DMA("sp", hT[0][:], h0T_v[:, :, 0:512], ["h0T_d"], ["na_hT0"])
                    for blk in range(NBLK):
                        b = blk % 2
                        hk = f"na_hT{b}"
                        if blk + 1 < NBLK:
                            DMA("sp", hT[1 - b][:], h0T_v[:, :, (blk + 1) * 512:(blk + 2) * 512], ["h0T_d"], [f"na_hT{1 - b}"])
                        for p in range(4):
                            pk = bank(p % 2)
                            for k in range(8):
                                MM(pk, Wk[:, k, p * 128:(p + 1) * 128], hT[b][:, k, :], k == 0, k == 7, [hk, "na_Wk"], [("B", p % 2)])
                            CP("act" if p % 2 == 0 else "dve", kT[:, p, blk * 512:(blk + 1) * 512], pk, [("B", p % 2)], ["na_kT"])
                        for t in range(4):
                            pv = bank(2 + t % 2)
                            for k in range(8):
                                MM(pv, hT[b][:, k, t * 128:(t + 1) * 128], Wv[:, k, :], k == 0, k == 7, [hk, "na_Wv"], [("B", 2 + t % 2)])
                            CP("dve" if t % 2 == 0 else "act", Vx[:, blk * 4 + t, :, 0:64], pv.rearrange("p (h d) -> p h d", h=8),
                               [("B", 2 + t % 2)], ["na_V"])
                S.barrier()
                with ExitStack() as es2:
                    tts = sbuf(es2, "na_tt", [128, 16, 512], BF16)
                    DMA("sp", tts[:], tt_d, [], ["na_tt"])
                    Wq = load_w(es2, "na_Wq", IN_OFF["na_q"], 512)
                    qbd = sbuf(es2, "na_qbd", [128, 4, 8, 128], BF16)
                    Eb = [sbuf(es2, f"na_E{i}", [128, 1280], BF16) for i in range(2)]
                    rec = sbuf(es2, "na_rec", [128, 4], F32)
                    asb = [sbuf(es2, f"na_a{i}", [128, 512], BF16) for i in range(2)]
                    aT1 = sbuf(es2, "na_aT", [128, 4, 512], BF16)
                    aT = [aT1, aT1]
                    MSET("pool", qbd[:], 0.0, ["na_qbd"])
                    DMA("sp", hT[0][:], h0T_v[:, :, 0:512], ["h0T_d"], ["na_hT0"])

                    def tiles_of(r):
                        rs = min(max(r - 4, 0), 120)
                        if rs % 2 == 0:
                            return [(rs // 2 + i, 2 * (rs // 2 + i) - r + 7) for i in range(4)]
                        m0 = (rs - 1) // 2
                        return [(m0, 14)] + [(m0 + i, 2 * (m0 + i) - r + 7) for i in range(1, 4)] + [(m0 + 4, 15)]

                    def qk(u, rl):
                        r, h2 = u // 2, u % 2
                        tl = tiles_of(r)
                        nch = len(tl)
                        sbk = PS[:, (u % 2) * 1536:(u % 2) * 1536 + 1536]
                        keys = [("B", (u % 2) * 3 + i) for i in range(3)]
                        for pi in range(2):
                            p = 2 * h2 + pi
                            for ci, (m, slab) in enumerate(tl):
                                slot = pi * nch + ci
                                o = sbk[:, slot * 128:(slot + 1) * 128]
                                MM(o, kT[:, p, m * 128:(m + 1) * 128], qbd[:, p, rl, :], True, False, ["na_kT", "na_qbd"], keys)
                                MM(o, ident, tts[:, slab, p * 128:(p + 1) * 128], False, True, ["cst", "na_tt"], keys)

                    def rest(u):
                        r, h2 = u // 2, u % 2
                        tl = tiles_of(r)
                        nch = len(tl)
                        sbk = PS[:, (u % 2) * 1536:(u % 2) * 1536 + 1536]
                        keys = [("B", (u % 2) * 3 + i) for i in range(3)]
                        E = Eb[u % 2]
                        ek = f"na_E{u % 2}"
                        n = 2 * nch * 128
                        ACT(E[:, 0:n], sbk[:, 0:n], AF.Exp, keys, [ek])
                        par = r % 2
                        prt = slice(par * 64, (par + 1) * 64)
                        pO = bank(6)
                        for pi in range(2):
                            for hh in range(2):
                                hq = pi * 2 + hh
                                head = 4 * h2 + hq
                                for ci, (m, slab) in enumerate(tl):
                                    c0 = (pi * nch + ci) * 128 + hh * 64
                                    MM(pO[prt, hq * 65:(hq + 1) * 65], E[:, c0:c0 + 64], Vx[:, m, head, :], ci == 0, ci == nch - 1,
                                       [ek, "na_V"], [("B", 6)])
                        pov = pO[prt, 0:260].rearrange("p (h d) -> p h d", d=65)
                        S.add("dve", lambda e: e.reciprocal(out=rec[prt, :], in_=pov[:, :, 64]), r=[("B", 6)], w=["na_rec"])
                        tb = (r // 2) % 2
                        TT("dve", asb[tb][prt, h2 * 256:(h2 + 1) * 256].rearrange("p (h d) -> p h d", d=64), pov[:, :, 0:64],
                           rec[prt, :].unsqueeze(2).to_broadcast([64, 4, 64]), ALU.mult, [("B", 6), "na_rec"], [f"na_a{tb}"])

                    for blk in range(NBLK):
                        b = blk % 2
                        hk = f"na_hT{b}"
                        if blk + 1 < NBLK:
                            DMA("sp", hT[1 - b][:], h0T_v[:, :, (blk + 1) * 512:(blk + 2) * 512], ["h0T_d"], [f"na_hT{1 - b}"])
                        for p in range(4):
                            pq = bank(7)
                            for k in range(8):
                                MM(pq, Wq[:, k, p * 128:(p + 1) * 128], hT[b][:, k, :], k == 0, k == 7, [hk, "na_Wq"], [("B", 7)])
                            ACT(qbd[0:64, p, :, 0:64], pq[0:64, :].rearrange("p (r c) -> p r c", r=8), AF.Copy, [("B", 7)], ["na_qbd"], scale=0.125)
                            TS("dve", qbd[64:128, p, :, 64:128], pq[64:128, :].rearrange("p (r c) -> p r c", r=8), 0.125, None, ALU.mult, None,
                               [("B", 7)], ["na_qbd"])
                        u0 = blk * 16
                        qk(u0, 0)
                        for ul in range(16):
                            u = u0 + ul
                            if ul + 1 < 16:
                                qk(u + 1, (ul + 1) // 2)
                            rest(u)
                            if ul % 4 == 3:
                                t = ul // 4
                                tb = ((u // 2) // 2) % 2
                                pT = bank16(7)
                                for c in range(4):
                                    TR(pT[:, c * 128:(c + 1) * 128], asb[tb][:, c * 128:(c + 1) * 128], ident, [f"na_a{tb}", "cst"], [("B", 7)])
                                CP("act", aT[b][:, :, t * 128:(t + 1) * 128], pT[:, 0:512].rearrange("p (c t) -> p c t", c=4), [("B", 7)], ["na_aT"])
                        DMA("sp", aT_v[:, :, blk * 512:(blk + 1) * 512], aT[b][:], ["na_aT"], ["aT_d"])

        def phaseM():
            with ExitStack() as es:
                ba_bf = sbuf(es, "m_ba", [1, D], BF16)
                with ExitStack() as est:
                    tmpb = sbuf(est, "m_tmpb", [1, D], F32)
                    DMA("sp", tmpb[:], lnp_d[0:1, 1, :], [], ["m_tmpb"])
                    TS("dve", tmpb[:], tmpb[:], ALPHA, None, ALU.mult, None, ["m_tmpb"], ["m_tmpb"])
                    CP("dve", ba_bf[:], tmpb[:], ["m_tmpb"], ["m_ba"])
                S.barrier()
                lnp = sbuf(es, "m_lnp", [128, 3, D], F32)
                DMA("sp", lnp[:, 0, :], lnp_d[:, 0, :], [], ["m_lnp"])
                DMA("sp", lnp[:, 1:3, :], lnp_d[:, 2:4, :], [], ["m_lnp"])
                TS("pool", lnp[:, 0, :], lnp[:, 0, :], ALPHA, None, ALU.mult, None, ["m_lnp"], ["m_lnp"])
                Wga = load_w(es, "m_Wga", IN_OFF["gate_a"], 1024)
                Wgb = load_w(es, "m_Wgb", IN_OFF["gate_b"], 1024)
                Wa = sbuf(es, "m_Wa", [128, 4, D], BF16)
                Wb = sbuf(es, "m_Wb", [128, 4, D], BF16)
                Wo = sbuf(es, "m_Wo", [128, 8, D], BF16)
                for k in range(4):
                    DMA("pool", Wa[:, k, :], w_pa_d[k * 128:(k + 1) * 128, :], [], ["m_Wa"])
                    DMA("pool", Wb[:, k, :], w_pb_d[k * 128:(k + 1) * 128, :], [], ["m_Wb"])
                for k in range(8):
                    DMA("pool", Wo[:, k, :], w_out_d[k * 128:(k + 1) * 128, :], [], ["m_Wo"])
                wr = sbuf(es, "m_wr", [128, 8, 36], F32)
                rb = sbuf(es, "m_rb", [128, 4, 36], F32)
                DMA("sp", wr[:], wr_d, [], ["m_wr"])
                DMA("sp", rb[:], rb_d, [], ["m_rb"])
                hT = [sbuf(es, f"m_hT{i}", [128, 8, 512], BF16) for i in range(2)]
                aTs1 = sbuf(es, "m_aT", [128, 4, 512], BF16)
                aTs = [aTs1, aTs1]
                cTs1 = sbuf(es, "m_cT", [128, 4, 512], BF16)
                cTs = [cTs1, cTs1]
                xt = sbuf(es, "m_xt", [128, 4, D], F32)
                sgb_ = sbuf(es, "m_sgb", [128, 512], F32)
                sg4 = [(sbuf(es, f"m_sga{i}", [128, 512], F32), sgb_) for i in range(2)]
                m1 = sbuf(es, "m_m1", [128, 512], F32)
                m2 = sbuf(es, "m_m2", [128, 512], F32)
                mT = sbuf(es, "m_mT", [128, 8, 512], BF16)
                zz = [sbuf(es, f"m_z{i}", [128, 4, D], F32) for i in range(2)]
                hx2 = [sbuf(es, f"m_hx{i}", [128, D], F32) for i in range(2)]
                h1b = sbuf(es, "m_h1b", [128, 4, D], BF16)
                h1T = sbuf(es, "m_h1T", [128, 4, 128], F32)
                st1 = sbuf(es, "m_st1", [128, 4, 12], F32)
                mv1 = sbuf(es, "m_mv1", [128, 4, 2], F32)
                rs1 = sbuf(es, "m_rs1", [128, 4], F32)
                nm1 = sbuf(es, "m_nm1", [128, 4], F32)
                L = sbuf(es, "m_L", [128, 4, 36], F32)
                gmax = sbuf(es, "m_gmax", [128, 4], F32)
                gm = sbuf(es, "m_gm", [128, 4, 4], F32)
                gd = sbuf(es, "m_gd", [128, 4, 4], F32)
                gw = sbuf(es, "m_gw", [128, 4], F32)
                EM = sbuf(es, "m_EM", [128, 4, 32], F32)
                EM2 = sbuf(es, "m_EM2", [128, 4, 32], F32)
                top1 = sbuf(es, "m_top1", [128, 4], F32)
                top2 = sbuf(es, "m_top2", [128, 4], F32)
                oh = [sbuf(es, f"m_oh{i}", [128, 4, 32], F32) for i in range(2)]
                cnt = sbuf(es, "m_cnt", [128, 4, 32], BF16)
                rank = sbuf(es, "m_rank", [128, 4, 32], F32)
                slot = sbuf(es, "m_slot", [128, 4, 32], F32)
                tmp = sbuf(es, "m_tmp", [128, 4, 32], F32)
                tot = sbuf(es, "m_tot", [128, 32], F32)
                sm = sbuf(es, "m_sm", [128, 8, 4], F32)
                MSET("dve", tot[:], 0.0, ["m_tot"])
                if "fakeidx" in dbg:
                    fake_ix = sbuf(es, "m_fake", [128, NTILE, 2], I32)
                    S.add("pool", lambda e: e.iota(fake_ix[:].rearrange("p n k -> p (n k)"), pattern=[[128, 128]], base=0, channel_multiplier=1),
                          w=["m_fake"])
                ident32 = cst32[:, 2, 0:128]
                ustr = cst[:, 3, 0:128]
                ebase = cst32[:, 2, 128:160]
                pidx = cst32[:, 2, 160:161]

                def loads_ac(blk):
                    DMA("sp", aTs[0][:], aT_v[:, :, blk * 512:(blk + 1) * 512], ["aT_d"], ["m_aT"])
                    DMA("sp", cTs[0][:], cT_v[:, :, blk * 512:(blk + 1) * 512], ["cT_d"], ["m_cT"])

                def loads(blk, b):
                    DMA("sp", hT[b][:], h0T_v[:, :, blk * 512:(blk + 1) * 512], ["h0T_d"], [f"m_hT{b}"])

                def stageA(blk):
                    b = blk % 2
                    z = zz[b]
                    if blk + 1 < NBLK:
                        loads(blk + 1, 1 - b)
                    rs0 = xstat[:, blk * 4:(blk + 1) * 4, 0]
                    nm0 = xstat[:, blk * 4:(blk + 1) * 4, 1]
                    for c in range(8):
                        ga_, gb_ = 0, 1
                        pa_, pb_ = (2, 3) if c % 2 == 0 else (4, 5)
                        cs = slice(c * 128, (c + 1) * 128)
                        for k in range(8):
                            MM(bank(ga_), Wga[:, k, cs], hT[b][:, k, :], k == 0, k == 7, [f"m_hT{b}", "m_Wga"], [("B", ga_)])
                        for k in range(4):
                            MM(bank(pa_), Wa[:, k, cs], aTs[b][:, k, :], k == 0, k == 3, ["m_aT", "m_Wa"], [("B", pa_)])
                        for k in range(8):
                            MM(bank(gb_), Wgb[:, k, cs], hT[b][:, k, :], k == 0, k == 7, [f"m_hT{b}", "m_Wgb"], [("B", gb_)])
                        for k in range(4):
                            MM(bank(pb_), Wb[:, k, cs], cTs[b][:, k, :], k == 0, k == 3, ["m_cT", "m_Wb"], [("B", pb_)])
                        sga, sgb = sg4[c % 2]
                        ACT(sga[:], bank(ga_), AF.Sigmoid, [("B", ga_)], [f"m_sga{c % 2}"])
                        ACT(sgb[:], bank(gb_), AF.Sigmoid, [("B", gb_)], ["m_sgb"])
                        TT("dve", m1[:], bank(pa_), sga[:], ALU.mult, [("B", pa_), f"m_sga{c % 2}"], ["m_m1"])
                        TT("dve", m2[:], bank(pb_), sgb[:], ALU.mult, [("B", pb_), "m_sgb"], ["m_m2"])
                        TT("pool", mT[:, c, :], m1[:], m2[:], ALU.add, ["m_m1", "m_m2"], [("m_mT", c)])
                    if blk + 1 < NBLK:
                        loads_ac(blk + 1)
                    mtk = [("m_mT", c) for c in range(8)]
                    for t in range(4):
                        hxx = hx2[t % 2]
                        hk_ = f"m_hx{t % 2}"
                        ACT(hxx[:], xt[:, t, :], AF.Identity, ["m_xt", "xstat"], [hk_], scale=rs0[:, t:t + 1], bias=nm0[:, t:t + 1])
                        TT("dve", hxx[:], hxx[:], lnp[:, 0, :], ALU.mult, [hk_, "m_lnp"], [hk_])
                        for n in range(2):
                            o = (2, 3, 4, 5)[(t * 2 + n) % 4]
                            ns = slice(n * 512, (n + 1) * 512)
                            MM(bank(o), cst[0:1, 4, 0:128], ba_bf[0:1, ns], True, False, ["cst", "m_ba"], [("B", o)])
                            for k in range(8):
                                MM(bank(o), mT[:, k, t * 128:(t + 1) * 128], Wo[:, k, ns], False, k == 7, mtk + ["m_Wo"], [("B", o)])
                            TT("dve", z[:, t, ns], hxx[:, ns], bank(o), ALU.add, [hk_, ("B", o)], [("m_z", b, t)])
                    if blk + 1 < NBLK:
                        DMA("sp", xt[:], x_v[blk + 1], [], ["m_xt"])

                def stageB(blk):
                    b = blk % 2
                    z = zz[b]
                    zk = [("m_z", b, t) for t in range(4)]
                    for t in range(4):
                        for c in range(2):
                            S.add("dve", lambda e, t=t, c=c: e.bn_stats(out=st1[:, t, c * 6:(c + 1) * 6], in_=z[:, t, c * 512:(c + 1) * 512]),
                                  r=[("m_z", b, t)], w=["m1st"])
                        S.add("dve", lambda e, t=t: e.bn_aggr(out=mv1[:, t, :], in_=st1[:, t, :]), r=["m1st"], w=["m1mv"])
                    ACT(rs1[:], mv1[:, :, 1], AF.Ln, ["m1mv"], ["m1rs"], bias=LN_EPS)
                    ACT(rs1[:], rs1[:], AF.Exp, ["m1rs"], ["m1rs"], scale=-0.5)
                    STT(nm1[:], mv1[:, :, 0], -1.0, rs1[:], ALU.mult, ALU.mult, ["m1mv", "m1rs"], ["m1nm"])
                    for t in range(4):
                        ACT(z[:, t, :], z[:, t, :], AF.Identity, [("m_z", b, t), "m1rs", "m1nm"], [("m_z", b, t)], scale=rs1[:, t:t + 1], bias=nm1[:, t:t + 1])
                        TT("dve", z[:, t, :], z[:, t, :], lnp[:, 1, :], ALU.mult, [("m_z", b, t), "m_lnp"], [("m_z", b, t)])
                        TT("pool", z[:, t, :], z[:, t, :], lnp[:, 2, :], ALU.add, [("m_z", b, t), "m_lnp"], [("m_z", b, t)])
                        CP("act", h1b[:, t, :], z[:, t, :], [("m_z", b, t)], [("m_h1b", t)])
                    DMA("sp", h1_v[blk], z[:], zk, ["h1_d"])
                    pL = bank(7)
                    for t in range(4):
                        for g in range(2):
                            for kk_ in range(4):
                                k = g * 4 + kk_
                                TR(bank(6)[:, kk_ * 128:(kk_ + 1) * 128], z[:, t, k * 128:(k + 1) * 128], ident32, [("m_z", b, t), "cst32"], [("B", 6)])
                            CP("act" if g == 0 else "dve", h1T[:, 0:4, :], bank(6).rearrange("p (k t) -> p k t", k=4),
                               [("B", 6)], ["m_h1T"])
                            for kk_ in range(4):
                                k = g * 4 + kk_
                                MM(pL[:, t * 36:(t + 1) * 36], h1T[:, kk_, :], wr[:, k, :], k == 0, k == 7, ["m_h1T", "m_wr"], [("B", 7)])
                    TT("dve", L[:], pL[:, 0:144].rearrange("p (t n) -> p t n", n=36), rb[:], ALU.add, [("B", 7), "m_rb"], ["m_L"])
                    GL = L[:, :, 0:4]
                    EL = L[:, :, 4:36].rearrange("p t (g e) -> p t g e", g=4)
                    S.add("dve", lambda e: e.tensor_reduce(out=gmax[:], in_=GL, axis=AX.X, op=ALU.max), r=["m_L"], w=["m_gmax"])
                    TT("dve", gm[:], GL, gmax[:].unsqueeze(2).to_broadcast([128, 4, 4]), ALU.is_equal, ["m_L", "m_gmax"], ["m_gm"])
                    TT("dve", gd[:], GL, gmax[:].unsqueeze(2).to_broadcast([128, 4, 4]), ALU.subtract, ["m_L", "m_gmax"], ["m_gd"])
                    ACT(gd[:], gd[:], AF.Exp, ["m_gd"], ["m_gd"])
                    S.add("dve", lambda e: e.tensor_reduce(out=gw[:], in_=gd[:], axis=AX.X, op=ALU.add), r=["m_gd"], w=["m_gw"])
                    S.add("dve", lambda e: e.reciprocal(out=gw[:], in_=gw[:]), r=["m_gw"], w=["m_gw"])
                    TS("dve", gm[:], gm[:], 1e9, -1e9, ALU.mult, ALU.add, ["m_gm"], ["m_gm"])
                    TT("dve", EM[:].rearrange("p t (g e) -> p t g e", g=4), EL, gm[:].unsqueeze(3).to_broadcast([128, 4, 4, 8]), ALU.add,
                       ["m_L", "m_gm"], ["m_EM"])
                    S.add("dve", lambda e: e.tensor_reduce(out=top1[:], in_=EM[:], axis=AX.X, op=ALU.max), r=["m_EM"], w=["m_top1"])
                    TT("dve", oh[0][:], EM[:], top1[:].unsqueeze(2).to_broadcast([128, 4, 32]), ALU.is_equal, ["m_EM", "m_top1"], ["m_oh0"])
                    STT(EM2[:], oh[0][:], -1e9, EM[:], ALU.mult, ALU.add, ["m_oh0", "m_EM"], ["m_EM2"])
                    S.add("dve", lambda e: e.tensor_reduce(out=top2[:], in_=EM2[:], axis=AX.X, op=ALU.max), r=["m_EM2"], w=["m_top2"])
                    TT("dve", oh[1][:], EM2[:], top2[:].unsqueeze(2).to_broadcast([128, 4, 32]), ALU.is_equal, ["m_EM2", "m_top2"], ["m_oh1"])
                    w1 = wall[:, blk * 4:(blk + 1) * 4, 0]
                    w2 = wall[:, blk * 4:(blk + 1) * 4, 1]
                    TT("dve", sm[:, 0, :], top2[:], top1[:], ALU.subtract, ["m_top1", "m_top2"], ["m_sm0"])
                    ACT(sm[:, 0, :], sm[:, 0, :], AF.Exp, ["m_sm0"], ["m_sm0"])
                    TS("dve", sm[:, 0, :], sm[:, 0, :], 1.0, None, ALU.add, None, ["m_sm0"], ["m_sm0"])
                    S.add("dve", lambda e: e.reciprocal(out=sm[:, 1, :], in_=sm[:, 0, :]), r=["m_sm0"], w=["m_sm1"])
                    TT("dve", w1, sm[:, 1, :], gw[:], ALU.mult, ["m_sm1", "m_gw"], [("wall", blk)])
                    TT("dve", w2, gw[:], w1, ALU.subtract, ["m_gw", ("wall", blk)], [("wall", blk)])
                    TT("dve", cnt[:], oh[0][:], oh[1][:], ALU.add, ["m_oh0", "m_oh1"], ["m_cnt"])
                    pR = bank(7)[:, 256:512]
                    for t in range(4):
                        MM(pR[:, t * 32:(t + 1) * 32], ustr, cnt[:, t, :], True, t == 0, ["m_cnt", "cst"], [("B", 7)])
                        for t2 in range(t):
                            MM(pR[:, t * 32:(t + 1) * 32], ones_bf, cnt[:, t2, :], False, t2 == t - 1, ["m_cnt", "cst"], [("B", 7)])
                    for t in range(4):
                        MM(pR[:, 128:160], ones_bf, cnt[:, t, :], t == 0, t == 3, ["m_cnt", "cst"], [("B", 7)])
                    TT("dve", rank[:], pR[:, 0:128].rearrange("p (t n) -> p t n", n=32), tot[:].unsqueeze(1).to_broadcast([128, 4, 32]), ALU.add,
                       [("B", 7), "m_tot"], ["m_rank"])
                    TT("dve", tot[:], tot[:], pR[:, 128:160], ALU.add, ["m_tot", ("B", 7)], ["m_tot"])
                    TT("dve", slot[:], rank[:], ebase.unsqueeze(1).to_broadcast([128, 4, 32]), ALU.add, ["m_rank", "cst32"], ["m_slot"])
                    for kx in range(2):
                        ohk = f"m_oh{kx}"
                        TT("dve", tmp[:], oh[kx][:], slot[:], ALU.mult, [ohk, "m_slot"], ["m_tmp"])
                        S.add("dve", lambda e: e.tensor_reduce(out=sm[:, 2, :], in_=tmp[:], axis=AX.X, op=ALU.add), r=["m_tmp"], w=["m_sm2"])
                        TT("dve", tmp[:], oh[kx][:], rank[:], ALU.mult, [ohk, "m_rank"], ["m_tmp"])
                        S.add("dve", lambda e: e.tensor_reduce(out=sm[:, 3, :], in_=tmp[:], axis=AX.X, op=ALU.add), r=["m_tmp"], w=["m_sm3"])
                        TS("dve", sm[:, 3, :], sm[:, 3, :], float(CAP), None, ALU.is_ge, None, ["m_sm3"], ["m_sm3"])
                        TS("dve", sm[:, 4, :], sm[:, 2, :], -1.0, pidx, ALU.mult, ALU.add, ["m_sm2", "cst32"], ["m_sm4"])
                        TT("dve", sm[:, 4, :], sm[:, 4, :], sm[:, 3, :], ALU.mult, ["m_sm4", "m_sm3"], ["m_sm4"])
                        TT("dve", sm[:, 2, :], sm[:, 2, :], sm[:, 4, :], ALU.add, ["m_sm2", "m_sm4"], ["m_sm2"])
                        CP("pool", posall[:, blk * 4:(blk + 1) * 4, kx], sm[:, 2, :], ["m_sm2"], [("posall", blk)])
                    for t in range(4):
                        if "noscatter" in dbg:
                            break
                        for kx in range(2):
                            ixap = posall[:, blk * 4 + t, kx:kx + 1]
                            if "fakeidx" in dbg:
                                ixap = fake_ix[:, blk * 4 + t, kx:kx + 1]
                            S.add("pool", lambda e, t=t, kx=kx, ixap=ixap: e.indirect_dma_start(
                                out=xbuf_d, out_offset=bass.IndirectOffsetOnAxis(ap=ixap, axis=0),
                                in_=h1b[:, t, :], in_offset=None), r=[("m_h1b", t), ("posall", blk)], w=["xbuf_d"], dma=True, cost=4.0)

                loads(0, 0)
                loads_ac(0)
                DMA("sp", xt[:], x_v[0], [], ["m_xt"])
                stageA(0)
                for blk in range(NBLK):
                    ra = None
                    if blk + 1 < NBLK:
                        S.record()
                        stageA(blk + 1)
                        ra = S.stop()
                    S.record()
                    stageB(blk)
                    rb_ = S.stop()
                    S.merge(ra, rb_)

        def dump_routing():
            pos_d = nc.dram_tensor("pos_d", [128, NTILE * 2], I32, kind="ExternalOutput").ap()
            wall_d = nc.dram_tensor("wall_d", [128, NTILE * 2], F32, kind="ExternalOutput").ap()
            allk = [("posall", b) for b in range(NBLK)]
            allw = [("wall", b) for b in range(NBLK)]
            DMA("sp", pos_d, posall[:].rearrange("p n k -> p (n k)"), allk, ["pos_d"])
            DMA("sp", wall_d, wall[:].rearrange("p n k -> p (n k)"), allw, ["wall_d"])

        def phaseE():
            with ExitStack() as es:
                wg = [sbuf(es, f"e_wg{i}", [128, 8, 512], BF16) for i in range(2)]
                wu = [sbuf(es, f"e_wu{i}", [128, 8, 512], BF16) for i in range(2)]
                wd = [sbuf(es, f"e_wd{i}", [128, 4, D], BF16) for i in range(2)]
                xs = [sbuf(es, f"e_xs{i}", [128, 6, D], BF16) for i in range(2)]
                xT2 = [sbuf(es, f"e_xT{i}", [128, 8, CAP], BF16) for i in range(2)]
                sg = sbuf(es, "e_sg", [128, 384], F32)
                tg = sbuf(es, "e_tg", [128, 384], F32)
                hT2 = [sbuf(es, f"e_hT{i}", [128, 4, CAP], BF16) for i in range(2)]
                ysb = [sbuf(es, f"e_y{i}", [128, D], F32) for i in range(2)]

                wst = [sbuf(es, f"e_wst{i}", [128, 4096], F32) for i in range(3)]

                def wload(e, b):
                    DMA("sp", wst[0][:].rearrange("p (k n) -> p k n", k=8), w_gate_d[e].rearrange("(k p) n -> p k n", p=128), [], ["e_wst0"])
                    DMA("sp", wst[1][:].rearrange("p (k n) -> p k n", k=8), w_up_d[e].rearrange("(k p) n -> p k n", p=128), [], ["e_wst1"])
                    DMA("sp", wst[2][:].rearrange("p (k n) -> p k n", k=4), w_down_d[e].rearrange("(k p) n -> p k n", p=128), [], ["e_wst2"])
                    CP("pool", wg[b][:].rearrange("p k n -> p (k n)"), wst[0][:], ["e_wst0"], [f"e_wg{b}"])
                    CP("pool", wu[b][:].rearrange("p k n -> p (k n)"), wst[1][:], ["e_wst1"], [f"e_wu{b}"])
                    CP("pool", wd[b][:].rearrange("p k n -> p (k n)"), wst[2][:], ["e_wst2"], [f"e_wd{b}"])
                    DMA("sp", xs[b][:], xbuf_d[e * CAP:(e + 1) * CAP, :].rearrange("(s p) d -> p s d", p=128), ["xbuf_d"], [f"e_xs{b}"])

                def stageA(e):
                    b = e % 2
                    for k in range(8):
                        pb = bank16(k % 2)
                        for s_ in range(6):
                            TR(pb[:, s_ * 128:(s_ + 1) * 128], xs[b][:, s_, k * 128:(k + 1) * 128], ident, [f"e_xs{b}", "cst"], [("B", k % 2)])
                        CP("act" if k % 2 == 0 else "dve", xT2[b][:, k, :], pb[:, 0:CAP], [("B", k % 2)], [("e_xT", b, k)])

                def stageGU(e):
                    b = e % 2
                    xT = xT2[b]
                    hTt = hT2[b]
                    xtk = [("e_xT", b, k) for k in range(8)]
                    for m in range(4):
                        for half in range(2):
                            u = m * 2 + half
                            o = 2 + 2 * (u % 2)
                            hs = slice(half * 384, (half + 1) * 384)
                            ms = slice(m * 128, (m + 1) * 128)
                            for k in range(8):
                                MM(bank(o)[:, 0:384], wg[b][:, k, ms], xT[:, k, hs], k == 0, k == 7, xtk + [f"e_wg{b}"], [("B", o)])
                            for k in range(8):
                                MM(bank(o + 1)[:, 0:384], wu[b][:, k, ms], xT[:, k, hs], k == 0, k == 7, xtk + [f"e_wu{b}"], [("B", o + 1)])
                            ACT(sg[:], bank(o)[:, 0:384], AF.Sigmoid, [("B", o)], ["e_sg"])
                            TT("dve", tg[:], bank(o)[:, 0:384], sg[:], ALU.mult, [("B", o), "e_sg"], ["e_tg"])
                            TT("dve", hTt[:, m, hs], bank(o + 1)[:, 0:384], tg[:], ALU.mult, [("B", o + 1), "e_tg"], [("e_hT", b, m)])

                def stageY(e):
                    b = e % 2
                    hTt = hT2[b]
                    htk = [("e_hT", b, m) for m in range(4)]
                    for s_ in range(6):
                        yb = (e * 6 + s_) % 2
                        for n in range(2):
                            yo = 6 + n
                            for m in range(4):
                                MM(bank(yo), hTt[:, m, s_ * 128:(s_ + 1) * 128], wd[b][:, m, n * 512:(n + 1) * 512], m == 0, m == 3,
                                   htk + [f"e_wd{b}"], [("B", yo)])
                            CP("act" if n == 0 else "dve", ysb[yb][:, n * 512:(n + 1) * 512], bank(yo), [("B", yo)], [(f"e_y{yb}", n)])
                        DMA("sp", ybuf_d[e * CAP + s_ * 128:e * CAP + (s_ + 1) * 128, :], ysb[yb][:], [(f"e_y{yb}", 0), (f"e_y{yb}", 1)], ["ybuf_d"])

                wload(0, 0)
                for e in range(32):
                    if e + 1 < 32:
                        wload(e + 1, (e + 1) % 2)
                    stageA(e)
                    stageGU(e)
                    stageY(e)

        def phaseG():
            with ExitStack() as es:
                lnp = sbuf(es, "g_lnp", [128, 2, D], F32)
                DMA("sp", lnp[:], lnp_d[:, 4:6, :], [], ["g_lnp"])
                y0 = [sbuf(es, f"g_y0{i}", [128, D], F32) for i in range(2)]
                y1 = [sbuf(es, f"g_y1{i}", [128, D], F32) for i in range(2)]
                h1t = [sbuf(es, f"g_h1{i}", [128, D], F32) for i in range(2)]
                zz = [sbuf(es, f"g_z{i}", [128, 1, D], F32) for i in range(2)]
                st2 = [sbuf(es, f"g_st{i}", [128, 1, 12], F32) for i in range(2)]
                mv2 = [sbuf(es, f"g_mv{i}", [128, 1, 2], F32) for i in range(2)]
                rs2 = [sbuf(es, f"g_rs{i}", [128, 1], F32) for i in range(2)]
                nm2 = [sbuf(es, f"g_nm{i}", [128, 1], F32) for i in range(2)]

                def gl(n, b):
                    S.add("pool", lambda e: e.indirect_dma_start(out=y0[b][:], out_offset=None, in_=ybuf_d,
                                                                 in_offset=bass.IndirectOffsetOnAxis(ap=posall[:, n, 0:1], axis=0)),
                          r=["ybuf_d"], w=[f"g_y0{b}"], dma=True, cost=5.0)
                    S.add("pool", lambda e: e.indirect_dma_start(out=y1[b][:], out_offset=None, in_=ybuf_d,
                                                                 in_offset=bass.IndirectOffsetOnAxis(ap=posall[:, n, 1:2], axis=0)),
                          r=["ybuf_d"], w=[f"g_y1{b}"], dma=True, cost=5.0)
                    DMA("sp", h1t[b][:], h1_tv[n], ["h1_d"], [f"g_h1{b}"])

                def stageA(n):
                    b = n % 2
                    zt = zz[b]
                    zk = f"g_z{b}"
                    TS("dve", zt[:, 0, :], y0[b][:], wall[:, n, 0:1], None, ALU.mult, None, [f"g_y0{b}"], [zk, zk + "a", zk + "b"])
                    STT(zt[:, 0, :], y1[b][:], wall[:, n, 1:2], zt[:, 0, :], ALU.mult, ALU.add, [f"g_y1{b}", zk], [zk])
                    STT(zt[:, 0, :], h1t[b][:], ALPHA, zt[:, 0, :], ALU.mult, ALU.add, [f"g_h1{b}", zk], [zk])
                    ln_stats(f"g{b}", zt, zk, st2[b], mv2[b], rs2[b], nm2[b], tiles=1)

                def stageB(n):
                    b = n % 2
                    zt = zz[b]
                    zk = f"g_z{b}"
                    ACT(zt[:, 0, :], zt[:, 0, :], AF.Identity, [zk, f"g{b}rs", f"g{b}nm"], [zk], scale=rs2[b][:, 0:1], bias=nm2[b][:, 0:1])
                    TT("dve", zt[:, 0, :], zt[:, 0, :], lnp[:, 0, :], ALU.mult, [zk, "g_lnp"], [zk])
                    TT("pool", zt[:, 0, 0:512], zt[:, 0, 0:512], lnp[:, 1, 0:512], ALU.add, [zk, "g_lnp"], [zk + "a"])
                    TT("dve", zt[:, 0, 512:1024], zt[:, 0, 512:1024], lnp[:, 1, 512:1024], ALU.add, [zk, "g_lnp"], [zk + "b"])
                    DMA("sp", out_v[n], zt[:, 0, :], [zk, zk + "a", zk + "b"], ["out"])

                gl(0, 0)
                gl(1, 1)
                for n in range(0, NTILE, 2):
                    sts = []
                    for n2 in (n, n + 1):
                        S.record()
                        stageA(n2)
                        if n2 + 2 < NTILE:
                            gl(n2 + 2, n2 % 2)
                        stageB(n2)
                        sts.append(S.stop())
                    S.merge(*sts)

        for ph in phases:
            if ph == "0":
                phase0()
            elif ph == "B":
                hgrn_phase(1)
            elif ph == "F":
                hgrn_phase(0)
            elif ph == "N":
                phaseN()
            elif ph == "M":
                phaseM()
                if "pos_d" in dbg:
                    dump_routing()
            elif ph == "E":
                phaseE()
            elif ph == "G":
                phaseG()
            S.barrier()
        S.emit(nc)
    return nc


def host_consts():
    bf = ml_dtypes.bfloat16
    cst = np.zeros((128, 5, 512), np.float32)
    eye = np.eye(128, dtype=np.float32)
    s = np.arange(128)[:, None]
    t = np.arange(128)[None, :]
    cst[:, 0, :] = np.tile(eye, (1, 4))
    cst[:, 1, :] = np.tile((s <= t).astype(np.float32), (1, 4))
    cst[:, 2, :] = np.tile((s >= t).astype(np.float32), (1, 4))
    cst[:, 3, :] = np.tile((s < t).astype(np.float32), (1, 4))
    cst[:, 4, :] = 1.0
    cst32 = np.zeros((128, 3, 512), np.float32)
    tt = np.arange(512)
    cst32[:, 0, :] = (tt % 128 != 0).astype(np.float32)[None, :]
    cst32[:, 1, :] = (tt % 128 != 127).astype(np.float32)[None, :]
    cst32[:, 2, 0:128] = eye
    cst32[:, 2, 128:160] = (np.arange(32) * CAP).astype(np.float32)[None, :]
    cst32[:, 2, 160] = 32 * CAP + np.arange(128)
    return cst.astype(bf), cst32


def host_tt(rpb):
    bf = ml_dtypes.bfloat16
    cols = np.arange(64)
    col_start = np.clip(cols - 8, 0, 48)
    c = cols[None, :]
    kc = cols[:, None]
    valid = (kc >= col_start[None, :]) & (kc < col_start[None, :] + 16)
    off = np.clip(kc - c + 15, 0, 30)
    full = np.full((15, 64, 8, 64), NEG, np.float32)
    for ro in range(15):
        for h in range(8):
            full[ro, :, h, :] = np.where(valid, rpb[h, ro][off], NEG)
    full = full.reshape(15, 64, 512)
    neg = np.full((64, 512), NEG, np.float32)
    tt = np.zeros((128, 16, 512), np.float32)
    for a in range(14):
        tt[0:64, a] = full[a]
        tt[64:128, a] = full[a + 1]
    tt[0:64, 14] = neg
    tt[64:128, 14] = full[3]
    tt[0:64, 15] = full[10]
    tt[64:128, 15] = neg
    return tt.astype(bf)


def make_in_maps(inp):
    cst, cst32 = host_consts()
    f = lambda a: np.ascontiguousarray(np.asarray(a, np.float32))
    lnp = np.stack([inp["emb_ln_g"], inp["emb_ln_b"], inp["ln1_g"][0], inp["ln1_b"][0], inp["ln2_g"][0], inp["ln2_b"][0]], 0)
    lnp = f(np.broadcast_to(lnp[None], (128, 6, D)))
    embp = f(np.concatenate([np.asarray(inp["emb_ln_g"]).reshape(8, 128).T, np.asarray(inp["emb_ln_b"]).reshape(8, 128).T], 1))
    lbraw = f(np.asarray(inp["hg_lb"]).reshape(2, 2, 4, 128).transpose(3, 0, 1, 2).reshape(128, 16))
    normg = f(np.asarray(inp["hg_norm_g"])[0].reshape(4, 128).T)
    wr = np.concatenate([np.asarray(inp["w_router_group"])[0], np.asarray(inp["w_router_expert"])[0]], 1)
    wr = f(wr.reshape(8, 128, 36).transpose(1, 0, 2))
    rb = np.concatenate([np.asarray(inp["b_router_group"])[0], np.asarray(inp["b_router_expert"])[0]], 0)
    rb = f(np.broadcast_to(rb[None, None], (128, 4, 36)))
    tt = host_tt(np.asarray(inp["na_rpb"], np.float32)[0])
    shared = dict(w_in=f(inp["w_in"][0]), w_proj_a=f(inp["w_proj_a"][0]), w_proj_b=f(inp["w_proj_b"][0]), w_out=f(inp["w_out"][0]),
                  w_gate=f(inp["w_gate"][0]), w_up=f(inp["w_up"][0]), w_down=f(inp["w_down"][0]),
                  lnp=lnp, embp=embp, lbraw=lbraw, normg=normg, wr=wr, rb=rb, tt=tt, cst=cst, cst32=cst32)
    x = np.asarray(inp["x"], np.float32)
    return [dict(shared, x=np.ascontiguousarray(x[b])) for b in range(x.shape[0])]


def kernel(**inputs):
    nc = build()
    in_maps = make_in_maps(inputs)
    res = run_bass_kernel_spmd(nc, in_maps, core_ids=list(range(8)))
    return np.stack([np.asarray(r["out"], np.float32) for r in res.results], 0)
```

```python
from contextlib import ExitStack
import numpy as np
import ml_dtypes
import concourse.bass as bass
import concourse.mybir as mybir
from concourse.bass_utils import run_bass_kernel_spmd

F32 = mybir.dt.float32
BF16 = mybir.dt.bfloat16
I32 = mybir.dt.int32
AF = mybir.ActivationFunctionType
ALU = mybir.AluOpType
AX = mybir.AxisListType

T = 8192
D = 1024
NBLK = 16
NTILE = 64
CAP = 768
NSLOT = 32 * CAP + 128
ALPHA = 2.0 ** 0.25
LN_EPS = 1e-5
RMS_EPS = 1e-6
NEG = -30000.0

ENGS = ("pe", "dve", "act", "pool", "sp")
EPOCH = 3000
NDMA_SEM = 8


class Op:
    __slots__ = ("eng", "fn", "deps", "sig", "sem", "val", "dma", "n")

    def __init__(self, eng, fn, dma):
        self.eng = eng
        self.fn = fn
        self.dma = dma
        self.deps = []
        self.sig = dma
        self.sem = None
        self.val = 0


class Sched:
    def __init__(self):
        self.q = {e: [] for e in ENGS}
        self.last_w = {}
        self.readers = {}
        self.dma_hist = {e: [] for e in ENGS}
        self.nops = 0
        self.bar = None
        self.bar_pending = set()
        self.rec = None
        self.m_wfin = {}
        self.m_rfin = {}
        self.m_efree = {}

    def barrier(self):
        ops = []
        for e in ENGS:
            last = None
            for op in reversed(self.q[e]):
                if not op.dma:
                    last = op
                    break
            if last is not None:
                ops.append(last)
            ops.extend(self.dma_hist[e][-NDMA_SEM:])
        self.bar = ops
        self.bar_pending = set(ENGS)

    def record(self):
        self.rec = []

    def stop(self):
        r = self.rec
        self.rec = None
        return r

    def merge(self, *streams):
        streams = [st for st in streams if st]
        pos = [0] * len(streams)
        total = sum(len(st) for st in streams)
        wfin = self.m_wfin
        rfin = self.m_rfin
        efree = self.m_efree
        for _ in range(total):
            best = None
            for i, st in enumerate(streams):
                if pos[i] >= len(st):
                    continue
                eng, fn, r, w, dma, cost = st[pos[i]]
                rdy = 0.0
                for k in r:
                    rdy = max(rdy, wfin.get(k, 0.0))
                for k in w:
                    rdy = max(rdy, wfin.get(k, 0.0), rfin.get(k, 0.0))
                start = max(efree.get(eng, 0.0), rdy + 0.1)
                rem = len(st) - pos[i]
                cand = (start, -rem, i)
                if best is None or cand < best[0]:
                    best = (cand, i, start)
            _, i, start = best
            eng, fn, r, w, dma, cost = streams[i][pos[i]]
            pos[i] += 1
            if dma:
                efree[eng] = start + 0.06
                fin = start + cost
            else:
                fin = start + cost
                efree[eng] = fin
            for k in r:
                rfin[k] = max(rfin.get(k, 0.0), fin)
            for k in w:
                wfin[k] = fin
                rfin[k] = 0.0
            self.add(eng, fn, r, w, dma, cost)

    def add(self, eng, fn, r=(), w=(), dma=False, cost=0.5):
        if self.rec is not None:
            self.rec.append((eng, fn, tuple(r), tuple(w), dma, cost))
            return None
        op = Op(eng, fn, dma)
        op.n = self.nops
        self.nops += 1
        deps = {}
        if eng in self.bar_pending:
            self.bar_pending.discard(eng)
            for p in self.bar:
                if p.dma or p.eng != eng:
                    deps[id(p)] = p
        for k in r:
            p = self.last_w.get(k)
            if p is not None and p is not op:
                if p.eng == eng and not p.dma and not dma:
                    if eng != "pe":
                        deps[id(p)] = p
                else:
                    deps[id(p)] = p
        for k in w:
            p = self.last_w.get(k)
            if p is not None and p is not op and (p.dma or dma or p.eng != eng or eng != "pe"):
                deps[id(p)] = p
            rd = self.readers.get(k)
            if rd:
                for p in rd.values():
                    if p is not op and (p.dma or dma or p.eng != eng or eng != "pe"):
                        deps[id(p)] = p
        for k in w:
            self.last_w[k] = op
            self.readers[k] = {}
        for k in r:
            d = self.readers.setdefault(k, {})
            d[("dma", op.n) if dma else eng] = op
        if dma:
            h = self.dma_hist[eng]
            if len(h) >= NDMA_SEM:
                p = h[-NDMA_SEM]
                deps[id(p)] = p
            h.append(op)
        op.deps = list(deps.values())
        for p in op.deps:
            p.sig = True
        self.q[eng].append(op)
        return op

    def emit(self, nc):
        with ExitStack() as es:
            for e in ENGS:
                cnt = 0
                sem = None
                dsem = [None] * NDMA_SEM
                dval = [0] * NDMA_SEM
                nd = 0
                ns = 0
                for op in self.q[e]:
                    if op.dma:
                        s = nd % NDMA_SEM
                        if dsem[s] is None:
                            dsem[s] = es.enter_context(nc.semaphore(f"d_{e}_{s}"))
                        dval[s] += 16
                        op.sem = dsem[s]
                        op.val = dval[s]
                        nd += 1
                    elif op.sig:
                        if sem is None or cnt >= EPOCH:
                            sem = es.enter_context(nc.semaphore(f"c_{e}_{ns}"))
                            ns += 1
                            cnt = 0
                        cnt += 1
                        op.sem = sem
                        op.val = cnt
            block = es.enter_context(nc.Block())
            engmap = {"pe": block.tensor, "dve": block.vector, "act": block.scalar,
                      "pool": block.gpsimd, "sp": block.sync}
            for e in ENGS:
                ops = self.q[e]
                if not ops:
                    continue

                def body(engine, ops=ops):
                    waited = {}
                    for op in ops:
                        for p in op.deps:
                            key = id(p.sem)
                            if waited.get(key, 0) >= p.val:
                                continue
                            waited[key] = p.val
                            engine.wait_ge(p.sem, p.val)
                        ins = op.fn(engine)
                        if op.sig:
                            ins.then_inc(op.sem, 16 if op.dma else 1)
                    last = {}
                    for op in ops:
                        if op.dma:
                            last[id(op.sem)] = op
                    for op in last.values():
                        if waited.get(id(op.sem), 0) < op.val:
                            engine.wait_ge(op.sem, op.val)

                engmap[e](body)


IN_OFF = dict(na_q=0, na_k=512, na_v=1024, hg_q=1536, hg_ff=2048, hg_fb=2560, hg_i=3072, hg_g=3584,
              gate_a=4096, gate_b=5120)


def build(dbg=(), phases="0BNFMEG"):
    nc = bass.Bass("TRN2", target_bir_lowering=False)
    S = Sched()

    def din(name, shape, dt):
        return nc.dram_tensor(name, list(shape), dt, kind="ExternalInput").ap()

    def dscr(name, shape, dt):
        kind = "ExternalOutput" if name in dbg else "Internal"
        return nc.dram_tensor(name, list(shape), dt, kind=kind).ap()

    x_d = din("x", [T, D], F32)
    w_in_d = din("w_in", [D, 6144], F32)
    w_pa_d = din("w_proj_a", [512, D], F32)
    w_pb_d = din("w_proj_b", [512, D], F32)
    w_out_d = din("w_out", [D, D], F32)
    w_gate_d = din("w_gate", [32, D, 512], F32)
    w_up_d = din("w_up", [32, D, 512], F32)
    w_down_d = din("w_down", [32, 512, D], F32)
    lnp_d = din("lnp", [128, 6, D], F32)
    embp_d = din("embp", [128, 16], F32)
    lbraw_d = din("lbraw", [128, 16], F32)
    normg_d = din("normg", [128, 4], F32)
    wr_d = din("wr", [128, 8, 36], F32)
    rb_d = din("rb", [128, 4, 36], F32)
    tt_d = din("tt", [128, 16, 512], BF16)
    cst_d = din("cst", [128, 5, 512], BF16)
    cst32_d = din("cst32", [128, 3, 512], F32)
    out_d = nc.dram_tensor("out", [T, D], F32, kind="ExternalOutput").ap()

    h0T_d = dscr("h0T_d", [D, T], BF16)
    obw_d = dscr("obw_d", [512, T], F32)
    aT_d = dscr("aT_d", [512, T], BF16)
    cT_d = dscr("cT_d", [512, T], BF16)
    h1_d = dscr("h1_d", [T, D], F32)
    xbuf_d = dscr("xbuf_d", [NSLOT, D], BF16)
    ybuf_d = dscr("ybuf_d", [NSLOT, D], F32)

    h0T_v = h0T_d.rearrange("(k p) t -> p k t", p=128)
    obw_v = obw_d.rearrange("(h p) t -> p h t", p=128)
    aT_v = aT_d.rearrange("(k p) t -> p k t", p=128)
    cT_v = cT_d.rearrange("(k p) t -> p k t", p=128)
    x_v = x_d.rearrange("(b t p) d -> b p t d", t=4, p=128)
    h1_v = h1_d.rearrange("(b t p) d -> b p t d", t=4, p=128)
    out_v = out_d.rearrange("(n p) d -> n p d", p=128)
    h1_tv = h1_d.rearrange("(n p) d -> n p d", p=128)
    w_in_v = w_in_d.rearrange("(k p) n -> p k n", p=128)

    with ExitStack() as ges:
        uid = [0]

        def sbuf(es, name, shape, dt):
            uid[0] += 1
            return es.enter_context(nc.sbuf_tensor(f"s{uid[0]}_{name}", list(shape), dt))

        PS = ges.enter_context(nc.psum_tensor("PS", [128, 4096], F32))

        def bank(i, n=1):
            return PS[:, i * 512:(i + n) * 512]

        def bank16(i):
            return PS[:, i * 512:(i + 1) * 512].bitcast(BF16)

        cst = sbuf(ges, "cst", [128, 5, 512], BF16)
        cst32 = sbuf(ges, "cst32", [128, 3, 512], F32)
        embp = sbuf(ges, "embp", [128, 16], F32)
        lbraw = sbuf(ges, "lbraw", [128, 16], F32)
        lbp = sbuf(ges, "lbp", [128, 3, 8], F32)
        normg = sbuf(ges, "normg", [128, 4], F32)
        posall = sbuf(ges, "posall", [128, NTILE, 2], I32)
        wall = sbuf(ges, "wall", [128, NTILE, 2], F32)
        xstat = sbuf(ges, "xstat", [128, NTILE, 2], F32)
        ident = cst[:, 0, 0:128]
        ones_bf = cst[:, 4, 0:128]

        def nfree(ap):
            n = 1
            for d in ap.shape[1:]:
                n *= d
            return n

        def DMA(eng, out, in_, r, w):
            S.add(eng, lambda e: e.dma_start(out=out, in_=in_), r=r, w=w, dma=True, cost=2.5 + nfree(out) * 128 * 4 / 150e3)

        def MM(out, lhsT, rhs, start, stop, r, w):
            c_ = max(64, nfree(rhs)) / 2400.0 * (4.0 if rhs.dtype == F32 else 1.0) + 0.01
            S.add("pe", lambda e: e.matmul(out, lhsT=lhsT, rhs=rhs, start=start, stop=stop), r=r, w=w, cost=c_)

        def TR(out, in_, idn, r, w):
            S.add("pe", lambda e: e.transpose(out, in_, idn), r=r, w=w, cost=0.08)

        def ACT(out, in_, func, r, w, scale=1.0, bias=0.0):
            S.add("act", lambda e: e.activation(out=out, in_=in_, func=func, bias=bias, scale=scale), r=r, w=w, cost=0.25 + nfree(out) / 1200.0)

        def vcost(eng, out):
            return (0.3 + nfree(out) / 330.0) if eng == "pool" else (0.16 + nfree(out) / 960.0)

        def TT(eng, out, in0, in1, op, r, w):
            S.add(eng, lambda e: e.tensor_tensor(out=out, in0=in0, in1=in1, op=op), r=r, w=w, cost=vcost(eng, out))

        def TS(eng, out, in0, s1, s2, op0, op1, r, w):
            if s2 is None:
                S.add(eng, lambda e: e.tensor_scalar(out=out, in0=in0, scalar1=s1, scalar2=None, op0=op0), r=r, w=w, cost=vcost(eng, out))
            else:
                S.add(eng, lambda e: e.tensor_scalar(out=out, in0=in0, scalar1=s1, scalar2=s2, op0=op0, op1=op1), r=r, w=w, cost=vcost(eng, out))

        def STT(out, in0, scalar, in1, op0, op1, r, w):
            S.add("dve", lambda e: e.scalar_tensor_tensor(out=out, in0=in0, scalar=scalar, in1=in1, op0=op0, op1=op1), r=r, w=w, cost=vcost("dve", out))

        def CP(eng, out, in_, r, w):
            if eng == "act":
                S.add("act", lambda e: e.activation(out=out, in_=in_, func=AF.Copy), r=r, w=w, cost=0.25 + nfree(out) / 1200.0)
            else:
                S.add(eng, lambda e: e.tensor_copy(out=out, in_=in_), r=r, w=w, cost=vcost(eng, out))

        def MSET(eng, ap, val, w):
            S.add(eng, lambda e: e.memset(ap, val), w=w)

        DMA("sp", cst[:], cst_d, [], ["cst"])
        DMA("sp", cst32[:], cst32_d, [], ["cst32"])
        DMA("sp", embp[:], embp_d, [], ["embp"])
        DMA("sp", lbraw[:], lbraw_d, [], ["lbraw"])
        DMA("sp", normg[:], normg_d, [], ["normg"])
        lbr = lbraw[:].rearrange("p (d l h) -> p d l h", d=2, l=2)
        lb_v = lbp[:, 0, :].rearrange("p (d h) -> p d h", d=2)
        TT("dve", lb_v, lbr[:, :, 1, :], lbr[:, :, 0, :], ALU.subtract, ["lbraw"], ["lbp"])
        ACT(lbp[:, 0, :], lbp[:, 0, :], AF.Exp, ["lbp"], ["lbp"])
        ACT(lbp[:, 0, :], lbp[:, 0, :], AF.Ln, ["lbp"], ["lbp"], bias=1.0)
        ACT(lbp[:, 0, :], lbp[:, 0, :], AF.Exp, ["lbp"], ["lbp"], scale=-1.0)
        TS("dve", lbp[:, 1, :], lbp[:, 0, :], -1.0, 1.0, ALU.mult, ALU.add, ["lbp"], ["lbp"])
        TS("dve", lbp[:, 2, :], lbp[:, 1, :], -1.0, None, ALU.mult, None, ["lbp"], ["lbp"])

        def ln_stats(es_name, xt, xkey, stats, mv, rstd, nmr, tiles=4):
            for t in range(tiles):
                for c in range(2):
                    S.add("dve", lambda e, t=t, c=c: e.bn_stats(out=stats[:, t, c * 6:(c + 1) * 6], in_=xt[:, t, c * 512:(c + 1) * 512]),
                          r=[xkey], w=[es_name + "st"])
                S.add("dve", lambda e, t=t: e.bn_aggr(out=mv[:, t, :], in_=stats[:, t, :]), r=[es_name + "st"], w=[es_name + "mv"])
            ACT(rstd[:, 0:tiles], mv[:, 0:tiles, 1], AF.Ln, [es_name + "mv"], [es_name + "rs", "xstat"], bias=LN_EPS)
            ACT(rstd[:, 0:tiles], rstd[:, 0:tiles], AF.Exp, [es_name + "rs"], [es_name + "rs", "xstat"], scale=-0.5)
            STT(nmr[:, 0:tiles], mv[:, 0:tiles, 0], -1.0, rstd[:, 0:tiles], ALU.mult, ALU.mult, [es_name + "mv", es_name + "rs"], [es_name + "nm", "xstat"])

        def phase0():
            with ExitStack() as es:
                xt = [sbuf(es, f"p0_xt{i}", [128, 4, D], F32) for i in range(2)]
                xn2 = [sbuf(es, f"p0_xn{i}", [128, 4, D], BF16) for i in range(2)]
                hT = [sbuf(es, f"p0_hT{i}", [128, 8, 512], BF16) for i in range(2)]
                stats2 = [sbuf(es, f"p0_stats{i}", [128, 4, 12], F32) for i in range(2)]
                mv2 = [sbuf(es, f"p0_mv{i}", [128, 4, 2], F32) for i in range(2)]
                def stageA(blk):
                    b = blk % 2
                    if blk + 1 < NBLK:
                        DMA("sp", xt[1 - b][:], x_v[blk + 1], [], [f"xt{1 - b}"])
                    rstd = xstat[:, blk * 4:(blk + 1) * 4, 0]
                    nmr = xstat[:, blk * 4:(blk + 1) * 4, 1]
                    ln_stats(f"p0{b}", xt[b], f"xt{b}", stats2[b], mv2[b], rstd, nmr)
                    for t in range(4):
                        ACT(xn2[b][:, t, :], xt[b][:, t, :], AF.Identity, [f"xt{b}", f"p0{b}rs", f"p0{b}nm"], [("xn", b, t)],
                            scale=rstd[:, t:t + 1], bias=nmr[:, t:t + 1])

                def stageB(blk):
                    b = blk % 2
                    xn = xn2[b]
                    for k in range(8):
                        pb = bank16(k % 2)
                        for t in range(4):
                            TR(pb[:, t * 128:(t + 1) * 128], xn[:, t, k * 128:(k + 1) * 128], ident, [("xn", b, t), "cst"], [("B", k % 2)])
                        if k % 2 == 0:
                            ACT(hT[b][:, k, :], pb[:, 0:512], AF.Identity, [("B", k % 2), "embp"], [f"hT{b}"],
                                scale=embp[:, k:k + 1], bias=embp[:, 8 + k:9 + k])
                        else:
                            TS("dve", hT[b][:, k, :], pb[:, 0:512], embp[:, k:k + 1], embp[:, 8 + k:9 + k], ALU.mult, ALU.add,
                               [("B", k % 2), "embp"], [f"hT{b}"])
                    DMA("sp", h0T_v[:, :, blk * 512:(blk + 1) * 512], hT[b][:], [f"hT{b}"], ["h0T_d"])

                DMA("sp", xt[0][:], x_v[0], [], ["xt0"])
                stageA(0)
                for blk in range(NBLK):
                    S.record()
                    if blk + 1 < NBLK:
                        stageA(blk + 1)
                    ra = S.stop()
                    S.record()
                    stageB(blk)
                    rb_ = S.stop()
                    S.merge(ra, rb_)

        def load_w(es, name, col0, ncol):
            w = sbuf(es, name, [128, 8, ncol], BF16)
            for k in range(8):
                DMA("pool", w[:, k, :], w_in_v[:, k, col0:col0 + ncol], [], [name])
            return w

        def hgrn_phase(direction):
            fwd = direction == 0
            with ExitStack() as es:
                Wq = load_w(es, "hg_Wq", IN_OFF["hg_q"], 512)
                Wf = load_w(es, "hg_Wf", IN_OFF["hg_ff"] if fwd else IN_OFF["hg_fb"], 512)
                Wi = load_w(es, "hg_Wi", IN_OFF["hg_i"], 512)
                Wg = load_w(es, "hg_Wg", IN_OFF["hg_g"], 512) if fwd else None
                hT = [sbuf(es, f"hg_hT{i}", [128, 8, 512], BF16) for i in range(2)]
                tnames = ["e1", "l1", "l2", "sq", "qf", "g", "fg", "kk", "bc", "eb", "enb", "kd32"]
                tmps = [{n: sbuf(es, f"hg_{n}{i}", [128, 512], F32) for n in tnames} for i in range(2)]
                ebl2 = [sbuf(es, f"hg_ebl{i}", [128, 4, 4], F32) for i in range(2)]
                Qd2 = [sbuf(es, f"hg_Qd{i}", [128, 4, 512], BF16) for i in range(2)]
                Kd2 = [sbuf(es, f"hg_Kd{i}", [128, 4, 512], BF16) for i in range(2)]
                Kl2 = [sbuf(es, f"hg_Kl{i}", [128, 4, 512], BF16) for i in range(2)]
                vsb2 = [sbuf(es, f"hg_v{i}", [128, 4, 512], BF16) for i in range(2)]
                klt = sbuf(es, "hg_klt", [128, 512], BF16)
                at = sbuf(es, "hg_at", [128, 512], BF16)
                osb = [sbuf(es, f"hg_o{i}", [128, 4, 512], F32) for i in range(2)]
                St = sbuf(es, "hg_S", [128, 4, 128], F32)
                Sb2 = [sbuf(es, f"hg_Sb{i}", [128, 4, 128], BF16) for i in range(2)]
                if fwd:
                    obw = sbuf(es, "hg_obw", [128, 4, 512], F32)
                    osq = sbuf(es, "hg_osq", [128, 4, 512], BF16)
                    rs = sbuf(es, "hg_rs", [128, 512], F32)
                    sg2 = [sbuf(es, f"hg_sg{i}", [128, 4, 512], F32) for i in range(2)]
                    cT = [sbuf(es, f"hg_cT{i}", [128, 4, 512], BF16) for i in range(2)]
                zlist = []
                if not fwd:
                    zt = sbuf(es, "hg_zero", [128, 8192], BF16)
                    MSET("pool", zt[:], 0.0, ["hg_zero"])
                    xb_flat = xbuf_d.rearrange("(p a) d -> p (a d)", p=128)
                    ncol = NSLOT * D // 128
                    c0 = 0
                    while c0 < ncol:
                        cw = min(8192, ncol - c0)
                        zlist.append((c0, cw))
                        c0 += cw
                MSET("dve", St[:], 0.0, ["hg_S"])
                MSET("dve", Sb2[0][:], 0.0, ["hg_Sb0"])
                MSET("dve", Sb2[1][:], 0.0, ["hg_Sb1"])
                gcount = [0]
                di = 0 if fwd else 1
                mask = cst[:, 1 if fwd else 2, :]
                blocks = list(range(NBLK)) if fwd else list(range(NBLK - 1, -1, -1))
                tiles = [0, 1, 2, 3] if fwd else [3, 2, 1, 0]
                DMA("sp", hT[0][:], h0T_v[:, :, blocks[0] * 512:(blocks[0] + 1) * 512], ["h0T_d"], ["hg_hT0"])

                def vproj(bi, tl, bk):
                    b = bi % 2
                    hk = f"hg_hT{b}"
                    vsb = vsb2[b]
                    for t in tl:
                        pv = bank(bk)
                        for k in range(8):
                            MM(pv, hT[b][:, k, t * 128:(t + 1) * 128], Wi[:, k, :], k == 0, k == 7, [hk, "hg_Wi"], [("B", bk)])
                        CP("act" if t % 2 == 0 else "dve", vsb[:, t, :], pv, [("B", bk)], [("hg_v", b, t)])

                def head(bi, h, si):
                    b = bi % 2
                    hk = f"hg_hT{b}"
                    Qd, Kd, Kl, ebl = Qd2[b], Kd2[b], Kl2[b], ebl2[b]
                    tm = tmps[si]
                    e1, l1, l2, sq, qf, gg, fg, kk, bc, eb, enb, kd32 = [tm[n] for n in tnames]
                    K_ = lambda n: f"hg_{n}{si}"
                    pf = bank(2 * si)
                    pq = bank(2 * si + 1)
                    pfk = ("B", 2 * si)
                    pqk = ("B", 2 * si + 1)
                    for k in range(8):
                        MM(pf, Wf[:, k, h * 128:(h + 1) * 128], hT[b][:, k, :], k == 0, k == 7, [hk, "hg_Wf"], [pfk])
                    for k in range(8):
                        MM(pq, Wq[:, k, h * 128:(h + 1) * 128], hT[b][:, k, :], k == 0, k == 7, [hk, "hg_Wq"], [pqk])
                    lb = lbp[:, 0, di * 4 + h:di * 4 + h + 1]
                    ACT(e1[:], pf, AF.Exp, [pfk], [K_("e1")], scale=-1.0)
                    ACT(l1[:], e1[:], AF.Ln, [K_("e1")], [K_("l1")], bias=1.0)
                    ACT(l2[:], e1[:], AF.Ln, [K_("e1"), "lbp"], [K_("l2")], scale=lb, bias=1.0)
                    TT("pool", gg[:], l2[:], l1[:], ALU.subtract, [K_("l1"), K_("l2")], [K_("g")])
                    ACT(fg[:], gg[:], AF.Exp, [K_("g")], [K_("fg")])
                    TS("pool", kk[:], fg[:], -1.0, 1.0, ALU.mult, ALU.add, [K_("fg")], [K_("kk")])
                    ACT(e1[:], pq, AF.Exp, [pqk], [K_("e1")], scale=-1.0)
                    ACT(l1[:], e1[:], AF.Ln, [K_("e1")], [K_("l1")], bias=1.0)
                    ACT(sq[:], l1[:], AF.Exp, [K_("l1")], [K_("sq")], scale=-1.0)
                    TT("dve", qf[:], pq, sq[:], ALU.mult, [pqk, K_("sq")], [K_("qf")])
                    if fwd:
                        S.add("dve", lambda e: e.tensor_tensor_scan(out=bc[:], data0=cst32[:, 0, :], data1=gg[:], initial=0.0,
                                                                    op0=ALU.mult, op1=ALU.add), r=[K_("g"), "cst32"], w=[K_("bc")], cost=1.25)
                        blast = bc[:].rearrange("p (c t) -> p c t", t=128)[:, :, 127]
                    else:
                        S.add("dve", lambda e: e.tensor_tensor_scan(out=bc[:, ::-1], data0=cst32[:, 1, ::-1], data1=gg[:, ::-1], initial=0.0,
                                                                    op0=ALU.mult, op1=ALU.add), r=[K_("g"), "cst32"], w=[K_("bc")], cost=1.25)
                        blast = bc[:].rearrange("p (c t) -> p c t", t=128)[:, :, 0]
                    ACT(eb[:], bc[:], AF.Exp, [K_("bc")], [K_("eb")])
                    ACT(enb[:], bc[:], AF.Exp, [K_("bc")], [K_("enb")], scale=-1.0)
                    ACT(ebl[:, h, :], blast, AF.Exp, [K_("bc")], [("hg_ebl", b, h)])
                    TT("dve", Qd[:, h, :], qf[:], eb[:], ALU.mult, [K_("qf"), K_("eb")], [("hg_Qd", b, h)])
                    TT("pool", kd32[:], kk[:], enb[:], ALU.mult, [K_("kk"), K_("enb")], [K_("kd32")])
                    CP("pool", Kd[:, h, :], kd32[:], [K_("kd32")], [("hg_Kd", b, h)])
                    TT("dve", Kl[:, h, :].rearrange("p (c t) -> p c t", t=128), kd32[:].rearrange("p (c t) -> p c t", t=128),
                       ebl[:, h, :].unsqueeze(2).to_broadcast([128, 4, 128]), ALU.mult, [K_("kd32"), ("hg_ebl", b, h)], [("hg_Kl", b, h)])
                    if fwd:
                        for k in range(8):
                            MM(pq, Wg[:, k, h * 128:(h + 1) * 128], hT[b][:, k, :], k == 0, k == 7, [hk, "hg_Wg"], [pqk])
                        ACT(e1[:], pq, AF.Exp, [pqk], [K_("e1")], scale=-1.0)
                        ACT(l1[:], e1[:], AF.Ln, [K_("e1")], [K_("l1")], bias=1.0)
                        ACT(sq[:], l1[:], AF.Exp, [K_("l1")], [K_("sq")], scale=-1.0)
                        TT("dve", sg2[b][:, h, :], pq, sq[:], ALU.mult, [pqk, K_("sq")], [("hg_sg", b, h)])

                def streamsA(bi):
                    b = bi % 2
                    out = []
                    for si in range(2):
                        S.record()
                        if si == 0 and bi + 1 < NBLK:
                            nb = blocks[bi + 1]
                            DMA("sp", hT[1 - b][:], h0T_v[:, :, nb * 512:(nb + 1) * 512], ["h0T_d"], [f"hg_hT{1 - b}"])
                        vproj(bi, [2 * si, 2 * si + 1], 2 * si)
                        head(bi, si, si)
                        head(bi, si + 2, si)
                        out.append(S.stop())
                    return out

                def stageB(bi):
                    b = bi % 2
                    blk = blocks[bi]
                    Qd, Kd, Kl, vsb, ebl = Qd2[b], Kd2[b], Kl2[b], vsb2[b], ebl2[b]
                    allq = [("hg_Qd", b, h) for h in range(4)]
                    allk = [("hg_Kd", b, h) for h in range(4)]
                    alll = [("hg_Kl", b, h) for h in range(4)]
                    ob = osb[b]
                    if fwd:
                        DMA("sp", obw[:], obw_v[:, :, blk * 512:(blk + 1) * 512], ["obw_d"], ["hg_obw"])
                    for t in tiles:
                        ts = slice(t * 128, (t + 1) * 128)
                        g = gcount[0]
                        gcount[0] += 1
                        Sold, Snew = Sb2[(g + 1) % 2], Sb2[g % 2]
                        soldk, snewk = f"hg_Sb{(g + 1) % 2}", f"hg_Sb{g % 2}"
                        pk = bank16(4)
                        for h in range(4):
                            TR(pk[:, h * 128:(h + 1) * 128], Kl[:, h, ts], ident, alll + ["cst"], [("B", 4)])
                        CP("act", klt[:], pk[:, 0:512], [("B", 4)], ["hg_klt"])
                        pa = bank(5)
                        for h in range(4):
                            MM(pa[:, h * 128:(h + 1) * 128], Kd[:, h, ts], Qd[:, h, ts], True, True, allq + allk, [("B", 5)])
                        TT("dve", at[:], pa, mask, ALU.mult, [("B", 5), "cst"], ["hg_at"])
                        pS = bank(7)
                        for h in range(4):
                            MM(pS[:, h * 128:(h + 1) * 128], klt[:, h * 128:(h + 1) * 128], vsb[:, t, h * 128:(h + 1) * 128], True, True,
                               ["hg_klt", ("hg_v", b, t)], [("B", 7)])
                        po = bank(6)
                        for h in range(4):
                            MM(po[:, h * 128:(h + 1) * 128], vsb[:, t, h * 128:(h + 1) * 128], at[:, h * 128:(h + 1) * 128], True, False,
                               [("hg_v", b, t), "hg_at"], [("B", 6)])
                            MM(po[:, h * 128:(h + 1) * 128], Sold[:, h, :], Qd[:, h, ts], False, True, [soldk] + allq, [("B", 6)])
                        for h in range(4):
                            STT(St[:, h, :], St[:, h, :], ebl[:, h, t:t + 1], pS[:, h * 128:(h + 1) * 128], ALU.mult, ALU.add,
                                ["hg_S", ("hg_ebl", b, h), ("B", 7)], ["hg_S"])
                        CP("act", Snew[:], St[:], ["hg_S"], [snewk])
                        CP("act", ob[:, :, ts], po.rearrange("p (h t) -> p h t", h=4), [("B", 6)], [f"hg_o{b}"])
                    if not fwd:
                        DMA("sp", obw_v[:, :, blk * 512:(blk + 1) * 512], ob[:], [f"hg_o{b}"], ["obw_d"])
                        for _ in range(2):
                            if zlist:
                                c0, cw = zlist.pop(0)
                                DMA("sp", xb_flat[:, c0:c0 + cw], zt[:, 0:cw], ["hg_zero"], [("xbuf_z", c0)])
                        if bi == 0:
                            DMA("sp", ybuf_d[32 * CAP:32 * CAP + 128, :], zt[:, 0:2048].bitcast(F32), ["hg_zero"], ["ybuf_z"])
                    else:
                        TT("pool", ob[:], ob[:], obw[:], ALU.add, [f"hg_o{b}", "hg_obw"], [f"hg_o{b}"])
                        TT("pool", osq[:], ob[:], ob[:], ALU.mult, [f"hg_o{b}"], ["hg_osq"])
                        for h in range(4):
                            pr = bank(4)
                            MM(pr, ones_bf, osq[:, h, :], True, True, ["hg_osq", "cst"], [("B", 4)])
                            ACT(rs[:], pr, AF.Ln, [("B", 4)], ["hg_rs"], scale=1.0 / 128.0, bias=RMS_EPS)
                            ACT(rs[:], rs[:], AF.Exp, ["hg_rs"], ["hg_rs"], scale=-0.5)
                            TT("dve", rs[:], rs[:], ob[:, h, :], ALU.mult, ["hg_rs", f"hg_o{b}"], ["hg_rs"])
                            STT(cT[b][:, h, :], rs[:], normg[:, h:h + 1], sg2[b][:, h, :], ALU.mult, ALU.mult,
                                ["hg_rs", ("hg_sg", b, h), "normg"], [f"hg_cT{b}"])
                        DMA("sp", cT_v[:, :, blk * 512:(blk + 1) * 512], cT[b][:], [f"hg_cT{b}"], ["cT_d"])

                for st_ in streamsA(0):
                    S.merge(st_)
                for bi in range(NBLK):
                    sts = streamsA(bi + 1) if bi + 1 < NBLK else []
                    S.record()
                    stageB(bi)
                    rb_ = S.stop()
                    S.merge(*(sts + [rb_]))

        def phaseN():
            with ExitStack() as es:
                kT = sbuf(es, "na_kT", [128, 4, T], BF16)
                Vx = sbuf(es, "na_V", [128, NTILE, 8, 65], BF16)
                hT = [sbuf(es, f"na_hT{i}", [128, 8, 512], BF16) for i in range(2)]
                MSET("pool", Vx[:, :, :, 64:65], 1.0, ["na_V"])
                with ExitStack() as es1:
                    Wk = load_w(es1, "na_Wk", IN_OFF["na_k"], 512)
                    Wv = load_w(es1, "na_Wv", IN_OFF["na_v"], 512)
                    DMA("sp", hT[0][:], h0T_v[:, :, 0:512], ["h0T_d"], ["na_hT0"])
                    for blk in range(NBLK):
                        b = blk % 2
                        hk = f"na_hT{b}"
                        if blk + 1 < NBLK:
                            DMA("sp", hT[1 - b][:], h0T_v[:, :, (blk + 1) * 512:(blk + 2) * 512], ["h0T_d"], [f"na_hT{1 - b}"])
                        for p in range(4):
                            pk = bank(p % 2)
                            for k in range(8):
                                MM(pk, Wk[:, k, p * 128:(p + 1) * 128], hT[b][:, k, :], k == 0, k == 7, [hk, "na_Wk"], [("B", p % 2)])
                            CP("act" if p % 2 == 0 else "dve", kT[:, p, blk * 512:(blk + 1) * 512], pk, [("B", p % 2)], ["na_kT"])
                        for t in range(4):
                            pv = bank(2 + t % 2)
                            for k in range(8):
                                MM(pv, hT[b][:, k, t * 128:(t + 1) * 128], Wv[:, k, :], k == 0, k == 7, [hk, "na_Wv"], [("B", 2 + t % 2)])
                            CP("dve" if t % 2 == 0 else "act", Vx[:, blk * 4 + t, :, 0:64], pv.rearrange("p (h d) -> p h d", h=8),
                               [("B", 2 + t % 2)], ["na_V"])
                S.barrier()
                with ExitStack() as es2:
                    tts = sbuf(es2, "na_tt", [128, 16, 512], BF16)
                    DMA("sp", tts[:], tt_d, [], ["na_tt"])
                    Wq = load_w(es2, "na_Wq", IN_OFF["na_q"], 512)
                    qbd = sbuf(es2, "na_qbd", [128, 4, 8, 128], BF16)
                    Eb = [sbuf(es2, f"na_E{i}", [128, 1280], BF16) for i in range(2)]
                    rec = sbuf(es2, "na_rec", [128, 4], F32)
                    asb = [sbuf(es2, f"na_a{i}", [128, 512], BF16) for i in range(2)]
                    aT1 = sbuf(es2, "na_aT", [128, 4, 512], BF16)
                    aT = [aT1, aT1]
                    MSET("pool", qbd[:], 0.0, ["na_qbd"])
                    DMA("sp", hT[0][:], h0T_v[:, :, 0:512], ["h0T_d"], ["na_hT0"])

                    def tiles_of(r):
                        rs = min(max(r - 4, 0), 120)
                        if rs % 2 == 0:
                            return [(rs // 2 + i, 2 * (rs // 2 + i) - r + 7) for i in range(4)]
                        m0 = (rs - 1) // 2
                        return [(m0, 14)] + [(m0 + i, 2 * (m0 + i) - r + 7) for i in range(1, 4)] + [(m0 + 4, 15)]

                    def qk(u, rl):
                        r, h2 = u // 2, u % 2
                        tl = tiles_of(r)
                        nch = len(tl)
                        sbk = PS[:, (u % 2) * 1536:(u % 2) * 1536 + 1536]
                        keys = [("B", (u % 2) * 3 + i) for i in range(3)]
                        for pi in range(2):
                            p = 2 * h2 + pi
                            for ci, (m, slab) in enumerate(tl):
                                slot = pi * nch + ci
                                o = sbk[:, slot * 128:(slot + 1) * 128]
                                MM(o, kT[:, p, m * 128:(m + 1) * 128], qbd[:, p, rl, :], True, False, ["na_kT", "na_qbd"], keys)
                                MM(o, ident, tts[:, slab, p * 128:(p + 1) * 128], False, True, ["cst", "na_tt"], keys)

                    def rest(u):
                        r, h2 = u // 2, u % 2
                        tl = tiles_of(r)
                        nch = len(tl)
                        sbk = PS[:, (u % 2) * 1536:(u % 2) * 1536 + 1536]
                        keys = [("B", (u % 2) * 3 + i) for i in range(3)]
                        E = Eb[u % 2]
                        ek = f"na_E{u % 2}"
                        n = 2 * nch * 128
                        ACT(E[:, 0:n], sbk[:, 0:n], AF.Exp, keys, [ek])
                        par = r % 2
                        prt = slice(par * 64, (par + 1) * 64)
                        pO = bank(6)
                        for pi in range(2):
                            for hh in range(2):
                                hq = pi * 2 + hh
                                head = 4 * h2 + hq
                                for ci, (m, slab) in enumerate(tl):
                                    c0 = (pi * nch + ci) * 128 + hh * 64
                                    MM(pO[prt, hq * 65:(hq + 1) * 65], E[:, c0:c0 + 64], Vx[:, m, head, :], ci == 0, ci == nch - 1,
                                       [ek, "na_V"], [("B", 6)])
                        pov = pO[prt, 0:260].rearrange("p (h d) -> p h d", d=65)
                        S.add("dve", lambda e: e.reciprocal(out=rec[prt, :], in_=pov[:, :, 64]), r=[("B", 6)], w=["na_rec"])
                        tb = (r // 2) % 2
                        TT("dve", asb[tb][prt, h2 * 256:(h2 + 1) * 256].rearrange("p (h d) -> p h d", d=64), pov[:, :, 0:64],
                           rec[prt, :].unsqueeze(2).to_broadcast([64, 4, 64]), ALU.mult, [("B", 6), "na_rec"], [f"na_a{tb}"])

                    for blk in range(NBLK):
                        b = blk % 2
                        hk = f"na_hT{b}"
                        if blk + 1 < NBLK:
                            DMA("sp", hT[1 - b][:], h0T_v[:, :, (blk + 1) * 512:(blk + 2) * 512], ["h0T_d"], [f"na_hT{1 - b}"])
                        for p in range(4):
                            pq = bank(7)
                            for k in range(8):
                                MM(pq, Wq[:, k, p * 128:(p + 1) * 128], hT[b][:, k, :], k == 0, k == 7, [hk, "na_Wq"], [("B", 7)])
                            ACT(qbd[0:64, p, :, 0:64], pq[0:64, :].rearrange("p (r c) -> p r c", r=8), AF.Copy, [("B", 7)], ["na_qbd"], scale=0.125)
                            TS("dve", qbd[64:128, p, :, 64:128], pq[64:128, :].rearrange("p (r c) -> p r c", r=8), 0.125, None, ALU.mult, None,
                               [("B", 7)], ["na_qbd"])
                        u0 = blk * 16
                        qk(u0, 0)
                        for ul in range(16):
                            u = u0 + ul
                            if ul + 1 < 16:
                                qk(u + 1, (ul + 1) // 2)
                            rest(u)
                            if ul % 4 == 3:
                                t = ul // 4
                                tb = ((u // 2) // 2) % 2
                                pT = bank16(7)
                                for c in range(4):
                                    TR(pT[:, c * 128:(c + 1) * 128], asb[tb][:, c * 128:(c + 1) * 128], ident, [f"na_a{tb}", "cst"], [("B", 7)])
                                CP("act", aT[b][:, :, t * 128:(t + 1) * 128], pT[:, 0:512].rearrange("p (c t) -> p c t", c=4), [("B", 7)], ["na_aT"])
                        DMA("sp", aT_v[:, :, blk * 512:(blk + 1) * 512], aT[b][:], ["na_aT"], ["aT_d"])

        def phaseM():
            with ExitStack() as es:
                ba_bf = sbuf(es, "m_ba", [1, D], BF16)
                with ExitStack() as est:
                    tmpb = sbuf(est, "m_tmpb", [1, D], F32)
                    DMA("sp", tmpb[:], lnp_d[0:1, 1, :], [], ["m_tmpb"])
                    TS("dve", tmpb[:], tmpb[:], ALPHA, None, ALU.mult, None, ["m_tmpb"], ["m_tmpb"])
                    CP("dve", ba_bf[:], tmpb[:], ["m_tmpb"], ["m_ba"])
                S.barrier()
                lnp = sbuf(es, "m_lnp", [128, 3, D], F32)
                DMA("sp", lnp[:, 0, :], lnp_d[:, 0, :], [], ["m_lnp"])
                DMA("sp", lnp[:, 1:3, :], lnp_d[:, 2:4, :], [], ["m_lnp"])
                TS("pool", lnp[:, 0, :], lnp[:, 0, :], ALPHA, None, ALU.mult, None, ["m_lnp"], ["m_lnp"])
                Wga = load_w(es, "m_Wga", IN_OFF["gate_a"], 1024)
                Wgb = load_w(es, "m_Wgb", IN_OFF["gate_b"], 1024)
                Wa = sbuf(es, "m_Wa", [128, 4, D], BF16)
                Wb = sbuf(es, "m_Wb", [128, 4, D], BF16)
                Wo = sbuf(es, "m_Wo", [128, 8, D], BF16)
                for k in range(4):
                    DMA("pool", Wa[:, k, :], w_pa_d[k * 128:(k + 1) * 128, :], [], ["m_Wa"])
                    DMA("pool", Wb[:, k, :], w_pb_d[k * 128:(k + 1) * 128, :], [], ["m_Wb"])
                for k in range(8):
                    DMA("pool", Wo[:, k, :], w_out_d[k * 128:(k + 1) * 128, :], [], ["m_Wo"])
                wr = sbuf(es, "m_wr", [128, 8, 36], F32)
                rb = sbuf(es, "m_rb", [128, 4, 36], F32)
                DMA("sp", wr[:], wr_d, [], ["m_wr"])
                DMA("sp", rb[:], rb_d, [], ["m_rb"])
                hT = [sbuf(es, f"m_hT{i}", [128, 8, 512], BF16) for i in range(2)]
                aTs1 = sbuf(es, "m_aT", [128, 4, 512], BF16)
                aTs = [aTs1, aTs1]
                cTs1 = sbuf(es, "m_cT", [128, 4, 512], BF16)
                cTs = [cTs1, cTs1]
                xt = sbuf(es, "m_xt", [128, 4, D], F32)
                sgb_ = sbuf(es, "m_sgb", [128, 512], F32)
                sg4 = [(sbuf(es, f"m_sga{i}", [128, 512], F32), sgb_) for i in range(2)]
                m1 = sbuf(es, "m_m1", [128, 512], F32)
                m2 = sbuf(es, "m_m2", [128, 512], F32)
                mT = sbuf(es, "m_mT", [128, 8, 512], BF16)
                zz = [sbuf(es, f"m_z{i}", [128, 4, D], F32) for i in range(2)]
                hx2 = [sbuf(es, f"m_hx{i}", [128, D], F32) for i in range(2)]
                h1b = sbuf(es, "m_h1b", [128, 4, D], BF16)
                h1T = sbuf(es, "m_h1T", [128, 4, 128], F32)
                st1 = sbuf(es, "m_st1", [128, 4, 12], F32)
                mv1 = sbuf(es, "m_mv1", [128, 4, 2], F32)
                rs1 = sbuf(es, "m_rs1", [128, 4], F32)
                nm1 = sbuf(es, "m_nm1", [128, 4], F32)
                L = sbuf(es, "m_L", [128, 4, 36], F32)
                gmax = sbuf(es, "m_gmax", [128, 4], F32)
                gm = sbuf(es, "m_gm", [128, 4, 4], F32)
                gd = sbuf(es, "m_gd", [128, 4, 4], F32)
                gw = sbuf(es, "m_gw", [128, 4], F32)
                EM = sbuf(es, "m_EM", [128, 4, 32], F32)
                EM2 = sbuf(es, "m_EM2", [128, 4, 32], F32)
                top1 = sbuf(es, "m_top1", [128, 4], F32)
                top2 = sbuf(es, "m_top2", [128, 4], F32)
                oh = [sbuf(es, f"m_oh{i}", [128, 4, 32], F32) for i in range(2)]
                cnt = sbuf(es, "m_cnt", [128, 4, 32], BF16)
                rank = sbuf(es, "m_rank", [128, 4, 32], F32)
                slot = sbuf(es, "m_slot", [128, 4, 32], F32)
                tmp = sbuf(es, "m_tmp", [128, 4, 32], F32)
                tot = sbuf(es, "m_tot", [128, 32], F32)
                sm = sbuf(es, "m_sm", [128, 8, 4], F32)
                MSET("dve", tot[:], 0.0, ["m_tot"])
                if "fakeidx" in dbg:
                    fake_ix = sbuf(es, "m_fake", [128, NTILE, 2], I32)
                    S.add("pool", lambda e: e.iota(fake_ix[:].rearrange("p n k -> p (n k)"), pattern=[[128, 128]], base=0, channel_multiplier=1),
                          w=["m_fake"])
                ident32 = cst32[:, 2, 0:128]
                ustr = cst[:, 3, 0:128]
                ebase = cst32[:, 2, 128:160]
                pidx = cst32[:, 2, 160:161]

                def loads_ac(blk):
                    DMA("sp", aTs[0][:], aT_v[:, :, blk * 512:(blk + 1) * 512], ["aT_d"], ["m_aT"])
                    DMA("sp", cTs[0][:], cT_v[:, :, blk * 512:(blk + 1) * 512], ["cT_d"], ["m_cT"])

                def loads(blk, b):
                    DMA("sp", hT[b][:], h0T_v[:, :, blk * 512:(blk + 1) * 512], ["h0T_d"], [f"m_hT{b}"])

                def stageA(blk):
                    b = blk % 2
                    z = zz[b]
                    if blk + 1 < NBLK:
                        loads(blk + 1, 1 - b)
                    rs0 = xstat[:, blk * 4:(blk + 1) * 4, 0]
                    nm0 = xstat[:, blk * 4:(blk + 1) * 4, 1]
                    for c in range(8):
                        ga_, gb_ = 0, 1
                        pa_, pb_ = (2, 3) if c % 2 == 0 else (4, 5)
                        cs = slice(c * 128, (c + 1) * 128)
                        for k in range(8):
                            MM(bank(ga_), Wga[:, k, cs], hT[b][:, k, :], k == 0, k == 7, [f"m_hT{b}", "m_Wga"], [("B", ga_)])
                        for k in range(4):
                            MM(bank(pa_), Wa[:, k, cs], aTs[b][:, k, :], k == 0, k == 3, ["m_aT", "m_Wa"], [("B", pa_)])
                        for k in range(8):
                            MM(bank(gb_), Wgb[:, k, cs], hT[b][:, k, :], k == 0, k == 7, [f"m_hT{b}", "m_Wgb"], [("B", gb_)])
                        for k in range(4):
                            MM(bank(pb_), Wb[:, k, cs], cTs[b][:, k, :], k == 0, k == 3, ["m_cT", "m_Wb"], [("B", pb_)])
                        sga, sgb = sg4[c % 2]
                        ACT(sga[:], bank(ga_), AF.Sigmoid, [("B", ga_)], [f"m_sga{c % 2}"])
                        ACT(sgb[:], bank(gb_), AF.Sigmoid, [("B", gb_)], ["m_sgb"])
                        TT("dve", m1[:], bank(pa_), sga[:], ALU.mult, [("B", pa_), f"m_sga{c % 2}"], ["m_m1"])
                        TT("dve", m2[:], bank(pb_), sgb[:], ALU.mult, [("B", pb_), "m_sgb"], ["m_m2"])
                        TT("pool", mT[:, c, :], m1[:], m2[:], ALU.add, ["m_m1", "m_m2"], [("m_mT", c)])
                    if blk + 1 < NBLK:
                        loads_ac(blk + 1)
                    mtk = [("m_mT", c) for c in range(8)]
                    for t in range(4):
                        hxx = hx2[t % 2]
                        hk_ = f"m_hx{t % 2}"
                        ACT(hxx[:], xt[:, t, :], AF.Identity, ["m_xt", "xstat"], [hk_], scale=rs0[:, t:t + 1], bias=nm0[:, t:t + 1])
                        TT("dve", hxx[:], hxx[:], lnp[:, 0, :], ALU.mult, [hk_, "m_lnp"], [hk_])
                        for n in range(2):
                            o = (2, 3, 4, 5)[(t * 2 + n) % 4]
                            ns = slice(n * 512, (n + 1) * 512)
                            MM(bank(o), cst[0:1, 4, 0:128], ba_bf[0:1, ns], True, False, ["cst", "m_ba"], [("B", o)])
                            for k in range(8):
                                MM(bank(o), mT[:, k, t * 128:(t + 1) * 128], Wo[:, k, ns], False, k == 7, mtk + ["m_Wo"], [("B", o)])
                            TT("dve", z[:, t, ns], hxx[:, ns], bank(o), ALU.add, [hk_, ("B", o)], [("m_z", b, t)])
                    if blk + 1 < NBLK:
                        DMA("sp", xt[:], x_v[blk + 1], [], ["m_xt"])

                def stageB(blk):
                    b = blk % 2
                    z = zz[b]
                    zk = [("m_z", b, t) for t in range(4)]
                    for t in range(4):
                        for c in range(2):
                            S.add("dve", lambda e, t=t, c=c: e.bn_stats(out=st1[:, t, c * 6:(c + 1) * 6], in_=z[:, t, c * 512:(c + 1) * 512]),
                                  r=[("m_z", b, t)], w=["m1st"])
                        S.add("dve", lambda e, t=t: e.bn_aggr(out=mv1[:, t, :], in_=st1[:, t, :]), r=["m1st"], w=["m1mv"])
                    ACT(rs1[:], mv1[:, :, 1], AF.Ln, ["m1mv"], ["m1rs"], bias=LN_EPS)
                    ACT(rs1[:], rs1[:], AF.Exp, ["m1rs"], ["m1rs"], scale=-0.5)
                    STT(nm1[:], mv1[:, :, 0], -1.0, rs1[:], ALU.mult, ALU.mult, ["m1mv", "m1rs"], ["m1nm"])
                    for t in range(4):
                        ACT(z[:, t, :], z[:, t, :], AF.Identity, [("m_z", b, t), "m1rs", "m1nm"], [("m_z", b, t)], scale=rs1[:, t:t + 1], bias=nm1[:, t:t + 1])
                        TT("dve", z[:, t, :], z[:, t, :], lnp[:, 1, :], ALU.mult, [("m_z", b, t), "m_lnp"], [("m_z", b, t)])
                        TT("pool", z[:, t, :], z[:, t, :], lnp[:, 2, :], ALU.add, [("m_z", b, t), "m_lnp"], [("m_z", b, t)])
                        CP("act", h1b[:, t, :], z[:, t, :], [("m_z", b, t)], [("m_h1b", t)])
                    DMA("sp", h1_v[blk], z[:], zk, ["h1_d"])
                    pL = bank(7)
                    for t in range(4):
                        for g in range(2):
                            for kk_ in range(4):
                                k = g * 4 + kk_
                                TR(bank(6)[:, kk_ * 128:(kk_ + 1) * 128], z[:, t, k * 128:(k + 1) * 128], ident32, [("m_z", b, t), "cst32"], [("B", 6)])
                            CP("act" if g == 0 else "dve", h1T[:, 0:4, :], bank(6).rearrange("p (k t) -> p k t", k=4),
                               [("B", 6)], ["m_h1T"])
                            for kk_ in range(4):
                                k = g * 4 + kk_
                                MM(pL[:, t * 36:(t + 1) * 36], h1T[:, kk_, :], wr[:, k, :], k == 0, k == 7, ["m_h1T", "m_wr"], [("B", 7)])
                    TT("dve", L[:], pL[:, 0:144].rearrange("p (t n) -> p t n", n=36), rb[:], ALU.add, [("B", 7), "m_rb"], ["m_L"])
                    GL = L[:, :, 0:4]
                    EL = L[:, :, 4:36].rearrange("p t (g e) -> p t g e", g=4)
                    S.add("dve", lambda e: e.tensor_reduce(out=gmax[:], in_=GL, axis=AX.X, op=ALU.max), r=["m_L"], w=["m_gmax"])
                    TT("dve", gm[:], GL, gmax[:].unsqueeze(2).to_broadcast([128, 4, 4]), ALU.is_equal, ["m_L", "m_gmax"], ["m_gm"])
                    TT("dve", gd[:], GL, gmax[:].unsqueeze(2).to_broadcast([128, 4, 4]), ALU.subtract, ["m_L", "m_gmax"], ["m_gd"])
                    ACT(gd[:], gd[:], AF.Exp, ["m_gd"], ["m_gd"])
                    S.add("dve", lambda e: e.tensor_reduce(out=gw[:], in_=gd[:], axis=AX.X, op=ALU.add), r=["m_gd"], w=["m_gw"])
                    S.add("dve", lambda e: e.reciprocal(out=gw[:], in_=gw[:]), r=["m_gw"], w=["m_gw"])
                    TS("dve", gm[:], gm[:], 1e9, -1e9, ALU.mult, ALU.add, ["m_gm"], ["m_gm"])
                    TT("dve", EM[:].rearrange("p t (g e) -> p t g e", g=4), EL, gm[:].unsqueeze(3).to_broadcast([128, 4, 4, 8]), ALU.add,
                       ["m_L", "m_gm"], ["m_EM"])
                    S.add("dve", lambda e: e.tensor_reduce(out=top1[:], in_=EM[:], axis=AX.X, op=ALU.max), r=["m_EM"], w=["m_top1"])
                    TT("dve", oh[0][:], EM[:], top1[:].unsqueeze(2).to_broadcast([128, 4, 32]), ALU.is_equal, ["m_EM", "m_top1"], ["m_oh0"])
                    STT(EM2[:], oh[0][:], -1e9, EM[:], ALU.mult, ALU.add, ["m_oh0", "m_EM"], ["m_EM2"])
                    S.add("dve", lambda e: e.tensor_reduce(out=top2[:], in_=EM2[:], axis=AX.X, op=ALU.max), r=["m_EM2"], w=["m_top2"])
                    TT("dve", oh[1][:], EM2[:], top2[:].unsqueeze(2).to_broadcast([128, 4, 32]), ALU.is_equal, ["m_EM2", "m_top2"], ["m_oh1"])
                    w1 = wall[:, blk * 4:(blk + 1) * 4, 0]
                    w2 = wall[:, blk * 4:(blk + 1) * 4, 1]
                    TT("dve", sm[:, 0, :], top2[:], top1[:], ALU.subtract, ["m_top1", "m_top2"], ["m_sm0"])
                    ACT(sm[:, 0, :], sm[:, 0, :], AF.Exp, ["m_sm0"], ["m_sm0"])
                    TS("dve", sm[:, 0, :], sm[:, 0, :], 1.0, None, ALU.add, None, ["m_sm0"], ["m_sm0"])
                    S.add("dve", lambda e: e.reciprocal(out=sm[:, 1, :], in_=sm[:, 0, :]), r=["m_sm0"], w=["m_sm1"])
                    TT("dve", w1, sm[:, 1, :], gw[:], ALU.mult, ["m_sm1", "m_gw"], [("wall", blk)])
                    TT("dve", w2, gw[:], w1, ALU.subtract, ["m_gw", ("wall", blk)], [("wall", blk)])
                    TT("dve", cnt[:], oh[0][:], oh[1][:], ALU.add, ["m_oh0", "m_oh1"], ["m_cnt"])
                    pR = bank(7)[:, 256:512]
                    for t in range(4):
                        MM(pR[:, t * 32:(t + 1) * 32], ustr, cnt[:, t, :], True, t == 0, ["m_cnt", "cst"], [("B", 7)])
                        for t2 in range(t):
                            MM(pR[:, t * 32:(t + 1) * 32], ones_bf, cnt[:, t2, :], False, t2 == t - 1, ["m_cnt", "cst"], [("B", 7)])
                    for t in range(4):
                        MM(pR[:, 128:160], ones_bf, cnt[:, t, :], t == 0, t == 3, ["m_cnt", "cst"], [("B", 7)])
                    TT("dve", rank[:], pR[:, 0:128].rearrange("p (t n) -> p t n", n=32), tot[:].unsqueeze(1).to_broadcast([128, 4, 32]), ALU.add,
                       [("B", 7), "m_tot"], ["m_rank"])
                    TT("dve", tot[:], tot[:], pR[:, 128:160], ALU.add, ["m_tot", ("B", 7)], ["m_tot"])
                    TT("dve", slot[:], rank[:], ebase.unsqueeze(1).to_broadcast([128, 4, 32]), ALU.add, ["m_rank", "cst32"], ["m_slot"])
                    for kx in range(2):
                        ohk = f"m_oh{kx}"
                        TT("dve", tmp[:], oh[kx][:], slot[:], ALU.mult, [ohk, "m_slot"], ["m_tmp"])
                        S.add("dve", lambda e: e.tensor_reduce(out=sm[:, 2, :], in_=tmp[:], axis=AX.X, op=ALU.add), r=["m_tmp"], w=["m_sm2"])
                        TT("dve", tmp[:], oh[kx][:], rank[:], ALU.mult, [ohk, "m_rank"], ["m_tmp"])
                        S.add("dve", lambda e: e.tensor_reduce(out=sm[:, 3, :], in_=tmp[:], axis=AX.X, op=ALU.add), r=["m_tmp"], w=["m_sm3"])
                        TS("dve", sm[:, 3, :], sm[:, 3, :], float(CAP), None, ALU.is_ge, None, ["m_sm3"], ["m_sm3"])
                        TS("dve", sm[:, 4, :], sm[:, 2, :], -1.0, pidx, ALU.mult, ALU.add, ["m_sm2", "cst32"], ["m_sm4"])
                        TT("dve", sm[:, 4, :], sm[:, 4, :], sm[:, 3, :], ALU.mult, ["m_sm4", "m_sm3"], ["m_sm4"])
                        TT("dve", sm[:, 2, :], sm[:, 2, :], sm[:, 4, :], ALU.add, ["m_sm2", "m_sm4"], ["m_sm2"])
                        CP("pool", posall[:, blk * 4:(blk + 1) * 4, kx], sm[:, 2, :], ["m_sm2"], [("posall", blk)])
                    for t in range(4):
                        if "noscatter" in dbg:
                            break
                        for kx in range(2):
                            ixap = posall[:, blk * 4 + t, kx:kx + 1]
                            if "fakeidx" in dbg:
                                ixap = fake_ix[:, blk * 4 + t, kx:kx + 1]
                            S.add("pool", lambda e, t=t, kx=kx, ixap=ixap: e.indirect_dma_start(
                                out=xbuf_d, out_offset=bass.IndirectOffsetOnAxis(ap=ixap, axis=0),
                                in_=h1b[:, t, :], in_offset=None), r=[("m_h1b", t), ("posall", blk)], w=["xbuf_d"], dma=True, cost=4.0)

                loads(0, 0)
                loads_ac(0)
                DMA("sp", xt[:], x_v[0], [], ["m_xt"])
                stageA(0)
                for blk in range(NBLK):
                    ra = None
                    if blk + 1 < NBLK:
                        S.record()
                        stageA(blk + 1)
                        ra = S.stop()
                    S.record()
                    stageB(blk)
                    rb_ = S.stop()
                    S.merge(ra, rb_)

        def dump_routing():
            pos_d = nc.dram_tensor("pos_d", [128, NTILE * 2], I32, kind="ExternalOutput").ap()
            wall_d = nc.dram_tensor("wall_d", [128, NTILE * 2], F32, kind="ExternalOutput").ap()
            allk = [("posall", b) for b in range(NBLK)]
            allw = [("wall", b) for b in range(NBLK)]
            DMA("sp", pos_d, posall[:].rearrange("p n k -> p (n k)"), allk, ["pos_d"])
            DMA("sp", wall_d, wall[:].rearrange("p n k -> p (n k)"), allw, ["wall_d"])

        def phaseE():
            with ExitStack() as es:
                wg = [sbuf(es, f"e_wg{i}", [128, 8, 512], BF16) for i in range(2)]
                wu = [sbuf(es, f"e_wu{i}", [128, 8, 512], BF16) for i in range(2)]
                wd = [sbuf(es, f"e_wd{i}", [128, 4, D], BF16) for i in range(2)]
                xs = [sbuf(es, f"e_xs{i}", [128, 6, D], BF16) for i in range(2)]
                xT2 = [sbuf(es, f"e_xT{i}", [128, 8, CAP], BF16) for i in range(2)]
                sg = sbuf(es, "e_sg", [128, 384], F32)
                tg = sbuf(es, "e_tg", [128, 384], F32)
                hT2 = [sbuf(es, f"e_hT{i}", [128, 4, CAP], BF16) for i in range(2)]
                ysb = [sbuf(es, f"e_y{i}", [128, D], F32) for i in range(2)]

                wst = [sbuf(es, f"e_wst{i}", [128, 4096], F32) for i in range(3)]

                def wload(e, b):
                    DMA("sp", wst[0][:].rearrange("p (k n) -> p k n", k=8), w_gate_d[e].rearrange("(k p) n -> p k n", p=128), [], ["e_wst0"])
                    DMA("sp", wst[1][:].rearrange("p (k n) -> p k n", k=8), w_up_d[e].rearrange("(k p) n -> p k n", p=128), [], ["e_wst1"])
                    DMA("sp", wst[2][:].rearrange("p (k n) -> p k n", k=4), w_down_d[e].rearrange("(k p) n -> p k n", p=128), [], ["e_wst2"])
                    CP("pool", wg[b][:].rearrange("p k n -> p (k n)"), wst[0][:], ["e_wst0"], [f"e_wg{b}"])
                    CP("pool", wu[b][:].rearrange("p k n -> p (k n)"), wst[1][:], ["e_wst1"], [f"e_wu{b}"])
                    CP("pool", wd[b][:].rearrange("p k n -> p (k n)"), wst[2][:], ["e_wst2"], [f"e_wd{b}"])
                    DMA("sp", xs[b][:], xbuf_d[e * CAP:(e + 1) * CAP, :].rearrange("(s p) d -> p s d", p=128), ["xbuf_d"], [f"e_xs{b}"])

                def stageA(e):
                    b = e % 2
                    for k in range(8):
                        pb = bank16(k % 2)
                        for s_ in range(6):
                            TR(pb[:, s_ * 128:(s_ + 1) * 128], xs[b][:, s_, k * 128:(k + 1) * 128], ident, [f"e_xs{b}", "cst"], [("B", k % 2)])
                        CP("act" if k % 2 == 0 else "dve", xT2[b][:, k, :], pb[:, 0:CAP], [("B", k % 2)], [("e_xT", b, k)])

                def stageGU(e):
                    b = e % 2
                    xT = xT2[b]
                    hTt = hT2[b]
                    xtk = [("e_xT", b, k) for k in range(8)]
                    for m in range(4):
                        for half in range(2):
                            u = m * 2 + half
                            o = 2 + 2 * (u % 2)
                            hs = slice(half * 384, (half + 1) * 384)
                            ms = slice(m * 128, (m + 1) * 128)
                            for k in range(8):
                                MM(bank(o)[:, 0:384], wg[b][:, k, ms], xT[:, k, hs], k == 0, k == 7, xtk + [f"e_wg{b}"], [("B", o)])
                            for k in range(8):
                                MM(bank(o + 1)[:, 0:384], wu[b][:, k, ms], xT[:, k, hs], k == 0, k == 7, xtk + [f"e_wu{b}"], [("B", o + 1)])
                            ACT(sg[:], bank(o)[:, 0:384], AF.Sigmoid, [("B", o)], ["e_sg"])
                            TT("dve", tg[:], bank(o)[:, 0:384], sg[:], ALU.mult, [("B", o), "e_sg"], ["e_tg"])
                            TT("dve", hTt[:, m, hs], bank(o + 1)[:, 0:384], tg[:], ALU.mult, [("B", o + 1), "e_tg"], [("e_hT", b, m)])

                def stageY(e):
                    b = e % 2
                    hTt = hT2[b]
                    htk = [("e_hT", b, m) for m in range(4)]
                    for s_ in range(6):
                        yb = (e * 6 + s_) % 2
                        for n in range(2):
                            yo = 6 + n
                            for m in range(4):
                                MM(bank(yo), hTt[:, m, s_ * 128:(s_ + 1) * 128], wd[b][:, m, n * 512:(n + 1) * 512], m == 0, m == 3,
                                   htk + [f"e_wd{b}"], [("B", yo)])
                            CP("act" if n == 0 else "dve", ysb[yb][:, n * 512:(n + 1) * 512], bank(yo), [("B", yo)], [(f"e_y{yb}", n)])
                        DMA("sp", ybuf_d[e * CAP + s_ * 128:e * CAP + (s_ + 1) * 128, :], ysb[yb][:], [(f"e_y{yb}", 0), (f"e_y{yb}", 1)], ["ybuf_d"])

                wload(0, 0)
                for e in range(32):
                    if e + 1 < 32:
                        wload(e + 1, (e + 1) % 2)
                    stageA(e)
                    stageGU(e)
                    stageY(e)

        def phaseG():
            with ExitStack() as es:
                lnp = sbuf(es, "g_lnp", [128, 2, D], F32)
                DMA("sp", lnp[:], lnp_d[:, 4:6, :], [], ["g_lnp"])
                y0 = [sbuf(es, f"g_y0{i}", [128, D], F32) for i in range(2)]
                y1 = [sbuf(es, f"g_y1{i}", [128, D], F32) for i in range(2)]
                h1t = [sbuf(es, f"g_h1{i}", [128, D], F32) for i in range(2)]
                zz = [sbuf(es, f"g_z{i}", [128, 1, D], F32) for i in range(2)]
                st2 = [sbuf(es, f"g_st{i}", [128, 1, 12], F32) for i in range(2)]
                mv2 = [sbuf(es, f"g_mv{i}", [128, 1, 2], F32) for i in range(2)]
                rs2 = [sbuf(es, f"g_rs{i}", [128, 1], F32) for i in range(2)]
                nm2 = [sbuf(es, f"g_nm{i}", [128, 1], F32) for i in range(2)]

                def gl(n, b):
                    S.add("pool", lambda e: e.indirect_dma_start(out=y0[b][:], out_offset=None, in_=ybuf_d,
                                                                 in_offset=bass.IndirectOffsetOnAxis(ap=posall[:, n, 0:1], axis=0)),
                          r=["ybuf_d"], w=[f"g_y0{b}"], dma=True, cost=5.0)
                    S.add("pool", lambda e: e.indirect_dma_start(out=y1[b][:], out_offset=None, in_=ybuf_d,
                                                                 in_offset=bass.IndirectOffsetOnAxis(ap=posall[:, n, 1:2], axis=0)),
                          r=["ybuf_d"], w=[f"g_y1{b}"], dma=True, cost=5.0)
                    DMA("sp", h1t[b][:], h1_tv[n], ["h1_d"], [f"g_h1{b}"])

                def stageA(n):
                    b = n % 2
                    zt = zz[b]
                    zk = f"g_z{b}"
                    TS("dve", zt[:, 0, :], y0[b][:], wall[:, n, 0:1], None, ALU.mult, None, [f"g_y0{b}"], [zk, zk + "a", zk + "b"])
                    STT(zt[:, 0, :], y1[b][:], wall[:, n, 1:2], zt[:, 0, :], ALU.mult, ALU.add, [f"g_y1{b}", zk], [zk])
                    STT(zt[:, 0, :], h1t[b][:], ALPHA, zt[:, 0, :], ALU.mult, ALU.add, [f"g_h1{b}", zk], [zk])
                    ln_stats(f"g{b}", zt, zk, st2[b], mv2[b], rs2[b], nm2[b], tiles=1)

                def stageB(n):
                    b = n % 2
                    zt = zz[b]
                    zk = f"g_z{b}"
                    ACT(zt[:, 0, :], zt[:, 0, :], AF.Identity, [zk, f"g{b}rs", f"g{b}nm"], [zk], scale=rs2[b][:, 0:1], bias=nm2[b][:, 0:1])
                    TT("dve", zt[:, 0, :], zt[:, 0, :], lnp[:, 0, :], ALU.mult, [zk, "g_lnp"], [zk])
                    TT("pool", zt[:, 0, 0:512], zt[:, 0, 0:512], lnp[:, 1, 0:512], ALU.add, [zk, "g_lnp"], [zk + "a"])
                    TT("dve", zt[:, 0, 512:1024], zt[:, 0, 512:1024], lnp[:, 1, 512:1024], ALU.add, [zk, "g_lnp"], [zk + "b"])
                    DMA("sp", out_v[n], zt[:, 0, :], [zk, zk + "a", zk + "b"], ["out"])

                gl(0, 0)
                gl(1, 1)
                for n in range(0, NTILE, 2):
                    sts = []
                    for n2 in (n, n + 1):
                        S.record()
                        stageA(n2)
                        if n2 + 2 < NTILE:
                            gl(n2 + 2, n2 % 2)
                        stageB(n2)
                        sts.append(S.stop())
                    S.merge(*sts)

        for ph in phases:
            if ph == "0":
                phase0()
            elif ph == "B":
                hgrn_phase(1)
            elif ph == "F":
                hgrn_phase(0)
            elif ph == "N":
                phaseN()
            elif ph == "M":
                phaseM()
                if "pos_d" in dbg:
                    dump_routing()
            elif ph == "E":
                phaseE()
            elif ph == "G":
                phaseG()
            S.barrier()
        S.emit(nc)
    return nc


def host_consts():
    bf = ml_dtypes.bfloat16
    cst = np.zeros((128, 5, 512), np.float32)
    eye = np.eye(128, dtype=np.float32)
    s = np.arange(128)[:, None]
    t = np.arange(128)[None, :]
    cst[:, 0, :] = np.tile(eye, (1, 4))
    cst[:, 1, :] = np.tile((s <= t).astype(np.float32), (1, 4))
    cst[:, 2, :] = np.tile((s >= t).astype(np.float32), (1, 4))
    cst[:, 3, :] = np.tile((s < t).astype(np.float32), (1, 4))
    cst[:, 4, :] = 1.0
    cst32 = np.zeros((128, 3, 512), np.float32)
    tt = np.arange(512)
    cst32[:, 0, :] = (tt % 128 != 0).astype(np.float32)[None, :]
    cst32[:, 1, :] = (tt % 128 != 127).astype(np.float32)[None, :]
    cst32[:, 2, 0:128] = eye
    cst32[:, 2, 128:160] = (np.arange(32) * CAP).astype(np.float32)[None, :]
    cst32[:, 2, 160] = 32 * CAP + np.arange(128)
    return cst.astype(bf), cst32


def host_tt(rpb):
    bf = ml_dtypes.bfloat16
    cols = np.arange(64)
    col_start = np.clip(cols - 8, 0, 48)
    c = cols[None, :]
    kc = cols[:, None]
    valid = (kc >= col_start[None, :]) & (kc < col_start[None, :] + 16)
    off = np.clip(kc - c + 15, 0, 30)
    full = np.full((15, 64, 8, 64), NEG, np.float32)
    for ro in range(15):
        for h in range(8):
            full[ro, :, h, :] = np.where(valid, rpb[h, ro][off], NEG)
    full = full.reshape(15, 64, 512)
    neg = np.full((64, 512), NEG, np.float32)
    tt = np.zeros((128, 16, 512), np.float32)
    for a in range(14):
        tt[0:64, a] = full[a]
        tt[64:128, a] = full[a + 1]
    tt[0:64, 14] = neg
    tt[64:128, 14] = full[3]
    tt[0:64, 15] = full[10]
    tt[64:128, 15] = neg
    return tt.astype(bf)


def make_in_maps(inp):
    cst, cst32 = host_consts()
    f = lambda a: np.ascontiguousarray(np.asarray(a, np.float32))
    lnp = np.stack([inp["emb_ln_g"], inp["emb_ln_b"], inp["ln1_g"][0], inp["ln1_b"][0], inp["ln2_g"][0], inp["ln2_b"][0]], 0)
    lnp = f(np.broadcast_to(lnp[None], (128, 6, D)))
    embp = f(np.concatenate([np.asarray(inp["emb_ln_g"]).reshape(8, 128).T, np.asarray(inp["emb_ln_b"]).reshape(8, 128).T], 1))
    lbraw = f(np.asarray(inp["hg_lb"]).reshape(2, 2, 4, 128).transpose(3, 0, 1, 2).reshape(128, 16))
    normg = f(np.asarray(inp["hg_norm_g"])[0].reshape(4, 128).T)
    wr = np.concatenate([np.asarray(inp["w_router_group"])[0], np.asarray(inp["w_router_expert"])[0]], 1)
    wr = f(wr.reshape(8, 128, 36).transpose(1, 0, 2))
    rb = np.concatenate([np.asarray(inp["b_router_group"])[0], np.asarray(inp["b_router_expert"])[0]], 0)
    rb = f(np.broadcast_to(rb[None, None], (128, 4, 36)))
    tt = host_tt(np.asarray(inp["na_rpb"], np.float32)[0])
    shared = dict(w_in=f(inp["w_in"][0]), w_proj_a=f(inp["w_proj_a"][0]), w_proj_b=f(inp["w_proj_b"][0]), w_out=f(inp["w_out"][0]),
                  w_gate=f(inp["w_gate"][0]), w_up=f(inp["w_up"][0]), w_down=f(inp["w_down"][0]),
                  lnp=lnp, embp=embp, lbraw=lbraw, normg=normg, wr=wr, rb=rb, tt=tt, cst=cst, cst32=cst32)
    x = np.asarray(inp["x"], np.float32)
    return [dict(shared, x=np.ascontiguousarray(x[b])) for b in range(x.shape[0])]


def kernel(**inputs):
    nc = build()
    in_maps = make_in_maps(inputs)
    res = run_bass_kernel_spmd(nc, in_maps, core_ids=list(range(8)))
    return np.stack([np.asarray(r["out"], np.float32) for r in res.results], 0)
```

```python
from contextlib import ExitStack
import numpy as np
import ml_dtypes
import concourse.bass as bass
import concourse.mybir as mybir
from concourse.bass_utils import run_bass_kernel_spmd

F32 = mybir.dt.float32
BF16 = mybir.dt.bfloat16
I32 = mybir.dt.int32
AF = mybir.ActivationFunctionType
ALU = mybir.AluOpType
AX = mybir.AxisListType

T = 8192
D = 1024
NBLK = 16
NTILE = 64
CAP = 768
NSLOT = 32 * CAP + 128
ALPHA = 2.0 ** 0.25
LN_EPS = 1e-5
RMS_EPS = 1e-6
NEG = -30000.0

ENGS = ("pe", "dve", "act", "pool", "sp")
EPOCH = 3000
NDMA_SEM = 8


class Op:
    __slots__ = ("eng", "fn", "deps", "sig", "sem", "val", "dma", "n")

    def __init__(self, eng, fn, dma):
        self.eng = eng
        self.fn = fn
        self.dma = dma
        self.deps = []
        self.sig = dma
        self.sem = None
        self.val = 0


class Sched:
    def __init__(self):
        self.q = {e: [] for e in ENGS}
        self.last_w = {}
        self.readers = {}
        self.dma_hist = {e: [] for e in ENGS}
        self.nops = 0
        self.bar = None
        self.bar_pending = set()
        self.rec = None
        self.m_wfin = {}
        self.m_rfin = {}
        self.m_efree = {}

    def barrier(self):
        ops = []
        for e in ENGS:
            last = None
            for op in reversed(self.q[e]):
                if not op.dma:
                    last = op
                    break
            if last is not None:
                ops.append(last)
            ops.extend(self.dma_hist[e][-NDMA_SEM:])
        self.bar = ops
        self.bar_pending = set(ENGS)

    def record(self):
        self.rec = []

    def stop(self):
        r = self.rec
        self.rec = None
        return r

    def merge(self, *streams):
        streams = [st for st in streams if st]
        pos = [0] * len(streams)
        total = sum(len(st) for st in streams)
        wfin = self.m_wfin
        rfin = self.m_rfin
        efree = self.m_efree
        for _ in range(total):
            best = None
            for i, st in enumerate(streams):
                if pos[i] >= len(st):
                    continue
                eng, fn, r, w, dma, cost = st[pos[i]]
                rdy = 0.0
                for k in r:
                    rdy = max(rdy, wfin.get(k, 0.0))
                for k in w:
                    rdy = max(rdy, wfin.get(k, 0.0), rfin.get(k, 0.0))
                start = max(efree.get(eng, 0.0), rdy + 0.1)
                rem = len(st) - pos[i]
                cand = (start, -rem, i)
                if best is None or cand < best[0]:
                    best = (cand, i, start)
            _, i, start = best
            eng, fn, r, w, dma, cost = streams[i][pos[i]]
            pos[i] += 1
            if dma:
                efree[eng] = start + 0.06
                fin = start + cost
            else:
                fin = start + cost
                efree[eng] = fin
            for k in r:
                rfin[k] = max(rfin.get(k, 0.0), fin)
            for k in w:
                wfin[k] = fin
                rfin[k] = 0.0
            self.add(eng, fn, r, w, dma, cost)

    def add(self, eng, fn, r=(), w=(), dma=False, cost=0.5):
        if self.rec is not None:
            self.rec.append((eng, fn, tuple(r), tuple(w), dma, cost))
            return None
        op = Op(eng, fn, dma)
        op.n = self.nops
        self.nops += 1
        deps = {}
        if eng in self.bar_pending:
            self.bar_pending.discard(eng)
            for p in self.bar:
                if p.dma or p.eng != eng:
                    deps[id(p)] = p
        for k in r:
            p = self.last_w.get(k)
            if p is not None and p is not op:
                if p.eng == eng and not p.dma and not dma:
                    if eng != "pe":
                        deps[id(p)] = p
                else:
                    deps[id(p)] = p
        for k in w:
            p = self.last_w.get(k)
            if p is not None and p is not op and (p.dma or dma or p.eng != eng or eng != "pe"):
                deps[id(p)] = p
            rd = self.readers.get(k)
            if rd:
                for p in rd.values():
                    if p is not op and (p.dma or dma or p.eng != eng or eng != "pe"):
                        deps[id(p)] = p
        for k in w:
            self.last_w[k] = op
            self.readers[k] = {}
        for k in r:
            d = self.readers.setdefault(k, {})
            d[("dma", op.n) if dma else eng] = op
        if dma:
            h = self.dma_hist[eng]
            if len(h) >= NDMA_SEM:
                p = h[-NDMA_SEM]
                deps[id(p)] = p
            h.append(op)
        op.deps = list(deps.values())
        for p in op.deps:
            p.sig = True
        self.q[eng].append(op)
        return op

    def emit(self, nc):
        with ExitStack() as es:
            for e in ENGS:
                cnt = 0
                sem = None
                dsem = [None] * NDMA_SEM
                dval = [0] * NDMA_SEM
                nd = 0
                ns = 0
                for op in self.q[e]:
                    if op.dma:
                        s = nd % NDMA_SEM
                        if dsem[s] is None:
                            dsem[s] = es.enter_context(nc.semaphore(f"d_{e}_{s}"))
                        dval[s] += 16
                        op.sem = dsem[s]
                        op.val = dval[s]
                        nd += 1
                    elif op.sig:
                        if sem is None or cnt >= EPOCH:
                            sem = es.enter_context(nc.semaphore(f"c_{e}_{ns}"))
                            ns += 1
                            cnt = 0
                        cnt += 1
                        op.sem = sem
                        op.val = cnt
            block = es.enter_context(nc.Block())
            engmap = {"pe": block.tensor, "dve": block.vector, "act": block.scalar,
                      "pool": block.gpsimd, "sp": block.sync}
            for e in ENGS:
                ops = self.q[e]
                if not ops:
                    continue

                def body(engine, ops=ops):
                    waited = {}
                    for op in ops:
                        for p in op.deps:
                            key = id(p.sem)
                            if waited.get(key, 0) >= p.val:
                                continue
                            waited[key] = p.val
                            engine.wait_ge(p.sem, p.val)
                        ins = op.fn(engine)
                        if op.sig:
                            ins.then_inc(op.sem, 16 if op.dma else 1)
                    last = {}
                    for op in ops:
                        if op.dma:
                            last[id(op.sem)] = op
                    for op in last.values():
                        if waited.get(id(op.sem), 0) < op.val:
                            engine.wait_ge(op.sem, op.val)

                engmap[e](body)


IN_OFF = dict(na_q=0, na_k=512, na_v=1024, hg_q=1536, hg_ff=2048, hg_fb=2560, hg_i=3072, hg_g=3584,
              gate_a=4096, gate_b=5120)


def build(dbg=(), phases="0BNFMEG"):
    nc = bass.Bass("TRN2", target_bir_lowering=False)
    S = Sched()

    def din(name, shape, dt):
        return nc.dram_tensor(name, list(shape), dt, kind="ExternalInput").ap()

    def dscr(name, shape, dt):
        kind = "ExternalOutput" if name in dbg else "Internal"
        return nc.dram_tensor(name, list(shape), dt, kind=kind).ap()

    x_d = din("x", [T, D], F32)
    w_in_d = din("w_in", [D, 6144], F32)
    w_pa_d = din("w_proj_a", [512, D], F32)
    w_pb_d = din("w_proj_b", [512, D], F32)
    w_out_d = din("w_out", [D, D], F32)
    w_gate_d = din("w_gate", [32, D, 512], F32)
    w_up_d = din("w_up", [32, D, 512], F32)
    w_down_d = din("w_down", [32, 512, D], F32)
    lnp_d = din("lnp", [128, 6, D], F32)
    embp_d = din("embp", [128, 16], F32)
    lbraw_d = din("lbraw", [128, 16], F32)
    normg_d = din("normg", [128, 4], F32)
    wr_d = din("wr", [128, 8, 36], F32)
    rb_d = din("rb", [128, 4, 36], F32)
    tt_d = din("tt", [128, 16, 512], BF16)
    cst_d = din("cst", [128, 5, 512], BF16)
    cst32_d = din("cst32", [128, 3, 512], F32)
    out_d = nc.dram_tensor("out", [T, D], F32, kind="ExternalOutput").ap()

    h0T_d = dscr("h0T_d", [D, T], BF16)
    obw_d = dscr("obw_d", [512, T], F32)
    aT_d = dscr("aT_d", [512, T], BF16)
    cT_d = dscr("cT_d", [512, T], BF16)
    h1_d = dscr("h1_d", [T, D], F32)
    xbuf_d = dscr("xbuf_d", [NSLOT, D], BF16)
    ybuf_d = dscr("ybuf_d", [NSLOT, D], F32)

    h0T_v = h0T_d.rearrange("(k p) t -> p k t", p=128)
    obw_v = obw_d.rearrange("(h p) t -> p h t", p=128)
    aT_v = aT_d.rearrange("(k p) t -> p k t", p=128)
    cT_v = cT_d.rearrange("(k p) t -> p k t", p=128)
    x_v = x_d.rearrange("(b t p) d -> b p t d", t=4, p=128)
    h1_v = h1_d.rearrange("(b t p) d -> b p t d", t=4, p=128)
    out_v = out_d.rearrange("(n p) d -> n p d", p=128)
    h1_tv = h1_d.rearrange("(n p) d -> n p d", p=128)
    w_in_v = w_in_d.rearrange("(k p) n -> p k n", p=128)

    with ExitStack() as ges:
        uid = [0]

        def sbuf(es, name, shape, dt):
            uid[0] += 1
            return es.enter_context(nc.sbuf_tensor(f"s{uid[0]}_{name}", list(shape), dt))

        PS = ges.enter_context(nc.psum_tensor("PS", [128, 4096], F32))

        def bank(i, n=1):
            return PS[:, i * 512:(i + n) * 512]

        def bank16(i):
            return PS[:, i * 512:(i + 1) * 512].bitcast(BF16)

        cst = sbuf(ges, "cst", [128, 5, 512], BF16)
        cst32 = sbuf(ges, "cst32", [128, 3, 512], F32)
        embp = sbuf(ges, "embp", [128, 16], F32)
        lbraw = sbuf(ges, "lbraw", [128, 16], F32)
        lbp = sbuf(ges, "lbp", [128, 3, 8], F32)
        normg = sbuf(ges, "normg", [128, 4], F32)
        posall = sbuf(ges, "posall", [128, NTILE, 2], I32)
        wall = sbuf(ges, "wall", [128, NTILE, 2], F32)
        xstat = sbuf(ges, "xstat", [128, NTILE, 2], F32)
        ident = cst[:, 0, 0:128]
        ones_bf = cst[:, 4, 0:128]

        def nfree(ap):
            n = 1
            for d in ap.shape[1:]:
                n *= d
            return n

        def DMA(eng, out, in_, r, w):
            S.add(eng, lambda e: e.dma_start(out=out, in_=in_), r=r, w=w, dma=True, cost=2.5 + nfree(out) * 128 * 4 / 150e3)

        def MM(out, lhsT, rhs, start, stop, r, w):
            c_ = max(64, nfree(rhs)) / 2400.0 * (4.0 if rhs.dtype == F32 else 1.0) + 0.01
            S.add("pe", lambda e: e.matmul(out, lhsT=lhsT, rhs=rhs, start=start, stop=stop), r=r, w=w, cost=c_)

        def TR(out, in_, idn, r, w):
            S.add("pe", lambda e: e.transpose(out, in_, idn), r=r, w=w, cost=0.08)

        def ACT(out, in_, func, r, w, scale=1.0, bias=0.0):
            S.add("act", lambda e: e.activation(out=out, in_=in_, func=func, bias=bias, scale=scale), r=r, w=w, cost=0.25 + nfree(out) / 1200.0)

        def vcost(eng, out):
            return (0.3 + nfree(out) / 330.0) if eng == "pool" else (0.16 + nfree(out) / 960.0)

        def TT(eng, out, in0, in1, op, r, w):
            S.add(eng, lambda e: e.tensor_tensor(out=out, in0=in0, in1=in1, op=op), r=r, w=w, cost=vcost(eng, out))

        def TS(eng, out, in0, s1, s2, op0, op1, r, w):
            if s2 is None:
                S.add(eng, lambda e: e.tensor_scalar(out=out, in0=in0, scalar1=s1, scalar2=None, op0=op0), r=r, w=w, cost=vcost(eng, out))
            else:
                S.add(eng, lambda e: e.tensor_scalar(out=out, in0=in0, scalar1=s1, scalar2=s2, op0=op0, op1=op1), r=r, w=w, cost=vcost(eng, out))

        def STT(out, in0, scalar, in1, op0, op1, r, w):
            S.add("dve", lambda e: e.scalar_tensor_tensor(out=out, in0=in0, scalar=scalar, in1=in1, op0=op0, op1=op1), r=r, w=w, cost=vcost("dve", out))

        def CP(eng, out, in_, r, w):
            if eng == "act":
                S.add("act", lambda e: e.activation(out=out, in_=in_, func=AF.Copy), r=r, w=w, cost=0.25 + nfree(out) / 1200.0)
            else:
                S.add(eng, lambda e: e.tensor_copy(out=out, in_=in_), r=r, w=w, cost=vcost(eng, out))

        def MSET(eng, ap, val, w):
            S.add(eng, lambda e: e.memset(ap, val), w=w)

        DMA("sp", cst[:], cst_d, [], ["cst"])
        DMA("sp", cst32[:], cst32_d, [], ["cst32"])
        DMA("sp", embp[:], embp_d, [], ["embp"])
        DMA("sp", lbraw[:], lbraw_d, [], ["lbraw"])
        DMA("sp", normg[:], normg_d, [], ["normg"])
        lbr = lbraw[:].rearrange("p (d l h) -> p d l h", d=2, l=2)
        lb_v = lbp[:, 0, :].rearrange("p (d h) -> p d h", d=2)
        TT("dve", lb_v, lbr[:, :, 1, :], lbr[:, :, 0, :], ALU.subtract, ["lbraw"], ["lbp"])
        ACT(lbp[:, 0, :], lbp[:, 0, :], AF.Exp, ["lbp"], ["lbp"])
        ACT(lbp[:, 0, :], lbp[:, 0, :], AF.Ln, ["lbp"], ["lbp"], bias=1.0)
        ACT(lbp[:, 0, :], lbp[:, 0, :], AF.Exp, ["lbp"], ["lbp"], scale=-1.0)
        TS("dve", lbp[:, 1, :], lbp[:, 0, :], -1.0, 1.0, ALU.mult, ALU.add, ["lbp"], ["lbp"])
        TS("dve", lbp[:, 2, :], lbp[:, 1, :], -1.0, None, ALU.mult, None, ["lbp"], ["lbp"])

        def ln_stats(es_name, xt, xkey, stats, mv, rstd, nmr, tiles=4):
            for t in range(tiles):
                for c in range(2):
                    S.add("dve", lambda e, t=t, c=c: e.bn_stats(out=stats[:, t, c * 6:(c + 1) * 6], in_=xt[:, t, c * 512:(c + 1) * 512]),
                          r=[xkey], w=[es_name + "st"])
                S.add("dve", lambda e, t=t: e.bn_aggr(out=mv[:, t, :], in_=stats[:, t, :]), r=[es_name + "st"], w=[es_name + "mv"])
            ACT(rstd[:, 0:tiles], mv[:, 0:tiles, 1], AF.Ln, [es_name + "mv"], [es_name + "rs", "xstat"], bias=LN_EPS)
            ACT(rstd[:, 0:tiles], rstd[:, 0:tiles], AF.Exp, [es_name + "rs"], [es_name + "rs", "xstat"], scale=-0.5)
            STT(nmr[:, 0:tiles], mv[:, 0:tiles, 0], -1.0, rstd[:, 0:tiles], ALU.mult, ALU.mult, [es_name + "mv", es_name + "rs"], [es_name + "nm", "xstat"])

        def phase0():
            with ExitStack() as es:
                xt = [sbuf(es, f"p0_xt{i}", [128, 4, D], F32) for i in range(2)]
                xn2 = [sbuf(es, f"p0_xn{i}", [128, 4, D], BF16) for i in range(2)]
                hT = [sbuf(es, f"p0_hT{i}", [128, 8, 512], BF16) for i in range(2)]
                stats2 = [sbuf(es, f"p0_stats{i}", [128, 4, 12], F32) for i in range(2)]
                mv2 = [sbuf(es, f"p0_mv{i}", [128, 4, 2], F32) for i in range(2)]
                def stageA(blk):
                    b = blk % 2
                    if blk + 1 < NBLK:
                        DMA("sp", xt[1 - b][:], x_v[blk + 1], [], [f"xt{1 - b}"])
                    rstd = xstat[:, blk * 4:(blk + 1) * 4, 0]
                    nmr = xstat[:, blk * 4:(blk + 1) * 4, 1]
                    ln_stats(f"p0{b}", xt[b], f"xt{b}", stats2[b], mv2[b], rstd, nmr)
                    for t in range(4):
                        ACT(xn2[b][:, t, :], xt[b][:, t, :], AF.Identity, [f"xt{b}", f"p0{b}rs", f"p0{b}nm"], [("xn", b, t)],
                            scale=rstd[:, t:t + 1], bias=nmr[:, t:t + 1])

                def stageB(blk):
                    b = blk % 2
                    xn = xn2[b]
                    for k in range(8):
                        pb = bank16(k % 2)
                        for t in range(4):
                            TR(pb[:, t * 128:(t + 1) * 128], xn[:, t, k * 128:(k + 1) * 128], ident, [("xn", b, t), "cst"], [("B", k % 2)])
                        if k % 2 == 0:
                            ACT(hT[b][:, k, :], pb[:, 0:512], AF.Identity, [("B", k % 2), "embp"], [f"hT{b}"],
                                scale=embp[:, k:k + 1], bias=embp[:, 8 + k:9 + k])
                        else:
                            TS("dve", hT[b][:, k, :], pb[:, 0:512], embp[:, k:k + 1], embp[:, 8 + k:9 + k], ALU.mult, ALU.add,
                               [("B", k % 2), "embp"], [f"hT{b}"])
                    DMA("sp", h0T_v[:, :, blk * 512:(blk + 1) * 512], hT[b][:], [f"hT{b}"], ["h0T_d"])

                DMA("sp", xt[0][:], x_v[0], [], ["xt0"])
                stageA(0)
                for blk in range(NBLK):
                    S.record()
                    if blk + 1 < NBLK:
                        stageA(blk + 1)
                    ra = S.stop()
                    S.record()
                    stageB(blk)
                    rb_ = S.stop()
                    S.merge(ra, rb_)

        def load_w(es, name, col0, ncol):
            w = sbuf(es, name, [128, 8, ncol], BF16)
            for k in range(8):
                DMA("pool", w[:, k, :], w_in_v[:, k, col0:col0 + ncol], [], [name])
            return w

        def hgrn_phase(direction):
            fwd = direction == 0
            with ExitStack() as es:
                Wq = load_w(es, "hg_Wq", IN_OFF["hg_q"], 512)
                Wf = load_w(es, "hg_Wf", IN_OFF["hg_ff"] if fwd else IN_OFF["hg_fb"], 512)
                Wi = load_w(es, "hg_Wi", IN_OFF["hg_i"], 512)
                Wg = load_w(es, "hg_Wg", IN_OFF["hg_g"], 512) if fwd else None
                hT = [sbuf(es, f"hg_hT{i}", [128, 8, 512], BF16) for i in range(2)]
                tnames = ["e1", "l1", "l2", "sq", "qf", "g", "fg", "kk", "bc", "eb", "enb", "kd32"]
                tmps = [{n: sbuf(es, f"hg_{n}{i}", [128, 512], F32) for n in tnames} for i in range(2)]
                ebl2 = [sbuf(es, f"hg_ebl{i}", [128, 4, 4], F32) for i in range(2)]
                Qd2 = [sbuf(es, f"hg_Qd{i}", [128, 4, 512], BF16) for i in range(2)]
                Kd2 = [sbuf(es, f"hg_Kd{i}", [128, 4, 512], BF16) for i in range(2)]
                Kl2 = [sbuf(es, f"hg_Kl{i}", [128, 4, 512], BF16) for i in range(2)]
                vsb2 = [sbuf(es, f"hg_v{i}", [128, 4, 512], BF16) for i in range(2)]
                klt = sbuf(es, "hg_klt", [128, 512], BF16)
                at = sbuf(es, "hg_at", [128, 512], BF16)
                osb = [sbuf(es, f"hg_o{i}", [128, 4, 512], F32) for i in range(2)]
                St = sbuf(es, "hg_S", [128, 4, 128], F32)
                Sb2 = [sbuf(es, f"hg_Sb{i}", [128, 4, 128], BF16) for i in range(2)]
                if fwd:
                    obw = sbuf(es, "hg_obw", [128, 4, 512], F32)
                    osq = sbuf(es, "hg_osq", [128, 4, 512], BF16)
                    rs = sbuf(es, "hg_rs", [128, 512], F32)
                    sg2 = [sbuf(es, f"hg_sg{i}", [128, 4, 512], F32) for i in range(2)]
                    cT = [sbuf(es, f"hg_cT{i}", [128, 4, 512], BF16) for i in range(2)]
                zlist = []
                if not fwd:
                    zt = sbuf(es, "hg_zero", [128, 8192], BF16)
                    MSET("pool", zt[:], 0.0, ["hg_zero"])
                    xb_flat = xbuf_d.rearrange("(p a) d -> p (a d)", p=128)
                    ncol = NSLOT * D // 128
                    c0 = 0
                    while c0 < ncol:
                        cw = min(8192, ncol - c0)
                        zlist.append((c0, cw))
                        c0 += cw
                MSET("dve", St[:], 0.0, ["hg_S"])
                MSET("dve", Sb2[0][:], 0.0, ["hg_Sb0"])
                MSET("dve", Sb2[1][:], 0.0, ["hg_Sb1"])
                gcount = [0]
                di = 0 if fwd else 1
                mask = cst[:, 1 if fwd else 2, :]
                blocks = list(range(NBLK)) if fwd else list(range(NBLK - 1, -1, -1))
                tiles = [0, 1, 2, 3] if fwd else [3, 2, 1, 0]
                DMA("sp", hT[0][:], h0T_v[:, :, blocks[0] * 512:(blocks[0] + 1) * 512], ["h0T_d"], ["hg_hT0"])

                def vproj(bi, tl, bk):
                    b = bi % 2
                    hk = f"hg_hT{b}"
                    vsb = vsb2[b]
                    for t in tl:
                        pv = bank(bk)
                        for k in range(8):
                            MM(pv, hT[b][:, k, t * 128:(t + 1) * 128], Wi[:, k, :], k == 0, k == 7, [hk, "hg_Wi"], [("B", bk)])
                        CP("act" if t % 2 == 0 else "dve", vsb[:, t, :], pv, [("B", bk)], [("hg_v", b, t)])

                def head(bi, h, si):
                    b = bi % 2
                    hk = f"hg_hT{b}"
                    Qd, Kd, Kl, ebl = Qd2[b], Kd2[b], Kl2[b], ebl2[b]
                    tm = tmps[si]
                    e1, l1, l2, sq, qf, gg, fg, kk, bc, eb, enb, kd32 = [tm[n] for n in tnames]
                    K_ = lambda n: f"hg_{n}{si}"
                    pf = bank(2 * si)
                    pq = bank(2 * si + 1)
                    pfk = ("B", 2 * si)
                    pqk = ("B", 2 * si + 1)
                    for k in range(8):
                        MM(pf, Wf[:, k, h * 128:(h + 1) * 128], hT[b][:, k, :], k == 0, k == 7, [hk, "hg_Wf"], [pfk])
                    for k in range(8):
                        MM(pq, Wq[:, k, h * 128:(h + 1) * 128], hT[b][:, k, :], k == 0, k == 7, [hk, "hg_Wq"], [pqk])
                    lb = lbp[:, 0, di * 4 + h:di * 4 + h + 1]
                    ACT(e1[:], pf, AF.Exp, [pfk], [K_("e1")], scale=-1.0)
                    ACT(l1[:], e1[:], AF.Ln, [K_("e1")], [K_("l1")], bias=1.0)
                    ACT(l2[:], e1[:], AF.Ln, [K_("e1"), "lbp"], [K_("l2")], scale=lb, bias=1.0)
                    TT("pool", gg[:], l2[:], l1[:], ALU.subtract, [K_("l1"), K_("l2")], [K_("g")])
                    ACT(fg[:], gg[:], AF.Exp, [K_("g")], [K_("fg")])
                    TS("pool", kk[:], fg[:], -1.0, 1.0, ALU.mult, ALU.add, [K_("fg")], [K_("kk")])
                    ACT(e1[:], pq, AF.Exp, [pqk], [K_("e1")], scale=-1.0)
                    ACT(l1[:], e1[:], AF.Ln, [K_("e1")], [K_("l1")], bias=1.0)
                    ACT(sq[:], l1[:], AF.Exp, [K_("l1")], [K_("sq")], scale=-1.0)
                    TT("dve", qf[:], pq, sq[:], ALU.mult, [pqk, K_("sq")], [K_("qf")])
                    if fwd:
                        S.add("dve", lambda e: e.tensor_tensor_scan(out=bc[:], data0=cst32[:, 0, :], data1=gg[:], initial=0.0,
                                                                    op0=ALU.mult, op1=ALU.add), r=[K_("g"), "cst32"], w=[K_("bc")], cost=1.25)
                        blast = bc[:].rearrange("p (c t) -> p c t", t=128)[:, :, 127]
                    else:
                        S.add("dve", lambda e: e.tensor_tensor_scan(out=bc[:, ::-1], data0=cst32[:, 1, ::-1], data1=gg[:, ::-1], initial=0.0,
                                                                    op0=ALU.mult, op1=ALU.add), r=[K_("g"), "cst32"], w=[K_("bc")], cost=1.25)
                        blast = bc[:].rearrange("p (c t) -> p c t", t=128)[:, :, 0]
                    ACT(eb[:], bc[:], AF.Exp, [K_("bc")], [K_("eb")])
                    ACT(enb[:], bc[:], AF.Exp, [K_("bc")], [K_("enb")], scale=-1.0)
                    ACT(ebl[:, h, :], blast, AF.Exp, [K_("bc")], [("hg_ebl", b, h)])
                    TT("dve", Qd[:, h, :], qf[:], eb[:], ALU.mult, [K_("qf"), K_("eb")], [("hg_Qd", b, h)])
                    TT("pool", kd32[:], kk[:], enb[:], ALU.mult, [K_("kk"), K_("enb")], [K_("kd32")])
                    CP("pool", Kd[:, h, :], kd32[:], [K_("kd32")], [("hg_Kd", b, h)])
                    TT("dve", Kl[:, h, :].rearrange("p (c t) -> p c t", t=128), kd32[:].rearrange("p (c t) -> p c t", t=128),
                       ebl[:, h, :].unsqueeze(2).to_broadcast([128, 4, 128]), ALU.mult, [K_("kd32"), ("hg_ebl", b, h)], [("hg_Kl", b, h)])
                    if fwd:
                        for k in range(8):
                            MM(pq, Wg[:, k, h * 128:(h + 1) * 128], hT[b][:, k, :], k == 0, k == 7, [hk, "hg_Wg"], [pqk])
                        ACT(e1[:], pq, AF.Exp, [pqk], [K_("e1")], scale=-1.0)
                        ACT(l1[:], e1[:], AF.Ln, [K_("e1")], [K_("l1")], bias=1.0)
                        ACT(sq[:], l1[:], AF.Exp, [K_("l1")], [K_("sq")], scale=-1.0)
                        TT("dve", sg2[b][:, h, :], pq, sq[:], ALU.mult, [pqk, K_("sq")], [("hg_sg", b, h)])

                def streamsA(bi):
                    b = bi % 2
                    out = []
                    for si in range(2):
                        S.record()
                        if si == 0 and bi + 1 < NBLK:
                            nb = blocks[bi + 1]
                            DMA("sp", hT[1 - b][:], h0T_v[:, :, nb * 512:(nb + 1) * 512], ["h0T_d"], [f"hg_hT{1 - b}"])
                        vproj(bi, [2 * si, 2 * si + 1], 2 * si)
                        head(bi, si, si)
                        head(bi, si + 2, si)
                        out.append(S.stop())
                    return out

                def stageB(bi):
                    b = bi % 2
                    blk = blocks[bi]
                    Qd, Kd, Kl, vsb, ebl = Qd2[b], Kd2[b], Kl2[b], vsb2[b], ebl2[b]
                    allq = [("hg_Qd", b, h) for h in range(4)]
                    allk = [("hg_Kd", b, h) for h in range(4)]
                    alll = [("hg_Kl", b, h) for h in range(4)]
                    ob = osb[b]
                    if fwd:
                        DMA("sp", obw[:], obw_v[:, :, blk * 512:(blk + 1) * 512], ["obw_d"], ["hg_obw"])
                    for t in tiles:
                        ts = slice(t * 128, (t + 1) * 128)
                        g = gcount[0]
                        gcount[0] += 1
                        Sold, Snew = Sb2[(g + 1) % 2], Sb2[g % 2]
                        soldk, snewk = f"hg_Sb{(g + 1) % 2}", f"hg_Sb{g % 2}"
                        pk = bank16(4)
                        for h in range(4):
                            TR(pk[:, h * 128:(h + 1) * 128], Kl[:, h, ts], ident, alll + ["cst"], [("B", 4)])
                        CP("act", klt[:], pk[:, 0:512], [("B", 4)], ["hg_klt"])
                        pa = bank(5)
                        for h in range(4):
                            MM(pa[:, h * 128:(h + 1) * 128], Kd[:, h, ts], Qd[:, h, ts], True, True, allq + allk, [("B", 5)])
                        TT("dve", at[:], pa, mask, ALU.mult, [("B", 5), "cst"], ["hg_at"])
                        pS = bank(7)
                        for h in range(4):
                            MM(pS[:, h * 128:(h + 1) * 128], klt[:, h * 128:(h + 1) * 128], vsb[:, t, h * 128:(h + 1) * 128], True, True,
                               ["hg_klt", ("hg_v", b, t)], [("B", 7)])
                        po = bank(6)
                        for h in range(4):
                            MM(po[:, h * 128:(h + 1) * 128], vsb[:, t, h * 128:(h + 1) * 128], at[:, h * 128:(h + 1) * 128], True, False,
                               [("hg_v", b, t), "hg_at"], [("B", 6)])
                            MM(po[:, h * 128:(h + 1) * 128], Sold[:, h, :], Qd[:, h, ts], False, True, [soldk] + allq, [("B", 6)])
                        for h in range(4):
                            STT(St[:, h, :], St[:, h, :], ebl[:, h, t:t + 1], pS[:, h * 128:(h + 1) * 128], ALU.mult, ALU.add,
                                ["hg_S", ("hg_ebl", b, h), ("B", 7)], ["hg_S"])
                        CP("act", Snew[:], St[:], ["hg_S"], [snewk])
                        CP("act", ob[:, :, ts], po.rearrange("p (h t) -> p h t", h=4), [("B", 6)], [f"hg_o{b}"])
                    if not fwd:
                        DMA("sp", obw_v[:, :, blk * 512:(blk + 1) * 512], ob[:], [f"hg_o{b}"], ["obw_d"])
                        for _ in range(2):
                            if zlist:
                                c0, cw = zlist.pop(0)
                                DMA("sp", xb_flat[:, c0:c0 + cw], zt[:, 0:cw], ["hg_zero"], [("xbuf_z", c0)])
                        if bi == 0:
                            DMA("sp", ybuf_d[32 * CAP:32 * CAP + 128, :], zt[:, 0:2048].bitcast(F32), ["hg_zero"], ["ybuf_z"])
                    else:
                        TT("pool", ob[:], ob[:], obw[:], ALU.add, [f"hg_o{b}", "hg_obw"], [f"hg_o{b}"])
                        TT("pool", osq[:], ob[:], ob[:], ALU.mult, [f"hg_o{b}"], ["hg_osq"])
                        for h in range(4):
                            pr = bank(4)
                            MM(pr, ones_bf, osq[:, h, :], True, True, ["hg_osq", "cst"], [("B", 4)])
                            ACT(rs[:], pr, AF.Ln, [("B", 4)], ["hg_rs"], scale=1.0 / 128.0, bias=RMS_EPS)
                            ACT(rs[:], rs[:], AF.Exp, ["hg_rs"], ["hg_rs"], scale=-0.5)
                            TT("dve", rs[:], rs[:], ob[:, h, :], ALU.mult, ["hg_rs", f"hg_o{b}"], ["hg_rs"])
                            STT(cT[b][:, h, :], rs[:], normg[:, h:h + 1], sg2[b][:, h, :], ALU.mult, ALU.mult,
                                ["hg_rs", ("hg_sg", b, h), "normg"], [f"hg_cT{b}"])
                        DMA("sp", cT_v[:, :, blk * 512:(blk + 1) * 512], cT[b][:], [f"hg_cT{b}"], ["cT_d"])

                for st_ in streamsA(0):
                    S.merge(st_)
                for bi in range(NBLK):
                    sts = streamsA(bi + 1) if bi + 1 < NBLK else []
                    S.record()
                    stageB(bi)
                    rb_ = S.stop()
                    S.merge(*(sts + [rb_]))

        def phaseN():
            with ExitStack() as es:
                kT = sbuf(es, "na_kT", [128, 4, T], BF16)
                Vx = sbuf(es, "na_V", [128, NTILE, 8, 65], BF16)
                hT = [sbuf(es, f"na_hT{i}", [128, 8, 512], BF16) for i in range(2)]
                MSET("pool", Vx[:, :, :, 64:65], 1.0, ["na_V"])
                with ExitStack() as es1:
                    Wk = load_w(es1, "na_Wk", IN_OFF["na_k"], 512)
                    Wv = load_w(es1, "na_Wv", IN_OFF["na_v"], 512)
                    DMA("sp", hT[0][:], h0T_v[:, :, 0:512], ["h0T_d"], ["na_hT0"])
                    for blk in range(NBLK):
                        b = blk % 2
                        hk = f"na_hT{b}"
                        if blk + 1 < NBLK:
                            DMA("sp", hT[1 - b][:], h0T_v[:, :, (blk + 1) * 512:(blk + 2) * 512], ["h0T_d"], [f"na_hT{1 - b}"])
                        for p in range(4):
                            pk = bank(p % 2)
                            for k in range(8):
                                MM(pk, Wk[:, k, p * 128:(p + 1) * 128], hT[b][:, k, :], k == 0, k == 7, [hk, "na_Wk"], [("B", p % 2)])
                            CP("act" if p % 2 == 0 else "dve", kT[:, p, blk * 512:(blk + 1) * 512], pk, [("B", p % 2)], ["na_kT"])
                        for t in range(4):
                            pv = bank(2 + t % 2)
                            for k in range(8):
                                MM(pv, hT[b][:, k, t * 128:(t + 1) * 128], Wv[:, k, :], k == 0, k == 7, [hk, "na_Wv"], [("B", 2 + t % 2)])
                            CP("dve" if t % 2 == 0 else "act", Vx[:, blk * 4 + t, :, 0:64], pv.rearrange("p (h d) -> p h d", h=8),
                               [("B", 2 + t % 2)], ["na_V"])
                S.barrier()
                with ExitStack() as es2:
                    tts = sbuf(es2, "na_tt", [128, 16, 512], BF16)
                    DMA("sp", tts[:], tt_d, [], ["na_tt"])
                    Wq = load_w(es2, "na_Wq", IN_OFF["na_q"], 512)
                    qbd = sbuf(es2, "na_qbd", [128, 4, 8, 128], BF16)
                    Eb = [sbuf(es2, f"na_E{i}", [128, 1280], BF16) for i in range(2)]
                    rec = sbuf(es2, "na_rec", [128, 4], F32)
                    asb = [sbuf(es2, f"na_a{i}", [128, 512], BF16) for i in range(2)]
                    aT1 = sbuf(es2, "na_aT", [128, 4, 512], BF16)
                    aT = [aT1, aT1]
                    MSET("pool", qbd[:], 0.0, ["na_qbd"])
                    DMA("sp", hT[0][:], h0T_v[:, :, 0:512], ["h0T_d"], ["na_hT0"])

                    def tiles_of(r):
                        rs = min(max(r - 4, 0), 120)
                        if rs % 2 == 0:
                            return [(rs // 2 + i, 2 * (rs // 2 + i) - r + 7) for i in range(4)]
                        m0 = (rs - 1) // 2
                        return [(m0, 14)] + [(m0 + i, 2 * (m0 + i) - r + 7) for i in range(1, 4)] + [(m0 + 4, 15)]

                    def qk(u, rl):
                        r, h2 = u // 2, u % 2
                        tl = tiles_of(r)
                        nch = len(tl)
                        sbk = PS[:, (u % 2) * 1536:(u % 2) * 1536 + 1536]
                        keys = [("B", (u % 2) * 3 + i) for i in range(3)]
                        for pi in range(2):
                            p = 2 * h2 + pi
                            for ci, (m, slab) in enumerate(tl):
                                slot = pi * nch + ci
                                o = sbk[:, slot * 128:(slot + 1) * 128]
                                MM(o, kT[:, p, m * 128:(m + 1) * 128], qbd[:, p, rl, :], True, False, ["na_kT", "na_qbd"], keys)
                                MM(o, ident, tts[:, slab, p * 128:(p + 1) * 128], False, True, ["cst", "na_tt"], keys)

                    def rest(u):
                        r, h2 = u // 2, u % 2
                        tl = tiles_of(r)
                        nch = len(tl)
                        sbk = PS[:, (u % 2) * 1536:(u % 2) * 1536 + 1536]
                        keys = [("B", (u % 2) * 3 + i) for i in range(3)]
                        E = Eb[u % 2]
                        ek = f"na_E{u % 2}"
                        n = 2 * nch * 128
                        ACT(E[:, 0:n], sbk[:, 0:n], AF.Exp, keys, [ek])
                        par = r % 2
                        prt = slice(par * 64, (par + 1) * 64)
                        pO = bank(6)
                        for pi in range(2):
                            for hh in range(2):
                                hq = pi * 2 + hh
                                head = 4 * h2 + hq
                                for ci, (m, slab) in enumerate(tl):
                                    c0 = (pi * nch + ci) * 128 + hh * 64
                                    MM(pO[prt, hq * 65:(hq + 1) * 65], E[:, c0:c0 + 64], Vx[:, m, head, :], ci == 0, ci == nch - 1,
                                       [ek, "na_V"], [("B", 6)])
                        pov = pO[prt, 0:260].rearrange("p (h d) -> p h d", d=65)
                        S.add("dve", lambda e: e.reciprocal(out=rec[prt, :], in_=pov[:, :, 64]), r=[("B", 6)], w=["na_rec"])
                        tb = (r // 2) % 2
                        TT("dve", asb[tb][prt, h2 * 256:(h2 + 1) * 256].rearrange("p (h d) -> p h d", d=64), pov[:, :, 0:64],
                           rec[prt, :].unsqueeze(2).to_broadcast([64, 4, 64]), ALU.mult, [("B", 6), "na_rec"], [f"na_a{tb}"])

                    for blk in range(NBLK):
                        b = blk % 2
                        hk = f"na_hT{b}"
                        if blk + 1 < NBLK:
                            DMA("sp", hT[1 - b][:], h0T_v[:, :, (blk + 1) * 512:(blk + 2) * 512], ["h0T_d"], [f"na_hT{1 - b}"])
                        for p in range(4):
                            pq = bank(7)
                            for k in range(8):
                                MM(pq, Wq[:, k, p * 128:(p + 1) * 128], hT[b][:, k, :], k == 0, k == 7, [hk, "na_Wq"], [("B", 7)])
                            ACT(qbd[0:64, p, :, 0:64], pq[0:64, :].rearrange("p (r c) -> p r c", r=8), AF.Copy, [("B", 7)], ["na_qbd"], scale=0.125)
                            TS("dve", qbd[64:128, p, :, 64:128], pq[64:128, :].rearrange("p (r c) -> p r c", r=8), 0.125, None, ALU.mult, None,
                               [("B", 7)], ["na_qbd"])
                        u0 = blk * 16
                        qk(u0, 0)
                        for ul in range(16):
                            u = u0 + ul
                            if ul + 1 < 16:
                                qk(u + 1, (ul + 1) // 2)
                            rest(u)
                            if ul % 4 == 3:
                                t = ul // 4
                                tb = ((u // 2) // 2) % 2
                                pT = bank16(7)
                                for c in range(4):
                                    TR(pT[:, c * 128:(c + 1) * 128], asb[tb][:, c * 128:(c + 1) * 128], ident, [f"na_a{tb}", "cst"], [("B", 7)])
                                CP("act", aT[b][:, :, t * 128:(t + 1) * 128], pT[:, 0:512].rearrange("p (c t) -> p c t", c=4), [("B", 7)], ["na_aT"])
                        DMA("sp", aT_v[:, :, blk * 512:(blk + 1) * 512], aT[b][:], ["na_aT"], ["aT_d"])

        def phaseM():
            with ExitStack() as es:
                ba_bf = sbuf(es, "m_ba", [1, D], BF16)
                with ExitStack() as est:
                    tmpb = sbuf(est, "m_tmpb", [1, D], F32)
                    DMA("sp", tmpb[:], lnp_d[0:1, 1, :], [], ["m_tmpb"])
                    TS("dve", tmpb[:], tmpb[:], ALPHA, None, ALU.mult, None, ["m_tmpb"], ["m_tmpb"])
                    CP("dve", ba_bf[:], tmpb[:], ["m_tmpb"], ["m_ba"])
                S.barrier()
                lnp = sbuf(es, "m_lnp", [128, 3, D], F32)
                DMA("sp", lnp[:, 0, :], lnp_d[:, 0, :], [], ["m_lnp"])
                DMA("sp", lnp[:, 1:3, :], lnp_d[:, 2:4, :], [], ["m_lnp"])
                TS("pool", lnp[:, 0, :], lnp[:, 0, :], ALPHA, None, ALU.mult, None, ["m_lnp"], ["m_lnp"])
                Wga = load_w(es, "m_Wga", IN_OFF["gate_a"], 1024)
                Wgb = load_w(es, "m_Wgb", IN_OFF["gate_b"], 1024)
                Wa = sbuf(es, "m_Wa", [128, 4, D], BF16)
                Wb = sbuf(es, "m_Wb", [128, 4, D], BF16)
                Wo = sbuf(es, "m_Wo", [128, 8, D], BF16)
                for k in range(4):
                    DMA("pool", Wa[:, k, :], w_pa_d[k * 128:(k + 1) * 128, :], [], ["m_Wa"])
                    DMA("pool", Wb[:, k, :], w_pb_d[k * 128:(k + 1) * 128, :], [], ["m_Wb"])
                for k in range(8):
                    DMA("pool", Wo[:, k, :], w_out_d[k * 128:(k + 1) * 128, :], [], ["m_Wo"])
                wr = sbuf(es, "m_wr", [128, 8, 36], F32)
                rb = sbuf(es, "m_rb", [128, 4, 36], F32)
                DMA("sp", wr[:], wr_d, [], ["m_wr"])
                DMA("sp", rb[:], rb_d, [], ["m_rb"])
                hT = [sbuf(es, f"m_hT{i}", [128, 8, 512], BF16) for i in range(2)]
                aTs1 = sbuf(es, "m_aT", [128, 4, 512], BF16)
                aTs = [aTs1, aTs1]
                cTs1 = sbuf(es, "m_cT", [128, 4, 512], BF16)
                cTs = [cTs1, cTs1]
                xt = sbuf(es, "m_xt", [128, 4, D], F32)
                sgb_ = sbuf(es, "m_sgb", [128, 512], F32)
                sg4 = [(sbuf(es, f"m_sga{i}", [128, 512], F32), sgb_) for i in range(2)]
                m1 = sbuf(es, "m_m1", [128, 512], F32)
                m2 = sbuf(es, "m_m2", [128, 512], F32)
                mT = sbuf(es, "m_mT", [128, 8, 512], BF16)
                zz = [sbuf(es, f"m_z{i}", [128, 4, D], F32) for i in range(2)]
                hx2 = [sbuf(es, f"m_hx{i}", [128, D], F32) for i in range(2)]
                h1b = sbuf(es, "m_h1b", [128, 4, D], BF16)
                h1T = sbuf(es, "m_h1T", [128, 4, 128], F32)
                st1 = sbuf(es, "m_st1", [128, 4, 12], F32)
                mv1 = sbuf(es, "m_mv1", [128, 4, 2], F32)
                rs1 = sbuf(es, "m_rs1", [128, 4], F32)
                nm1 = sbuf(es, "m_nm1", [128, 4], F32)
                L = sbuf(es, "m_L", [128, 4, 36], F32)
                gmax = sbuf(es, "m_gmax", [128, 4], F32)
                gm = sbuf(es, "m_gm", [128, 4, 4], F32)
                gd = sbuf(es, "m_gd", [128, 4, 4], F32)
                gw = sbuf(es, "m_gw", [128, 4], F32)
                EM = sbuf(es, "m_EM", [128, 4, 32], F32)
                EM2 = sbuf(es, "m_EM2", [128, 4, 32], F32)
                top1 = sbuf(es, "m_top1", [128, 4], F32)
                top2 = sbuf(es, "m_top2", [128, 4], F32)
                oh = [sbuf(es, f"m_oh{i}", [128, 4, 32], F32) for i in range(2)]
                cnt = sbuf(es, "m_cnt", [128, 4, 32], BF16)
                rank = sbuf(es, "m_rank", [128, 4, 32], F32)
                slot = sbuf(es, "m_slot", [128, 4, 32], F32)
                tmp = sbuf(es, "m_tmp", [128, 4, 32], F32)
                tot = sbuf(es, "m_tot", [128, 32], F32)
                sm = sbuf(es, "m_sm", [128, 8, 4], F32)
                MSET("dve", tot[:], 0.0, ["m_tot"])
                if "fakeidx" in dbg:
                    fake_ix = sbuf(es, "m_fake", [128, NTILE, 2], I32)
                    S.add("pool", lambda e: e.iota(fake_ix[:].rearrange("p n k -> p (n k)"), pattern=[[128, 128]], base=0, channel_multiplier=1),
                          w=["m_fake"])
                ident32 = cst32[:, 2, 0:128]
                ustr = cst[:, 3, 0:128]
                ebase = cst32[:, 2, 128:160]
                pidx = cst32[:, 2, 160:161]

                def loads_ac(blk):
                    DMA("sp", aTs[0][:], aT_v[:, :, blk * 512:(blk + 1) * 512], ["aT_d"], ["m_aT"])
                    DMA("sp", cTs[0][:], cT_v[:, :, blk * 512:(blk + 1) * 512], ["cT_d"], ["m_cT"])

                def loads(blk, b):
                    DMA("sp", hT[b][:], h0T_v[:, :, blk * 512:(blk + 1) * 512], ["h0T_d"], [f"m_hT{b}"])

                def stageA(blk):
                    b = blk % 2
                    z = zz[b]
                    if blk + 1 < NBLK:
                        loads(blk + 1, 1 - b)
                    rs0 = xstat[:, blk * 4:(blk + 1) * 4, 0]
                    nm0 = xstat[:, blk * 4:(blk + 1) * 4, 1]
                    for c in range(8):
                        ga_, gb_ = 0, 1
                        pa_, pb_ = (2, 3) if c % 2 == 0 else (4, 5)
                        cs = slice(c * 128, (c + 1) * 128)
                        for k in range(8):
                            MM(bank(ga_), Wga[:, k, cs], hT[b][:, k, :], k == 0, k == 7, [f"m_hT{b}", "m_Wga"], [("B", ga_)])
                        for k in range(4):
                            MM(bank(pa_), Wa[:, k, cs], aTs[b][:, k, :], k == 0, k == 3, ["m_aT", "m_Wa"], [("B", pa_)])
                        for k in range(8):
                            MM(bank(gb_), Wgb[:, k, cs], hT[b][:, k, :], k == 0, k == 7, [f"m_hT{b}", "m_Wgb"], [("B", gb_)])
                        for k in range(4):
                            MM(bank(pb_), Wb[:, k, cs], cTs[b][:, k, :], k == 0, k == 3, ["m_cT", "m_Wb"], [("B", pb_)])
                        sga, sgb = sg4[c % 2]
                        ACT(sga[:], bank(ga_), AF.Sigmoid, [("B", ga_)], [f"m_sga{c % 2}"])
                        ACT(sgb[:], bank(gb_), AF.Sigmoid, [("B", gb_)], ["m_sgb"])
                        TT("dve", m1[:], bank(pa_), sga[:], ALU.mult, [("B", pa_), f"m_sga{c % 2}"], ["m_m1"])
                        TT("dve", m2[:], bank(pb_), sgb[:], ALU.mult, [("B", pb_), "m_sgb"], ["m_m2"])
                        TT("pool", mT[:, c, :], m1[:], m2[:], ALU.add, ["m_m1", "m_m2"], [("m_mT", c)])
                    if blk + 1 < NBLK:
                        loads_ac(blk + 1)
                    mtk = [("m_mT", c) for c in range(8)]
                    for t in range(4):
                        hxx = hx2[t % 2]
                        hk_ = f"m_hx{t % 2}"
                        ACT(hxx[:], xt[:, t, :], AF.Identity, ["m_xt", "xstat"], [hk_], scale=rs0[:, t:t + 1], bias=nm0[:, t:t + 1])
                        TT("dve", hxx[:], hxx[:], lnp[:, 0, :], ALU.mult, [hk_, "m_lnp"], [hk_])
                        for n in range(2):
                            o = (2, 3, 4, 5)[(t * 2 + n) % 4]
                            ns = slice(n * 512, (n + 1) * 512)
                            MM(bank(o), cst[0:1, 4, 0:128], ba_bf[0:1, ns], True, False, ["cst", "m_ba"], [("B", o)])
                            for k in range(8):
                                MM(bank(o), mT[:, k, t * 128:(t + 1) * 128], Wo[:, k, ns], False, k == 7, mtk + ["m_Wo"], [("B", o)])
                            TT("dve", z[:, t, ns], hxx[:, ns], bank(o), ALU.add, [hk_, ("B", o)], [("m_z", b, t)])
                    if blk + 1 < NBLK:
                        DMA("sp", xt[:], x_v[blk + 1], [], ["m_xt"])

                def stageB(blk):
                    b = blk % 2
                    z = zz[b]
                    zk = [("m_z", b, t) for t in range(4)]
                    for t in range(4):
                        for c in range(2):
                            S.add("dve", lambda e, t=t, c=c: e.bn_stats(out=st1[:, t, c * 6:(c + 1) * 6], in_=z[:, t, c * 512:(c + 1) * 512]),
                                  r=[("m_z", b, t)], w=["m1st"])
                        S.add("dve", lambda e, t=t: e.bn_aggr(out=mv1[:, t, :], in_=st1[:, t, :]), r=["m1st"], w=["m1mv"])
                    ACT(rs1[:], mv1[:, :, 1], AF.Ln, ["m1mv"], ["m1rs"], bias=LN_EPS)
                    ACT(rs1[:], rs1[:], AF.Exp, ["m1rs"], ["m1rs"], scale=-0.5)
                    STT(nm1[:], mv1[:, :, 0], -1.0, rs1[:], ALU.mult, ALU.mult, ["m1mv", "m1rs"], ["m1nm"])
                    for t in range(4):
                        ACT(z[:, t, :], z[:, t, :], AF.Identity, [("m_z", b, t), "m1rs", "m1nm"], [("m_z", b, t)], scale=rs1[:, t:t + 1], bias=nm1[:, t:t + 1])
                        TT("dve", z[:, t, :], z[:, t, :], lnp[:, 1, :], ALU.mult, [("m_z", b, t), "m_lnp"], [("m_z", b, t)])
                        TT("pool", z[:, t, :], z[:, t, :], lnp[:, 2, :], ALU.add, [("m_z", b, t), "m_lnp"], [("m_z", b, t)])
                        CP("act", h1b[:, t, :], z[:, t, :], [("m_z", b, t)], [("m_h1b", t)])
                    DMA("sp", h1_v[blk], z[:], zk, ["h1_d"])
                    pL = bank(7)
                    for t in range(4):
                        for g in range(2):
                            for kk_ in range(4):
                                k = g * 4 + kk_
                                TR(bank(6)[:, kk_ * 128:(kk_ + 1) * 128], z[:, t, k * 128:(k + 1) * 128], ident32, [("m_z", b, t), "cst32"], [("B", 6)])
                            CP("act" if g == 0 else "dve", h1T[:, 0:4, :], bank(6).rearrange("p (k t) -> p k t", k=4),
                               [("B", 6)], ["m_h1T"])
                            for kk_ in range(4):
                                k = g * 4 + kk_
                                MM(pL[:, t * 36:(t + 1) * 36], h1T[:, kk_, :], wr[:, k, :], k == 0, k == 7, ["m_h1T", "m_wr"], [("B", 7)])
                    TT("dve", L[:], pL[:, 0:144].rearrange("p (t n) -> p t n", n=36), rb[:], ALU.add, [("B", 7), "m_rb"], ["m_L"])
                    GL = L[:, :, 0:4]
                    EL = L[:, :, 4:36].rearrange("p t (g e) -> p t g e", g=4)
                    S.add("dve", lambda e: e.tensor_reduce(out=gmax[:], in_=GL, axis=AX.X, op=ALU.max), r=["m_L"], w=["m_gmax"])
                    TT("dve", gm[:], GL, gmax[:].unsqueeze(2).to_broadcast([128, 4, 4]), ALU.is_equal, ["m_L", "m_gmax"], ["m_gm"])
                    TT("dve", gd[:], GL, gmax[:].unsqueeze(2).to_broadcast([128, 4, 4]), ALU.subtract, ["m_L", "m_gmax"], ["m_gd"])
                    ACT(gd[:], gd[:], AF.Exp, ["m_gd"], ["m_gd"])
                    S.add("dve", lambda e: e.tensor_reduce(out=gw[:], in_=gd[:], axis=AX.X, op=ALU.add), r=["m_gd"], w=["m_gw"])
                    S.add("dve", lambda e: e.reciprocal(out=gw[:], in_=gw[:]), r=["m_gw"], w=["m_gw"])
                    TS("dve", gm[:], gm[:], 1e9, -1e9, ALU.mult, ALU.add, ["m_gm"], ["m_gm"])
                    TT("dve", EM[:].rearrange("p t (g e) -> p t g e", g=4), EL, gm[:].unsqueeze(3).to_broadcast([128, 4, 4, 8]), ALU.add,
                       ["m_L", "m_gm"], ["m_EM"])
                    S.add("dve", lambda e: e.tensor_reduce(out=top1[:], in_=EM[:], axis=AX.X, op=ALU.max), r=["m_EM"], w=["m_top1"])
                    TT("dve", oh[0][:], EM[:], top1[:].unsqueeze(2).to_broadcast([128, 4, 32]), ALU.is_equal, ["m_EM", "m_top1"], ["m_oh0"])
                    STT(EM2[:], oh[0][:], -1e9, EM[:], ALU.mult, ALU.add, ["m_oh0", "m_EM"], ["m_EM2"])
                    S.add("dve", lambda e: e.tensor_reduce(out=top2[:], in_=EM2[:], axis=AX.X, op=ALU.max), r=["m_EM2"], w=["m_top2"])
                    TT("dve", oh[1][:], EM2[:], top2[:].unsqueeze(2).to_broadcast([128, 4, 32]), ALU.is_equal, ["m_EM2", "m_top2"], ["m_oh1"])
                    w1 = wall[:, blk * 4:(blk + 1) * 4, 0]
                    w2 = wall[:, blk * 4:(blk + 1) * 4, 1]
                    TT("dve", sm[:, 0, :], top2[:], top1[:], ALU.subtract, ["m_top1", "m_top2"], ["m_sm0"])
                    ACT(sm[:, 0, :], sm[:, 0, :], AF.Exp, ["m_sm0"], ["m_sm0"])
                    TS("dve", sm[:, 0, :], sm[:, 0, :], 1.0, None, ALU.add, None, ["m_sm0"], ["m_sm0"])
                    S.add("dve", lambda e: e.reciprocal(out=sm[:, 1, :], in_=sm[:, 0, :]), r=["m_sm0"], w=["m_sm1"])
                    TT("dve", w1, sm[:, 1, :], gw[:], ALU.mult, ["m_sm1", "m_gw"], [("wall", blk)])
                    TT("dve", w2, gw[:], w1, ALU.subtract, ["m_gw", ("wall", blk)], [("wall", blk)])
                    TT("dve", cnt[:], oh[0][:], oh[1][:], ALU.add, ["m_oh0", "m_oh1"], ["m_cnt"])
                    pR = bank(7)[:, 256:512]
                    for t in range(4):
                        MM(pR[:, t * 32:(t + 1) * 32], ustr, cnt[:, t, :], True, t == 0, ["m_cnt", "cst"], [("B", 7)])
                        for t2 in range(t):
                            MM(pR[:, t * 32:(t + 1) * 32], ones_bf, cnt[:, t2, :], False, t2 == t - 1, ["m_cnt", "cst"], [("B", 7)])
                    for t in range(4):
                        MM(pR[:, 128:160], ones_bf, cnt[:, t, :], t == 0, t == 3, ["m_cnt", "cst"], [("B", 7)])
                    TT("dve", rank[:], pR[:, 0:128].rearrange("p (t n) -> p t n", n=32), tot[:].unsqueeze(1).to_broadcast([128, 4, 32]), ALU.add,
                       [("B", 7), "m_tot"], ["m_rank"])
                    TT("dve", tot[:], tot[:], pR[:, 128:160], ALU.add, ["m_tot", ("B", 7)], ["m_tot"])
                    TT("dve", slot[:], rank[:], ebase.unsqueeze(1).to_broadcast([128, 4, 32]), ALU.add, ["m_rank", "cst32"], ["m_slot"])
                    for kx in range(2):
                        ohk = f"m_oh{kx}"
                        TT("dve", tmp[:], oh[kx][:], slot[:], ALU.mult, [ohk, "m_slot"], ["m_tmp"])
                        S.add("dve", lambda e: e.tensor_reduce(out=sm[:, 2, :], in_=tmp[:], axis=AX.X, op=ALU.add), r=["m_tmp"], w=["m_sm2"])
                        TT("dve", tmp[:], oh[kx][:], rank[:], ALU.mult, [ohk, "m_rank"], ["m_tmp"])
                        S.add("dve", lambda e: e.tensor_reduce(out=sm[:, 3, :], in_=tmp[:], axis=AX.X, op=ALU.add), r=["m_tmp"], w=["m_sm3"])
                        TS("dve", sm[:, 3, :], sm[:, 3, :], float(CAP), None, ALU.is_ge, None, ["m_sm3"], ["m_sm3"])
                        TS("dve", sm[:, 4, :], sm[:, 2, :], -1.0, pidx, ALU.mult, ALU.add, ["m_sm2", "cst32"], ["m_sm4"])
                        TT("dve", sm[:, 4, :], sm[:, 4, :], sm[:, 3, :], ALU.mult, ["m_sm4", "m_sm3"], ["m_sm4"])
                        TT("dve", sm[:, 2, :], sm[:, 2, :], sm[:, 4, :], ALU.add, ["m_sm2", "m_sm4"], ["m_sm2"])
                        CP("pool", posall[:, blk * 4:(blk + 1) * 4, kx], sm[:, 2, :], ["m_sm2"], [("posall", blk)])
                    for t in range(4):
                        if "noscatter" in dbg:
                            break
                        for kx in range(2):
                            ixap = posall[:, blk * 4 + t, kx:kx + 1]
                            if "fakeidx" in dbg:
                                ixap = fake_ix[:, blk * 4 + t, kx:kx + 1]
                            S.add("pool", lambda e, t=t, kx=kx, ixap=ixap: e.indirect_dma_start(
                                out=xbuf_d, out_offset=bass.IndirectOffsetOnAxis(ap=ixap, axis=0),
                                in_=h1b[:, t, :], in_offset=None), r=[("m_h1b", t), ("posall", blk)], w=["xbuf_d"], dma=True, cost=4.0)

                loads(0, 0)
                loads_ac(0)
                DMA("sp", xt[:], x_v[0], [], ["m_xt"])
                stageA(0)
                for blk in range(NBLK):
                    ra = None
                    if blk + 1 < NBLK:
                        S.record()
                        stageA(blk + 1)
                        ra = S.stop()
                    S.record()
                    stageB(blk)
                    rb_ = S.stop()
                    S.merge(ra, rb_)

        def dump_routing():
            pos_d = nc.dram_tensor("pos_d", [128, NTILE * 2], I32, kind="ExternalOutput").ap()
            wall_d = nc.dram_tensor("wall_d", [128, NTILE * 2], F32, kind="ExternalOutput").ap()
            allk = [("posall", b) for b in range(NBLK)]
            allw = [("wall", b) for b in range(NBLK)]
            DMA("sp", pos_d, posall[:].rearrange("p n k -> p (n k)"), allk, ["pos_d"])
            DMA("sp", wall_d, wall[:].rearrange("p n k -> p (n k)"), allw, ["wall_d"])

        def phaseE():
            with ExitStack() as es:
                wg = [sbuf(es, f"e_wg{i}", [128, 8, 512], BF16) for i in range(2)]
                wu = [sbuf(es, f"e_wu{i}", [128, 8, 512], BF16) for i in range(2)]
                wd = [sbuf(es, f"e_wd{i}", [128, 4, D], BF16) for i in range(2)]
                xs = [sbuf(es, f"e_xs{i}", [128, 6, D], BF16) for i in range(2)]
                xT2 = [sbuf(es, f"e_xT{i}", [128, 8, CAP], BF16) for i in range(2)]
                sg = sbuf(es, "e_sg", [128, 384], F32)
                tg = sbuf(es, "e_tg", [128, 384], F32)
                hT2 = [sbuf(es, f"e_hT{i}", [128, 4, CAP], BF16) for i in range(2)]
                ysb = [sbuf(es, f"e_y{i}", [128, D], F32) for i in range(2)]

                wst = [sbuf(es, f"e_wst{i}", [128, 4096], F32) for i in range(3)]

                def wload(e, b):
                    DMA("sp", wst[0][:].rearrange("p (k n) -> p k n", k=8), w_gate_d[e].rearrange("(k p) n -> p k n", p=128), [], ["e_wst0"])
                    DMA("sp", wst[1][:].rearrange("p (k n) -> p k n", k=8), w_up_d[e].rearrange("(k p) n -> p k n", p=128), [], ["e_wst1"])
                    DMA("sp", wst[2][:].rearrange("p (k n) -> p k n", k=4), w_down_d[e].rearrange("(k p) n -> p k n", p=128), [], ["e_wst2"])
                    CP("pool", wg[b][:].rearrange("p k n -> p (k n)"), wst[0][:], ["e_wst0"], [f"e_wg{b}"])
                    CP("pool", wu[b][:].rearrange("p k n -> p (k n)"), wst[1][:], ["e_wst1"], [f"e_wu{b}"])
                    CP("pool", wd[b][:].rearrange("p k n -> p (k n)"), wst[2][:], ["e_wst2"], [f"e_wd{b}"])
                    DMA("sp", xs[b][:], xbuf_d[e * CAP:(e + 1) * CAP, :].rearrange("(s p) d -> p s d", p=128), ["xbuf_d"], [f"e_xs{b}"])

                def stageA(e):
                    b = e % 2
                    for k in range(8):
                        pb = bank16(k % 2)
                        for s_ in range(6):
                            TR(pb[:, s_ * 128:(s_ + 1) * 128], xs[b][:, s_, k * 128:(k + 1) * 128], ident, [f"e_xs{b}", "cst"], [("B", k % 2)])
                        CP("act" if k % 2 == 0 else "dve", xT2[b][:, k, :], pb[:, 0:CAP], [("B", k % 2)], [("e_xT", b, k)])

                def stageGU(e):
                    b = e % 2
                    xT = xT2[b]
                    hTt = hT2[b]
                    xtk = [("e_xT", b, k) for k in range(8)]
                    for m in range(4):
                        for half in range(2):
                            u = m * 2 + half
                            o = 2 + 2 * (u % 2)
                            hs = slice(half * 384, (half + 1) * 384)
                            ms = slice(m * 128, (m + 1) * 128)
                            for k in range(8):
                                MM(bank(o)[:, 0:384], wg[b][:, k, ms], xT[:, k, hs], k == 0, k == 7, xtk + [f"e_wg{b}"], [("B", o)])
                            for k in range(8):
                                MM(bank(o + 1)[:, 0:384], wu[b][:, k, ms], xT[:, k, hs], k == 0, k == 7, xtk + [f"e_wu{b}"], [("B", o + 1)])
                            ACT(sg[:], bank(o)[:, 0:384], AF.Sigmoid, [("B", o)], ["e_sg"])
                            TT("dve", tg[:], bank(o)[:, 0:384], sg[:], ALU.mult, [("B", o), "e_sg"], ["e_tg"])
                            TT("dve", hTt[:, m, hs], bank(o + 1)[:, 0:384], tg[:], ALU.mult, [("B", o + 1), "e_tg"], [("e_hT", b, m)])

                def stageY(e):
                    b = e % 2
                    hTt = hT2[b]
                    htk = [("e_hT", b, m) for m in range(4)]
                    for s_ in range(6):
                        yb = (e * 6 + s_) % 2
                        for n in range(2):
                            yo = 6 + n
                            for m in range(4):
                                MM(bank(yo), hTt[:, m, s_ * 128:(s_ + 1) * 128], wd[b][:, m, n * 512:(n + 1) * 512], m == 0, m == 3,
                                   htk + [f"e_wd{b}"], [("B", yo)])
                            CP("act" if n == 0 else "dve", ysb[yb][:, n * 512:(n + 1) * 512], bank(yo), [("B", yo)], [(f"e_y{yb}", n)])
                        DMA("sp", ybuf_d[e * CAP + s_ * 128:e * CAP + (s_ + 1) * 128, :], ysb[yb][:], [(f"e_y{yb}", 0), (f"e_y{yb}", 1)], ["ybuf_d"])

                wload(0, 0)
                for e in range(32):
                    if e + 1 < 32:
                        wload(e + 1, (e + 1) % 2)
                    stageA(e)
                    stageGU(e)
                    stageY(e)

        def phaseG():
            with ExitStack() as es:
                lnp = sbuf(es, "g_lnp", [128, 2, D], F32)
                DMA("sp", lnp[:], lnp_d[:, 4:6, :], [], ["g_lnp"])
                y0 = [sbuf(es, f"g_y0{i}", [128, D], F32) for i in range(2)]
                y1 = [sbuf(es, f"g_y1{i}", [128, D], F32) for i in range(2)]
                h1t = [sbuf(es, f"g_h1{i}", [128, D], F32) for i in range(2)]
                zz = [sbuf(es, f"g_z{i}", [128, 1, D], F32) for i in range(2)]
                st2 = [sbuf(es, f"g_st{i}", [128, 1, 12], F32) for i in range(2)]
                mv2 = [sbuf(es, f"g_mv{i}", [128, 1, 2], F32) for i in range(2)]
                rs2 = [sbuf(es, f"g_rs{i}", [128, 1], F32) for i in range(2)]
                nm2 = [sbuf(es, f"g_nm{i}", [128, 1], F32) for i in range(2)]

                def gl(n, b):
                    S.add("pool", lambda e: e.indirect_dma_start(out=y0[b][:], out_offset=None, in_=ybuf_d,
                                                                 in_offset=bass.IndirectOffsetOnAxis(ap=posall[:, n, 0:1], axis=0)),
                          r=["ybuf_d"], w=[f"g_y0{b}"], dma=True, cost=5.0)
                    S.add("pool", lambda e: e.indirect_dma_start(out=y1[b][:], out_offset=None, in_=ybuf_d,
                                                                 in_offset=bass.IndirectOffsetOnAxis(ap=posall[:, n, 1:2], axis=0)),
                          r=["ybuf_d"], w=[f"g_y1{b}"], dma=True, cost=5.0)
                    DMA("sp", h1t[b][:], h1_tv[n], ["h1_d"], [f"g_h1{b}"])

                def stageA(n):
                    b = n % 2
                    zt = zz[b]
                    zk = f"g_z{b}"
                    ACT(zt[:, 0, :], y0[b][:], AF.Copy, [f"g_y0{b}"], [zk, zk + "a", zk + "b"], scale=wall[:, n, 0:1])
                    STT(zt[:, 0, :], y1[b][:], wall[:, n, 1:2], zt[:, 0, :], ALU.mult, ALU.add, [f"g_y1{b}", zk], [zk])
                    STT(zt[:, 0, :], h1t[b][:], ALPHA, zt[:, 0, :], ALU.mult, ALU.add, [f"g_h1{b}", zk], [zk])
                    ln_stats(f"g{b}", zt, zk, st2[b], mv2[b], rs2[b], nm2[b], tiles=1)

                def stageB(n):
                    b = n % 2
                    zt = zz[b]
                    zk = f"g_z{b}"
                    ACT(zt[:, 0, :], zt[:, 0, :], AF.Identity, [zk, f"g{b}rs", f"g{b}nm"], [zk], scale=rs2[b][:, 0:1], bias=nm2[b][:, 0:1])
                    TT("dve", zt[:, 0, :], zt[:, 0, :], lnp[:, 0, :], ALU.mult, [zk, "g_lnp"], [zk])
                    TT("pool", zt[:, 0, 0:512], zt[:, 0, 0:512], lnp[:, 1, 0:512], ALU.add, [zk, "g_lnp"], [zk + "a"])
                    TT("dve", zt[:, 0, 512:1024], zt[:, 0, 512:1024], lnp[:, 1, 512:1024], ALU.add, [zk, "g_lnp"], [zk + "b"])
                    DMA("sp", out_v[n], zt[:, 0, :], [zk, zk + "a", zk + "b"], ["out"])

                gl(0, 0)
                gl(1, 1)
                for n in range(0, NTILE, 2):
                    sts = []
                    for n2 in (n, n + 1):
                        S.record()
                        stageA(n2)
                        if n2 + 2 < NTILE:
                            gl(n2 + 2, n2 % 2)
                        stageB(n2)
                        sts.append(S.stop())
                    S.merge(*sts)

        for ph in phases:
            if ph == "0":
                phase0()
            elif ph == "B":
                hgrn_phase(1)
            elif ph == "F":
                hgrn_phase(0)
            elif ph == "N":
                phaseN()
            elif ph == "M":
                phaseM()
                if "pos_d" in dbg:
                    dump_routing()
            elif ph == "E":
                phaseE()
            elif ph == "G":
                phaseG()
            S.barrier()
        S.emit(nc)
    return nc


def host_consts():
    bf = ml_dtypes.bfloat16
    cst = np.zeros((128, 5, 512), np.float32)
    eye = np.eye(128, dtype=np.float32)
    s = np.arange(128)[:, None]
    t = np.arange(128)[None, :]
    cst[:, 0, :] = np.tile(eye, (1, 4))
    cst[:, 1, :] = np.tile((s <= t).astype(np.float32), (1, 4))
    cst[:, 2, :] = np.tile((s >= t).astype(np.float32), (1, 4))
    cst[:, 3, :] = np.tile((s < t).astype(np.float32), (1, 4))
    cst[:, 4, :] = 1.0
    cst32 = np.zeros((128, 3, 512), np.float32)
    tt = np.arange(512)
    cst32[:, 0, :] = (tt % 128 != 0).astype(np.float32)[None, :]
    cst32[:, 1, :] = (tt % 128 != 127).astype(np.float32)[None, :]
    cst32[:, 2, 0:128] = eye
    cst32[:, 2, 128:160] = (np.arange(32) * CAP).astype(np.float32)[None, :]
    cst32[:, 2, 160] = 32 * CAP + np.arange(128)
    return cst.astype(bf), cst32


def host_tt(rpb):
    bf = ml_dtypes.bfloat16
    cols = np.arange(64)
    col_start = np.clip(cols - 8, 0, 48)
    c = cols[None, :]
    kc = cols[:, None]
    valid = (kc >= col_start[None, :]) & (kc < col_start[None, :] + 16)
    off = np.clip(kc - c + 15, 0, 30)
    full = np.full((15, 64, 8, 64), NEG, np.float32)
    for ro in range(15):
        for h in range(8):
            full[ro, :, h, :] = np.where(valid, rpb[h, ro][off], NEG)
    full = full.reshape(15, 64, 512)
    neg = np.full((64, 512), NEG, np.float32)
    tt = np.zeros((128, 16, 512), np.float32)
    for a in range(14):
        tt[0:64, a] = full[a]
        tt[64:128, a] = full[a + 1]
    tt[0:64, 14] = neg
    tt[64:128, 14] = full[3]
    tt[0:64, 15] = full[10]
    tt[64:128, 15] = neg
    return tt.astype(bf)


def make_in_maps(inp):
    cst, cst32 = host_consts()
    f = lambda a: np.ascontiguousarray(np.asarray(a, np.float32))
    lnp = np.stack([inp["emb_ln_g"], inp["emb_ln_b"], inp["ln1_g"][0], inp["ln1_b"][0], inp["ln2_g"][0], inp["ln2_b"][0]], 0)
    lnp = f(np.broadcast_to(lnp[None], (128, 6, D)))
    embp = f(np.concatenate([np.asarray(inp["emb_ln_g"]).reshape(8, 128).T, np.asarray(inp["emb_ln_b"]).reshape(8, 128).T], 1))
    lbraw = f(np.asarray(inp["hg_lb"]).reshape(2, 2, 4, 128).transpose(3, 0, 1, 2).reshape(128, 16))
    normg = f(np.asarray(inp["hg_norm_g"])[0].reshape(4, 128).T)
    wr = np.concatenate([np.asarray(inp["w_router_group"])[0], np.asarray(inp["w_router_expert"])[0]], 1)
    wr = f(wr.reshape(8, 128, 36).transpose(1, 0, 2))
    rb = np.concatenate([np.asarray(inp["b_router_group"])[0], np.asarray(inp["b_router_expert"])[0]], 0)
    rb = f(np.broadcast_to(rb[None, None], (128, 4, 36)))
    tt = host_tt(np.asarray(inp["na_rpb"], np.float32)[0])
    shared = dict(w_in=f(inp["w_in"][0]), w_proj_a=f(inp["w_proj_a"][0]), w_proj_b=f(inp["w_proj_b"][0]), w_out=f(inp["w_out"][0]),
                  w_gate=f(inp["w_gate"][0]), w_up=f(inp["w_up"][0]), w_down=f(inp["w_down"][0]),
                  lnp=lnp, embp=embp, lbraw=lbraw, normg=normg, wr=wr, rb=rb, tt=tt, cst=cst, cst32=cst32)
    x = np.asarray(inp["x"], np.float32)
    return [dict(shared, x=np.ascontiguousarray(x[b])) for b in range(x.shape[0])]


def kernel(**inputs):
    nc = build()
    in_maps = make_in_maps(inputs)
    res = run_bass_kernel_spmd(nc, in_maps, core_ids=list(range(8)))
    return np.stack([np.asarray(r["out"], np.float32) for r in res.results], 0)
```
